# Optimizing a Trainium2 kernel written in Bass

```python
import jax, jax.numpy as jnp
from jax import lax
import numpy as np

D_MODEL = 1024
BATCH = 8
SEQ = 2048
DEPTH = 2

CTX_LEN = 256
GRID_W = 64
HEAD_DIM = 64
ATTN_HEADS = 8
ATTN_KV_HEADS = 2
ATTN_GROUP = ATTN_HEADS // ATTN_KV_HEADS
WINDOW = 128
ATTN_BLOCK = 128
RET_HEADS = 8
RET_DK = 64
RET_DV = 64
RET_CHUNK = 128
FOURIER_GROUPS = 4
FOURIER_DIM = 128
ATTN_WIDTH = ATTN_HEADS * HEAD_DIM
KV_WIDTH = ATTN_KV_HEADS * HEAD_DIM
RET_QK_WIDTH = RET_HEADS * RET_DK
RET_WIDTH = RET_HEADS * RET_DV
FOURIER_WIDTH = FOURIER_GROUPS * FOURIER_DIM
N_BRANCH = 3
IN_WIDTHS = (ATTN_WIDTH, KV_WIDTH, KV_WIDTH, RET_QK_WIDTH, RET_QK_WIDTH, RET_WIDTH, RET_WIDTH, FOURIER_WIDTH, N_BRANCH * D_MODEL)
IN_COLS = sum(IN_WIDTHS)
N_GROUPS = 4
EXPERTS_PER_GROUP = 8
N_EXPERTS = N_GROUPS * EXPERTS_PER_GROUP
TOP_K = 2
EXPERT_HIDDEN = 512
MOE_BLOCK = 128
ROPE_BASE = 10000.0
NORM_EPS = 1e-6
GN_EPS = 1e-5

kernel_name = "hybrid_flow_backbone"

F32 = jnp.float32


def split_points():
    pts, acc = [], 0
    for w in IN_WIDTHS[:-1]:
        acc += w
        pts.append(acc)
    return pts


def rmsnorm(x, g):
    xf = x.astype(F32)
    y = xf * lax.rsqrt(jnp.mean(xf * xf, axis=-1, keepdims=True) + NORM_EPS)
    return (y * g.astype(F32)).astype(x.dtype)


def modulate(h, shift, scale):
    return h * (1.0 + scale) + shift


def rope_table(pos, n_freq):
    inv = ROPE_BASE ** (-jnp.arange(n_freq, dtype=F32) / n_freq)
    ang = pos[:, None] * inv[None, :]
    return jnp.cos(ang), jnp.sin(ang)


def rotate(x, cos, sin):
    f = x.shape[-1] // 2
    x1, x2 = x[..., :f], x[..., f:]
    c = cos[None, :, None, :].astype(x.dtype)
    s = sin[None, :, None, :].astype(x.dtype)
    return jnp.concatenate([x1 * c - x2 * s, x1 * s + x2 * c], axis=-1)


def axial_rope(x, row_cs, col_cs):
    h = x.shape[-1] // 2
    return jnp.concatenate([rotate(x[..., :h], *row_cs), rotate(x[..., h:], *col_cs)], axis=-1)


def window_attention(q, k, v, kc, vc, sink):
    B, N, H, Dh = q.shape
    L = kc.shape[1]
    nb = N // ATTN_BLOCK
    nw = 3 * ATTN_BLOCK
    scale = Dh ** -0.5
    qb = q.reshape(B, nb, ATTN_BLOCK, ATTN_KV_HEADS, ATTN_GROUP, Dh)

    def windows(t):
        tp = jnp.pad(t, ((0, 0), (ATTN_BLOCK, ATTN_BLOCK), (0, 0), (0, 0)))
        tb = tp.reshape(B, nb + 2, ATTN_BLOCK, ATTN_KV_HEADS, Dh)
        return jnp.concatenate([tb[:, :-2], tb[:, 1:-1], tb[:, 2:]], axis=2)

    kw, vw = windows(k), windows(v)
    kpos = jnp.arange(-ATTN_BLOCK, N + ATTN_BLOCK).reshape(nb + 2, ATTN_BLOCK)
    kpos = jnp.concatenate([kpos[:-2], kpos[1:-1], kpos[2:]], axis=1)
    qpos = jnp.arange(N).reshape(nb, ATTN_BLOCK)
    kp = kpos[:, None, :]
    valid = (jnp.abs(qpos[:, :, None] - kp) <= WINDOW) & (kp >= 0) & (kp < N)
    s_loc = jnp.einsum('bnqhgd,bnkhd->bnhgqk', qb, kw).astype(F32) * scale
    s_loc = jnp.where(valid[None, :, None, None], s_loc, -jnp.inf)
    s_ctx = jnp.einsum('bnqhgd,blhd->bnhgql', qb, kc).astype(F32) * scale
    s_snk = jnp.broadcast_to(sink.astype(F32).reshape(1, 1, ATTN_KV_HEADS, ATTN_GROUP, 1, 1), s_loc.shape[:-1] + (1,))
    p = jax.nn.softmax(jnp.concatenate([s_loc, s_ctx, s_snk], axis=-1), axis=-1).astype(v.dtype)
    o = (jnp.einsum('bnhgqk,bnkhd->bnqhgd', p[..., :nw], vw)
         + jnp.einsum('bnhgql,blhd->bnqhgd', p[..., nw:nw + L], vc))
    return o.reshape(B, N, H * Dh)


def context_attention(q, k, v, sink):
    B, L, H, Dh = q.shape
    qg = q.reshape(B, L, ATTN_KV_HEADS, ATTN_GROUP, Dh)
    s = jnp.einsum('bqhgd,bkhd->bhgqk', qg, k).astype(F32) * (Dh ** -0.5)
    s_snk = jnp.broadcast_to(sink.astype(F32).reshape(1, ATTN_KV_HEADS, ATTN_GROUP, 1, 1), s.shape[:-1] + (1,))
    p = jax.nn.softmax(jnp.concatenate([s, s_snk], axis=-1), axis=-1)[..., :L].astype(v.dtype)
    o = jnp.einsum('bhgqk,bkhd->bqhgd', p, v)
    return o.reshape(B, L, H * Dh)


def fourier_mix(u):
    B, N, _ = u.shape
    ug = u.astype(F32).reshape(B, N, FOURIER_GROUPS, FOURIER_DIM)
    z = jnp.fft.fft2(ug, axes=(1, 3), norm='ortho')
    return jnp.real(z).reshape(B, N, FOURIER_WIDTH).astype(u.dtype)


def retention_chunked(q, k, v, lg, s0, inclusive):
    B, N, H, dk = q.shape
    dv = v.shape[-1]
    nc = N // RET_CHUNK
    qc = q.astype(F32).reshape(B, nc, RET_CHUNK, H, dk)
    kc = k.astype(F32).reshape(B, nc, RET_CHUNK, H, dk)
    vc = v.astype(F32).reshape(B, nc, RET_CHUNK, H, dv)
    i = jnp.arange(RET_CHUNK)
    diff = i[:, None] - i[None, :]
    mask = (diff >= 0) if inclusive else (diff > 0)
    dec = jnp.where(mask[None], jnp.exp(jnp.where(mask, diff, 0).astype(F32)[None] * lg[:, None, None]), 0.0)
    inner = jnp.einsum('bcihd,bcjhd->bchij', qc, kc) * dec[None, None]
    o_inner = jnp.einsum('bchij,bcjhe->bcihe', inner, vc)
    fi = i.astype(F32)
    zeta = jnp.exp((RET_CHUNK - 1 - fi)[:, None] * lg[None, :])
    u = jnp.einsum('bcjhd,jh,bcjhe->bchde', kc, zeta, vc)
    g_chunk = jnp.exp(RET_CHUNK * lg)[None, :, None, None]

    def step(s, u_c):
        return g_chunk * s + u_c, s

    _, s_prev = lax.scan(step, s0.astype(F32), jnp.moveaxis(u, 1, 0))
    s_prev = jnp.moveaxis(s_prev, 0, 1)
    xi = jnp.exp((fi + 1.0)[:, None] * lg[None, :])
    o_cross = jnp.einsum('bcihd,ih,bchde->bcihe', qc, xi, s_prev)
    return (o_inner + o_cross).reshape(B, N, H, dv)


def context_states(k, v, lg_f, lg_b):
    L = k.shape[1]
    m = jnp.arange(L, dtype=F32)
    w_f = jnp.exp((L - 1 - m)[:, None] * lg_f[None, :])
    w_b = jnp.exp(m[:, None] * lg_b[None, :])
    kf, vf = k.astype(F32), v.astype(F32)
    s_f = jnp.einsum('blhd,lh,blhe->bhde', kf, w_f, vf)
    s_b = jnp.einsum('blhd,lh,blhe->bhde', kf, w_b, vf)
    return s_f, s_b


def bidir_retention(q, k, v, lg_f, lg_b, s_f, s_b):
    flip = lambda t: jnp.flip(t, axis=1)
    o_f = retention_chunked(q, k, v, lg_f, s_f, True)
    o_b = flip(retention_chunked(flip(q), flip(k), flip(v), lg_b, s_b, False))
    return o_f + o_b


def retention_out(o, g):
    B, T = o.shape[0], o.shape[1]
    mu = jnp.mean(o, axis=-1, keepdims=True)
    var = jnp.mean(jnp.square(o - mu), axis=-1, keepdims=True)
    on = ((o - mu) * lax.rsqrt(var + GN_EPS)).reshape(B, T, RET_WIDTH)
    return (jax.nn.silu(g.astype(F32)) * on).astype(g.dtype)


def token_mixer(hc, hl, w_in, sink, dec_f, dec_b, w_ba, w_bf, w_br, w_out, row_cs, col_cs, ret_cs, need_ctx):
    B, L, _ = hc.shape
    h = jnp.concatenate([hc, hl], axis=1)
    T = h.shape[1]
    proj = h @ w_in
    qa, ka, va, qr, kr, vr, gr, fu, gates = jnp.split(proj, split_points(), axis=-1)
    qa = qa.reshape(B, T, ATTN_HEADS, HEAD_DIM)
    ka = ka.reshape(B, T, ATTN_KV_HEADS, HEAD_DIM)
    va = va.reshape(B, T, ATTN_KV_HEADS, HEAD_DIM)
    qr = qr.reshape(B, T, RET_HEADS, RET_DK)
    kr = kr.reshape(B, T, RET_HEADS, RET_DK) * (RET_DK ** -0.5)
    vr = vr.reshape(B, T, RET_HEADS, RET_DV)
    oa_l = window_attention(axial_rope(qa[:, L:], row_cs, col_cs), axial_rope(ka[:, L:], row_cs, col_cs),
                            va[:, L:], ka[:, :L], va[:, :L], sink)
    of_l = fourier_mix(fu[:, L:])
    lg_f = jax.nn.log_sigmoid(dec_f.astype(F32))
    lg_b = jax.nn.log_sigmoid(dec_b.astype(F32))
    s_f, s_b = context_states(kr[:, :L], vr[:, :L], lg_f, lg_b)
    or_l = bidir_retention(rotate(qr[:, L:], *ret_cs), rotate(kr[:, L:], *ret_cs), vr[:, L:], lg_f, lg_b, s_f, s_b)
    if need_ctx:
        zero = jnp.zeros_like(s_f)
        oa = jnp.concatenate([context_attention(qa[:, :L], ka[:, :L], va[:, :L], sink), oa_l], axis=1)
        of = jnp.concatenate([fourier_mix(fu[:, :L]), of_l], axis=1)
        orr = jnp.concatenate([bidir_retention(qr[:, :L], kr[:, :L], vr[:, :L], lg_f, lg_b, zero, zero), or_l], axis=1)
        g_r, g_m = gr, gates
    else:
        oa, of, orr = oa_l, of_l, or_l
        g_r, g_m = gr[:, L:], gates[:, L:]
    ret = retention_out(orr, g_r)
    ga, gf, gt = jnp.split(jax.nn.sigmoid(g_m), 3, axis=-1)
    y = (ga * (oa @ w_ba) + gf * (of @ w_bf) + gt * (ret @ w_br)) @ w_out
    if need_ctx:
        return y[:, :L], y[:, L:]
    return None, y


def expert_ffn(xb, e, w_g, w_u, w_d):
    return (jax.nn.silu(xb @ w_g[e]) * (xb @ w_u[e])) @ w_d[e]


def hier_moe(h, w_rg, b_rg, w_re, b_re, w_g, w_u, w_d):
    T, D = h.shape
    hf = h.astype(F32)
    lg_grp = hf @ w_rg.astype(F32) + b_rg.astype(F32)
    p_grp = jax.nn.softmax(lg_grp, axis=-1)
    grp = jnp.argmax(lg_grp, axis=-1)
    pg = jnp.take_along_axis(p_grp, grp[:, None], axis=1)
    lg_e = (hf @ w_re.astype(F32) + b_re.astype(F32)).reshape(T, N_GROUPS, EXPERTS_PER_GROUP)
    lg_in = jnp.take_along_axis(lg_e, grp[:, None, None], axis=1)[:, 0]
    top_v, top_i = lax.top_k(lg_in, TOP_K)
    w = pg * jax.nn.softmax(top_v, axis=-1)
    flat_e = (grp[:, None] * EXPERTS_PER_GROUP + top_i).reshape(-1).astype(jnp.int32)
    flat_w = w.reshape(-1)
    A = T * TOP_K
    flat_tok = jnp.repeat(jnp.arange(T, dtype=jnp.int32), TOP_K)
    order = jnp.argsort(flat_e)
    se, st, sw = flat_e[order], flat_tok[order], flat_w[order]
    counts = jnp.zeros((N_EXPERTS,), jnp.int32).at[flat_e].add(1)
    pcounts = (counts + MOE_BLOCK - 1) // MOE_BLOCK * MOE_BLOCK
    start = jnp.cumsum(counts) - counts
    pend = jnp.cumsum(pcounts)
    pstart = pend - pcounts
    dest = pstart[se] + jnp.arange(A, dtype=jnp.int32) - start[se]
    n_blocks = -(-A // MOE_BLOCK) + N_EXPERTS
    P = n_blocks * MOE_BLOCK
    buf_tok = jnp.full((P,), T, jnp.int32).at[dest].set(st)
    buf_w = jnp.zeros((P,), F32).at[dest].set(sw)
    blk_e = jnp.minimum(jnp.searchsorted(pend, jnp.arange(n_blocks, dtype=jnp.int32) * MOE_BLOCK, side='right'), N_EXPERTS - 1)
    h_pad = jnp.concatenate([h, jnp.zeros((1, D), h.dtype)], axis=0)
    xb = h_pad[buf_tok].reshape(n_blocks, MOE_BLOCK, D)
    y = lax.map(lambda a: expert_ffn(a[0], a[1], w_g, w_u, w_d), (xb, blk_e))
    out = jnp.zeros((T + 1, D), F32).at[buf_tok].add(y.reshape(P, D).astype(F32) * buf_w[:, None])
    return out[:T].astype(h.dtype)


def setup_inputs(seed: int = 0) -> dict:
    key = jax.random.key(seed)
    ks = jax.random.split(key, 24)
    D = D_MODEL
    nrm = lambda k, shape, s: jax.random.normal(k, shape, F32) * s
    base_logit = jnp.asarray(np.log(2.0 ** (5 + np.arange(RET_HEADS)) - 1.0).astype(np.float32))
    return {
        'x': nrm(ks[0], (BATCH, SEQ, D), 1.0),
        'c': nrm(ks[1], (BATCH, D), 1.0),
        'ctx': nrm(ks[2], (BATCH, CTX_LEN, D), 1.0),
        'c_ctx': nrm(ks[3], (D,), 1.0),
        'norm_mix': 1.0 + nrm(ks[4], (DEPTH, D), 0.05),
        'norm_ffn': 1.0 + nrm(ks[5], (DEPTH, D), 0.05),
        'w_ada': nrm(ks[6], (DEPTH, D, 6 * D), 0.5 * D ** -0.5),
        'b_ada': nrm(ks[7], (DEPTH, 6 * D), 0.02),
        'w_in': nrm(ks[8], (DEPTH, D, IN_COLS), D ** -0.5),
        'attn_sink': nrm(ks[9], (DEPTH, ATTN_HEADS), 0.5),
        'ret_decay_fwd': base_logit[None, :] + nrm(ks[10], (DEPTH, RET_HEADS), 0.1),
        'ret_decay_bwd': base_logit[None, :] + nrm(ks[11], (DEPTH, RET_HEADS), 0.1),
        'w_branch_attn': nrm(ks[12], (DEPTH, ATTN_WIDTH, D), ATTN_WIDTH ** -0.5),
        'w_branch_fourier': nrm(ks[13], (DEPTH, FOURIER_WIDTH, D), FOURIER_WIDTH ** -0.5),
        'w_branch_ret': nrm(ks[14], (DEPTH, RET_WIDTH, D), RET_WIDTH ** -0.5),
        'w_out': nrm(ks[15], (DEPTH, D, D), D ** -0.5),
        'w_router_group': nrm(ks[16], (DEPTH, D, N_GROUPS), D ** -0.5),
        'b_router_group': nrm(ks[17], (DEPTH, N_GROUPS), 0.01),
        'w_router_expert': nrm(ks[18], (DEPTH, D, N_EXPERTS), D ** -0.5),
        'b_router_expert': nrm(ks[19], (DEPTH, N_EXPERTS), 0.01),
        'w_exp_gate': nrm(ks[20], (DEPTH, N_EXPERTS, D, EXPERT_HIDDEN), D ** -0.5),
        'w_exp_up': nrm(ks[21], (DEPTH, N_EXPERTS, D, EXPERT_HIDDEN), D ** -0.5),
        'w_exp_down': nrm(ks[22], (DEPTH, N_EXPERTS, EXPERT_HIDDEN, D), EXPERT_HIDDEN ** -0.5),
        'norm_final': 1.0 + nrm(ks[23], (D,), 0.05),
    }


def reference(x, c, ctx, c_ctx, norm_mix, norm_ffn, w_ada, b_ada, w_in, attn_sink, ret_decay_fwd, ret_decay_bwd,
              w_branch_attn, w_branch_fourier, w_branch_ret, w_out, w_router_group, b_router_group,
              w_router_expert, b_router_expert, w_exp_gate, w_exp_up, w_exp_down, norm_final):
    B, N, D = x.shape
    L = ctx.shape[1]
    ROWS = N // GRID_W
    row = jnp.repeat(jnp.arange(ROWS, dtype=F32), GRID_W)
    col = jnp.tile(jnp.arange(GRID_W, dtype=F32), ROWS)
    row_cs = rope_table(row, HEAD_DIM // 4)
    col_cs = rope_table(col, HEAD_DIM // 4)
    ret_cs = rope_table(jnp.arange(N, dtype=F32), RET_DK // 2)
    xc, xl = ctx, x
    for l in range(DEPTH):
        need_ctx = l < DEPTH - 1
        mod_l = (jax.nn.silu(c) @ w_ada[l] + b_ada[l])[:, None, :]
        mod_c = (jax.nn.silu(c_ctx) @ w_ada[l] + b_ada[l])[None, None, :]
        sh1_l, sc1_l, g1_l, sh2_l, sc2_l, g2_l = jnp.split(mod_l, 6, axis=-1)
        sh1_c, sc1_c, g1_c, sh2_c, sc2_c, g2_c = jnp.split(mod_c, 6, axis=-1)
        hl = modulate(rmsnorm(xl, norm_mix[l]), sh1_l, sc1_l)
        hc = modulate(rmsnorm(xc, norm_mix[l]), sh1_c, sc1_c)
        yc, yl = token_mixer(hc, hl, w_in[l], attn_sink[l], ret_decay_fwd[l], ret_decay_bwd[l],
                             w_branch_attn[l], w_branch_fourier[l], w_branch_ret[l], w_out[l],
                             row_cs, col_cs, ret_cs, need_ctx)
        xl = xl + g1_l * yl
        hl2 = modulate(rmsnorm(xl, norm_ffn[l]), sh2_l, sc2_l)
        moe_args = (w_router_group[l], b_router_group[l], w_router_expert[l], b_router_expert[l],
                    w_exp_gate[l], w_exp_up[l], w_exp_down[l])
        if need_ctx:
            xc = xc + g1_c * yc
            hc2 = modulate(rmsnorm(xc, norm_ffn[l]), sh2_c, sc2_c)
            h2 = jnp.concatenate([hc2, hl2], axis=1)
            f = hier_moe(h2.reshape(-1, D), *moe_args).reshape(B, L + N, D)
            xc = xc + g2_c * f[:, :L]
            xl = xl + g2_l * f[:, L:]
        else:
            f = hier_moe(hl2.reshape(-1, D), *moe_args).reshape(B, N, D)
            xl = xl + g2_l * f
    return rmsnorm(xl, norm_final)
```

```python
import contextlib, os
import numpy as np
import ml_dtypes
import concourse.bass as bass
import concourse.mybir as mybir
from concourse.bass_utils import run_bass_kernel_spmd

F32 = mybir.dt.float32
BF16 = mybir.dt.bfloat16
AF = mybir.ActivationFunctionType
ALU = mybir.AluOpType
AX = mybir.AxisListType

ARENA_BASE = 20736
SBUF_BYTES = 229376 - ARENA_BASE - 128
SAME_ENGINE_SYNC = bool(int(os.environ.get("SES", "1")))
NDMA_SEM = 12
EPOCH = 30000


def _dsize(dt):
    return 2 if dt == BF16 else 4


class KB:
    def __init__(self, nc):
        self.nc = nc
        self.es = contextlib.ExitStack()
        self.eng = {'pe': nc.tensor, 'act': nc.scalar, 'dve': nc.vector, 'pool': nc.gpsimd, 'sp': nc.sync}
        self.cnt = {e: 0 for e in self.eng}
        self.sems = {e: [] for e in self.eng}
        self.seen = {e: {} for e in self.eng}
        self.res = {}
        self.dma_sems = {}
        self.dma_i = {}
        self.bot = ARENA_BASE
        self.top = ARENA_BASE + SBUF_BYTES
        self.nps = 0
        self.n_sem = 0
        self.peak = 0

    def __enter__(self):
        self.es.__enter__()
        return self

    def __exit__(self, *a):
        return self.es.__exit__(*a)

    def sb(self, name, shape, dtype, top=False):
        n = 1
        for s in shape[1:]:
            n *= s
        nbytes = (n * _dsize(dtype) + 63) // 64 * 64
        if top:
            self.top -= nbytes
            off = self.top
        else:
            off = self.bot
            self.bot += nbytes
        assert self.bot <= self.top, f"SBUF overflow allocating {name}: bot={self.bot} top={self.top}"
        self.peak = max(self.peak, self.bot + SBUF_BYTES - self.top)
        self.nps += 1
        return self.nc.alloc_sbuf_tensor_at(f"{name}_{self.nps}", list(shape), dtype, offset=off)

    def mark(self):
        return (self.bot, self.top)

    def release(self, m):
        self.barrier()
        self.bot, self.top = m

    def ps(self, name, shape, dtype=F32):
        return self.nc.alloc_psum_tensor(name, list(shape), dtype)

    def _newsem(self, name):
        self.n_sem += 1
        return self.es.enter_context(self.nc.semaphore(f"{name}_{self.n_sem}"))

    def _tick(self, e):
        c = self.cnt[e]
        ep, v = divmod(c, EPOCH)
        while len(self.sems[e]) <= ep:
            self.sems[e].append(self._newsem(f"s_{e}"))
        self.cnt[e] = c + 1
        return (self.sems[e][ep], v + 1, e)

    def _wait(self, e, tick):
        sem, val, owner = tick
        if owner == e and (e == 'pe' or not SAME_ENGINE_SYNC):
            return
        k = sem.num
        if self.seen[e].get(k, 0) >= val:
            return
        self.eng[e].wait_ge(sem, val)
        self.seen[e][k] = val

    def _deps(self, e, r, w):
        ticks = []
        for k in r:
            rec = self.res.get(k)
            if rec and rec[0]:
                ticks.append(rec[0])
        for k in w:
            rec = self.res.get(k)
            if rec:
                if rec[0]:
                    ticks.append(rec[0])
                ticks.extend(rec[1])
        for t in ticks:
            self._wait(e, t)

    def _record(self, tick, r, w):
        for k in r:
            rec = self.res.setdefault(k, [None, []])
            rec[1] = [t for t in rec[1] if not (t[0] is tick[0])] + [tick]
        for k in w:
            self.res[k] = [tick, []]

    def op(self, e, fn, r=(), w=()):
        self._deps(e, r, w)
        tick = self._tick(e)
        fn(self.eng[e]).then_inc(tick[0], 1)
        self._record(tick, r, w)

    def dma(self, q, out, in_, r=(), w=(), fn=None, **kw):
        self._deps(q, r, w)
        pool = self.dma_sems.setdefault(q, [])
        i = self.dma_i.get(q, 0)
        self.dma_i[q] = i + 1
        slot = i % NDMA_SEM
        if len(pool) <= slot:
            pool.append([self._newsem(f"d_{q}"), 0])
        ent = pool[slot]
        if ent[1] > 0:
            self._wait(q, (ent[0], ent[1], 'dma'))
        ent[1] += 16
        tick = (ent[0], ent[1], 'dma')
        if fn is not None:
            fn(self.eng[q]).then_inc(ent[0], 16)
        else:
            self.eng[q].dma_start(out=out, in_=in_, **kw).then_inc(ent[0], 16)
        self._record(tick, r, w)

    def all_ticks(self):
        ticks = []
        for e in self.eng:
            c = self.cnt[e]
            if c > 0:
                ep, v = divmod(c - 1, EPOCH)
                ticks.append((self.sems[e][ep], v + 1, e))
        for q, pool in self.dma_sems.items():
            for ent in pool:
                if ent[1] > 0:
                    ticks.append((ent[0], ent[1], 'dma'))
        return ticks

    def barrier(self):
        ticks = self.all_ticks()
        for e in self.eng:
            for t in ticks:
                if t[2] == e:
                    continue
                self._wait(e, t)
        self.res = {}

    def finish(self):
        ticks = self.all_ticks()
        for t in ticks:
            if t[2] != 'sp':
                self._wait('sp', t)

    def make_ident(self, ident):
        nc = self.nc
        self.op('pool', lambda e: e.memset(ident[:], 1.0), w=['ident'])
        self.op('pool', lambda e: e.affine_select(out=ident[:], in_=ident[:], pattern=[[-1, 128]],
                                                  compare_op=ALU.is_equal, fill=0.0, base=0,
                                                  channel_multiplier=1), r=['ident'], w=['ident'])

D = 1024; L = 256; N = 2048; T = 2304; NT = 18
QA, QAS, KA, KAS, VA, QR, QRS, KR, KRS, VR, GR, FU, GT = 0, 512, 1024, 1152, 1280, 1408, 1920, 2432, 2944, 3456, 3968, 4480, 4992
WEXT = 8064
CAP = int(os.environ.get('MOE_CAP', '1024'))
NS = CAP // 128
I32 = mybir.dt.int32
TB = [(0, 256), (256, 512), (768, 512), (1280, 512), (1792, 512)]


def bc(ap, free):
    return bass.AP(ap.tensor, ap.offset, [list(ap.ap[0])] + [list(f) for f in free])


def build(stop=None, nlayers=2):
    nc = bass.Bass("TRN2", target_bir_lowering=False)

    def din(name, shape, dt=F32):
        return nc.dram_tensor(name, list(shape), dt, kind="ExternalInput").ap()
    x_in = din("x", [N, D]); ctx_in = din("ctx", [L, D]); c_in = din("c", [D]); cctx_in = din("c_ctx", [D])
    norm_mix = din("norm_mix", [2, D]); norm_ffn = din("norm_ffn", [2, D])
    w_ada = din("w_ada", [2, D, 6 * D]); b_ada = din("b_ada", [2, 6 * D])
    w_ext = din("w_ext", [2, D, WEXT]); sink_in = din("attn_sink", [2, 8])
    dec_f = din("ret_decay_fwd", [2, 8]); dec_b = din("ret_decay_bwd", [2, 8])
    w_ba = din("w_branch_attn", [2, 512, D]); w_bf = din("w_branch_fourier", [2, 512, D]); w_br = din("w_branch_ret", [2, 512, D])
    w_out = din("w_out", [2, D, D]); w_rt = din("w_rt", [2, D, 36]); b_rt = din("b_rt", [2, 36])
    if stop is None or stop.startswith('moe'):
        w_eg = din("w_exp_gate", [2, 32, D, 512]); w_eu = din("w_exp_up", [2, 32, D, 512]); w_ed = din("w_exp_down", [2, 32, 512, D])
    norm_final = din("norm_final", [D])
    ropeA = din("ropeA", [2, 128, N]); ropeR = din("ropeR", [2, 128, N])
    dftN = din("dftN", [2, N, N], BF16); dft256 = din("dft256", [2, 256, 256], BF16); dftC = din("dftC", [128, 256], BF16)
    amask = din("amask", [128, 256], BF16); rconst = din("rconst", [128, 770])
    out = nc.dram_tensor("out", [N, D], F32, kind="ExternalOutput").ap()
    xbuf = nc.dram_tensor("xbuf", [T, D], F32).ap()
    mconst = din("mconst", [128, 32 + NT])
    h2d = nc.dram_tensor("h2d", [T, D], BF16).ap()
    rec_d = nc.dram_tensor("rec_d", [32 * CAP, 4], F32).ap()
    AB = nc.dram_tensor("AB", [2 * T, D], F32).ap()
    modbuf = nc.dram_tensor("modbuf", [2, 2, 6 * D], F32).ap()
    dbg = {}
    if stop is not None:
        dbg['d_x'] = nc.dram_tensor("d_x", [T, D], F32, kind="ExternalOutput").ap()
        dbg['d_mod'] = nc.dram_tensor("d_mod", [2, 6 * D], F32, kind="ExternalOutput").ap()
        dbg['d_hT'] = nc.dram_tensor("d_hT", [128, 8, T], BF16, kind="ExternalOutput").ap()
        dbg['d_oT'] = nc.dram_tensor("d_oT", [128, 4, T], BF16, kind="ExternalOutput").ap()
        dbg['d_mT'] = nc.dram_tensor("d_mT", [128, 8, T], BF16, kind="ExternalOutput").ap()

    kb = KB(nc)
    with kb:
        PSA = kb.ps("psa", [128, 7, 512], F32)
        PST = kb.ps("pst", [128, 8, 128], BF16)
        ident = kb.sb("ident", [128, 128], BF16)
        ident32 = kb.sb("ident32", [128, 128], F32)
        kb.make_ident(ident)
        kb.op('pool', lambda e: e.memset(ident32[:], 1.0), w=['ident32'])
        kb.op('pool', lambda e: e.affine_select(out=ident32[:], in_=ident32[:], pattern=[[-1, 128]], compare_op=ALU.is_equal,
                                                fill=0.0, base=0, channel_multiplier=1), r=['ident32'], w=['ident32'])
        cst = kb.sb("cst", [128, 4], F32)
        kb.op('pool', lambda e: e.memset(cst[:, 0:1], 1e-6), w=['cst'])
        kb.op('pool', lambda e: e.memset(cst[:, 1:2], 1e-5), w=['cst'])
        kb.op('pool', lambda e: e.memset(cst[:, 2:3], 1.0), w=['cst'])
        epsN = cst[:, 0:1]; epsG = cst[:, 1:2]; one1 = cst[:, 2:3]
        stg = [kb.sb(f"stg{i}", [128, 2048], F32) for i in range(2)]
        stg_i = [0]; cast_rr = [0]
        hT = kb.sb("hT", [128, 8, T], BF16)
        kb.barrier()

        def load_w(dst, dkey, src2d, K, W, engs=('pool',)):
            kk = max(1, 2048 // W)
            for k0 in range(0, K, kk):
                k1 = min(K, k0 + kk)
                i = stg_i[0] % 2; stg_i[0] += 1
                sv = stg[i][:, 0:(k1 - k0) * W].rearrange("p (k c) -> p k c", c=W)
                kb.dma('sp', sv, src2d[k0 * 128:k1 * 128, :].rearrange("(k p) c -> p k c", p=128), w=[('stg', i)])
                en = engs[cast_rr[0] % len(engs)]; cast_rr[0] += 1
                if en == 'act':
                    kb.op('act', lambda e: e.copy(out=dst[:, k0:k1, :], in_=sv), r=[('stg', i)], w=[dkey])
                else:
                    kb.op(en, lambda e: e.tensor_copy(out=dst[:, k0:k1, :], in_=sv), r=[('stg', i)], w=[dkey])

        def proj_fm(wt, wkey, c0, tok0, ntok, bank):
            for k in range(8):
                kb.op('pe', lambda e: e.matmul(PSA[:, bank, 0:ntok], lhsT=wt[:, k, c0:c0 + 128], rhs=hT[:, k, tok0:tok0 + ntok],
                                               start=(k == 0), stop=(k == 7)), r=[wkey, 'hT'], w=[('ps', bank)])

        def dump(name, src, r):
            kb.dma('sp', dbg[name], src, r=r, w=[name])

        def xdump(name, t, shape, dt):
            if stop is None or not os.environ.get('XDUMP'):
                return
            kb.barrier()
            d_ = nc.dram_tensor("x_" + name, list(shape), dt, kind="ExternalOutput").ap()
            kb.dma('sp', d_, t[:], w=["x_" + name])

        def phase_ada(l):
            m = kb.mark()
            cT = kb.sb('cT', [128, 8, 2], F32); cTb = kb.sb('cTb', [128, 8, 2], BF16)
            bsb = kb.sb('bsb', [2, 6 * D], F32); modsb = kb.sb('modsb', [2, 6 * D], F32)
            wa = [kb.sb(f'wa{i}', [128, 8, 512], BF16) for i in range(2)]
            kb.dma('sp', cT[:, :, 0], cctx_in.rearrange("(k p) -> p k", p=128), w=['cT'], allow_slow_non_contiguous=True)
            kb.dma('sp', cT[:, :, 1], c_in.rearrange("(k p) -> p k", p=128), w=['cT'], allow_slow_non_contiguous=True)
            kb.dma('sp', bsb[:], b_ada[l].partition_broadcast(2), w=['bsb'])
            kb.op('act', lambda e: e.activation(out=cTb[:], in_=cT[:], func=AF.Silu), r=['cT'], w=['cTb'])
            for nb in range(12):
                i = nb % 2
                load_w(wa[i], ('wa', i), w_ada[l][:, nb * 512:(nb + 1) * 512], 8, 512, engs=('pool', 'act'))
                for k in range(8):
                    kb.op('pe', lambda e: e.matmul(PSA[0:2, i, :], lhsT=cTb[:, k, :], rhs=wa[i][:, k, :], start=(k == 0), stop=(k == 7)),
                          r=['cTb', ('wa', i)], w=[('ps', i)])
                kb.op('dve', lambda e: e.tensor_tensor(out=modsb[:, nb * 512:(nb + 1) * 512], in0=PSA[0:2, i, :],
                                                       in1=bsb[:, nb * 512:(nb + 1) * 512], op=ALU.add), r=[('ps', i), 'bsb'], w=['modsb'])
            kb.dma('sp', modbuf[l], modsb[:], r=['modsb'], w=[('mod', l)])
            if stop == f'ada{l}':
                dump('d_mod', modsb[:], ['modsb'])
            kb.release(m)
            kb.res[('mod', l)] = None
            kb.res.pop(('mod', l))

        def phase_norm(l, which, logits=None, sparse=False):
            m = kb.mark()
            nw = norm_mix if which == 1 else norm_ffn
            si, ci = (0, 1) if which == 1 else (3, 4)
            tiles = range(NT) if (which == 1 or l == 0) else range(2, NT)
            nwb = kb.sb('nwb', [128, D], F32)
            kb.dma('sp', nwb[:], nw[l].partition_broadcast(128), w=['nwb'])
            G = []; S = []
            for row in (0, 1):
                g_ = kb.sb(f'G{row}', [128, D], F32); s_ = kb.sb(f'S{row}', [128, D], F32)
                kb.dma('sp', g_[:], modbuf[l, row, ci * D:(ci + 1) * D].partition_broadcast(128), w=[f'G{row}'])
                kb.dma('sp', s_[:], modbuf[l, row, si * D:(si + 1) * D].partition_broadcast(128), w=[f'S{row}'])
                kb.op('dve', lambda e: e.scalar_tensor_tensor(out=g_[:], in0=g_[:], scalar=1.0, in1=nwb[:], op0=ALU.add, op1=ALU.mult),
                      r=[f'G{row}', 'nwb'], w=[f'G{row}'])
                G.append(g_); S.append(s_)
            xt = [kb.sb(f'xt{i}', [128, D], F32) for i in range(2)]
            tmp = [kb.sb(f'tmp{i}', [128, D], F32) for i in range(2)]
            hb = [kb.sb(f'hb{i}', [128, D], BF16 if which == 1 else F32) for i in range(2)]
            ssq = [kb.sb(f'ssq{i}', [128, 1], F32) for i in range(2)]
            junk = kb.sb('junk', [128, D], F32)
            if which == 2:
                hbb = [kb.sb(f'hbb{i}', [128, D], BF16) for i in range(2)]
                h32 = [kb.sb(f'h32{i}', [128, 8, 128], F32) for i in range(2)]
                wrt32 = kb.sb('wrt32', [128, 8, 36], F32); brt = kb.sb('brt', [128, 36], F32)
                kb.dma('sp', wrt32[:], w_rt[l].rearrange("(k p) c -> p k c", p=128), w=['wrt32'])
                kb.dma('sp', brt[:], b_rt[l].partition_broadcast(128), w=['brt'])
            for t in tiles:
                i = t % 2; row = 0 if t < 2 else 1
                kb.dma('sp', xt[i][:], xbuf[t * 128:(t + 1) * 128, :], r=[('x', t)], w=[('xt', i)])
                kb.op('act', lambda e: e.activation(out=junk[:], in_=xt[i][:], func=AF.Square, accum_out=ssq[i][:]),
                      r=[('xt', i)], w=['junk', ('ssq', i)])
                kb.op('act', lambda e: e.activation(out=ssq[i][:], in_=ssq[i][:], func=AF.Sqrt, scale=1.0 / D, bias=epsN),
                      r=[('ssq', i)], w=[('ssq', i)])
                kb.op('dve', lambda e: e.reciprocal(out=ssq[i][:], in_=ssq[i][:]), r=[('ssq', i)], w=[('ssq', i)])
                kb.op('dve', lambda e: e.scalar_tensor_tensor(out=tmp[i][:], in0=xt[i][:], scalar=ssq[i][:, 0:1], in1=G[row][:],
                                                              op0=ALU.mult, op1=ALU.mult), r=[('xt', i), ('ssq', i), f'G{row}'], w=[('tmp', i)])
                kb.op('pool', lambda e: e.tensor_tensor(out=hb[i][:], in0=tmp[i][:], in1=S[row][:], op=ALU.add),
                      r=[('tmp', i), f'S{row}'], w=[('hb', i)])
                if which == 1:
                    for k in range(8):
                        kb.op('pe', lambda e: e.transpose(out=PST[:, k, :], in_=hb[i][:, k * 128:(k + 1) * 128], identity=ident[:]),
                              r=[('hb', i), 'ident'], w=['pst'])
                    kb.op('act', lambda e: e.copy(out=hT[:, :, t * 128:(t + 1) * 128], in_=PST[:]), r=['pst'], w=['hT'])
                else:
                    for k in range(8):
                        kb.op('pe', lambda e: e.matmul(PSA[:, 5 + k // 4, (k % 4) * 128:(k % 4 + 1) * 128], lhsT=hb[i][:, k * 128:(k + 1) * 128],
                                                       rhs=ident32[:], start=True, stop=True), r=[('hb', i), 'ident32'], w=[('ps', 5 + k // 4)])
                    pv = PSA[:, 5:7, :].rearrange("p a (b c) -> p (a b) c", c=128)
                    kb.op('dve', lambda e: e.tensor_copy(out=h32[i][:], in_=pv), r=[('ps', 5), ('ps', 6)], w=[('h32', i)])
                    if sparse:
                        kb.op('act', lambda e: e.copy(out=hbb[i][:], in_=hb[i][:]), r=[('hb', i)], w=[('hbb', i)])
                        kb.dma('sp', h2d[t * 128:(t + 1) * 128, :], hbb[i][:], r=[('hbb', i)], w=[('h2d', t)])
                    else:
                        kb.op('act', lambda e: e.copy(out=hT[:, :, t * 128:(t + 1) * 128], in_=h32[i][:]), r=[('h32', i)], w=['hT'])
                    for k in range(8):
                        kb.op('pe', lambda e: e.matmul(PSA[:, 4, 0:36], lhsT=h32[i][:, k, :], rhs=wrt32[:, k, :], start=(k == 0), stop=(k == 7)),
                              r=[('h32', i), 'wrt32'], w=[('ps', 4)])
                    kb.op('dve', lambda e: e.tensor_tensor(out=logits[:, t, :], in0=PSA[:, 4, 0:36], in1=brt[:], op=ALU.add),
                          r=[('ps', 4), 'brt'], w=['logits'])
            kb.release(m)

        def merge(l, gate_off, wb_dram, oT, okey, mT, first):
            m = kb.mark()
            wg = [kb.sb(f'mwg{i}', [128, 8, 128], BF16) for i in range(2)]
            wb = [kb.sb(f'mwb{i}', [128, 4, 128], BF16) for i in range(2)]
            sig = [kb.sb(f'sig{i}', [128, 512], F32) for i in range(2)]
            mtmp = [kb.sb(f'mtmp{i}', [128, 512], F32) for i in range(2)]
            it = 0
            for fc in range(8):
                j = fc % 2
                load_w(wg[j], ('mwg', j), w_ext[l][:, GT + gate_off + fc * 128: GT + gate_off + (fc + 1) * 128], 8, 128)
                load_w(wb[j], ('mwb', j), wb_dram[l][:, fc * 128:(fc + 1) * 128], 4, 128)
                for (s0, n) in (TB if l == 0 else TB[1:]):
                    i = it % 2; it += 1
                    proj_fm(wg[j], ('mwg', j), 0, s0, n, i)
                    kb.op('act', lambda e: e.activation(out=sig[i][:, 0:n], in_=PSA[:, i, 0:n], func=AF.Sigmoid), r=[('ps', i)], w=[('sig', i)])
                    for k in range(4):
                        kb.op('pe', lambda e: e.matmul(PSA[:, 2 + i, 0:n], lhsT=wb[j][:, k, :], rhs=oT[:, k, s0:s0 + n], start=(k == 0), stop=(k == 3)),
                              r=[('mwb', j), okey], w=[('ps', 2 + i)])
                    if first:
                        kb.op('dve', lambda e: e.tensor_tensor(out=mT[:, fc, s0:s0 + n], in0=sig[i][:, 0:n], in1=PSA[:, 2 + i, 0:n], op=ALU.mult),
                              r=[('sig', i), ('ps', 2 + i)], w=['mT'])
                    else:
                        kb.op('dve', lambda e: e.tensor_tensor(out=mtmp[i][:, 0:n], in0=sig[i][:, 0:n], in1=PSA[:, 2 + i, 0:n], op=ALU.mult),
                              r=[('sig', i), ('ps', 2 + i)], w=[('mtmp', i)])
                        kb.op('pool', lambda e: e.tensor_tensor(out=mT[:, fc, s0:s0 + n], in0=mT[:, fc, s0:s0 + n], in1=mtmp[i][:, 0:n], op=ALU.add),
                              r=[('mtmp', i), 'mT'], w=['mT'])
            kb.release(m)

        def rope_evac(bA, bB, dst, dkey, tC, tS, n, t1, t2, j):
            kb.op('dve', lambda e: e.tensor_tensor(out=t1[:, 0:n], in0=PSA[:, bA, 0:n], in1=tC[:, 0:n], op=ALU.mult), r=[('ps', bA), ('rc', j)], w=[('t1', j)])
            kb.op('dve', lambda e: e.tensor_tensor(out=t2[:, 0:n], in0=PSA[:, bB, 0:n], in1=tS[:, 0:n], op=ALU.mult), r=[('ps', bB), ('rs', j)], w=[('t2', j)])
            kb.op('pool', lambda e: e.tensor_tensor(out=dst, in0=t1[:, 0:n], in1=t2[:, 0:n], op=ALU.add), r=[('t1', j), ('t2', j)], w=[dkey])

        def phase_ret(l, retT):
            need_ctx = (l == 0)
            m = kb.mark()
            RC = kb.sb('RC', [128, 770], F32)
            kb.dma('sp', RC[:], rconst[:, :], w=['RC'])
            dpos = RC[:, 0:128]; dneg = RC[:, 128:256]; mge = RC[:, 256:384]; mlt = RC[:, 384:512]
            io1 = RC[:, 512:640]; iob = RC[:, 640:768]; pc127 = RC[:, 768:769]; pcol = RC[:, 769:770]
            lg = kb.sb('lg', [128, 16], F32)
            kb.dma('sp', lg[:, 0:8], dec_f[l].partition_broadcast(128), w=['lg'])
            kb.dma('sp', lg[:, 8:16], dec_b[l].partition_broadcast(128), w=['lg'])
            kb.op('act', lambda e: e.activation(out=lg[:], in_=lg[:], func=AF.Exp, scale=-1.0), r=['lg'], w=['lg'])
            kb.op('act', lambda e: e.activation(out=lg[:], in_=lg[:], func=AF.Ln, bias=one1), r=['lg', 'cst'], w=['lg'])
            kb.op('dve', lambda e: e.tensor_scalar(out=lg[:], in0=lg[:], scalar1=-1.0, scalar2=None, op0=ALU.mult), r=['lg'], w=['lg'])
            lgp = kb.sb('lgp', [128, 8], F32)
            for r in range(4):
                for hh in range(2):
                    for d in range(2):
                        kb.op('pool', lambda e: e.tensor_copy(out=lgp[hh * 64:(hh + 1) * 64, d * 4 + r:d * 4 + r + 1],
                                                              in_=lg[hh * 64:(hh + 1) * 64, d * 8 + 2 * r + hh:d * 8 + 2 * r + hh + 1]), r=['lg'], w=['lgp'])
            g128 = kb.sb('g128', [128, 8], F32)
            kb.op('act', lambda e: e.activation(out=g128[:], in_=lgp[:], func=AF.Exp, scale=128.0), r=['lgp'], w=['g128'])
            Z = kb.sb('Z', [128, 16], F32)
            kb.op('act', lambda e: e.activation(out=Z[:, 0:8], in_=lg[:, 0:8], func=AF.Exp, scale=pc127), r=['lg', 'RC'], w=['Z'])
            kb.op('act', lambda e: e.activation(out=Z[:, 8:16], in_=lg[:, 8:16], func=AF.Exp, scale=pcol), r=['lg', 'RC'], w=['Z'])
            kb.op('dve', lambda e: e.tensor_scalar(out=Z[:], in0=Z[:], scalar1=0.125, scalar2=None, op0=ALU.mult), r=['Z'], w=['Z'])
            DecT = kb.sb('DecT', [128, 8, 128], F32)
            d1 = kb.sb('d1', [128, 128], F32); d2 = kb.sb('d2', [128, 128], F32)
            for h in range(8):
                kb.op('act', lambda e: e.activation(out=d1[:], in_=dpos, func=AF.Exp, scale=lg[:, h:h + 1]), r=['lg', 'RC'], w=['d1'])
                kb.op('pool', lambda e: e.tensor_tensor(out=d1[:], in0=d1[:], in1=mge, op=ALU.mult), r=['d1', 'RC'], w=['d1'])
                kb.op('act', lambda e: e.activation(out=d2[:], in_=dneg, func=AF.Exp, scale=lg[:, 8 + h:9 + h]), r=['lg', 'RC'], w=['d2'])
                kb.op('pool', lambda e: e.tensor_tensor(out=d2[:], in0=d2[:], in1=mlt, op=ALU.mult), r=['d2', 'RC'], w=['d2'])
                kb.op('pool', lambda e: e.tensor_tensor(out=d1[:], in0=d1[:], in1=d2[:], op=ALU.add), r=['d1', 'd2'], w=['d1'])
                kb.op('dve', lambda e: e.tensor_scalar(out=DecT[:, h, :], in0=d1[:], scalar1=0.125, scalar2=None, op0=ALU.mult), r=['d1'], w=['DecT'])
            X = kb.sb('X', [128, 8, 128], F32)
            for r in range(4):
                kb.op('act', lambda e: e.activation(out=X[:, r, :], in_=io1, func=AF.Exp, scale=lgp[:, r:r + 1]), r=['lgp', 'RC'], w=['X'])
                kb.op('act', lambda e: e.activation(out=X[:, 4 + r, :], in_=iob, func=AF.Exp, scale=lgp[:, 4 + r:5 + r]), r=['lgp', 'RC'], w=['X'])
            if os.environ.get('RET_CUT') == '1':
                kb.release(m); return
            qT = kb.sb('qT', [128, T], BF16); kT = kb.sb('kT', [128, T], BF16)
            ktok = kb.sb('ktok', [128, NT, 128], BF16); vtok = kb.sb('vtok', [128, NT, 128], BF16)
            vf = kb.sb('vf', [128, NT, 128], BF16); vb = kb.sb('vb', [128, NT, 128], BF16)
            sg = kb.sb('sg', [128, NT, 128], F32)
            Sf = kb.sb('Sf', [128, NT, 128], BF16); Rb = kb.sb('Rb', [128, NT, 128], BF16)
            Srun = kb.sb('Srun', [128, 128], F32); Rrun = kb.sb('Rrun', [128, 128], F32)
            ws = {nm: kb.sb('w' + nm, [128, 8, 128], BF16) for nm in ('q', 'qs', 'k', 'ks', 'v', 'g')}
            rc = [kb.sb(f'rc{i}', [128, 512], F32) for i in range(2)]; rs = [kb.sb(f'rs{i}', [128, 512], F32) for i in range(2)]
            t1 = [kb.sb(f't1{i}', [128, 512], F32) for i in range(2)]; t2 = [kb.sb(f't2{i}', [128, 512], F32) for i in range(2)]
            AT = [kb.sb(f'AT{i}', [128, 2, 128], BF16) for i in range(2)]
            qxf = [kb.sb(f'qxf{i}', [128, 128], BF16) for i in range(2)]; qxb = [kb.sb(f'qxb{i}', [128, 128], BF16) for i in range(2)]
            oc = [kb.sb(f'oc{i}', [128, 128], F32) for i in range(2)]; sq = [kb.sb(f'sq{i}', [128, 128], F32) for i in range(2)]
            st = [kb.sb(f'st{i}', [128, 4], F32) for i in range(2)]
            rtok = [kb.sb(f'rtok{i}', [128, 128], BF16) for i in range(2)]
            for r in range(4):
                for nm, c0 in (('q', QR), ('qs', QRS), ('k', KR), ('ks', KRS), ('v', VR), ('g', GR)):
                    load_w(ws[nm], 'w' + nm, w_ext[l][:, c0 + r * 128:c0 + (r + 1) * 128], 8, 128)
                for bi, (s0, n) in enumerate(TB):
                    if s0 == 0:
                        proj_fm(ws['q'], 'wq', 0, s0, n, 0)
                        kb.op('act', lambda e: e.copy(out=qT[:, s0:s0 + n], in_=PSA[:, 0, 0:n]), r=[('ps', 0)], w=['qT'])
                        proj_fm(ws['k'], 'wk', 0, s0, n, 1)
                        kb.op('act', lambda e: e.copy(out=kT[:, s0:s0 + n], in_=PSA[:, 1, 0:n]), r=[('ps', 1)], w=['kT'])
                    else:
                        j = bi % 2; p0 = s0 - 256
                        kb.dma('sp', rc[j][:, 0:n], ropeR[0, :, p0:p0 + n], w=[('rc', j)])
                        kb.dma('sp', rs[j][:, 0:n], ropeR[1, :, p0:p0 + n], w=[('rs', j)])
                        proj_fm(ws['q'], 'wq', 0, s0, n, 0); proj_fm(ws['qs'], 'wqs', 0, s0, n, 1)
                        rope_evac(0, 1, qT[:, s0:s0 + n], 'qT', rc[j], rs[j], n, t1[j], t2[j], j)
                        proj_fm(ws['k'], 'wk', 0, s0, n, 2); proj_fm(ws['ks'], 'wks', 0, s0, n, 3)
                        rope_evac(2, 3, kT[:, s0:s0 + n], 'kT', rc[j], rs[j], n, t1[j], t2[j], j)
                    for t in range(s0 // 128, (s0 + n) // 128):
                        bank = 4 + (t % 2)
                        for k in range(8):
                            kb.op('pe', lambda e: e.matmul(PSA[:, bank, 0:128], lhsT=hT[:, k, t * 128:(t + 1) * 128], rhs=ws['v'][:, k, :],
                                                           start=(k == 0), stop=(k == 7)), r=['hT', 'wv'], w=[('ps', bank)])
                        for k in range(8):
                            kb.op('pe', lambda e: e.matmul(PSA[:, bank, 128:256], lhsT=hT[:, k, t * 128:(t + 1) * 128], rhs=ws['g'][:, k, :],
                                                           start=(k == 0), stop=(k == 7)), r=['hT', 'wg'], w=[('ps', bank)])
                        kb.op('act', lambda e: e.copy(out=vtok[:, t, :], in_=PSA[:, bank, 0:128]), r=[('ps', bank)], w=['vtok'])
                        kb.op('act', lambda e: e.activation(out=sg[:, t, :], in_=PSA[:, bank, 128:256], func=AF.Silu), r=[('ps', bank)], w=['sg'])
                if os.environ.get('RET_CUT') == '2':
                    kb.release(m); return
                for t in range(NT):
                    kb.op('pe', lambda e: e.transpose(out=PST[:, t % 8, :], in_=kT[:, t * 128:(t + 1) * 128], identity=ident[:]), r=['kT', 'ident'], w=['pst'])
                    if t % 8 == 7 or t == NT - 1:
                        n8 = t % 8 + 1; t0 = t - n8 + 1
                        kb.op('act', lambda e: e.copy(out=ktok[:, t0:t + 1, :], in_=PST[:, 0:n8, :]), r=['pst'], w=['ktok'])
                v4 = lambda a: a[:].rearrange("p c (h e) -> p c h e", h=2)
                kb.op('pool', lambda e: e.tensor_tensor(out=v4(vf), in0=v4(vtok), in1=bc(Z[:, 2 * r:2 * r + 2], [[0, NT], [1, 2], [0, 64]]), op=ALU.mult),
                      r=['vtok', 'Z'], w=['vf'])
                kb.op('pool', lambda e: e.tensor_tensor(out=v4(vb), in0=v4(vtok), in1=bc(Z[:, 8 + 2 * r:8 + 2 * r + 2], [[0, NT], [1, 2], [0, 64]]), op=ALU.mult),
                      r=['vtok', 'Z'], w=['vb'])
                if os.environ.get('RET_CUT') == '3':
                    kb.release(m); return
                kb.op('pool', lambda e: e.memset(Srun[:], 0.0), w=['Srun'])
                kb.op('pool', lambda e: e.memset(Sf[:, 0, :], 0.0), w=['Sf'])
                for c in range(NT - 1):
                    bank = 4 + c % 2
                    kb.op('pe', lambda e: e.matmul(PSA[:, bank, 0:128], lhsT=ktok[:, c, :], rhs=vf[:, c, :], start=True, stop=True),
                          r=['ktok', 'vf'], w=[('ps', bank)])
                    kb.op('dve', lambda e: e.scalar_tensor_tensor(out=Srun[:], in0=Srun[:], scalar=g128[:, r:r + 1], in1=PSA[:, bank, 0:128],
                                                                  op0=ALU.mult, op1=ALU.add), r=['Srun', 'g128', ('ps', bank)], w=['Srun'])
                    kb.op('act', lambda e: e.copy(out=Sf[:, c + 1, :], in_=Srun[:]), r=['Srun'], w=['Sf'])
                kb.op('pool', lambda e: e.memset(Rrun[:], 0.0), w=['Rrun'])
                kb.op('pool', lambda e: e.memset(Rb[:, 1, :], 0.0), w=['Rb'])
                order = [1, 0] + list(range(17, 2, -1)); dest = [0, 17] + list(range(16, 1, -1))
                for ii, (c, dd) in enumerate(zip(order, dest)):
                    bank = 4 + ii % 2
                    kb.op('pe', lambda e: e.matmul(PSA[:, bank, 0:128], lhsT=ktok[:, c, :], rhs=vb[:, c, :], start=True, stop=True),
                          r=['ktok', 'vb'], w=[('ps', bank)])
                    kb.op('dve', lambda e: e.scalar_tensor_tensor(out=Rrun[:], in0=Rrun[:], scalar=g128[:, 4 + r:5 + r], in1=PSA[:, bank, 0:128],
                                                                  op0=ALU.mult, op1=ALU.add), r=['Rrun', 'g128', ('ps', bank)], w=['Rrun'])
                    kb.op('act', lambda e: e.copy(out=Rb[:, dd, :], in_=Rrun[:]), r=['Rrun'], w=['Rb'])
                if os.environ.get('RET_CUT') == '4':
                    kb.release(m); return
                for c in (range(NT) if need_ctx else range(2, NT)):
                    i = c % 2; cs = slice(c * 128, (c + 1) * 128)
                    SK = os.environ.get('RET_SKIP', '')
                    for hh in range(2):
                        if 'I' in SK:
                            break
                        ps_ = slice(hh * 64, (hh + 1) * 64)
                        kb.op('pe', lambda e: e.matmul(PSA[:, i + 4 * hh, 0:128], lhsT=kT[ps_, cs], rhs=qT[ps_, cs], start=True, stop=True),
                              r=['kT', 'qT'], w=[('ps', i + 4 * hh)])
                    if 'A' not in SK:
                        for hh in range(2):
                            kb.op('dve', lambda e: e.tensor_tensor(out=AT[i][:, hh, :], in0=PSA[:, i + 4 * hh, 0:128],
                                                                   in1=DecT[:, 2 * r + hh, :], op=ALU.mult), r=[('ps', i + 4 * hh), 'DecT'], w=[('AT', i)])
                    if 'Q' not in SK:
                        kb.op('pool', lambda e: e.tensor_tensor(out=qxf[i][:], in0=qT[:, cs], in1=X[:, r, :], op=ALU.mult), r=['qT', 'X'], w=[('qxf', i)])
                        kb.op('pool', lambda e: e.tensor_tensor(out=qxb[i][:], in0=qT[:, cs], in1=X[:, 4 + r, :], op=ALU.mult), r=['qT', 'X'], w=[('qxb', i)])
                    for hh in range(2):
                        if 'O' in SK:
                            break
                        ps_ = slice(hh * 64, (hh + 1) * 64)
                        o_ = PSA[:, 2 + i, hh * 64:(hh + 1) * 64]
                        if os.environ.get('RET_X') == 'A':
                            kb.op('pe', lambda e: e.matmul(o_, lhsT=AT[i][:, hh, :], rhs=vtok[:, c, hh * 64:(hh + 1) * 64], start=True, stop=True),
                                  r=[('AT', i), 'vtok'], w=[('ps', 2 + i)])
                            continue
                        kb.op('pe', lambda e: e.matmul(o_, lhsT=AT[i][:, hh, :], rhs=vtok[:, c, hh * 64:(hh + 1) * 64], start=True, stop=False),
                              r=[('AT', i), 'vtok'], w=[('ps', 2 + i)])
                        kb.op('pe', lambda e: e.matmul(o_, lhsT=qxf[i][ps_, :], rhs=Sf[ps_, c, hh * 64:(hh + 1) * 64], start=False, stop=False),
                              r=[('qxf', i), 'Sf'], w=[('ps', 2 + i)])
                        kb.op('pe', lambda e: e.matmul(o_, lhsT=qxb[i][ps_, :], rhs=Rb[ps_, c, hh * 64:(hh + 1) * 64], start=False, stop=True),
                              r=[('qxb', i), 'Rb'], w=[('ps', 2 + i)])
                    if os.environ.get('RET_CUT') == '6':
                        continue
                    o3 = lambda a: a[:].rearrange("p (h e) -> p h e", h=2)
                    kb.op('act', lambda e: e.copy(out=oc[i][:], in_=PSA[:, 2 + i, 0:128]), r=[('ps', 2 + i)], w=[('oc', i)])
                    kb.op('dve', lambda e: e.reduce_sum(out=st[i][:, 0:2], in_=o3(oc[i]), axis=AX.X), r=[('oc', i)], w=[('st', i)])
                    kb.op('dve', lambda e: e.tensor_scalar(out=st[i][:, 0:2], in0=st[i][:, 0:2], scalar1=-1.0 / 64, scalar2=None, op0=ALU.mult),
                          r=[('st', i)], w=[('st', i)])
                    kb.op('pool', lambda e: e.tensor_tensor(out=o3(oc[i]), in0=o3(oc[i]), in1=bc(st[i][:, 0:2], [[1, 2], [0, 64]]), op=ALU.add),
                          r=[('oc', i), ('st', i)], w=[('oc', i)])
                    kb.op('pool', lambda e: e.tensor_tensor(out=sq[i][:], in0=oc[i][:], in1=oc[i][:], op=ALU.mult), r=[('oc', i)], w=[('sq', i)])
                    kb.op('dve', lambda e: e.reduce_sum(out=st[i][:, 2:4], in_=o3(sq[i]), axis=AX.X), r=[('sq', i)], w=[('st', i)])
                    kb.op('act', lambda e: e.activation(out=st[i][:, 2:4], in_=st[i][:, 2:4], func=AF.Sqrt, scale=1.0 / 64, bias=epsG),
                          r=[('st', i)], w=[('st', i)])
                    kb.op('dve', lambda e: e.reciprocal(out=st[i][:, 2:4], in_=st[i][:, 2:4]), r=[('st', i)], w=[('st', i)])
                    kb.op('pool', lambda e: e.tensor_tensor(out=o3(oc[i]), in0=o3(oc[i]), in1=bc(st[i][:, 2:4], [[1, 2], [0, 64]]), op=ALU.mult),
                          r=[('oc', i), ('st', i)], w=[('oc', i)])
                    if os.environ.get('RET_CUT') == '7':
                        continue
                    kb.op('pool', lambda e: e.tensor_tensor(out=rtok[i][:], in0=oc[i][:], in1=sg[:, c, :], op=ALU.mult), r=[('oc', i), 'sg'], w=[('rtok', i)])
                    kb.op('pe', lambda e: e.transpose(out=PST[:, 0, :], in_=rtok[i][:], identity=ident[:]), r=[('rtok', i), 'ident'], w=['pst'])
                    kb.op('act', lambda e: e.copy(out=retT[:, r, cs], in_=PST[:, 0, :]), r=['pst'], w=['retT'])
                if os.environ.get('RET_CUT') in ('5', '6', '7'):
                    kb.release(m); return
                if r == 3:
                    for nm_, t_, sh_, dt_ in (('sg', sg, [128, NT, 128], F32), ('DecT', DecT, [128, 8, 128], F32), ('X', X, [128, 8, 128], F32),
                                              ('Z', Z, [128, 16], F32), ('g128', g128, [128, 8], F32), ('lg', lg, [128, 16], F32),
                                              ('Sf', Sf, [128, NT, 128], BF16), ('Rb', Rb, [128, NT, 128], BF16), ('qT', qT, [128, T], BF16),
                                              ('kT', kT, [128, T], BF16), ('vtok', vtok, [128, NT, 128], BF16), ('ktok', ktok, [128, NT, 128], BF16),
                                              ('vf', vf, [128, NT, 128], BF16), ('AT1', AT[1], [128, 2, 128], BF16), ('qxf1', qxf[1], [128, 128], BF16),
                                              ('qxb1', qxb[1], [128, 128], BF16), ('oc1', oc[1], [128, 128], F32), ('st1', st[1], [128, 4], F32),
                                              ('sq1', sq[1], [128, 128], F32), ('rtok1', rtok[1], [128, 128], BF16)):
                        xdump(nm_, t_, sh_, dt_)
            kb.release(m)

        def phase_attn(l, oaT):
            need_ctx = (l == 0)
            m = kb.mark()
            qT = kb.sb('aqT', [128, 4, T], BF16); kT = kb.sb('akT', [128, T], BF16)
            Va = kb.sb('Va', [128, NT, 2, 66], BF16)
            msk = kb.sb('msk', [128, 256], BF16)
            kb.dma('sp', msk[:], amask[:, :], w=['msk'])
            snk = kb.sb('snk', [128, 8], F32)
            kb.dma('sp', snk[:], sink_in[l].partition_broadcast(128), w=['snk'])
            kb.op('act', lambda e: e.activation(out=snk[:], in_=snk[:], func=AF.Exp), r=['snk'], w=['snk'])
            kb.op('pool', lambda e: e.memset(Va[:, :, :, 64:66], 1.0), w=['Va'])
            wq = kb.sb('awq', [128, 8, 1024], BF16); wk = kb.sb('awk', [128, 8, 256], BF16); wv = kb.sb('awv', [128, 8, 128], BF16)
            load_w(wq, 'awq', w_ext[l][:, QA:QA + 1024], 8, 1024)
            load_w(wk, 'awk', w_ext[l][:, KA:KA + 256], 8, 256)
            load_w(wv, 'awv', w_ext[l][:, VA:VA + 128], 8, 128)
            rc = [kb.sb(f'arc{i}', [128, 512], F32) for i in range(2)]; rs = [kb.sb(f'ars{i}', [128, 512], F32) for i in range(2)]
            t1 = [kb.sb(f'at1{i}', [128, 512], F32) for i in range(2)]; t2 = [kb.sb(f'at2{i}', [128, 512], F32) for i in range(2)]
            for bi, (s0, n) in enumerate(TB):
                if s0 == 0:
                    for g in range(4):
                        proj_fm(wq, 'awq', g * 128, s0, n, g % 2)
                        kb.op('act', lambda e: e.copy(out=qT[:, g, s0:s0 + n], in_=PSA[:, g % 2, 0:n]), r=[('ps', g % 2)], w=['aqT'])
                    proj_fm(wk, 'awk', 0, s0, n, 2)
                    kb.op('act', lambda e: e.copy(out=kT[:, s0:s0 + n], in_=PSA[:, 2, 0:n]), r=[('ps', 2)], w=['akT'])
                else:
                    j = bi % 2; p0 = s0 - 256
                    kb.dma('sp', rc[j][:, 0:n], ropeA[0, :, p0:p0 + n], w=[('rc', j)])
                    kb.dma('sp', rs[j][:, 0:n], ropeA[1, :, p0:p0 + n], w=[('rs', j)])
                    for g in range(4):
                        b0 = 2 * (g % 2)
                        proj_fm(wq, 'awq', g * 128, s0, n, b0); proj_fm(wq, 'awq', 512 + g * 128, s0, n, b0 + 1)
                        rope_evac(b0, b0 + 1, qT[:, g, s0:s0 + n], 'aqT', rc[j], rs[j], n, t1[j], t2[j], j)
                    proj_fm(wk, 'awk', 0, s0, n, 4); proj_fm(wk, 'awk', 128, s0, n, 5)
                    rope_evac(4, 5, kT[:, s0:s0 + n], 'akT', rc[j], rs[j], n, t1[j], t2[j], j)
                for t in range(s0 // 128, (s0 + n) // 128):
                    for k in range(8):
                        kb.op('pe', lambda e: e.matmul(PSA[:, 6, 0:128], lhsT=hT[:, k, t * 128:(t + 1) * 128], rhs=wv[:, k, :],
                                                       start=(k == 0), stop=(k == 7)), r=['hT', 'awv'], w=[('ps', 6)])
                    kb.op('act', lambda e: e.copy(out=Va[:, t, :, 0:64], in_=PSA[:, 6, 0:128].rearrange("p (h e) -> p h e", h=2)),
                          r=[('ps', 6)], w=['Va'])
            PT = [[kb.sb(f'PT{i}_{j}', [128, 4, 128], BF16) for j in range(5)] for i in range(2)]
            oat = [kb.sb(f'oat{i}', [128, 8, 64], BF16) for i in range(2)]
            den = [kb.sb(f'den{i}', [128, 4], F32) for i in range(2)]
            it = 0
            for t in (range(NT) if need_ctx else range(2, NT)):
                if t < 2:
                    keys = [(0, None), (1, None)]
                else:
                    keys = []
                    if t > 2: keys.append((t - 1, 0))
                    keys.append((t, None))
                    if t < NT - 1: keys.append((t + 1, 1))
                    keys += [(0, None), (1, None)]
                ti = t % 2
                for h2 in range(2):
                    i = it % 2; it += 1
                    ps_ = slice(h2 * 64, (h2 + 1) * 64)
                    for ki, (kt, mk) in enumerate(keys):
                        bank = ki % 3
                        kb.op('pe', lambda e: e.matmul(PSA[:, bank, :].rearrange("p (g q) -> p g q", g=4), lhsT=kT[ps_, kt * 128:(kt + 1) * 128],
                                                       rhs=qT[ps_, :, t * 128:(t + 1) * 128], start=True, stop=True), r=['akT', 'aqT'], w=[('ps', bank)])
                        kb.op('act', lambda e: e.activation(out=PT[i][ki][:], in_=PSA[:, bank, :].rearrange("p (g q) -> p g q", g=4), func=AF.Exp, scale=0.125),
                              r=[('ps', bank)], w=[('PT', i, ki)])
                        if mk is not None:
                            kb.op('pool', lambda e: e.tensor_tensor(out=PT[i][ki][:], in0=PT[i][ki][:], in1=bc(msk[:, mk * 128:(mk + 1) * 128], [[0, 4], [1, 128]]),
                                                                    op=ALU.mult), r=[('PT', i, ki), 'msk'], w=[('PT', i, ki)])
                    ob = 3 + i
                    for g in range(4):
                        for ki, (kt, mk) in enumerate(keys):
                            kb.op('pe', lambda e: e.matmul(PSA[:, ob, g * 66:g * 66 + 65], lhsT=PT[i][ki][:, g, :], rhs=Va[:, kt, h2, 0:65],
                                                           start=(ki == 0), stop=(ki == len(keys) - 1)), r=[('PT', i, ki), 'Va'], w=[('ps', ob)])
                    ov = PSA[:, ob, 0:264].rearrange("p (g e) -> p g e", g=4)
                    kb.op('dve', lambda e: e.tensor_tensor(out=den[i][:], in0=ov[:, :, 64], in1=snk[:, h2 * 4:(h2 + 1) * 4], op=ALU.add),
                          r=[('ps', ob), 'snk'], w=[('den', i)])
                    kb.op('dve', lambda e: e.reciprocal(out=den[i][:], in_=den[i][:]), r=[('den', i)], w=[('den', i)])
                    kb.op('dve', lambda e: e.tensor_tensor(out=oat[ti][:, h2 * 4:(h2 + 1) * 4, :], in0=ov[:, :, 0:64], in1=bc(den[i][:, 0:4], [[1, 4], [0, 64]]),
                                                           op=ALU.mult), r=[('ps', ob), ('den', i)], w=[('oat', ti)])
                for k in range(4):
                    kb.op('pe', lambda e: e.transpose(out=PST[:, k, :], in_=oat[ti][:, 2 * k:2 * k + 2, :].rearrange("p h e -> p (h e)"), identity=ident[:]),
                          r=[('oat', ti), 'ident'], w=['pst'])
                kb.op('act', lambda e: e.copy(out=oaT[:, :, t * 128:(t + 1) * 128], in_=PST[:, 0:4, :]), r=['pst'], w=['oaT'])
            kb.release(m)

        def phase_four(l, ofT):
            need_ctx = (l == 0)
            m = kb.mark()
            wfu = kb.sb('wfu', [128, 8, 512], BF16)
            load_w(wfu, 'wfu', w_ext[l][:, FU:FU + 512], 8, 512)
            dC = kb.sb('dC', [128, 256], BF16)
            kb.dma('sp', dC[:], dftC[:, :], w=['dC'])
            W = kb.sb('W', [128, NT, 4, 256], BF16)
            uT = [kb.sb(f'uT{i}', [128, T], BF16) for i in range(2)]
            for g in range(4):
                i = g % 2
                for bi, (s0, n) in enumerate(TB):
                    proj_fm(wfu, 'wfu', g * 128, s0, n, bi % 2)
                    kb.op('act', lambda e: e.copy(out=uT[i][:, s0:s0 + n], in_=PSA[:, bi % 2, 0:n]), r=[('ps', bi % 2)], w=[('uT', i)])
                for t in range(NT):
                    bank = 2 + t % 2
                    kb.op('pe', lambda e: e.matmul(PSA[:, bank, 0:256], lhsT=uT[i][:, t * 128:(t + 1) * 128], rhs=dC[:], start=True, stop=True),
                          r=[('uT', i), 'dC'], w=[('ps', bank)])
                    kb.op('dve', lambda e: e.tensor_copy(out=W[:, t, g, :], in_=PSA[:, bank, 0:256]), r=[('ps', bank)], w=['W'])
            Cb = [kb.sb(f'Cb{i}', [128, 16, 256], BF16) for i in range(2)]
            Nb = [kb.sb(f'Nb{i}', [128, 16, 256], BF16) for i in range(2)]
            it = 0
            for nb in range(8):
                i = nb % 2
                kb.dma('sp', Cb[i][:], dftN[0, :, nb * 256:(nb + 1) * 256].rearrange("(t p) c -> p t c", p=128), w=[('Cb', i)])
                kb.dma('sp', Nb[i][:], dftN[1, :, nb * 256:(nb + 1) * 256].rearrange("(t p) c -> p t c", p=128), w=[('Nb', i)])
                for g in range(4):
                    bank = 4 + it % 2; it += 1
                    for t in range(16):
                        kb.op('pe', lambda e: e.matmul(PSA[:, bank, 0:256], lhsT=W[:, 2 + t, g, 0:128], rhs=Cb[i][:, t, :], start=(t == 0), stop=False),
                              r=['W', ('Cb', i)], w=[('ps', bank)])
                        kb.op('pe', lambda e: e.matmul(PSA[:, bank, 0:256], lhsT=W[:, 2 + t, g, 128:256], rhs=Nb[i][:, t, :], start=False, stop=(t == 15)),
                              r=['W', ('Nb', i)], w=[('ps', bank)])
                    kb.op('act', lambda e: e.copy(out=ofT[:, g, 256 + nb * 256:256 + (nb + 1) * 256], in_=PSA[:, bank, 0:256]), r=[('ps', bank)], w=['ofT'])
            if need_ctx:
                kb.dma('sp', Cb[0][:, 0:2, :], dft256[0].rearrange("(t p) c -> p t c", p=128), w=[('Cb', 0)])
                kb.dma('sp', Nb[0][:, 0:2, :], dft256[1].rearrange("(t p) c -> p t c", p=128), w=[('Nb', 0)])
                for g in range(4):
                    bank = 4 + g % 2
                    for t in range(2):
                        kb.op('pe', lambda e: e.matmul(PSA[:, bank, 0:256], lhsT=W[:, t, g, 0:128], rhs=Cb[0][:, t, :], start=(t == 0), stop=False),
                              r=['W', ('Cb', 0)], w=[('ps', bank)])
                        kb.op('pe', lambda e: e.matmul(PSA[:, bank, 0:256], lhsT=W[:, t, g, 128:256], rhs=Nb[0][:, t, :], start=False, stop=(t == 1)),
                              r=['W', ('Nb', 0)], w=[('ps', bank)])
                    kb.op('act', lambda e: e.copy(out=ofT[:, g, 0:256], in_=PSA[:, bank, 0:256]), r=[('ps', bank)], w=['ofT'])
            kb.release(m)

        def resid_update(l, gi, tiles, src_fn, src_keys):
            pass

        def phase_out(l, mT):
            m = kb.mark()
            wo = kb.sb('wo', [128, 8, D], BF16)
            load_w(wo, 'wo', w_out[l][:, :], 8, D)
            g1 = []
            for row in (0, 1):
                g_ = kb.sb(f'g1_{row}', [128, D], F32)
                kb.dma('sp', g_[:], modbuf[l, row, 2 * D:3 * D].partition_broadcast(128), w=[f'g1_{row}'])
                g1.append(g_)
            xt = [kb.sb(f'oxt{i}', [128, D], F32) for i in range(2)]
            yt = [kb.sb(f'oyt{i}', [128, D], F32) for i in range(2)]
            for t in (range(NT) if l == 0 else range(2, NT)):
                i = t % 2; row = 0 if t < 2 else 1
                kb.dma('sp', xt[i][:], xbuf[t * 128:(t + 1) * 128, :], r=[('x', t)], w=[('oxt', i)])
                for hf in range(2):
                    bank = 2 * i + hf
                    for fc in range(8):
                        kb.op('pe', lambda e: e.matmul(PSA[:, bank, :], lhsT=mT[:, fc, t * 128:(t + 1) * 128], rhs=wo[:, fc, hf * 512:(hf + 1) * 512],
                                                       start=(fc == 0), stop=(fc == 7)), r=['mT', 'wo'], w=[('ps', bank)])
                    kb.op('dve', lambda e: e.tensor_tensor(out=yt[i][:, hf * 512:(hf + 1) * 512], in0=PSA[:, bank, :], in1=g1[row][:, hf * 512:(hf + 1) * 512],
                                                           op=ALU.mult), r=[('ps', bank), f'g1_{row}'], w=[('oyt', i)])
                kb.op('pool', lambda e: e.tensor_tensor(out=yt[i][:], in0=yt[i][:], in1=xt[i][:], op=ALU.add), r=[('oyt', i), ('oxt', i)], w=[('oyt', i)])
                kb.dma('sp', xbuf[t * 128:(t + 1) * 128, :], yt[i][:], r=[('oyt', i)], w=[('x', t)])
            kb.release(m)

        def phase_moe(l):
            m = kb.mark()
            tiles = list(range(NT) if l == 0 else range(2, NT))
            blocks = TB if l == 0 else TB[1:]
            logits = kb.sb('logits', [128, NT, 36], F32)
            Wt = kb.sb('Wt', [128, NT, 32], F32)
            kb.op('pool', lambda e: e.memset(logits[:], 0.0), w=['logits'])
            phase_norm(l, 2, logits)
            if os.environ.get('MOE_CUT') == '1':
                kb.release(m); return
            m2 = kb.mark()
            lgG = logits[:, :, 0:4]; lgE = logits[:, :, 4:36]
            gmax = kb.sb('gmax', [128, NT], F32); ohg = kb.sb('ohg', [128, NT, 4], F32); eg = kb.sb('eg', [128, NT, 4], F32)
            pg = kb.sb('pg', [128, NT], F32); me = kb.sb('me', [128, NT, 32], F32); oh1 = kb.sb('oh1', [128, NT, 32], F32)
            oh2 = kb.sb('oh2', [128, NT, 32], F32); m1 = kb.sb('m1', [128, NT], F32); m2_ = kb.sb('m2', [128, NT], F32)
            w1 = kb.sb('w1', [128, NT], F32); w2 = kb.sb('w2', [128, NT], F32)
            b1 = lambda a, n_: bc(a, [[1, NT], [0, n_]])
            kb.op('dve', lambda e: e.reduce_max(out=gmax[:], in_=lgG, axis=AX.X), r=['logits'], w=['gmax'])
            kb.op('dve', lambda e: e.tensor_tensor(out=ohg[:], in0=lgG, in1=b1(gmax[:, 0:NT], 4), op=ALU.is_equal), r=['logits', 'gmax'], w=['ohg'])
            kb.op('dve', lambda e: e.tensor_tensor(out=eg[:], in0=lgG, in1=b1(gmax[:, 0:NT], 4), op=ALU.subtract), r=['logits', 'gmax'], w=['eg'])
            kb.op('act', lambda e: e.activation(out=eg[:], in_=eg[:], func=AF.Exp), r=['eg'], w=['eg'])
            kb.op('dve', lambda e: e.reduce_sum(out=pg[:], in_=eg[:], axis=AX.X), r=['eg'], w=['pg'])
            kb.op('dve', lambda e: e.reciprocal(out=pg[:], in_=pg[:]), r=['pg'], w=['pg'])
            kb.op('dve', lambda e: e.tensor_scalar(out=ohg[:], in0=ohg[:], scalar1=-1.0, scalar2=1e30, op0=ALU.add, op1=ALU.mult), r=['ohg'], w=['ohg'])
            kb.op('dve', lambda e: e.tensor_tensor(out=me[:].rearrange("p t (g x) -> p t g x", g=4), in0=lgE.rearrange("p t (g x) -> p t g x", g=4),
                                                   in1=bc(ohg[:, 0:NT, :], [[4, NT], [1, 4], [0, 8]]), op=ALU.add), r=['logits', 'ohg'], w=['me'])
            kb.op('dve', lambda e: e.reduce_max(out=m1[:], in_=me[:], axis=AX.X), r=['me'], w=['m1'])
            kb.op('dve', lambda e: e.tensor_tensor(out=oh1[:], in0=me[:], in1=b1(m1[:, 0:NT], 32), op=ALU.is_equal), r=['me', 'm1'], w=['oh1'])
            kb.op('dve', lambda e: e.scalar_tensor_tensor(out=me[:], in0=oh1[:], scalar=-1e30, in1=me[:], op0=ALU.mult, op1=ALU.add), r=['oh1', 'me'], w=['me'])
            kb.op('dve', lambda e: e.reduce_max(out=m2_[:], in_=me[:], axis=AX.X), r=['me'], w=['m2'])
            kb.op('dve', lambda e: e.tensor_tensor(out=oh2[:], in0=me[:], in1=b1(m2_[:, 0:NT], 32), op=ALU.is_equal), r=['me', 'm2'], w=['oh2'])
            kb.op('dve', lambda e: e.tensor_tensor(out=w1[:], in0=m1[:], in1=m2_[:], op=ALU.subtract), r=['m1', 'm2'], w=['w1'])
            kb.op('act', lambda e: e.activation(out=w2[:], in_=w1[:], func=AF.Sigmoid, scale=-1.0), r=['w1'], w=['w2'])
            kb.op('act', lambda e: e.activation(out=w1[:], in_=w1[:], func=AF.Sigmoid), r=['w1'], w=['w1'])
            kb.op('dve', lambda e: e.tensor_tensor(out=w1[:], in0=w1[:], in1=pg[:], op=ALU.mult), r=['w1', 'pg'], w=['w1'])
            kb.op('dve', lambda e: e.tensor_tensor(out=w2[:], in0=w2[:], in1=pg[:], op=ALU.mult), r=['w2', 'pg'], w=['w2'])
            kb.op('dve', lambda e: e.tensor_tensor(out=oh1[:], in0=oh1[:], in1=b1(w1[:, 0:NT], 32), op=ALU.mult), r=['oh1', 'w1'], w=['oh1'])
            kb.op('dve', lambda e: e.tensor_tensor(out=oh2[:], in0=oh2[:], in1=b1(w2[:, 0:NT], 32), op=ALU.mult), r=['oh2', 'w2'], w=['oh2'])
            kb.op('dve', lambda e: e.tensor_tensor(out=Wt[:], in0=oh1[:], in1=oh2[:], op=ALU.add), r=['oh1', 'oh2'], w=['Wt'])
            kb.release(m2)
            if os.environ.get('MOE_CUT') == '2':
                kb.release(m); return
            acc = kb.sb('acc', [128, NT, D], F32)
            m3 = kb.mark()
            wg = [kb.sb(f'ewg{i}', [128, 8, 512], BF16) for i in range(2)]
            wu = [kb.sb(f'ewu{i}', [128, 8, 512], BF16) for i in range(2)]
            wd = [kb.sb(f'ewd{i}', [128, 4, D], BF16) for i in range(2)]
            aT = [kb.sb(f'aT{i}', [128, 4, 512], BF16) for i in range(2)]
            sgt = [kb.sb(f'sgt{i}', [128, 512], F32) for i in range(2)]
            it = 0; ih = 0
            for ex in range(int(os.environ.get('MOE_NEXP', '32'))):
                j = ex % 2
                load_w(wg[j], ('ewg', j), w_eg[l, ex], 8, 512, engs=('pool', 'act'))
                load_w(wu[j], ('ewu', j), w_eu[l, ex], 8, 512, engs=('pool', 'act'))
                load_w(wd[j], ('ewd', j), w_ed[l, ex], 4, D, engs=('pool', 'act'))
                for (s0, n) in blocks:
                    i = it % 2; it += 1
                    for hc in range(4):
                        ii = ih % 2; ih += 1
                        for k in range(8):
                            kb.op('pe', lambda e: e.matmul(PSA[:, ii, 0:n], lhsT=wg[j][:, k, hc * 128:(hc + 1) * 128], rhs=hT[:, k, s0:s0 + n],
                                                           start=(k == 0), stop=(k == 7)), r=[('ewg', j), 'hT'], w=[('ps', ii)])
                        for k in range(8):
                            kb.op('pe', lambda e: e.matmul(PSA[:, 2 + ii, 0:n], lhsT=wu[j][:, k, hc * 128:(hc + 1) * 128], rhs=hT[:, k, s0:s0 + n],
                                                           start=(k == 0), stop=(k == 7)), r=[('ewu', j), 'hT'], w=[('ps', 2 + ii)])
                        kb.op('act', lambda e: e.activation(out=sgt[ii][:, 0:n], in_=PSA[:, ii, 0:n], func=AF.Silu), r=[('ps', ii)], w=[('sgt', ii)])
                        kb.op('dve', lambda e: e.tensor_tensor(out=aT[i][:, hc, 0:n], in0=sgt[ii][:, 0:n], in1=PSA[:, 2 + ii, 0:n], op=ALU.mult),
                              r=[('sgt', ii), ('ps', 2 + ii)], w=[('aT', i)])
                    for t in range(s0 // 128, (s0 + n) // 128):
                        tl = t * 128 - s0
                        for hf in range(2):
                            bank = 4 + (2 * t + hf) % 3
                            for hc in range(4):
                                kb.op('pe', lambda e: e.matmul(PSA[:, bank, :], lhsT=aT[i][:, hc, tl:tl + 128], rhs=wd[j][:, hc, hf * 512:(hf + 1) * 512],
                                                               start=(hc == 0), stop=(hc == 3)), r=[('aT', i), ('ewd', j)], w=[('ps', bank)])
                            a_ = acc[:, t, hf * 512:(hf + 1) * 512]
                            if ex == 0:
                                kb.op('dve', lambda e: e.tensor_scalar(out=a_, in0=PSA[:, bank, :], scalar1=Wt[:, t, ex:ex + 1], scalar2=None, op0=ALU.mult),
                                      r=[('ps', bank), 'Wt'], w=[('acc', t)])
                            else:
                                kb.op('dve', lambda e: e.scalar_tensor_tensor(out=a_, in0=PSA[:, bank, :], scalar=Wt[:, t, ex:ex + 1], in1=a_,
                                                                              op0=ALU.mult, op1=ALU.add), r=[('ps', bank), 'Wt', ('acc', t)], w=[('acc', t)])
            kb.release(m3)
            g2 = []
            for row in (0, 1):
                g_ = kb.sb(f'g2_{row}', [128, D], F32)
                kb.dma('sp', g_[:], modbuf[l, row, 5 * D:6 * D].partition_broadcast(128), w=[f'g2_{row}'])
                g2.append(g_)
            xt = [kb.sb(f'mxt{i}', [128, D], F32) for i in range(2)]
            for t in tiles:
                i = t % 2; row = 0 if t < 2 else 1
                kb.dma('sp', xt[i][:], xbuf[t * 128:(t + 1) * 128, :], r=[('x', t)], w=[('mxt', i)])
                kb.op('dve', lambda e: e.tensor_tensor(out=acc[:, t, :], in0=acc[:, t, :], in1=g2[row][:], op=ALU.mult), r=[('acc', t), f'g2_{row}'], w=[('acc', t)])
                kb.op('pool', lambda e: e.tensor_tensor(out=xt[i][:], in0=xt[i][:], in1=acc[:, t, :], op=ALU.add), r=[('acc', t), ('mxt', i)], w=[('mxt', i)])
                kb.dma('sp', xbuf[t * 128:(t + 1) * 128, :], xt[i][:], r=[('mxt', i)], w=[('x', t)])
            kb.release(m)

        moe_state = {}

        def phase_moe_sparse(l):
            IOA = bass.IndirectOffsetOnAxis
            ABv = AB.rearrange("r (h c) -> (r h) c", h=2)
            if 'bregs' not in moe_state:
                regs = {}
                for nm_, v_ in (('rec', 32 * CAP - 1), ('tok', T - 1), ('ab', 4 * T - 1)):
                    rg = nc.gpsimd.alloc_register('bnd_' + nm_)
                    nc.gpsimd.reg_mov(rg, v_)
                    regs[nm_] = rg
                moe_state['bregs'] = regs
            BR = moe_state['bregs']
            m = kb.mark()
            t0 = 0 if l == 0 else 2
            tiles = list(range(t0, NT))
            logits = kb.sb('logits', [128, NT, 36], F32)
            kb.op('pool', lambda e: e.memset(logits[:], 0.0), w=['logits'])
            phase_norm(l, 2, logits, sparse=True)
            MC = kb.sb('MC', [128, 32 + NT], F32)
            kb.dma('sp', MC[:], mconst[:, :], w=['MC'])
            eC = MC[:, 0:32]; tokid = MC[:, 32:32 + NT]
            zt = kb.sb('zt', [128, D], F32)
            kb.op('pool', lambda e: e.memset(zt[:], 0.0), w=['zt'])
            for q in range(2 * NT):
                kb.dma('sp', AB[q * 128:(q + 1) * 128, :], zt[:], r=['zt'], w=[('ABz', q)])
            ri_ = kb.sb('recinit', [128, (32 * CAP) // 128, 4], F32)
            kb.op('pool', lambda e: e.memset(ri_[:], 1.0e6), w=['recinit'])
            kb.op('pool', lambda e: e.memset(ri_[:, :, 2:3], 0.0), r=['recinit'], w=['recinit'])
            kb.dma('sp', rec_d.rearrange("(p s) c -> p s c", p=128), ri_[:], r=['recinit'], w=['rec_d'])
            lgG = logits[:, :, 0:4]; lgE = logits[:, :, 4:36]
            gmax = kb.sb('gmax', [128, NT], F32); ohg = kb.sb('ohg', [128, NT, 4], F32); eg = kb.sb('eg', [128, NT, 4], F32)
            pg = kb.sb('pg', [128, NT], F32); me = kb.sb('me', [128, NT, 32], F32); oh1 = kb.sb('oh1', [128, NT, 32], F32)
            oh2 = kb.sb('oh2', [128, NT, 32], F32); m1 = kb.sb('m1', [128, NT], F32); m2_ = kb.sb('m2', [128, NT], F32)
            w1 = kb.sb('w1', [128, NT], F32); w2 = kb.sb('w2', [128, NT], F32)
            b1 = lambda a, n_: bc(a, [[1, NT], [0, n_]])
            kb.op('dve', lambda e: e.reduce_max(out=gmax[:], in_=lgG, axis=AX.X), r=['logits'], w=['gmax'])
            kb.op('dve', lambda e: e.tensor_tensor(out=ohg[:], in0=lgG, in1=b1(gmax[:, 0:NT], 4), op=ALU.is_equal), r=['logits', 'gmax'], w=['ohg'])
            kb.op('dve', lambda e: e.tensor_tensor(out=eg[:], in0=lgG, in1=b1(gmax[:, 0:NT], 4), op=ALU.subtract), r=['logits', 'gmax'], w=['eg'])
            kb.op('act', lambda e: e.activation(out=eg[:], in_=eg[:], func=AF.Exp), r=['eg'], w=['eg'])
            kb.op('dve', lambda e: e.reduce_sum(out=pg[:], in_=eg[:], axis=AX.X), r=['eg'], w=['pg'])
            kb.op('dve', lambda e: e.reciprocal(out=pg[:], in_=pg[:]), r=['pg'], w=['pg'])
            kb.op('dve', lambda e: e.tensor_scalar(out=ohg[:], in0=ohg[:], scalar1=-1.0, scalar2=1e30, op0=ALU.add, op1=ALU.mult), r=['ohg'], w=['ohg'])
            kb.op('dve', lambda e: e.tensor_tensor(out=me[:].rearrange("p t (g x) -> p t g x", g=4), in0=lgE.rearrange("p t (g x) -> p t g x", g=4),
                                                   in1=bc(ohg[:, 0:NT, :], [[4, NT], [1, 4], [0, 8]]), op=ALU.add), r=['logits', 'ohg'], w=['me'])
            kb.op('dve', lambda e: e.reduce_max(out=m1[:], in_=me[:], axis=AX.X), r=['me'], w=['m1'])
            kb.op('dve', lambda e: e.tensor_tensor(out=oh1[:], in0=me[:], in1=b1(m1[:, 0:NT], 32), op=ALU.is_equal), r=['me', 'm1'], w=['oh1'])
            kb.op('dve', lambda e: e.scalar_tensor_tensor(out=me[:], in0=oh1[:], scalar=-1e30, in1=me[:], op0=ALU.mult, op1=ALU.add), r=['oh1', 'me'], w=['me'])
            kb.op('dve', lambda e: e.reduce_max(out=m2_[:], in_=me[:], axis=AX.X), r=['me'], w=['m2'])
            kb.op('dve', lambda e: e.tensor_tensor(out=oh2[:], in0=me[:], in1=b1(m2_[:, 0:NT], 32), op=ALU.is_equal), r=['me', 'm2'], w=['oh2'])
            kb.op('dve', lambda e: e.tensor_tensor(out=w1[:], in0=m1[:], in1=m2_[:], op=ALU.subtract), r=['m1', 'm2'], w=['w1'])
            kb.op('act', lambda e: e.activation(out=w2[:], in_=w1[:], func=AF.Sigmoid, scale=-1.0), r=['w1'], w=['w2'])
            kb.op('act', lambda e: e.activation(out=w1[:], in_=w1[:], func=AF.Sigmoid), r=['w1'], w=['w1'])
            kb.op('dve', lambda e: e.tensor_tensor(out=w1[:], in0=w1[:], in1=pg[:], op=ALU.mult), r=['w1', 'pg'], w=['w1'])
            kb.op('dve', lambda e: e.tensor_tensor(out=w2[:], in0=w2[:], in1=pg[:], op=ALU.mult), r=['w2', 'pg'], w=['w2'])
            if t0 > 0:
                kb.op('pool', lambda e: e.memset(oh1[:, 0:t0, :], 0.0), r=['oh1'], w=['oh1'])
                kb.op('pool', lambda e: e.memset(oh2[:, 0:t0, :], 0.0), r=['oh2'], w=['oh2'])
            selb = kb.sb('selb', [128, NT * 32], BF16)
            kb.op('pool', lambda e: e.tensor_tensor(out=selb[:], in0=oh1[:].rearrange("p t e -> p (t e)"), in1=oh2[:].rearrange("p t e -> p (t e)"), op=ALU.add),
                  r=['oh1', 'oh2'], w=['selb'])
            LT = kb.sb('LT', [128, 128], BF16); ones = kb.sb('ones', [128, 128], BF16)
            kb.op('pool', lambda e: e.memset(ones[:], 1.0), w=['ones'])
            kb.op('pool', lambda e: e.memset(LT[:], 1.0), w=['LT'])
            kb.op('pool', lambda e: e.affine_select(out=LT[:], in_=LT[:], pattern=[[1, 128]], compare_op=ALU.is_gt, fill=0.0, base=0,
                                                    channel_multiplier=-1), r=['LT'], w=['LT'])
            slot = kb.sb('slot', [128, NT, 32], F32); tot = kb.sb('tot', [128, NT, 32], F32); cum = kb.sb('cum', [128, NT, 32], F32)
            sl2 = slot[:].rearrange("p t e -> p (t e)"); to2 = tot[:].rearrange("p t e -> p (t e)")
            for (c0, c1, bank) in ((0, 512, 0), (512, NT * 32, 1)):
                kb.op('pe', lambda e: e.matmul(PSA[:, bank, 0:c1 - c0], lhsT=LT[:], rhs=selb[:, c0:c1], start=True, stop=True), r=['LT', 'selb'], w=[('ps', bank)])
                kb.op('dve', lambda e: e.tensor_copy(out=sl2[:, c0:c1], in_=PSA[:, bank, 0:c1 - c0]), r=[('ps', bank)], w=['slot'])
                kb.op('pe', lambda e: e.matmul(PSA[:, 2 + bank, 0:c1 - c0], lhsT=ones[:], rhs=selb[:, c0:c1], start=True, stop=True), r=['ones', 'selb'], w=[('ps', 2 + bank)])
                kb.op('dve', lambda e: e.tensor_copy(out=to2[:, c0:c1], in_=PSA[:, 2 + bank, 0:c1 - c0]), r=[('ps', 2 + bank)], w=['tot'])
            kb.op('pool', lambda e: e.memset(cum[:, 0, :], 0.0), w=['cum'])
            for t in range(1, NT):
                kb.op('dve', lambda e: e.tensor_tensor(out=cum[:, t, :], in0=cum[:, t - 1, :], in1=tot[:, t - 1, :], op=ALU.add), r=['cum', 'tot'], w=['cum'])
            kb.op('dve', lambda e: e.tensor_tensor(out=slot[:], in0=slot[:], in1=cum[:], op=ALU.add), r=['slot', 'cum'], w=['slot'])
            rec = kb.sb('rec', [128, NT, 2, 4], F32)
            rr = kb.sb('rr', [128, NT, 2], F32); rri = kb.sb('rri', [128, NT, 2], I32)
            s_ = kb.sb('s_', [128, NT], F32); e_ = kb.sb('e_', [128, NT], F32)
            kb.op('pool', lambda e: e.memset(rec[:], 0.0), w=['rec'])
            for k, (oh, wk) in enumerate(((oh1, w1), (oh2, w2))):
                kb.op('dve', lambda e: e.tensor_tensor(out=me[:], in0=oh[:], in1=slot[:], op=ALU.mult), r=['oh1', 'oh2', 'slot', 'me'], w=['me'])
                kb.op('dve', lambda e: e.reduce_sum(out=s_[:], in_=me[:], axis=AX.X), r=['me'], w=['s_'])
                kb.op('dve', lambda e: e.tensor_tensor(out=me[:], in0=oh[:], in1=bc(eC, [[0, NT], [1, 32]]), op=ALU.mult), r=['oh1', 'oh2', 'MC', 'me'], w=['me'])
                kb.op('dve', lambda e: e.reduce_sum(out=e_[:], in_=me[:], axis=AX.X), r=['me'], w=['e_'])
                kb.op('dve', lambda e: e.tensor_tensor(out=e_[:], in0=e_[:], in1=s_[:], op=ALU.add), r=['e_', 's_'], w=['e_'])
                kb.op('dve', lambda e: e.tensor_scalar(out=s_[:], in0=s_[:], scalar1=float(CAP), scalar2=1.0e6, op0=ALU.is_ge, op1=ALU.mult), r=['s_'], w=['s_'])
                kb.op('dve', lambda e: e.tensor_tensor(out=rr[:, :, k], in0=e_[:], in1=s_[:], op=ALU.add), r=['e_', 's_'], w=['rr'])
                kb.op('pool', lambda e: e.tensor_copy(out=rec[:, :, k, 0], in_=tokid), r=['MC', 'rec'], w=['rec'])
                kb.op('pool', lambda e: e.tensor_scalar(out=rec[:, :, k, 1], in0=tokid, scalar1=2.0, scalar2=float(2 * k * T), op0=ALU.mult, op1=ALU.add), r=['MC', 'rec'], w=['rec'])
                kb.op('pool', lambda e: e.tensor_scalar(out=rec[:, :, k, 3], in0=tokid, scalar1=2.0, scalar2=float(2 * k * T + 1), op0=ALU.mult, op1=ALU.add), r=['MC', 'rec'], w=['rec'])
                kb.op('pool', lambda e: e.tensor_copy(out=rec[:, :, k, 2], in_=wk[:]), r=['w1', 'w2', 'rec'], w=['rec'])
            kb.op('dve', lambda e: e.tensor_copy(out=rri[:], in_=rr[:]), r=['rr'], w=['rri'])
            if stop == f'moe{l}' and os.environ.get('XDUMP'):
                xdump('rr', rr, [128, NT, 2], F32); xdump('rri', rri, [128, NT, 2], I32); xdump('rec', rec, [128, NT, 2, 4], F32)
                xdump('slot', slot, [128, NT, 32], F32)
            kb.barrier()
            for t in tiles:
                for k in range(2):
                    kb.dma('pool', None, None, r=['rec', 'rri'], w=[('recs', t, k)],
                           fn=lambda g: g.indirect_dma_start(out=rec_d[:, :], out_offset=IOA(ap=rri[:, t, k:k + 1], axis=0), in_=rec[:, t, k, :],
                                                             in_offset=None, bounds_check=BR['rec'], oob_is_err=False))
            kb.barrier()
            if os.environ.get('MOE_CUT') == '3':
                kb.release(m); return
            m3 = kb.mark()
            wg = [kb.sb(f'ewg{i}', [128, 8, 512], BF16) for i in range(2)]
            wu = [kb.sb(f'ewu{i}', [128, 8, 512], BF16) for i in range(2)]
            wd = [kb.sb(f'ewd{i}', [128, 4, D], BF16) for i in range(2)]
            XT = [kb.sb(f'XT{i}', [128, 8, CAP], BF16) for i in range(2)]
            aT = kb.sb('aT', [128, 4, CAP], BF16)
            sgt = [kb.sb(f'sgt{i}', [128, 512], F32) for i in range(2)]
            rsb = [kb.sb(f'rsb{i}', [128, NS, 4], F32) for i in range(2)]
            gi = [kb.sb(f'gi{i}', [128, NS, 4], I32) for i in range(2)]
            xg = [kb.sb(f'xg{i}', [128, D], BF16) for i in range(3)]
            yw = [kb.sb(f'yw{i}', [128, D], F32) for i in range(3)]
            for i in range(3):
                kb.op('pool', lambda e: e.memset(xg[i][:], 0.0), w=[('xg', i)])
            ig = 0; iy = 0; ih = 0
            for ex in range(int(os.environ.get('MOE_NEXP', '32'))):
                j = ex % 2
                load_w(wg[j], ('ewg', j), w_eg[l, ex], 8, 512, engs=('act', 'dve'))
                load_w(wu[j], ('ewu', j), w_eu[l, ex], 8, 512, engs=('act', 'dve'))
                load_w(wd[j], ('ewd', j), w_ed[l, ex], 4, D, engs=('act', 'dve'))
                kb.dma('sp', rsb[j][:], rec_d[ex * CAP:(ex + 1) * CAP, :].rearrange("(s p) c -> p s c", p=128), w=[('rsb', j)])
                kb.op('dve', lambda e: e.tensor_copy(out=gi[j][:], in_=rsb[j][:]), r=[('rsb', j)], w=[('gi', j)])
                for s in range(NS):
                    q = ig % 3; ig += 1
                    kb.dma('pool', None, None, r=[('gi', j)], w=[('xg', q)],
                           fn=lambda g: g.indirect_dma_start(out=xg[q][:], out_offset=None, in_=h2d[:, :],
                                                             in_offset=IOA(ap=gi[j][:, s, 0:1], axis=0), bounds_check=BR['tok'], oob_is_err=False))
                    for k in range(8):
                        kb.op('pe', lambda e: e.transpose(out=PST[:, k, :], in_=xg[q][:, k * 128:(k + 1) * 128], identity=ident[:]),
                              r=[('xg', q), 'ident'], w=['pst'])
                    kb.op('act', lambda e: e.copy(out=XT[j][:, :, s * 128:(s + 1) * 128], in_=PST[:]), r=['pst'], w=[('XT', j)])
                for c0 in range(0, CAP, 512):
                    n = min(512, CAP - c0)
                    for hc in range(4):
                        ii = ih % 2; ih += 1
                        for k in range(8):
                            kb.op('pe', lambda e: e.matmul(PSA[:, ii, 0:n], lhsT=wg[j][:, k, hc * 128:(hc + 1) * 128], rhs=XT[j][:, k, c0:c0 + n],
                                                           start=(k == 0), stop=(k == 7)), r=[('ewg', j), ('XT', j)], w=[('ps', ii)])
                        for k in range(8):
                            kb.op('pe', lambda e: e.matmul(PSA[:, 2 + ii, 0:n], lhsT=wu[j][:, k, hc * 128:(hc + 1) * 128], rhs=XT[j][:, k, c0:c0 + n],
                                                           start=(k == 0), stop=(k == 7)), r=[('ewu', j), ('XT', j)], w=[('ps', 2 + ii)])
                        kb.op('act', lambda e: e.activation(out=sgt[ii][:, 0:n], in_=PSA[:, ii, 0:n], func=AF.Silu), r=[('ps', ii)], w=[('sgt', ii)])
                        kb.op('dve', lambda e: e.tensor_tensor(out=aT[:, hc, c0:c0 + n], in0=sgt[ii][:, 0:n], in1=PSA[:, 2 + ii, 0:n], op=ALU.mult),
                              r=[('sgt', ii), ('ps', 2 + ii)], w=['aT'])
                for s in range(NS):
                    q = iy % 3; iy += 1
                    for hf in range(2):
                        bank = 4 + (2 * s + hf) % 3
                        for hc in range(4):
                            kb.op('pe', lambda e: e.matmul(PSA[:, bank, :], lhsT=aT[:, hc, s * 128:(s + 1) * 128], rhs=wd[j][:, hc, hf * 512:(hf + 1) * 512],
                                                           start=(hc == 0), stop=(hc == 3)), r=['aT', ('ewd', j)], w=[('ps', bank)])
                        kb.op('dve', lambda e: e.tensor_scalar(out=yw[q][:, hf * 512:(hf + 1) * 512], in0=PSA[:, bank, :], scalar1=rsb[j][:, s, 2:3], scalar2=None,
                                                               op0=ALU.mult), r=[('ps', bank), ('rsb', j)], w=[('yw', q)])
                    for hf in range(2):
                        kb.dma('pool', None, None, r=[('yw', q), ('gi', j)], w=[('ABs', ex, s, hf)],
                               fn=lambda g: g.indirect_dma_start(out=ABv[:, :], out_offset=IOA(ap=gi[j][:, s, 1 + 2 * hf:2 + 2 * hf], axis=0),
                                                                 in_=yw[q][:, hf * 512:(hf + 1) * 512], in_offset=None, bounds_check=BR['ab'], oob_is_err=False))
            kb.release(m3)
            g2 = []
            for row in (0, 1):
                g_ = kb.sb(f'g2_{row}', [128, D], F32)
                kb.dma('sp', g_[:], modbuf[l, row, 5 * D:6 * D].partition_broadcast(128), w=[f'g2_{row}'])
                g2.append(g_)
            xt = [kb.sb(f'mxt{i}', [128, D], F32) for i in range(2)]
            ya = [kb.sb(f'mya{i}', [128, D], F32) for i in range(2)]
            yb = [kb.sb(f'myb{i}', [128, D], F32) for i in range(2)]
            for t in tiles:
                i = t % 2; row = 0 if t < 2 else 1
                kb.dma('sp', xt[i][:], xbuf[t * 128:(t + 1) * 128, :], r=[('x', t)], w=[('mxt', i)])
                kb.dma('sp', ya[i][:], AB[t * 128:(t + 1) * 128, :], w=[('mya', i)])
                kb.dma('sp', yb[i][:], AB[T + t * 128:T + (t + 1) * 128, :], w=[('myb', i)])
                kb.op('pool', lambda e: e.tensor_tensor(out=ya[i][:], in0=ya[i][:], in1=yb[i][:], op=ALU.add), r=[('mya', i), ('myb', i)], w=[('mya', i)])
                kb.op('dve', lambda e: e.tensor_tensor(out=ya[i][:], in0=ya[i][:], in1=g2[row][:], op=ALU.mult), r=[('mya', i), f'g2_{row}'], w=[('mya', i)])
                kb.op('pool', lambda e: e.tensor_tensor(out=xt[i][:], in0=xt[i][:], in1=ya[i][:], op=ALU.add), r=[('mya', i), ('mxt', i)], w=[('mxt', i)])
                kb.dma('sp', xbuf[t * 128:(t + 1) * 128, :], xt[i][:], r=[('mxt', i)], w=[('x', t)])
            kb.release(m)

        def phase_final():
            m = kb.mark()
            nwb = kb.sb('fnw', [128, D], F32)
            kb.dma('sp', nwb[:], norm_final.partition_broadcast(128), w=['fnw'])
            xt = [kb.sb(f'fxt{i}', [128, D], F32) for i in range(2)]
            yt = [kb.sb(f'fyt{i}', [128, D], F32) for i in range(2)]
            ssq = [kb.sb(f'fss{i}', [128, 1], F32) for i in range(2)]
            junk = kb.sb('fjunk', [128, D], F32)
            for t in range(2, NT):
                i = t % 2
                kb.dma('sp', xt[i][:], xbuf[t * 128:(t + 1) * 128, :], r=[('x', t)], w=[('fxt', i)])
                kb.op('act', lambda e: e.activation(out=junk[:], in_=xt[i][:], func=AF.Square, accum_out=ssq[i][:]), r=[('fxt', i)], w=['fjunk', ('fss', i)])
                kb.op('act', lambda e: e.activation(out=ssq[i][:], in_=ssq[i][:], func=AF.Sqrt, scale=1.0 / D, bias=epsN), r=[('fss', i)], w=[('fss', i)])
                kb.op('dve', lambda e: e.reciprocal(out=ssq[i][:], in_=ssq[i][:]), r=[('fss', i)], w=[('fss', i)])
                kb.op('dve', lambda e: e.scalar_tensor_tensor(out=yt[i][:], in0=xt[i][:], scalar=ssq[i][:, 0:1], in1=nwb[:], op0=ALU.mult, op1=ALU.mult),
                      r=[('fxt', i), ('fss', i), 'fnw'], w=[('fyt', i)])
                kb.dma('sp', out[(t - 2) * 128:(t - 1) * 128, :], yt[i][:], r=[('fyt', i)], w=[('out', t)])
            kb.release(m)

        def finish_dbg(hT_=True, oT=None, mT=None):
            kb.barrier()
            for t in range(NT):
                pass
            kb.dma('sp', dbg['d_x'], xbuf[:, :], w=['d_x'])
            if hT_:
                kb.dma('sp', dbg['d_hT'], hT[:], w=['d_hT'])
            if oT is not None:
                kb.dma('sp', dbg['d_oT'], oT[:], w=['d_oT'])
            if mT is not None:
                kb.dma('sp', dbg['d_mT'], mT[:], w=['d_mT'])
            kb.finish()
            return nc

        kb.dma('sp', xbuf[0:L, :], ctx_in[:, :], w=['xinit0'])
        kb.dma('sp', xbuf[L:T, :], x_in[:, :], w=['xinit1'])
        for l in range(nlayers):
            phase_ada(l)
        kb.barrier()
        if stop == 'ada':
            kb.dma('sp', dbg['d_mod'], modbuf[0], w=['d_mod'])
            return finish_dbg()
        for l in range(nlayers):
            phase_norm(l, 1)
            if stop == f'norm{l}':
                return finish_dbg()
            mm = kb.mark()
            oT = kb.sb('oT', [128, 4, T], BF16, top=True)
            phase_ret(l, oT)
            if stop == f'ret{l}':
                return finish_dbg(oT=oT)
            mT = kb.sb('mT', [128, 8, T], BF16)
            merge(l, 2048, w_br, oT, 'retT', mT, True)
            phase_attn(l, oT)
            if stop == f'attn{l}':
                return finish_dbg(oT=oT)
            merge(l, 0, w_ba, oT, 'oaT', mT, False)
            phase_four(l, oT)
            if stop == f'four{l}':
                return finish_dbg(oT=oT)
            merge(l, 1024, w_bf, oT, 'ofT', mT, False)
            if stop == f'merge{l}':
                return finish_dbg(mT=mT)
            phase_out(l, mT)
            kb.release(mm)
            if stop == f'mix{l}':
                return finish_dbg()
            if os.environ.get('MOE_DENSE'):
                phase_moe(l)
            else:
                phase_moe_sparse(l)
            if stop == f'moe{l}':
                return finish_dbg()
        phase_final()
        kb.finish()
    print("SBUF peak bytes", kb.peak, "instr counts", kb.cnt)
    return nc


def _consts():
    bf = ml_dtypes.bfloat16
    f32 = np.float32
    n = np.arange(N)
    inv16 = (10000.0 ** (-(np.arange(16, dtype=f32)) / f32(16))).astype(f32)
    row = (n // 64).astype(f32); col = (n % 64).astype(f32)
    ropeA = np.zeros((2, 128, N), f32)
    for p in range(128):
        d = p % 64
        pos = row if d < 32 else col
        dd = d % 32
        ang = (pos * inv16[dd % 16]).astype(f32)
        ropeA[0, p] = np.cos(ang); ropeA[1, p] = np.sin(ang) * (-1.0 if dd < 16 else 1.0)
    inv32 = (10000.0 ** (-(np.arange(32, dtype=f32)) / f32(32))).astype(f32)
    ropeR = np.zeros((2, 128, N), f32)
    for p in range(128):
        d = p % 64
        ang = (n.astype(f32) * inv32[d % 32]).astype(f32)
        ropeR[0, p] = np.cos(ang); ropeR[1, p] = np.sin(ang) * (-1.0 if d < 32 else 1.0)
    k = np.arange(N, dtype=np.int64)
    ph = (np.outer(k, k) % N).astype(np.float64) * (2 * np.pi / N)
    dftN = np.stack([np.cos(ph), -np.sin(ph)]) / np.sqrt(N)
    k2 = np.arange(256, dtype=np.int64)
    ph2 = (np.outer(k2, k2) % 256).astype(np.float64) * (2 * np.pi / 256)
    dft256 = np.stack([np.cos(ph2), -np.sin(ph2)]) / np.sqrt(256)
    k3 = np.arange(128, dtype=np.int64)
    ph3 = (np.outer(k3, k3) % 128).astype(np.float64) * (2 * np.pi / 128)
    dftC = np.concatenate([np.cos(ph3), np.sin(ph3)], axis=1) / np.sqrt(128)
    b = np.arange(128)[:, None]; a = np.arange(128)[None, :]
    amask = np.concatenate([(b >= a), (b <= a)], axis=1).astype(f32)
    j = b; i = a
    rconst = np.concatenate([np.maximum(i - j, 0), np.maximum(j - i, 0), (i >= j), (j > i), (i + 1) + 0 * j, (128 - i) + 0 * j,
                             127 - np.arange(128)[:, None], np.arange(128)[:, None]], axis=1).astype(f32)
    mconst = np.concatenate([np.tile((np.arange(32) * CAP)[None, :], (128, 1)), np.arange(128)[:, None] + 128 * np.arange(NT)[None, :]], axis=1).astype(f32)
    return dict(mconst=mconst, ropeA=ropeA, ropeR=ropeR, dftN=dftN.astype(bf), dft256=dft256.astype(bf), dftC=dftC.astype(bf),
                amask=amask.astype(bf), rconst=rconst)


def _wext_index():
    idx = []
    swa = np.array([d + 16 if (d % 32) < 16 else d - 16 for d in range(64)])
    swr = np.array([d + 32 if d < 32 else d - 32 for d in range(64)])
    base = 0
    qa = [np.concatenate([np.arange(g * 64, (g + 1) * 64), np.arange((4 + g) * 64, (5 + g) * 64)]) for g in range(4)]
    qas = [np.concatenate([g * 64 + swa, (4 + g) * 64 + swa]) for g in range(4)]
    idx += qa + qas
    idx += [512 + np.arange(128), 512 + np.concatenate([swa, 64 + swa])]
    idx += [640 + np.arange(128)]
    idx += [768 + np.arange(512), 768 + np.concatenate([h * 64 + swr for h in range(8)])]
    idx += [1280 + np.arange(512), 1280 + np.concatenate([h * 64 + swr for h in range(8)])]
    idx += [1792 + np.arange(512), 2304 + np.arange(512), 2816 + np.arange(512), 3328 + np.arange(3072)]
    idx = np.concatenate(idx)
    assert idx.shape[0] == WEXT
    return idx


_CACHE = {}


def kernel(x, c, ctx, c_ctx, norm_mix, norm_ffn, w_ada, b_ada, w_in, attn_sink, ret_decay_fwd, ret_decay_bwd,
           w_branch_attn, w_branch_fourier, w_branch_ret, w_out, w_router_group, b_router_group,
           w_router_expert, b_router_expert, w_exp_gate, w_exp_up, w_exp_down, norm_final):
    f = lambda a: np.ascontiguousarray(np.asarray(a, dtype=np.float32))
    if 'nc' not in _CACHE:
        _CACHE['nc'] = build()
        _CACHE['consts'] = _consts()
        _CACHE['idx'] = _wext_index()
    nc = _CACHE['nc']
    shared = dict(_CACHE['consts'])
    w_in = f(w_in)
    shared.update(
        c_ctx=f(c_ctx), norm_mix=f(norm_mix), norm_ffn=f(norm_ffn), w_ada=f(w_ada), b_ada=f(b_ada),
        w_ext=np.ascontiguousarray(w_in[:, :, _CACHE['idx']]), attn_sink=f(attn_sink),
        ret_decay_fwd=f(ret_decay_fwd), ret_decay_bwd=f(ret_decay_bwd), w_branch_attn=f(w_branch_attn),
        w_branch_fourier=f(w_branch_fourier), w_branch_ret=f(w_branch_ret), w_out=f(w_out),
        w_rt=np.ascontiguousarray(np.concatenate([f(w_router_group), f(w_router_expert)], axis=-1)),
        b_rt=np.ascontiguousarray(np.concatenate([f(b_router_group), f(b_router_expert)], axis=-1)),
        w_exp_gate=f(w_exp_gate), w_exp_up=f(w_exp_up), w_exp_down=f(w_exp_down), norm_final=f(norm_final))
    x = f(x); c = f(c); ctx = f(ctx)
    B = x.shape[0]
    in_maps = []
    for b in range(B):
        d = dict(shared)
        d.update(x=x[b], ctx=ctx[b], c=c[b])
        in_maps.append(d)
    res = run_bass_kernel_spmd(nc, in_maps, core_ids=list(range(B)))
    return np.stack([np.asarray(r["out"], dtype=np.float32) for r in res.results], axis=0)
```

```python
import contextlib, os
import numpy as np
import ml_dtypes
import concourse.bass as bass
import concourse.mybir as mybir
from concourse.bass_utils import run_bass_kernel_spmd

F32 = mybir.dt.float32
BF16 = mybir.dt.bfloat16
AF = mybir.ActivationFunctionType
ALU = mybir.AluOpType
AX = mybir.AxisListType

ARENA_BASE = 20736
SBUF_BYTES = 229376 - ARENA_BASE - 128
SAME_ENGINE_SYNC = bool(int(os.environ.get("SES", "1")))
NDMA_SEM = 12
EPOCH = 30000


def _dsize(dt):
    return 2 if dt == BF16 else 4


class KB:
    def __init__(self, nc):
        self.nc = nc
        self.es = contextlib.ExitStack()
        self.eng = {'pe': nc.tensor, 'act': nc.scalar, 'dve': nc.vector, 'pool': nc.gpsimd, 'sp': nc.sync}
        self.cnt = {e: 0 for e in self.eng}
        self.sems = {e: [] for e in self.eng}
        self.seen = {e: {} for e in self.eng}
        self.res = {}
        self.dma_sems = {}
        self.dma_i = {}
        self.bot = ARENA_BASE
        self.top = ARENA_BASE + SBUF_BYTES
        self.nps = 0
        self.n_sem = 0
        self.peak = 0
        self.off = {}

    def __enter__(self):
        self.es.__enter__()
        return self

    def __exit__(self, *a):
        return self.es.__exit__(*a)

    def sb(self, name, shape, dtype, top=False):
        n = 1
        for s in shape[1:]:
            n *= s
        nbytes = (n * _dsize(dtype) + 63) // 64 * 64
        if top:
            self.top -= nbytes
            off = self.top
        else:
            off = self.bot
            self.bot += nbytes
        assert self.bot <= self.top, f"SBUF overflow allocating {name}: bot={self.bot} top={self.top}"
        self.peak = max(self.peak, self.bot + SBUF_BYTES - self.top)
        self.nps += 1
        self.off[name] = off
        return self.nc.alloc_sbuf_tensor_at(f"{name}_{self.nps}", list(shape), dtype, offset=off)

    def alias(self, name, shape, dtype, off):
        self.nps += 1
        return self.nc.alloc_sbuf_tensor_at(f"{name}_{self.nps}", list(shape), dtype, offset=off)

    def mark(self):
        return (self.bot, self.top)

    def release(self, m):
        self.barrier()
        self.bot, self.top = m

    def ps(self, name, shape, dtype=F32):
        return self.nc.alloc_psum_tensor(name, list(shape), dtype)

    def _newsem(self, name):
        self.n_sem += 1
        return self.es.enter_context(self.nc.semaphore(f"{name}_{self.n_sem}"))

    def _tick(self, e):
        c = self.cnt[e]
        ep, v = divmod(c, EPOCH)
        while len(self.sems[e]) <= ep:
            self.sems[e].append(self._newsem(f"s_{e}"))
        self.cnt[e] = c + 1
        return (self.sems[e][ep], v + 1, e)

    def _wait(self, e, tick):
        sem, val, owner = tick
        if owner == e and (e == 'pe' or not SAME_ENGINE_SYNC):
            return
        k = sem.num
        if self.seen[e].get(k, 0) >= val:
            return
        self.eng[e].wait_ge(sem, val)
        self.seen[e][k] = val

    def _deps(self, e, r, w):
        ticks = []
        for k in r:
            rec = self.res.get(k)
            if rec and rec[0]:
                ticks.append(rec[0])
        for k in w:
            rec = self.res.get(k)
            if rec:
                if rec[0]:
                    ticks.append(rec[0])
                ticks.extend(rec[1])
        for t in ticks:
            self._wait(e, t)

    def _record(self, tick, r, w):
        for k in r:
            rec = self.res.setdefault(k, [None, []])
            rec[1] = [t for t in rec[1] if not (t[0] is tick[0])] + [tick]
        for k in w:
            self.res[k] = [tick, []]

    def op(self, e, fn, r=(), w=()):
        self._deps(e, r, w)
        tick = self._tick(e)
        fn(self.eng[e]).then_inc(tick[0], 1)
        self._record(tick, r, w)

    def dma(self, q, out, in_, r=(), w=(), fn=None, **kw):
        self._deps(q, r, w)
        pool = self.dma_sems.setdefault(q, [])
        i = self.dma_i.get(q, 0)
        self.dma_i[q] = i + 1
        slot = i % NDMA_SEM
        if len(pool) <= slot:
            pool.append([self._newsem(f"d_{q}"), 0])
        ent = pool[slot]
        if ent[1] > 0:
            self._wait(q, (ent[0], ent[1], 'dma'))
        ent[1] += 16
        tick = (ent[0], ent[1], 'dma')
        if fn is not None:
            fn(self.eng[q]).then_inc(ent[0], 16)
        else:
            self.eng[q].dma_start(out=out, in_=in_, **kw).then_inc(ent[0], 16)
        self._record(tick, r, w)

    def all_ticks(self):
        ticks = []
        for e in self.eng:
            c = self.cnt[e]
            if c > 0:
                ep, v = divmod(c - 1, EPOCH)
                ticks.append((self.sems[e][ep], v + 1, e))
        for q, pool in self.dma_sems.items():
            for ent in pool:
                if ent[1] > 0:
                    ticks.append((ent[0], ent[1], 'dma'))
        return ticks

    def barrier(self):
        ticks = self.all_ticks()
        for e in self.eng:
            for t in ticks:
                if t[2] == e:
                    continue
                self._wait(e, t)
        self.res = {}

    def finish(self):
        ticks = self.all_ticks()
        for t in ticks:
            if t[2] != 'sp':
                self._wait('sp', t)

    def make_ident(self, ident):
        nc = self.nc
        self.op('pool', lambda e: e.memset(ident[:], 1.0), w=['ident'])
        self.op('pool', lambda e: e.affine_select(out=ident[:], in_=ident[:], pattern=[[-1, 128]],
                                                  compare_op=ALU.is_equal, fill=0.0, base=0,
                                                  channel_multiplier=1), r=['ident'], w=['ident'])

D = 1024; L = 256; N = 2048; T = 2304; NT = 18
QA, QAS, KA, KAS, VA, QR, QRS, KR, KRS, VR, GR, FU, GT = 0, 512, 1024, 1152, 1280, 1408, 1920, 2432, 2944, 3456, 3968, 4480, 4992
WEXT = 8064
CAP = int(os.environ.get('MOE_CAP', '1024'))
NS = CAP // 128
I32 = mybir.dt.int32
TB = [(0, 256), (256, 512), (768, 512), (1280, 512), (1792, 512)]


def bc(ap, free):
    return bass.AP(ap.tensor, ap.offset, [list(ap.ap[0])] + [list(f) for f in free])


def build(stop=None, nlayers=2):
    nc = bass.Bass("TRN2", target_bir_lowering=False)

    def din(name, shape, dt=F32):
        return nc.dram_tensor(name, list(shape), dt, kind="ExternalInput").ap()
    x_in = din("x", [N, D]); ctx_in = din("ctx", [L, D]); c_in = din("c", [D]); cctx_in = din("c_ctx", [D])
    norm_mix = din("norm_mix", [2, D]); norm_ffn = din("norm_ffn", [2, D])
    w_ada = din("w_ada", [2, D, 6 * D]); b_ada = din("b_ada", [2, 6 * D])
    w_ext = din("w_ext", [2, D, WEXT]); sink_in = din("attn_sink", [2, 8])
    dec_f = din("ret_decay_fwd", [2, 8]); dec_b = din("ret_decay_bwd", [2, 8])
    w_ba = din("w_branch_attn", [2, 512, D]); w_bf = din("w_branch_fourier", [2, 512, D]); w_br = din("w_branch_ret", [2, 512, D])
    w_out = din("w_out", [2, D, D]); w_rt = din("w_rt", [2, D, 36]); b_rt = din("b_rt", [2, 36])
    if stop is None or stop.startswith('moe'):
        w_eg = din("w_exp_gate", [2, 32, D, 512]); w_eu = din("w_exp_up", [2, 32, D, 512]); w_ed = din("w_exp_down", [2, 32, 512, D])
    norm_final = din("norm_final", [D])
    ropeA = din("ropeA", [2, 128, N]); ropeR = din("ropeR", [2, 128, N])
    dftN = din("dftN", [2, N, N], BF16); dft256 = din("dft256", [2, 256, 256], BF16); dftC = din("dftC", [128, 256], BF16)
    amask = din("amask", [128, 256], BF16); rconst = din("rconst", [128, 770])
    out = nc.dram_tensor("out", [N, D], F32, kind="ExternalOutput").ap()
    xbuf = nc.dram_tensor("xbuf", [T, D], F32).ap()
    mconst = din("mconst", [128, 32 + NT])
    h2d = nc.dram_tensor("h2d", [T, D], BF16).ap()
    rec_d = nc.dram_tensor("rec_d", [32 * CAP, 4], F32).ap()
    AB = nc.dram_tensor("AB", [2 * T, D], F32).ap()
    modbuf = nc.dram_tensor("modbuf", [2, 2, 6 * D], F32).ap()
    dbg = {}
    if stop is not None:
        dbg['d_x'] = nc.dram_tensor("d_x", [T, D], F32, kind="ExternalOutput").ap()
        dbg['d_mod'] = nc.dram_tensor("d_mod", [2, 6 * D], F32, kind="ExternalOutput").ap()
        dbg['d_hT'] = nc.dram_tensor("d_hT", [128, 8, T], BF16, kind="ExternalOutput").ap()
        dbg['d_oT'] = nc.dram_tensor("d_oT", [128, 4, T], BF16, kind="ExternalOutput").ap()
        dbg['d_mT'] = nc.dram_tensor("d_mT", [128, 8, T], BF16, kind="ExternalOutput").ap()

    kb = KB(nc)
    with kb:
        PSA = kb.ps("psa", [128, 7, 512], F32)
        PST = kb.ps("pst", [128, 8, 128], BF16)
        ident = kb.sb("ident", [128, 128], BF16)
        ident32 = kb.sb("ident32", [128, 128], F32)
        kb.make_ident(ident)
        kb.op('pool', lambda e: e.memset(ident32[:], 1.0), w=['ident32'])
        kb.op('pool', lambda e: e.affine_select(out=ident32[:], in_=ident32[:], pattern=[[-1, 128]], compare_op=ALU.is_equal,
                                                fill=0.0, base=0, channel_multiplier=1), r=['ident32'], w=['ident32'])
        cst = kb.sb("cst", [128, 4], F32)
        kb.op('pool', lambda e: e.memset(cst[:, 0:1], 1e-6), w=['cst'])
        kb.op('pool', lambda e: e.memset(cst[:, 1:2], 1e-5), w=['cst'])
        kb.op('pool', lambda e: e.memset(cst[:, 2:3], 1.0), w=['cst'])
        epsN = cst[:, 0:1]; epsG = cst[:, 1:2]; one1 = cst[:, 2:3]
        stg = [kb.sb(f"stg{i}", [128, 2048], F32) for i in range(2)]
        stg_i = [0]; cast_rr = [0]
        hT = kb.sb("hT", [128, 8, T], BF16)
        kb.barrier()

        stg_all = [list(stg)]

        def load_w(dst, dkey, src2d, K, W, engs=('pool',)):
            kk = max(1, 2048 // W)
            stg = stg_all[0]
            for k0 in range(0, K, kk):
                k1 = min(K, k0 + kk)
                i = stg_i[0] % len(stg); stg_i[0] += 1
                sv = stg[i][:, 0:(k1 - k0) * W].rearrange("p (k c) -> p k c", c=W)
                kb.dma('sp', sv, src2d[k0 * 128:k1 * 128, :].rearrange("(k p) c -> p k c", p=128), w=[('stg', i)])
                en = engs[cast_rr[0] % len(engs)]; cast_rr[0] += 1
                if en == 'act':
                    kb.op('act', lambda e: e.copy(out=dst[:, k0:k1, :], in_=sv), r=[('stg', i)], w=[dkey])
                else:
                    kb.op(en, lambda e: e.tensor_copy(out=dst[:, k0:k1, :], in_=sv), r=[('stg', i)], w=[dkey])

        def proj_fm(wt, wkey, c0, tok0, ntok, bank):
            for k in range(8):
                kb.op('pe', lambda e: e.matmul(PSA[:, bank, 0:ntok], lhsT=wt[:, k, c0:c0 + 128], rhs=hT[:, k, tok0:tok0 + ntok],
                                               start=(k == 0), stop=(k == 7)), r=[wkey, 'hT'], w=[('ps', bank)])

        def dump(name, src, r):
            kb.dma('sp', dbg[name], src, r=r, w=[name])

        def xdump(name, t, shape, dt):
            if stop is None or not os.environ.get('XDUMP'):
                return
            kb.barrier()
            d_ = nc.dram_tensor("x_" + name, list(shape), dt, kind="ExternalOutput").ap()
            kb.dma('sp', d_, t[:], w=["x_" + name])

        def phase_ada(l):
            m = kb.mark()
            cT = kb.sb('cT', [128, 8, 2], F32); cTb = kb.sb('cTb', [128, 8, 2], BF16)
            bsb = kb.sb('bsb', [2, 6 * D], F32); modsb = kb.sb('modsb', [2, 6 * D], F32)
            wa = [kb.sb(f'wa{i}', [128, 8, 512], BF16) for i in range(2)]
            kb.dma('sp', cT[:, :, 0], cctx_in.rearrange("(k p) -> p k", p=128), w=['cT'], allow_slow_non_contiguous=True)
            kb.dma('sp', cT[:, :, 1], c_in.rearrange("(k p) -> p k", p=128), w=['cT'], allow_slow_non_contiguous=True)
            kb.dma('sp', bsb[:], b_ada[l].partition_broadcast(2), w=['bsb'])
            kb.op('act', lambda e: e.activation(out=cTb[:], in_=cT[:], func=AF.Silu), r=['cT'], w=['cTb'])
            for nb in range(12):
                i = nb % 2
                load_w(wa[i], ('wa', i), w_ada[l][:, nb * 512:(nb + 1) * 512], 8, 512, engs=('pool', 'act'))
                for k in range(8):
                    kb.op('pe', lambda e: e.matmul(PSA[0:2, i, :], lhsT=cTb[:, k, :], rhs=wa[i][:, k, :], start=(k == 0), stop=(k == 7)),
                          r=['cTb', ('wa', i)], w=[('ps', i)])
                kb.op('dve', lambda e: e.tensor_tensor(out=modsb[:, nb * 512:(nb + 1) * 512], in0=PSA[0:2, i, :],
                                                       in1=bsb[:, nb * 512:(nb + 1) * 512], op=ALU.add), r=[('ps', i), 'bsb'], w=['modsb'])
            kb.dma('sp', modbuf[l], modsb[:], r=['modsb'], w=[('mod', l)])
            if stop == f'ada{l}':
                dump('d_mod', modsb[:], ['modsb'])
            kb.release(m)
            kb.res[('mod', l)] = None
            kb.res.pop(('mod', l))

        def phase_norm(l, which, logits=None, sparse=False):
            m = kb.mark()
            nw = norm_mix if which == 1 else norm_ffn
            si, ci = (0, 1) if which == 1 else (3, 4)
            tiles = range(NT) if (which == 1 or l == 0) else range(2, NT)
            nwb = kb.sb('nwb', [128, D], F32)
            kb.dma('sp', nwb[:], nw[l].partition_broadcast(128), w=['nwb'])
            G = []; S = []
            for row in (0, 1):
                g_ = kb.sb(f'G{row}', [128, D], F32); s_ = kb.sb(f'S{row}', [128, D], F32)
                kb.dma('sp', g_[:], modbuf[l, row, ci * D:(ci + 1) * D].partition_broadcast(128), w=[f'G{row}'])
                kb.dma('sp', s_[:], modbuf[l, row, si * D:(si + 1) * D].partition_broadcast(128), w=[f'S{row}'])
                kb.op('dve', lambda e: e.scalar_tensor_tensor(out=g_[:], in0=g_[:], scalar=1.0, in1=nwb[:], op0=ALU.add, op1=ALU.mult),
                      r=[f'G{row}', 'nwb'], w=[f'G{row}'])
                G.append(g_); S.append(s_)
            xt = [kb.sb(f'xt{i}', [128, D], F32) for i in range(2)]
            tmp = [kb.sb(f'tmp{i}', [128, D], F32) for i in range(2)]
            hb = [kb.sb(f'hb{i}', [128, D], BF16 if which == 1 else F32) for i in range(2)]
            ssq = [kb.sb(f'ssq{i}', [128, 1], F32) for i in range(2)]
            junk = kb.sb('junk', [128, D], F32)
            if which == 2:
                hbb = [kb.sb(f'hbb{i}', [128, D], BF16) for i in range(2)]
                h32 = [kb.sb(f'h32{i}', [128, 8, 128], F32) for i in range(2)]
                wrt32 = kb.sb('wrt32', [128, 8, 36], F32); brt = kb.sb('brt', [128, 36], F32)
                kb.dma('sp', wrt32[:], w_rt[l].rearrange("(k p) c -> p k c", p=128), w=['wrt32'])
                kb.dma('sp', brt[:], b_rt[l].partition_broadcast(128), w=['brt'])
            for t in tiles:
                i = t % 2; row = 0 if t < 2 else 1
                kb.dma('sp', xt[i][:], xbuf[t * 128:(t + 1) * 128, :], r=[('x', t)], w=[('xt', i)])
                kb.op('act', lambda e: e.activation(out=junk[:], in_=xt[i][:], func=AF.Square, accum_out=ssq[i][:]),
                      r=[('xt', i)], w=['junk', ('ssq', i)])
                kb.op('act', lambda e: e.activation(out=ssq[i][:], in_=ssq[i][:], func=AF.Sqrt, scale=1.0 / D, bias=epsN),
                      r=[('ssq', i)], w=[('ssq', i)])
                kb.op('dve', lambda e: e.reciprocal(out=ssq[i][:], in_=ssq[i][:]), r=[('ssq', i)], w=[('ssq', i)])
                kb.op('dve', lambda e: e.scalar_tensor_tensor(out=tmp[i][:], in0=xt[i][:], scalar=ssq[i][:, 0:1], in1=G[row][:],
                                                              op0=ALU.mult, op1=ALU.mult), r=[('xt', i), ('ssq', i), f'G{row}'], w=[('tmp', i)])
                kb.op('pool', lambda e: e.tensor_tensor(out=hb[i][:], in0=tmp[i][:], in1=S[row][:], op=ALU.add),
                      r=[('tmp', i), f'S{row}'], w=[('hb', i)])
                if which == 1:
                    for k in range(8):
                        kb.op('pe', lambda e: e.transpose(out=PST[:, k, :], in_=hb[i][:, k * 128:(k + 1) * 128], identity=ident[:]),
                              r=[('hb', i), 'ident'], w=['pst'])
                    kb.op('act', lambda e: e.copy(out=hT[:, :, t * 128:(t + 1) * 128], in_=PST[:]), r=['pst'], w=['hT'])
                else:
                    for k in range(8):
                        kb.op('pe', lambda e: e.matmul(PSA[:, 5 + k // 4, (k % 4) * 128:(k % 4 + 1) * 128], lhsT=hb[i][:, k * 128:(k + 1) * 128],
                                                       rhs=ident32[:], start=True, stop=True), r=[('hb', i), 'ident32'], w=[('ps', 5 + k // 4)])
                    pv = PSA[:, 5:7, :].rearrange("p a (b c) -> p (a b) c", c=128)
                    kb.op('dve', lambda e: e.tensor_copy(out=h32[i][:], in_=pv), r=[('ps', 5), ('ps', 6)], w=[('h32', i)])
                    if sparse:
                        kb.op('act', lambda e: e.copy(out=hbb[i][:], in_=hb[i][:]), r=[('hb', i)], w=[('hbb', i)])
                        kb.dma('sp', h2d[t * 128:(t + 1) * 128, :], hbb[i][:], r=[('hbb', i)], w=[('h2d', t)])
                    else:
                        kb.op('act', lambda e: e.copy(out=hT[:, :, t * 128:(t + 1) * 128], in_=h32[i][:]), r=[('h32', i)], w=['hT'])
                    for k in range(8):
                        kb.op('pe', lambda e: e.matmul(PSA[:, 4, 0:36], lhsT=h32[i][:, k, :], rhs=wrt32[:, k, :], start=(k == 0), stop=(k == 7)),
                              r=[('h32', i), 'wrt32'], w=[('ps', 4)])
                    kb.op('dve', lambda e: e.tensor_tensor(out=logits[:, t, :], in0=PSA[:, 4, 0:36], in1=brt[:], op=ALU.add),
                          r=[('ps', 4), 'brt'], w=['logits'])
            kb.release(m)

        def merge(l, gate_off, wb_dram, oT, okey, mT, first):
            m = kb.mark()
            wg = [kb.sb(f'mwg{i}', [128, 8, 128], BF16) for i in range(2)]
            wb = [kb.sb(f'mwb{i}', [128, 4, 128], BF16) for i in range(2)]
            sig = [kb.sb(f'sig{i}', [128, 512], F32) for i in range(2)]
            mtmp = [kb.sb(f'mtmp{i}', [128, 512], F32) for i in range(2)]
            it = 0
            for fc in range(8):
                j = fc % 2
                load_w(wg[j], ('mwg', j), w_ext[l][:, GT + gate_off + fc * 128: GT + gate_off + (fc + 1) * 128], 8, 128)
                load_w(wb[j], ('mwb', j), wb_dram[l][:, fc * 128:(fc + 1) * 128], 4, 128)
                for (s0, n) in (TB if l == 0 else TB[1:]):
                    i = it % 2; it += 1
                    proj_fm(wg[j], ('mwg', j), 0, s0, n, i)
                    kb.op('act', lambda e: e.activation(out=sig[i][:, 0:n], in_=PSA[:, i, 0:n], func=AF.Sigmoid), r=[('ps', i)], w=[('sig', i)])
                    for k in range(4):
                        kb.op('pe', lambda e: e.matmul(PSA[:, 2 + i, 0:n], lhsT=wb[j][:, k, :], rhs=oT[:, k, s0:s0 + n], start=(k == 0), stop=(k == 3)),
                              r=[('mwb', j), okey], w=[('ps', 2 + i)])
                    if first:
                        kb.op('dve', lambda e: e.tensor_tensor(out=mT[:, fc, s0:s0 + n], in0=sig[i][:, 0:n], in1=PSA[:, 2 + i, 0:n], op=ALU.mult),
                              r=[('sig', i), ('ps', 2 + i)], w=['mT'])
                    else:
                        kb.op('dve', lambda e: e.tensor_tensor(out=mtmp[i][:, 0:n], in0=sig[i][:, 0:n], in1=PSA[:, 2 + i, 0:n], op=ALU.mult),
                              r=[('sig', i), ('ps', 2 + i)], w=[('mtmp', i)])
                        kb.op('pool', lambda e: e.tensor_tensor(out=mT[:, fc, s0:s0 + n], in0=mT[:, fc, s0:s0 + n], in1=mtmp[i][:, 0:n], op=ALU.add),
                              r=[('mtmp', i), 'mT'], w=['mT'])
            kb.release(m)

        def rope_evac(bA, bB, dst, dkey, tC, tS, n, t1, t2, j):
            kb.op('dve', lambda e: e.tensor_tensor(out=t1[:, 0:n], in0=PSA[:, bA, 0:n], in1=tC[:, 0:n], op=ALU.mult), r=[('ps', bA), ('rc', j)], w=[('t1', j)])
            kb.op('dve', lambda e: e.tensor_tensor(out=t2[:, 0:n], in0=PSA[:, bB, 0:n], in1=tS[:, 0:n], op=ALU.mult), r=[('ps', bB), ('rs', j)], w=[('t2', j)])
            kb.op('pool', lambda e: e.tensor_tensor(out=dst, in0=t1[:, 0:n], in1=t2[:, 0:n], op=ALU.add), r=[('t1', j), ('t2', j)], w=[dkey])

        def phase_ret(l, retT):
            need_ctx = (l == 0)
            m = kb.mark()
            RC = kb.sb('RC', [128, 770], F32)
            kb.dma('sp', RC[:], rconst[:, :], w=['RC'])
            dpos = RC[:, 0:128]; dneg = RC[:, 128:256]; mge = RC[:, 256:384]; mlt = RC[:, 384:512]
            io1 = RC[:, 512:640]; iob = RC[:, 640:768]; pc127 = RC[:, 768:769]; pcol = RC[:, 769:770]
            lg = kb.sb('lg', [128, 16], F32)
            kb.dma('sp', lg[:, 0:8], dec_f[l].partition_broadcast(128), w=['lg'])
            kb.dma('sp', lg[:, 8:16], dec_b[l].partition_broadcast(128), w=['lg'])
            kb.op('act', lambda e: e.activation(out=lg[:], in_=lg[:], func=AF.Exp, scale=-1.0), r=['lg'], w=['lg'])
            kb.op('act', lambda e: e.activation(out=lg[:], in_=lg[:], func=AF.Ln, bias=one1), r=['lg', 'cst'], w=['lg'])
            kb.op('dve', lambda e: e.tensor_scalar(out=lg[:], in0=lg[:], scalar1=-1.0, scalar2=None, op0=ALU.mult), r=['lg'], w=['lg'])
            lgp = kb.sb('lgp', [128, 8], F32)
            for r in range(4):
                for hh in range(2):
                    for d in range(2):
                        kb.op('pool', lambda e: e.tensor_copy(out=lgp[hh * 64:(hh + 1) * 64, d * 4 + r:d * 4 + r + 1],
                                                              in_=lg[hh * 64:(hh + 1) * 64, d * 8 + 2 * r + hh:d * 8 + 2 * r + hh + 1]), r=['lg'], w=['lgp'])
            g128 = kb.sb('g128', [128, 8], F32)
            kb.op('act', lambda e: e.activation(out=g128[:], in_=lgp[:], func=AF.Exp, scale=128.0), r=['lgp'], w=['g128'])
            Z = kb.sb('Z', [128, 16], F32)
            kb.op('act', lambda e: e.activation(out=Z[:, 0:8], in_=lg[:, 0:8], func=AF.Exp, scale=pc127), r=['lg', 'RC'], w=['Z'])
            kb.op('act', lambda e: e.activation(out=Z[:, 8:16], in_=lg[:, 8:16], func=AF.Exp, scale=pcol), r=['lg', 'RC'], w=['Z'])
            kb.op('dve', lambda e: e.tensor_scalar(out=Z[:], in0=Z[:], scalar1=0.125, scalar2=None, op0=ALU.mult), r=['Z'], w=['Z'])
            DecT = kb.sb('DecT', [128, 8, 128], F32)
            d1 = kb.sb('d1', [128, 128], F32); d2 = kb.sb('d2', [128, 128], F32)
            for h in range(8):
                kb.op('act', lambda e: e.activation(out=d1[:], in_=dpos, func=AF.Exp, scale=lg[:, h:h + 1]), r=['lg', 'RC'], w=['d1'])
                kb.op('pool', lambda e: e.tensor_tensor(out=d1[:], in0=d1[:], in1=mge, op=ALU.mult), r=['d1', 'RC'], w=['d1'])
                kb.op('act', lambda e: e.activation(out=d2[:], in_=dneg, func=AF.Exp, scale=lg[:, 8 + h:9 + h]), r=['lg', 'RC'], w=['d2'])
                kb.op('pool', lambda e: e.tensor_tensor(out=d2[:], in0=d2[:], in1=mlt, op=ALU.mult), r=['d2', 'RC'], w=['d2'])
                kb.op('pool', lambda e: e.tensor_tensor(out=d1[:], in0=d1[:], in1=d2[:], op=ALU.add), r=['d1', 'd2'], w=['d1'])
                kb.op('dve', lambda e: e.tensor_scalar(out=DecT[:, h, :], in0=d1[:], scalar1=0.125, scalar2=None, op0=ALU.mult), r=['d1'], w=['DecT'])
            X = kb.sb('X', [128, 8, 128], F32)
            for r in range(4):
                kb.op('act', lambda e: e.activation(out=X[:, r, :], in_=io1, func=AF.Exp, scale=lgp[:, r:r + 1]), r=['lgp', 'RC'], w=['X'])
                kb.op('act', lambda e: e.activation(out=X[:, 4 + r, :], in_=iob, func=AF.Exp, scale=lgp[:, 4 + r:5 + r]), r=['lgp', 'RC'], w=['X'])
            if os.environ.get('RET_CUT') == '1':
                kb.release(m); return
            qT = kb.sb('qT', [128, T], BF16); kT = kb.sb('kT', [128, T], BF16)
            ktok = kb.sb('ktok', [128, NT, 128], BF16); vtok = kb.sb('vtok', [128, NT, 128], BF16)
            vf = kb.sb('vf', [128, NT, 128], BF16); vb = kb.sb('vb', [128, NT, 128], BF16)
            sg = kb.sb('sg', [128, NT, 128], F32)
            Sf = kb.sb('Sf', [128, NT, 128], BF16); Rb = kb.sb('Rb', [128, NT, 128], BF16)
            Srun = kb.sb('Srun', [128, 128], F32); Rrun = kb.sb('Rrun', [128, 128], F32)
            ws = {nm: kb.sb('w' + nm, [128, 8, 128], BF16) for nm in ('q', 'qs', 'k', 'ks', 'v', 'g')}
            rc = [kb.sb(f'rc{i}', [128, 512], F32) for i in range(2)]; rs = [kb.sb(f'rs{i}', [128, 512], F32) for i in range(2)]
            t1 = [kb.sb(f't1{i}', [128, 512], F32) for i in range(2)]; t2 = [kb.sb(f't2{i}', [128, 512], F32) for i in range(2)]
            AT = [kb.sb(f'AT{i}', [128, 2, 128], BF16) for i in range(2)]
            qxf = [kb.sb(f'qxf{i}', [128, 128], BF16) for i in range(2)]; qxb = [kb.sb(f'qxb{i}', [128, 128], BF16) for i in range(2)]
            oc = [kb.sb(f'oc{i}', [128, 128], F32) for i in range(2)]; sq = [kb.sb(f'sq{i}', [128, 128], F32) for i in range(2)]
            st = [kb.sb(f'st{i}', [128, 4], F32) for i in range(2)]
            rtok = [kb.sb(f'rtok{i}', [128, 128], BF16) for i in range(2)]
            for r in range(4):
                for nm, c0 in (('q', QR), ('qs', QRS), ('k', KR), ('ks', KRS), ('v', VR), ('g', GR)):
                    load_w(ws[nm], 'w' + nm, w_ext[l][:, c0 + r * 128:c0 + (r + 1) * 128], 8, 128)
                for bi, (s0, n) in enumerate(TB):
                    if s0 == 0:
                        proj_fm(ws['q'], 'wq', 0, s0, n, 0)
                        kb.op('act', lambda e: e.copy(out=qT[:, s0:s0 + n], in_=PSA[:, 0, 0:n]), r=[('ps', 0)], w=['qT'])
                        proj_fm(ws['k'], 'wk', 0, s0, n, 1)
                        kb.op('act', lambda e: e.copy(out=kT[:, s0:s0 + n], in_=PSA[:, 1, 0:n]), r=[('ps', 1)], w=['kT'])
                    else:
                        j = bi % 2; p0 = s0 - 256
                        kb.dma('sp', rc[j][:, 0:n], ropeR[0, :, p0:p0 + n], w=[('rc', j)])
                        kb.dma('sp', rs[j][:, 0:n], ropeR[1, :, p0:p0 + n], w=[('rs', j)])
                        proj_fm(ws['q'], 'wq', 0, s0, n, 0); proj_fm(ws['qs'], 'wqs', 0, s0, n, 1)
                        rope_evac(0, 1, qT[:, s0:s0 + n], 'qT', rc[j], rs[j], n, t1[j], t2[j], j)
                        proj_fm(ws['k'], 'wk', 0, s0, n, 2); proj_fm(ws['ks'], 'wks', 0, s0, n, 3)
                        rope_evac(2, 3, kT[:, s0:s0 + n], 'kT', rc[j], rs[j], n, t1[j], t2[j], j)
                    for t in range(s0 // 128, (s0 + n) // 128):
                        bank = 4 + (t % 2)
                        for k in range(8):
                            kb.op('pe', lambda e: e.matmul(PSA[:, bank, 0:128], lhsT=hT[:, k, t * 128:(t + 1) * 128], rhs=ws['v'][:, k, :],
                                                           start=(k == 0), stop=(k == 7)), r=['hT', 'wv'], w=[('ps', bank)])
                        for k in range(8):
                            kb.op('pe', lambda e: e.matmul(PSA[:, bank, 128:256], lhsT=hT[:, k, t * 128:(t + 1) * 128], rhs=ws['g'][:, k, :],
                                                           start=(k == 0), stop=(k == 7)), r=['hT', 'wg'], w=[('ps', bank)])
                        kb.op('act', lambda e: e.copy(out=vtok[:, t, :], in_=PSA[:, bank, 0:128]), r=[('ps', bank)], w=['vtok'])
                        kb.op('act', lambda e: e.activation(out=sg[:, t, :], in_=PSA[:, bank, 128:256], func=AF.Silu), r=[('ps', bank)], w=['sg'])
                if os.environ.get('RET_CUT') == '2':
                    kb.release(m); return
                for t in range(NT):
                    kb.op('pe', lambda e: e.transpose(out=PST[:, t % 8, :], in_=kT[:, t * 128:(t + 1) * 128], identity=ident[:]), r=['kT', 'ident'], w=['pst'])
                    if t % 8 == 7 or t == NT - 1:
                        n8 = t % 8 + 1; t0 = t - n8 + 1
                        kb.op('act', lambda e: e.copy(out=ktok[:, t0:t + 1, :], in_=PST[:, 0:n8, :]), r=['pst'], w=['ktok'])
                v4 = lambda a: a[:].rearrange("p c (h e) -> p c h e", h=2)
                kb.op('pool', lambda e: e.tensor_tensor(out=v4(vf), in0=v4(vtok), in1=bc(Z[:, 2 * r:2 * r + 2], [[0, NT], [1, 2], [0, 64]]), op=ALU.mult),
                      r=['vtok', 'Z'], w=['vf'])
                kb.op('pool', lambda e: e.tensor_tensor(out=v4(vb), in0=v4(vtok), in1=bc(Z[:, 8 + 2 * r:8 + 2 * r + 2], [[0, NT], [1, 2], [0, 64]]), op=ALU.mult),
                      r=['vtok', 'Z'], w=['vb'])
                if os.environ.get('RET_CUT') == '3':
                    kb.release(m); return
                kb.op('pool', lambda e: e.memset(Srun[:], 0.0), w=['Srun'])
                kb.op('pool', lambda e: e.memset(Sf[:, 0, :], 0.0), w=['Sf'])
                for c in range(NT - 1):
                    bank = 4 + c % 2
                    kb.op('pe', lambda e: e.matmul(PSA[:, bank, 0:128], lhsT=ktok[:, c, :], rhs=vf[:, c, :], start=True, stop=True),
                          r=['ktok', 'vf'], w=[('ps', bank)])
                    kb.op('dve', lambda e: e.scalar_tensor_tensor(out=Srun[:], in0=Srun[:], scalar=g128[:, r:r + 1], in1=PSA[:, bank, 0:128],
                                                                  op0=ALU.mult, op1=ALU.add), r=['Srun', 'g128', ('ps', bank)], w=['Srun'])
                    kb.op('act', lambda e: e.copy(out=Sf[:, c + 1, :], in_=Srun[:]), r=['Srun'], w=['Sf'])
                kb.op('pool', lambda e: e.memset(Rrun[:], 0.0), w=['Rrun'])
                kb.op('pool', lambda e: e.memset(Rb[:, 1, :], 0.0), w=['Rb'])
                order = [1, 0] + list(range(17, 2, -1)); dest = [0, 17] + list(range(16, 1, -1))
                for ii, (c, dd) in enumerate(zip(order, dest)):
                    bank = 4 + ii % 2
                    kb.op('pe', lambda e: e.matmul(PSA[:, bank, 0:128], lhsT=ktok[:, c, :], rhs=vb[:, c, :], start=True, stop=True),
                          r=['ktok', 'vb'], w=[('ps', bank)])
                    kb.op('dve', lambda e: e.scalar_tensor_tensor(out=Rrun[:], in0=Rrun[:], scalar=g128[:, 4 + r:5 + r], in1=PSA[:, bank, 0:128],
                                                                  op0=ALU.mult, op1=ALU.add), r=['Rrun', 'g128', ('ps', bank)], w=['Rrun'])
                    kb.op('act', lambda e: e.copy(out=Rb[:, dd, :], in_=Rrun[:]), r=['Rrun'], w=['Rb'])
                if os.environ.get('RET_CUT') == '4':
                    kb.release(m); return
                for c in (range(NT) if need_ctx else range(2, NT)):
                    i = c % 2; cs = slice(c * 128, (c + 1) * 128)
                    SK = os.environ.get('RET_SKIP', '')
                    for hh in range(2):
                        if 'I' in SK:
                            break
                        ps_ = slice(hh * 64, (hh + 1) * 64)
                        kb.op('pe', lambda e: e.matmul(PSA[:, i + 4 * hh, 0:128], lhsT=kT[ps_, cs], rhs=qT[ps_, cs], start=True, stop=True),
                              r=['kT', 'qT'], w=[('ps', i + 4 * hh)])
                    if 'A' not in SK:
                        for hh in range(2):
                            kb.op('dve', lambda e: e.tensor_tensor(out=AT[i][:, hh, :], in0=PSA[:, i + 4 * hh, 0:128],
                                                                   in1=DecT[:, 2 * r + hh, :], op=ALU.mult), r=[('ps', i + 4 * hh), 'DecT'], w=[('AT', i)])
                    if 'Q' not in SK:
                        kb.op('pool', lambda e: e.tensor_tensor(out=qxf[i][:], in0=qT[:, cs], in1=X[:, r, :], op=ALU.mult), r=['qT', 'X'], w=[('qxf', i)])
                        kb.op('pool', lambda e: e.tensor_tensor(out=qxb[i][:], in0=qT[:, cs], in1=X[:, 4 + r, :], op=ALU.mult), r=['qT', 'X'], w=[('qxb', i)])
                    for hh in range(2):
                        if 'O' in SK:
                            break
                        ps_ = slice(hh * 64, (hh + 1) * 64)
                        o_ = PSA[:, 2 + i, hh * 64:(hh + 1) * 64]
                        if os.environ.get('RET_X') == 'A':
                            kb.op('pe', lambda e: e.matmul(o_, lhsT=AT[i][:, hh, :], rhs=vtok[:, c, hh * 64:(hh + 1) * 64], start=True, stop=True),
                                  r=[('AT', i), 'vtok'], w=[('ps', 2 + i)])
                            continue
                        kb.op('pe', lambda e: e.matmul(o_, lhsT=AT[i][:, hh, :], rhs=vtok[:, c, hh * 64:(hh + 1) * 64], start=True, stop=False),
                              r=[('AT', i), 'vtok'], w=[('ps', 2 + i)])
                        kb.op('pe', lambda e: e.matmul(o_, lhsT=qxf[i][ps_, :], rhs=Sf[ps_, c, hh * 64:(hh + 1) * 64], start=False, stop=False),
                              r=[('qxf', i), 'Sf'], w=[('ps', 2 + i)])
                        kb.op('pe', lambda e: e.matmul(o_, lhsT=qxb[i][ps_, :], rhs=Rb[ps_, c, hh * 64:(hh + 1) * 64], start=False, stop=True),
                              r=[('qxb', i), 'Rb'], w=[('ps', 2 + i)])
                    if os.environ.get('RET_CUT') == '6':
                        continue
                    o3 = lambda a: a[:].rearrange("p (h e) -> p h e", h=2)
                    kb.op('act', lambda e: e.copy(out=oc[i][:], in_=PSA[:, 2 + i, 0:128]), r=[('ps', 2 + i)], w=[('oc', i)])
                    kb.op('dve', lambda e: e.reduce_sum(out=st[i][:, 0:2], in_=o3(oc[i]), axis=AX.X), r=[('oc', i)], w=[('st', i)])
                    kb.op('dve', lambda e: e.tensor_scalar(out=st[i][:, 0:2], in0=st[i][:, 0:2], scalar1=-1.0 / 64, scalar2=None, op0=ALU.mult),
                          r=[('st', i)], w=[('st', i)])
                    kb.op('pool', lambda e: e.tensor_tensor(out=o3(oc[i]), in0=o3(oc[i]), in1=bc(st[i][:, 0:2], [[1, 2], [0, 64]]), op=ALU.add),
                          r=[('oc', i), ('st', i)], w=[('oc', i)])
                    kb.op('pool', lambda e: e.tensor_tensor(out=sq[i][:], in0=oc[i][:], in1=oc[i][:], op=ALU.mult), r=[('oc', i)], w=[('sq', i)])
                    kb.op('dve', lambda e: e.reduce_sum(out=st[i][:, 2:4], in_=o3(sq[i]), axis=AX.X), r=[('sq', i)], w=[('st', i)])
                    kb.op('act', lambda e: e.activation(out=st[i][:, 2:4], in_=st[i][:, 2:4], func=AF.Sqrt, scale=1.0 / 64, bias=epsG),
                          r=[('st', i)], w=[('st', i)])
                    kb.op('dve', lambda e: e.reciprocal(out=st[i][:, 2:4], in_=st[i][:, 2:4]), r=[('st', i)], w=[('st', i)])
                    kb.op('pool', lambda e: e.tensor_tensor(out=o3(oc[i]), in0=o3(oc[i]), in1=bc(st[i][:, 2:4], [[1, 2], [0, 64]]), op=ALU.mult),
                          r=[('oc', i), ('st', i)], w=[('oc', i)])
                    if os.environ.get('RET_CUT') == '7':
                        continue
                    kb.op('pool', lambda e: e.tensor_tensor(out=rtok[i][:], in0=oc[i][:], in1=sg[:, c, :], op=ALU.mult), r=[('oc', i), 'sg'], w=[('rtok', i)])
                    kb.op('pe', lambda e: e.transpose(out=PST[:, 0, :], in_=rtok[i][:], identity=ident[:]), r=[('rtok', i), 'ident'], w=['pst'])
                    kb.op('act', lambda e: e.copy(out=retT[:, r, cs], in_=PST[:, 0, :]), r=['pst'], w=['retT'])
                if os.environ.get('RET_CUT') in ('5', '6', '7'):
                    kb.release(m); return
                if r == 3:
                    for nm_, t_, sh_, dt_ in (('sg', sg, [128, NT, 128], F32), ('DecT', DecT, [128, 8, 128], F32), ('X', X, [128, 8, 128], F32),
                                              ('Z', Z, [128, 16], F32), ('g128', g128, [128, 8], F32), ('lg', lg, [128, 16], F32),
                                              ('Sf', Sf, [128, NT, 128], BF16), ('Rb', Rb, [128, NT, 128], BF16), ('qT', qT, [128, T], BF16),
                                              ('kT', kT, [128, T], BF16), ('vtok', vtok, [128, NT, 128], BF16), ('ktok', ktok, [128, NT, 128], BF16),
                                              ('vf', vf, [128, NT, 128], BF16), ('AT1', AT[1], [128, 2, 128], BF16), ('qxf1', qxf[1], [128, 128], BF16),
                                              ('qxb1', qxb[1], [128, 128], BF16), ('oc1', oc[1], [128, 128], F32), ('st1', st[1], [128, 4], F32),
                                              ('sq1', sq[1], [128, 128], F32), ('rtok1', rtok[1], [128, 128], BF16)):
                        xdump(nm_, t_, sh_, dt_)
            kb.release(m)

        def phase_attn(l, oaT):
            need_ctx = (l == 0)
            m = kb.mark()
            qT = kb.sb('aqT', [128, 4, T], BF16); kT = kb.sb('akT', [128, T], BF16)
            Va = kb.sb('Va', [128, NT, 2, 66], BF16)
            msk = kb.sb('msk', [128, 256], BF16)
            kb.dma('sp', msk[:], amask[:, :], w=['msk'])
            snk = kb.sb('snk', [128, 8], F32)
            kb.dma('sp', snk[:], sink_in[l].partition_broadcast(128), w=['snk'])
            kb.op('act', lambda e: e.activation(out=snk[:], in_=snk[:], func=AF.Exp), r=['snk'], w=['snk'])
            kb.op('pool', lambda e: e.memset(Va[:, :, :, 64:66], 1.0), w=['Va'])
            wq = kb.sb('awq', [128, 8, 1024], BF16); wk = kb.sb('awk', [128, 8, 256], BF16); wv = kb.sb('awv', [128, 8, 128], BF16)
            load_w(wq, 'awq', w_ext[l][:, QA:QA + 1024], 8, 1024)
            load_w(wk, 'awk', w_ext[l][:, KA:KA + 256], 8, 256)
            load_w(wv, 'awv', w_ext[l][:, VA:VA + 128], 8, 128)
            rc = [kb.sb(f'arc{i}', [128, 512], F32) for i in range(2)]; rs = [kb.sb(f'ars{i}', [128, 512], F32) for i in range(2)]
            t1 = [kb.sb(f'at1{i}', [128, 512], F32) for i in range(2)]; t2 = [kb.sb(f'at2{i}', [128, 512], F32) for i in range(2)]
            for bi, (s0, n) in enumerate(TB):
                if s0 == 0:
                    for g in range(4):
                        proj_fm(wq, 'awq', g * 128, s0, n, g % 2)
                        kb.op('act', lambda e: e.copy(out=qT[:, g, s0:s0 + n], in_=PSA[:, g % 2, 0:n]), r=[('ps', g % 2)], w=['aqT'])
                    proj_fm(wk, 'awk', 0, s0, n, 2)
                    kb.op('act', lambda e: e.copy(out=kT[:, s0:s0 + n], in_=PSA[:, 2, 0:n]), r=[('ps', 2)], w=['akT'])
                else:
                    j = bi % 2; p0 = s0 - 256
                    kb.dma('sp', rc[j][:, 0:n], ropeA[0, :, p0:p0 + n], w=[('rc', j)])
                    kb.dma('sp', rs[j][:, 0:n], ropeA[1, :, p0:p0 + n], w=[('rs', j)])
                    for g in range(4):
                        b0 = 2 * (g % 2)
                        proj_fm(wq, 'awq', g * 128, s0, n, b0); proj_fm(wq, 'awq', 512 + g * 128, s0, n, b0 + 1)
                        rope_evac(b0, b0 + 1, qT[:, g, s0:s0 + n], 'aqT', rc[j], rs[j], n, t1[j], t2[j], j)
                    proj_fm(wk, 'awk', 0, s0, n, 4); proj_fm(wk, 'awk', 128, s0, n, 5)
                    rope_evac(4, 5, kT[:, s0:s0 + n], 'akT', rc[j], rs[j], n, t1[j], t2[j], j)
                for t in range(s0 // 128, (s0 + n) // 128):
                    for k in range(8):
                        kb.op('pe', lambda e: e.matmul(PSA[:, 6, 0:128], lhsT=hT[:, k, t * 128:(t + 1) * 128], rhs=wv[:, k, :],
                                                       start=(k == 0), stop=(k == 7)), r=['hT', 'awv'], w=[('ps', 6)])
                    kb.op('act', lambda e: e.copy(out=Va[:, t, :, 0:64], in_=PSA[:, 6, 0:128].rearrange("p (h e) -> p h e", h=2)),
                          r=[('ps', 6)], w=['Va'])
            PT = [[kb.sb(f'PT{i}_{j}', [128, 4, 128], BF16) for j in range(5)] for i in range(2)]
            oat = [kb.sb(f'oat{i}', [128, 8, 64], BF16) for i in range(2)]
            den = [kb.sb(f'den{i}', [128, 4], F32) for i in range(2)]
            it = 0
            for t in (range(NT) if need_ctx else range(2, NT)):
                if t < 2:
                    keys = [(0, None), (1, None)]
                else:
                    keys = []
                    if t > 2: keys.append((t - 1, 0))
                    keys.append((t, None))
                    if t < NT - 1: keys.append((t + 1, 1))
                    keys += [(0, None), (1, None)]
                ti = t % 2
                for h2 in range(2):
                    i = it % 2; it += 1
                    ps_ = slice(h2 * 64, (h2 + 1) * 64)
                    for ki, (kt, mk) in enumerate(keys):
                        bank = ki % 3
                        kb.op('pe', lambda e: e.matmul(PSA[:, bank, :].rearrange("p (g q) -> p g q", g=4), lhsT=kT[ps_, kt * 128:(kt + 1) * 128],
                                                       rhs=qT[ps_, :, t * 128:(t + 1) * 128], start=True, stop=True), r=['akT', 'aqT'], w=[('ps', bank)])
                        kb.op('act', lambda e: e.activation(out=PT[i][ki][:], in_=PSA[:, bank, :].rearrange("p (g q) -> p g q", g=4), func=AF.Exp, scale=0.125),
                              r=[('ps', bank)], w=[('PT', i, ki)])
                        if mk is not None:
                            kb.op('pool', lambda e: e.tensor_tensor(out=PT[i][ki][:], in0=PT[i][ki][:], in1=bc(msk[:, mk * 128:(mk + 1) * 128], [[0, 4], [1, 128]]),
                                                                    op=ALU.mult), r=[('PT', i, ki), 'msk'], w=[('PT', i, ki)])
                    ob = 3 + i
                    for g in range(4):
                        for ki, (kt, mk) in enumerate(keys):
                            kb.op('pe', lambda e: e.matmul(PSA[:, ob, g * 66:g * 66 + 65], lhsT=PT[i][ki][:, g, :], rhs=Va[:, kt, h2, 0:65],
                                                           start=(ki == 0), stop=(ki == len(keys) - 1)), r=[('PT', i, ki), 'Va'], w=[('ps', ob)])
                    ov = PSA[:, ob, 0:264].rearrange("p (g e) -> p g e", g=4)
                    kb.op('dve', lambda e: e.tensor_tensor(out=den[i][:], in0=ov[:, :, 64], in1=snk[:, h2 * 4:(h2 + 1) * 4], op=ALU.add),
                          r=[('ps', ob), 'snk'], w=[('den', i)])
                    kb.op('dve', lambda e: e.reciprocal(out=den[i][:], in_=den[i][:]), r=[('den', i)], w=[('den', i)])
                    kb.op('dve', lambda e: e.tensor_tensor(out=oat[ti][:, h2 * 4:(h2 + 1) * 4, :], in0=ov[:, :, 0:64], in1=bc(den[i][:, 0:4], [[1, 4], [0, 64]]),
                                                           op=ALU.mult), r=[('ps', ob), ('den', i)], w=[('oat', ti)])
                for k in range(4):
                    kb.op('pe', lambda e: e.transpose(out=PST[:, k, :], in_=oat[ti][:, 2 * k:2 * k + 2, :].rearrange("p h e -> p (h e)"), identity=ident[:]),
                          r=[('oat', ti), 'ident'], w=['pst'])
                kb.op('act', lambda e: e.copy(out=oaT[:, :, t * 128:(t + 1) * 128], in_=PST[:, 0:4, :]), r=['pst'], w=['oaT'])
            kb.release(m)

        def phase_four(l, ofT):
            need_ctx = (l == 0)
            m = kb.mark()
            wfu = kb.sb('wfu', [128, 8, 512], BF16)
            load_w(wfu, 'wfu', w_ext[l][:, FU:FU + 512], 8, 512)
            dC = kb.sb('dC', [128, 256], BF16)
            kb.dma('sp', dC[:], dftC[:, :], w=['dC'])
            W = kb.sb('W', [128, NT, 4, 256], BF16)
            uT = [kb.sb(f'uT{i}', [128, T], BF16) for i in range(2)]
            for g in range(4):
                i = g % 2
                for bi, (s0, n) in enumerate(TB):
                    proj_fm(wfu, 'wfu', g * 128, s0, n, bi % 2)
                    kb.op('act', lambda e: e.copy(out=uT[i][:, s0:s0 + n], in_=PSA[:, bi % 2, 0:n]), r=[('ps', bi % 2)], w=[('uT', i)])
                for t in range(NT):
                    bank = 2 + t % 2
                    kb.op('pe', lambda e: e.matmul(PSA[:, bank, 0:256], lhsT=uT[i][:, t * 128:(t + 1) * 128], rhs=dC[:], start=True, stop=True),
                          r=[('uT', i), 'dC'], w=[('ps', bank)])
                    kb.op('dve', lambda e: e.tensor_copy(out=W[:, t, g, :], in_=PSA[:, bank, 0:256]), r=[('ps', bank)], w=['W'])
            Cb = [kb.sb(f'Cb{i}', [128, 16, 256], BF16) for i in range(2)]
            Nb = [kb.sb(f'Nb{i}', [128, 16, 256], BF16) for i in range(2)]
            it = 0
            for nb in range(8):
                i = nb % 2
                kb.dma('sp', Cb[i][:], dftN[0, :, nb * 256:(nb + 1) * 256].rearrange("(t p) c -> p t c", p=128), w=[('Cb', i)])
                kb.dma('sp', Nb[i][:], dftN[1, :, nb * 256:(nb + 1) * 256].rearrange("(t p) c -> p t c", p=128), w=[('Nb', i)])
                for g in range(4):
                    bank = 4 + it % 2; it += 1
                    for t in range(16):
                        kb.op('pe', lambda e: e.matmul(PSA[:, bank, 0:256], lhsT=W[:, 2 + t, g, 0:128], rhs=Cb[i][:, t, :], start=(t == 0), stop=False),
                              r=['W', ('Cb', i)], w=[('ps', bank)])
                        kb.op('pe', lambda e: e.matmul(PSA[:, bank, 0:256], lhsT=W[:, 2 + t, g, 128:256], rhs=Nb[i][:, t, :], start=False, stop=(t == 15)),
                              r=['W', ('Nb', i)], w=[('ps', bank)])
                    kb.op('act', lambda e: e.copy(out=ofT[:, g, 256 + nb * 256:256 + (nb + 1) * 256], in_=PSA[:, bank, 0:256]), r=[('ps', bank)], w=['ofT'])
            if need_ctx:
                kb.dma('sp', Cb[0][:, 0:2, :], dft256[0].rearrange("(t p) c -> p t c", p=128), w=[('Cb', 0)])
                kb.dma('sp', Nb[0][:, 0:2, :], dft256[1].rearrange("(t p) c -> p t c", p=128), w=[('Nb', 0)])
                for g in range(4):
                    bank = 4 + g % 2
                    for t in range(2):
                        kb.op('pe', lambda e: e.matmul(PSA[:, bank, 0:256], lhsT=W[:, t, g, 0:128], rhs=Cb[0][:, t, :], start=(t == 0), stop=False),
                              r=['W', ('Cb', 0)], w=[('ps', bank)])
                        kb.op('pe', lambda e: e.matmul(PSA[:, bank, 0:256], lhsT=W[:, t, g, 128:256], rhs=Nb[0][:, t, :], start=False, stop=(t == 1)),
                              r=['W', ('Nb', 0)], w=[('ps', bank)])
                    kb.op('act', lambda e: e.copy(out=ofT[:, g, 0:256], in_=PSA[:, bank, 0:256]), r=[('ps', bank)], w=['ofT'])
            kb.release(m)

        def resid_update(l, gi, tiles, src_fn, src_keys):
            pass

        def phase_out(l, mT):
            m = kb.mark()
            wo = kb.sb('wo', [128, 8, D], BF16)
            load_w(wo, 'wo', w_out[l][:, :], 8, D)
            g1 = []
            for row in (0, 1):
                g_ = kb.sb(f'g1_{row}', [128, D], F32)
                kb.dma('sp', g_[:], modbuf[l, row, 2 * D:3 * D].partition_broadcast(128), w=[f'g1_{row}'])
                g1.append(g_)
            xt = [kb.sb(f'oxt{i}', [128, D], F32) for i in range(2)]
            yt = [kb.sb(f'oyt{i}', [128, D], F32) for i in range(2)]
            for t in (range(NT) if l == 0 else range(2, NT)):
                i = t % 2; row = 0 if t < 2 else 1
                kb.dma('sp', xt[i][:], xbuf[t * 128:(t + 1) * 128, :], r=[('x', t)], w=[('oxt', i)])
                for hf in range(2):
                    bank = 2 * i + hf
                    for fc in range(8):
                        kb.op('pe', lambda e: e.matmul(PSA[:, bank, :], lhsT=mT[:, fc, t * 128:(t + 1) * 128], rhs=wo[:, fc, hf * 512:(hf + 1) * 512],
                                                       start=(fc == 0), stop=(fc == 7)), r=['mT', 'wo'], w=[('ps', bank)])
                    kb.op('dve', lambda e: e.tensor_tensor(out=yt[i][:, hf * 512:(hf + 1) * 512], in0=PSA[:, bank, :], in1=g1[row][:, hf * 512:(hf + 1) * 512],
                                                           op=ALU.mult), r=[('ps', bank), f'g1_{row}'], w=[('oyt', i)])
                kb.op('pool', lambda e: e.tensor_tensor(out=yt[i][:], in0=yt[i][:], in1=xt[i][:], op=ALU.add), r=[('oyt', i), ('oxt', i)], w=[('oyt', i)])
                kb.dma('sp', xbuf[t * 128:(t + 1) * 128, :], yt[i][:], r=[('oyt', i)], w=[('x', t)])
            kb.release(m)

        def phase_moe(l):
            m = kb.mark()
            tiles = list(range(NT) if l == 0 else range(2, NT))
            blocks = TB if l == 0 else TB[1:]
            logits = kb.sb('logits', [128, NT, 36], F32)
            Wt = kb.sb('Wt', [128, NT, 32], F32)
            kb.op('pool', lambda e: e.memset(logits[:], 0.0), w=['logits'])
            phase_norm(l, 2, logits)
            if os.environ.get('MOE_CUT') == '1':
                kb.release(m); return
            m2 = kb.mark()
            lgG = logits[:, :, 0:4]; lgE = logits[:, :, 4:36]
            gmax = kb.sb('gmax', [128, NT], F32); ohg = kb.sb('ohg', [128, NT, 4], F32); eg = kb.sb('eg', [128, NT, 4], F32)
            pg = kb.sb('pg', [128, NT], F32); me = kb.sb('me', [128, NT, 32], F32); oh1 = kb.sb('oh1', [128, NT, 32], F32)
            oh2 = kb.sb('oh2', [128, NT, 32], F32); m1 = kb.sb('m1', [128, NT], F32); m2_ = kb.sb('m2', [128, NT], F32)
            w1 = kb.sb('w1', [128, NT], F32); w2 = kb.sb('w2', [128, NT], F32)
            b1 = lambda a, n_: bc(a, [[1, NT], [0, n_]])
            kb.op('dve', lambda e: e.reduce_max(out=gmax[:], in_=lgG, axis=AX.X), r=['logits'], w=['gmax'])
            kb.op('dve', lambda e: e.tensor_tensor(out=ohg[:], in0=lgG, in1=b1(gmax[:, 0:NT], 4), op=ALU.is_equal), r=['logits', 'gmax'], w=['ohg'])
            kb.op('dve', lambda e: e.tensor_tensor(out=eg[:], in0=lgG, in1=b1(gmax[:, 0:NT], 4), op=ALU.subtract), r=['logits', 'gmax'], w=['eg'])
            kb.op('act', lambda e: e.activation(out=eg[:], in_=eg[:], func=AF.Exp), r=['eg'], w=['eg'])
            kb.op('dve', lambda e: e.reduce_sum(out=pg[:], in_=eg[:], axis=AX.X), r=['eg'], w=['pg'])
            kb.op('dve', lambda e: e.reciprocal(out=pg[:], in_=pg[:]), r=['pg'], w=['pg'])
            kb.op('dve', lambda e: e.tensor_scalar(out=ohg[:], in0=ohg[:], scalar1=-1.0, scalar2=1e30, op0=ALU.add, op1=ALU.mult), r=['ohg'], w=['ohg'])
            kb.op('dve', lambda e: e.tensor_tensor(out=me[:].rearrange("p t (g x) -> p t g x", g=4), in0=lgE.rearrange("p t (g x) -> p t g x", g=4),
                                                   in1=bc(ohg[:, 0:NT, :], [[4, NT], [1, 4], [0, 8]]), op=ALU.add), r=['logits', 'ohg'], w=['me'])
            kb.op('dve', lambda e: e.reduce_max(out=m1[:], in_=me[:], axis=AX.X), r=['me'], w=['m1'])
            kb.op('dve', lambda e: e.tensor_tensor(out=oh1[:], in0=me[:], in1=b1(m1[:, 0:NT], 32), op=ALU.is_equal), r=['me', 'm1'], w=['oh1'])
            kb.op('dve', lambda e: e.scalar_tensor_tensor(out=me[:], in0=oh1[:], scalar=-1e30, in1=me[:], op0=ALU.mult, op1=ALU.add), r=['oh1', 'me'], w=['me'])
            kb.op('dve', lambda e: e.reduce_max(out=m2_[:], in_=me[:], axis=AX.X), r=['me'], w=['m2'])
            kb.op('dve', lambda e: e.tensor_tensor(out=oh2[:], in0=me[:], in1=b1(m2_[:, 0:NT], 32), op=ALU.is_equal), r=['me', 'm2'], w=['oh2'])
            kb.op('dve', lambda e: e.tensor_tensor(out=w1[:], in0=m1[:], in1=m2_[:], op=ALU.subtract), r=['m1', 'm2'], w=['w1'])
            kb.op('act', lambda e: e.activation(out=w2[:], in_=w1[:], func=AF.Sigmoid, scale=-1.0), r=['w1'], w=['w2'])
            kb.op('act', lambda e: e.activation(out=w1[:], in_=w1[:], func=AF.Sigmoid), r=['w1'], w=['w1'])
            kb.op('dve', lambda e: e.tensor_tensor(out=w1[:], in0=w1[:], in1=pg[:], op=ALU.mult), r=['w1', 'pg'], w=['w1'])
            kb.op('dve', lambda e: e.tensor_tensor(out=w2[:], in0=w2[:], in1=pg[:], op=ALU.mult), r=['w2', 'pg'], w=['w2'])
            kb.op('dve', lambda e: e.tensor_tensor(out=oh1[:], in0=oh1[:], in1=b1(w1[:, 0:NT], 32), op=ALU.mult), r=['oh1', 'w1'], w=['oh1'])
            kb.op('dve', lambda e: e.tensor_tensor(out=oh2[:], in0=oh2[:], in1=b1(w2[:, 0:NT], 32), op=ALU.mult), r=['oh2', 'w2'], w=['oh2'])
            kb.op('dve', lambda e: e.tensor_tensor(out=Wt[:], in0=oh1[:], in1=oh2[:], op=ALU.add), r=['oh1', 'oh2'], w=['Wt'])
            kb.release(m2)
            if os.environ.get('MOE_CUT') == '2':
                kb.release(m); return
            acc = kb.sb('acc', [128, NT, D], F32)
            m3 = kb.mark()
            wg = [kb.sb(f'ewg{i}', [128, 8, 512], BF16) for i in range(2)]
            wu = [kb.sb(f'ewu{i}', [128, 8, 512], BF16) for i in range(2)]
            wd = [kb.sb(f'ewd{i}', [128, 4, D], BF16) for i in range(2)]
            aT = [kb.sb(f'aT{i}', [128, 4, 512], BF16) for i in range(2)]
            sgt = [kb.sb(f'sgt{i}', [128, 512], F32) for i in range(2)]
            it = 0; ih = 0
            for ex in range(int(os.environ.get('MOE_NEXP', '32'))):
                j = ex % 2
                load_w(wg[j], ('ewg', j), w_eg[l, ex], 8, 512, engs=('pool', 'act'))
                load_w(wu[j], ('ewu', j), w_eu[l, ex], 8, 512, engs=('pool', 'act'))
                load_w(wd[j], ('ewd', j), w_ed[l, ex], 4, D, engs=('pool', 'act'))
                for (s0, n) in blocks:
                    i = it % 2; it += 1
                    for hc in range(4):
                        ii = ih % 2; ih += 1
                        for k in range(8):
                            kb.op('pe', lambda e: e.matmul(PSA[:, ii, 0:n], lhsT=wg[j][:, k, hc * 128:(hc + 1) * 128], rhs=hT[:, k, s0:s0 + n],
                                                           start=(k == 0), stop=(k == 7)), r=[('ewg', j), 'hT'], w=[('ps', ii)])
                        for k in range(8):
                            kb.op('pe', lambda e: e.matmul(PSA[:, 2 + ii, 0:n], lhsT=wu[j][:, k, hc * 128:(hc + 1) * 128], rhs=hT[:, k, s0:s0 + n],
                                                           start=(k == 0), stop=(k == 7)), r=[('ewu', j), 'hT'], w=[('ps', 2 + ii)])
                        kb.op('act', lambda e: e.activation(out=sgt[ii][:, 0:n], in_=PSA[:, ii, 0:n], func=AF.Silu), r=[('ps', ii)], w=[('sgt', ii)])
                        kb.op('dve', lambda e: e.tensor_tensor(out=aT[i][:, hc, 0:n], in0=sgt[ii][:, 0:n], in1=PSA[:, 2 + ii, 0:n], op=ALU.mult),
                              r=[('sgt', ii), ('ps', 2 + ii)], w=[('aT', i)])
                    for t in range(s0 // 128, (s0 + n) // 128):
                        tl = t * 128 - s0
                        for hf in range(2):
                            bank = 4 + (2 * t + hf) % 3
                            for hc in range(4):
                                kb.op('pe', lambda e: e.matmul(PSA[:, bank, :], lhsT=aT[i][:, hc, tl:tl + 128], rhs=wd[j][:, hc, hf * 512:(hf + 1) * 512],
                                                               start=(hc == 0), stop=(hc == 3)), r=[('aT', i), ('ewd', j)], w=[('ps', bank)])
                            a_ = acc[:, t, hf * 512:(hf + 1) * 512]
                            if ex == 0:
                                kb.op('dve', lambda e: e.tensor_scalar(out=a_, in0=PSA[:, bank, :], scalar1=Wt[:, t, ex:ex + 1], scalar2=None, op0=ALU.mult),
                                      r=[('ps', bank), 'Wt'], w=[('acc', t)])
                            else:
                                kb.op('dve', lambda e: e.scalar_tensor_tensor(out=a_, in0=PSA[:, bank, :], scalar=Wt[:, t, ex:ex + 1], in1=a_,
                                                                              op0=ALU.mult, op1=ALU.add), r=[('ps', bank), 'Wt', ('acc', t)], w=[('acc', t)])
            kb.release(m3)
            g2 = []
            for row in (0, 1):
                g_ = kb.sb(f'g2_{row}', [128, D], F32)
                kb.dma('sp', g_[:], modbuf[l, row, 5 * D:6 * D].partition_broadcast(128), w=[f'g2_{row}'])
                g2.append(g_)
            xt = [kb.sb(f'mxt{i}', [128, D], F32) for i in range(2)]
            for t in tiles:
                i = t % 2; row = 0 if t < 2 else 1
                kb.dma('sp', xt[i][:], xbuf[t * 128:(t + 1) * 128, :], r=[('x', t)], w=[('mxt', i)])
                kb.op('dve', lambda e: e.tensor_tensor(out=acc[:, t, :], in0=acc[:, t, :], in1=g2[row][:], op=ALU.mult), r=[('acc', t), f'g2_{row}'], w=[('acc', t)])
                kb.op('pool', lambda e: e.tensor_tensor(out=xt[i][:], in0=xt[i][:], in1=acc[:, t, :], op=ALU.add), r=[('acc', t), ('mxt', i)], w=[('mxt', i)])
                kb.dma('sp', xbuf[t * 128:(t + 1) * 128, :], xt[i][:], r=[('mxt', i)], w=[('x', t)])
            kb.release(m)

        moe_state = {}

        def phase_moe_sparse(l):
            IOA = bass.IndirectOffsetOnAxis
            ABv = AB.rearrange("r (h c) -> (r h) c", h=2)
            if 'bregs' not in moe_state:
                regs = {}
                for nm_, v_ in (('rec', 32 * CAP - 1), ('tok', T - 1), ('ab', 2 * T - 1)):
                    rg = nc.gpsimd.alloc_register('bnd_' + nm_)
                    nc.gpsimd.reg_mov(rg, v_)
                    regs[nm_] = rg
                moe_state['bregs'] = regs
            BR = moe_state['bregs']
            m = kb.mark()
            t0 = 0 if l == 0 else 2
            tiles = list(range(t0, NT))
            logits = kb.sb('logits', [128, NT, 36], F32)
            kb.op('pool', lambda e: e.memset(logits[:], 0.0), w=['logits'])
            phase_norm(l, 2, logits, sparse=True)
            MC = kb.sb('MC', [128, 32 + NT], F32)
            kb.dma('sp', MC[:], mconst[:, :], w=['MC'])
            eC = MC[:, 0:32]; tokid = MC[:, 32:32 + NT]
            zt = kb.sb('zt', [128, D], F32)
            kb.op('pool', lambda e: e.memset(zt[:], 0.0), w=['zt'])
            for q in range(2 * NT):
                kb.dma('sp', AB[q * 128:(q + 1) * 128, :], zt[:], r=['zt'], w=[('ABz', q)])
            ri_ = kb.sb('recinit', [128, (32 * CAP) // 128, 4], F32)
            kb.op('pool', lambda e: e.memset(ri_[:], 1.0e6), w=['recinit'])
            kb.op('pool', lambda e: e.memset(ri_[:, :, 2:3], 0.0), r=['recinit'], w=['recinit'])
            kb.dma('sp', rec_d.rearrange("(p s) c -> p s c", p=128), ri_[:], r=['recinit'], w=['rec_d'])
            lgG = logits[:, :, 0:4]; lgE = logits[:, :, 4:36]
            gmax = kb.sb('gmax', [128, NT], F32); ohg = kb.sb('ohg', [128, NT, 4], F32); eg = kb.sb('eg', [128, NT, 4], F32)
            pg = kb.sb('pg', [128, NT], F32); me = kb.sb('me', [128, NT, 32], F32); oh1 = kb.sb('oh1', [128, NT, 32], F32)
            oh2 = kb.sb('oh2', [128, NT, 32], F32); m1 = kb.sb('m1', [128, NT], F32); m2_ = kb.sb('m2', [128, NT], F32)
            w1 = kb.sb('w1', [128, NT], F32); w2 = kb.sb('w2', [128, NT], F32)
            b1 = lambda a, n_: bc(a, [[1, NT], [0, n_]])
            kb.op('dve', lambda e: e.reduce_max(out=gmax[:], in_=lgG, axis=AX.X), r=['logits'], w=['gmax'])
            kb.op('dve', lambda e: e.tensor_tensor(out=ohg[:], in0=lgG, in1=b1(gmax[:, 0:NT], 4), op=ALU.is_equal), r=['logits', 'gmax'], w=['ohg'])
            kb.op('dve', lambda e: e.tensor_tensor(out=eg[:], in0=lgG, in1=b1(gmax[:, 0:NT], 4), op=ALU.subtract), r=['logits', 'gmax'], w=['eg'])
            kb.op('act', lambda e: e.activation(out=eg[:], in_=eg[:], func=AF.Exp), r=['eg'], w=['eg'])
            kb.op('dve', lambda e: e.reduce_sum(out=pg[:], in_=eg[:], axis=AX.X), r=['eg'], w=['pg'])
            kb.op('dve', lambda e: e.reciprocal(out=pg[:], in_=pg[:]), r=['pg'], w=['pg'])
            kb.op('dve', lambda e: e.tensor_scalar(out=ohg[:], in0=ohg[:], scalar1=-1.0, scalar2=1e30, op0=ALU.add, op1=ALU.mult), r=['ohg'], w=['ohg'])
            kb.op('dve', lambda e: e.tensor_tensor(out=me[:].rearrange("p t (g x) -> p t g x", g=4), in0=lgE.rearrange("p t (g x) -> p t g x", g=4),
                                                   in1=bc(ohg[:, 0:NT, :], [[4, NT], [1, 4], [0, 8]]), op=ALU.add), r=['logits', 'ohg'], w=['me'])
            kb.op('dve', lambda e: e.reduce_max(out=m1[:], in_=me[:], axis=AX.X), r=['me'], w=['m1'])
            kb.op('dve', lambda e: e.tensor_tensor(out=oh1[:], in0=me[:], in1=b1(m1[:, 0:NT], 32), op=ALU.is_equal), r=['me', 'm1'], w=['oh1'])
            kb.op('dve', lambda e: e.scalar_tensor_tensor(out=me[:], in0=oh1[:], scalar=-1e30, in1=me[:], op0=ALU.mult, op1=ALU.add), r=['oh1', 'me'], w=['me'])
            kb.op('dve', lambda e: e.reduce_max(out=m2_[:], in_=me[:], axis=AX.X), r=['me'], w=['m2'])
            kb.op('dve', lambda e: e.tensor_tensor(out=oh2[:], in0=me[:], in1=b1(m2_[:, 0:NT], 32), op=ALU.is_equal), r=['me', 'm2'], w=['oh2'])
            kb.op('dve', lambda e: e.tensor_tensor(out=w1[:], in0=m1[:], in1=m2_[:], op=ALU.subtract), r=['m1', 'm2'], w=['w1'])
            kb.op('act', lambda e: e.activation(out=w2[:], in_=w1[:], func=AF.Sigmoid, scale=-1.0), r=['w1'], w=['w2'])
            kb.op('act', lambda e: e.activation(out=w1[:], in_=w1[:], func=AF.Sigmoid), r=['w1'], w=['w1'])
            kb.op('dve', lambda e: e.tensor_tensor(out=w1[:], in0=w1[:], in1=pg[:], op=ALU.mult), r=['w1', 'pg'], w=['w1'])
            kb.op('dve', lambda e: e.tensor_tensor(out=w2[:], in0=w2[:], in1=pg[:], op=ALU.mult), r=['w2', 'pg'], w=['w2'])
            if t0 > 0:
                kb.op('pool', lambda e: e.memset(oh1[:, 0:t0, :], 0.0), r=['oh1'], w=['oh1'])
                kb.op('pool', lambda e: e.memset(oh2[:, 0:t0, :], 0.0), r=['oh2'], w=['oh2'])
            selb = kb.sb('selb', [128, NT * 32], BF16)
            kb.op('pool', lambda e: e.tensor_tensor(out=selb[:], in0=oh1[:].rearrange("p t e -> p (t e)"), in1=oh2[:].rearrange("p t e -> p (t e)"), op=ALU.add),
                  r=['oh1', 'oh2'], w=['selb'])
            LT = kb.sb('LT', [128, 128], BF16); ones = kb.sb('ones', [128, 128], BF16)
            kb.op('pool', lambda e: e.memset(ones[:], 1.0), w=['ones'])
            kb.op('pool', lambda e: e.memset(LT[:], 1.0), w=['LT'])
            kb.op('pool', lambda e: e.affine_select(out=LT[:], in_=LT[:], pattern=[[1, 128]], compare_op=ALU.is_gt, fill=0.0, base=0,
                                                    channel_multiplier=-1), r=['LT'], w=['LT'])
            slot = kb.sb('slot', [128, NT, 32], F32); tot = kb.sb('tot', [128, NT, 32], F32); cum = kb.sb('cum', [128, NT, 32], F32)
            sl2 = slot[:].rearrange("p t e -> p (t e)"); to2 = tot[:].rearrange("p t e -> p (t e)")
            for (c0, c1, bank) in ((0, 512, 0), (512, NT * 32, 1)):
                kb.op('pe', lambda e: e.matmul(PSA[:, bank, 0:c1 - c0], lhsT=LT[:], rhs=selb[:, c0:c1], start=True, stop=True), r=['LT', 'selb'], w=[('ps', bank)])
                kb.op('dve', lambda e: e.tensor_copy(out=sl2[:, c0:c1], in_=PSA[:, bank, 0:c1 - c0]), r=[('ps', bank)], w=['slot'])
                kb.op('pe', lambda e: e.matmul(PSA[:, 2 + bank, 0:c1 - c0], lhsT=ones[:], rhs=selb[:, c0:c1], start=True, stop=True), r=['ones', 'selb'], w=[('ps', 2 + bank)])
                kb.op('dve', lambda e: e.tensor_copy(out=to2[:, c0:c1], in_=PSA[:, 2 + bank, 0:c1 - c0]), r=[('ps', 2 + bank)], w=['tot'])
            kb.op('pool', lambda e: e.memset(cum[:, 0, :], 0.0), w=['cum'])
            for t in range(1, NT):
                kb.op('dve', lambda e: e.tensor_tensor(out=cum[:, t, :], in0=cum[:, t - 1, :], in1=tot[:, t - 1, :], op=ALU.add), r=['cum', 'tot'], w=['cum'])
            kb.op('dve', lambda e: e.tensor_tensor(out=slot[:], in0=slot[:], in1=cum[:], op=ALU.add), r=['slot', 'cum'], w=['slot'])
            rec = kb.sb('rec', [128, NT, 2, 4], F32)
            rr = kb.sb('rr', [128, NT, 2], F32); rri = kb.sb('rri', [128, NT, 2], I32)
            s_ = kb.sb('s_', [128, NT], F32); e_ = kb.sb('e_', [128, NT], F32)
            kb.op('pool', lambda e: e.memset(rec[:], 0.0), w=['rec'])
            for k, (oh, wk) in enumerate(((oh1, w1), (oh2, w2))):
                kb.op('dve', lambda e: e.tensor_tensor(out=me[:], in0=oh[:], in1=slot[:], op=ALU.mult), r=['oh1', 'oh2', 'slot', 'me'], w=['me'])
                kb.op('dve', lambda e: e.reduce_sum(out=s_[:], in_=me[:], axis=AX.X), r=['me'], w=['s_'])
                kb.op('dve', lambda e: e.tensor_tensor(out=me[:], in0=oh[:], in1=bc(eC, [[0, NT], [1, 32]]), op=ALU.mult), r=['oh1', 'oh2', 'MC', 'me'], w=['me'])
                kb.op('dve', lambda e: e.reduce_sum(out=e_[:], in_=me[:], axis=AX.X), r=['me'], w=['e_'])
                kb.op('dve', lambda e: e.tensor_tensor(out=e_[:], in0=e_[:], in1=s_[:], op=ALU.add), r=['e_', 's_'], w=['e_'])
                kb.op('dve', lambda e: e.tensor_scalar(out=s_[:], in0=s_[:], scalar1=float(CAP), scalar2=1.0e6, op0=ALU.is_ge, op1=ALU.mult), r=['s_'], w=['s_'])
                kb.op('dve', lambda e: e.tensor_tensor(out=rr[:, :, k], in0=e_[:], in1=s_[:], op=ALU.add), r=['e_', 's_'], w=['rr'])
                kb.op('pool', lambda e: e.tensor_copy(out=rec[:, :, k, 0], in_=tokid), r=['MC', 'rec'], w=['rec'])
                kb.op('pool', lambda e: e.tensor_scalar(out=rec[:, :, k, 1], in0=tokid, scalar1=float(k * T), scalar2=None, op0=ALU.add), r=['MC', 'rec'], w=['rec'])
                kb.op('pool', lambda e: e.tensor_scalar(out=rec[:, :, k, 3], in0=tokid, scalar1=2.0, scalar2=float(2 * k * T + 1), op0=ALU.mult, op1=ALU.add), r=['MC', 'rec'], w=['rec'])
                kb.op('pool', lambda e: e.tensor_copy(out=rec[:, :, k, 2], in_=wk[:]), r=['w1', 'w2', 'rec'], w=['rec'])
            kb.op('dve', lambda e: e.tensor_copy(out=rri[:], in_=rr[:]), r=['rr'], w=['rri'])
            if stop == f'moe{l}' and os.environ.get('XDUMP'):
                xdump('rr', rr, [128, NT, 2], F32); xdump('rri', rri, [128, NT, 2], I32); xdump('rec', rec, [128, NT, 2, 4], F32)
                xdump('slot', slot, [128, NT, 32], F32)
            kb.barrier()
            for t in tiles:
                for k in range(2):
                    kb.dma('pool', None, None, r=['rec', 'rri'], w=[('recs', t, k)],
                           fn=lambda g: g.indirect_dma_start(out=rec_d[:, :], out_offset=IOA(ap=rri[:, t, k:k + 1], axis=0), in_=rec[:, t, k, :],
                                                             in_offset=None, bounds_check=BR['rec'], oob_is_err=False))
            kb.barrier()
            if os.environ.get('MOE_CUT') == '3':
                kb.release(m); return
            m3 = kb.mark()
            stg_all[0] = list(stg) + [kb.alias(f'stgx{i}', [128, 2048], F32, kb.off['hT'] + i * 8192) for i in range(4)]
            wg = [kb.sb(f'ewg{i}', [128, 8, 512], BF16) for i in range(2)]
            wu = [kb.sb(f'ewu{i}', [128, 8, 512], BF16) for i in range(2)]
            wd = [kb.sb(f'ewd{i}', [128, 4, D], BF16) for i in range(2)]
            XT = [kb.sb(f'XT{i}', [128, 8, CAP], BF16) for i in range(2)]
            aT = kb.sb('aT', [128, 4, CAP], BF16)
            sgt = [kb.sb(f'sgt{i}', [128, 512], F32) for i in range(2)]
            rsb = [kb.sb(f'rsb{i}', [128, NS, 4], F32) for i in range(2)]
            gi = [kb.sb(f'gi{i}', [128, NS, 4], I32) for i in range(2)]
            xg = [kb.sb(f'xg{i}', [128, D], BF16) for i in range(NS)]
            yw = [kb.sb(f'yw{i}', [128, D], F32) for i in range(3)]
            for i in range(NS):
                kb.op('pool', lambda e: e.memset(xg[i][:], 0.0), w=[('xg', i)])
            PST2 = PSA[:, 6, :].bitcast(BF16).rearrange("p (k c) -> p k c", c=128)
            stgs = stg_all[0]
            nexp = int(os.environ.get('MOE_NEXP', '32'))
            cnt = {'iy': 0, 'ih': 0}

            def w_issue(ex):
                j = ex % 2; pieces = []
                for (dst, dkey, src, K, W) in ((wg[j], ('ewg', j), w_eg[l, ex], 8, 512), (wu[j], ('ewu', j), w_eu[l, ex], 8, 512), (wd[j], ('ewd', j), w_ed[l, ex], 4, D)):
                    kk = 2048 // W
                    for k0 in range(0, K, kk):
                        i = len(pieces)
                        sv = stgs[i][:, 0:kk * W].rearrange("p (k c) -> p k c", c=W)
                        kb.dma('sp', sv, src[k0 * 128:(k0 + kk) * 128, :].rearrange("(k p) c -> p k c", p=128), w=[('stg', i)])
                        pieces.append((dst, dkey, k0, k0 + kk, sv, i))
                return pieces

            def w_cast(pieces):
                for n_, (dst, dkey, k0, k1, sv, i) in enumerate(pieces):
                    if n_ % 2 == 0:
                        kb.op('act', lambda e: e.copy(out=dst[:, k0:k1, :], in_=sv), r=[('stg', i)], w=[dkey])
                    else:
                        kb.op('dve', lambda e: e.tensor_copy(out=dst[:, k0:k1, :], in_=sv), r=[('stg', i)], w=[dkey])

            def G(ex):
                j = ex % 2
                kb.dma('sp', rsb[j][:], rec_d[ex * CAP:(ex + 1) * CAP, :].rearrange("(s p) c -> p s c", p=128), w=[('rsb', j)])
                kb.op('dve', lambda e: e.tensor_copy(out=gi[j][:], in_=rsb[j][:]), r=[('rsb', j)], w=[('gi', j)])
                for s_i in range(NS):
                    kb.dma('pool', None, None, r=[('gi', j)], w=[('xg', s_i)],
                           fn=lambda g: g.indirect_dma_start(out=xg[s_i][:], out_offset=None, in_=h2d[:, :],
                                                             in_offset=IOA(ap=gi[j][:, s_i, 0:1], axis=0), bounds_check=BR['tok'], oob_is_err=False))

            def TR(ex):
                j = ex % 2
                for s_i in range(NS):
                    pt_, pk_ = (PST, 'pst') if s_i % 2 == 0 else (PST2, ('ps', 6))
                    for k in range(8):
                        kb.op('pe', lambda e: e.transpose(out=pt_[:, k, :], in_=xg[s_i][:, k * 128:(k + 1) * 128], identity=ident[:]),
                              r=[('xg', s_i), 'ident'], w=[pk_])
                    kb.op('act', lambda e: e.copy(out=XT[j][:, :, s_i * 128:(s_i + 1) * 128], in_=pt_[:]), r=[pk_], w=[('XT', j)])

            def F1(ex):
                j = ex % 2
                for c0 in range(0, CAP, 512):
                    n = min(512, CAP - c0)
                    for hc in range(4):
                        ii = cnt['ih'] % 2; cnt['ih'] += 1
                        for k in range(8):
                            kb.op('pe', lambda e: e.matmul(PSA[:, ii, 0:n], lhsT=wg[j][:, k, hc * 128:(hc + 1) * 128], rhs=XT[j][:, k, c0:c0 + n],
                                                           start=(k == 0), stop=(k == 7)), r=[('ewg', j), ('XT', j)], w=[('ps', ii)])
                        for k in range(8):
                            kb.op('pe', lambda e: e.matmul(PSA[:, 2 + ii, 0:n], lhsT=wu[j][:, k, hc * 128:(hc + 1) * 128], rhs=XT[j][:, k, c0:c0 + n],
                                                           start=(k == 0), stop=(k == 7)), r=[('ewu', j), ('XT', j)], w=[('ps', 2 + ii)])
                        kb.op('act', lambda e: e.activation(out=sgt[ii][:, 0:n], in_=PSA[:, ii, 0:n], func=AF.Silu), r=[('ps', ii)], w=[('sgt', ii)])
                        kb.op('dve', lambda e: e.tensor_tensor(out=aT[:, hc, c0:c0 + n], in0=sgt[ii][:, 0:n], in1=PSA[:, 2 + ii, 0:n], op=ALU.mult),
                              r=[('sgt', ii), ('ps', 2 + ii)], w=['aT'])

            def F2(ex):
                j = ex % 2
                for s_i in range(NS):
                    q = cnt['iy'] % 3; cnt['iy'] += 1
                    for hf in range(2):
                        bank = 4 + hf
                        for hc in range(4):
                            kb.op('pe', lambda e: e.matmul(PSA[:, bank, :], lhsT=aT[:, hc, s_i * 128:(s_i + 1) * 128], rhs=wd[j][:, hc, hf * 512:(hf + 1) * 512],
                                                           start=(hc == 0), stop=(hc == 3)), r=['aT', ('ewd', j)], w=[('ps', bank)])
                        kb.op('dve', lambda e: e.tensor_scalar(out=yw[q][:, hf * 512:(hf + 1) * 512], in0=PSA[:, bank, :], scalar1=rsb[j][:, s_i, 2:3], scalar2=None,
                                                               op0=ALU.mult), r=[('ps', bank), ('rsb', j)], w=[('yw', q)])
                    kb.dma('pool', None, None, r=[('yw', q), ('gi', j)], w=[('ABs', ex, s_i)],
                           fn=lambda g: g.indirect_dma_start(out=AB[:, :], out_offset=IOA(ap=gi[j][:, s_i, 1:2], axis=0),
                                                             in_=yw[q][:], in_offset=None, bounds_check=BR['ab'], oob_is_err=False))

            w_cast(w_issue(0)); G(0); TR(0)
            for ex in range(nexp):
                nxt = ex + 1 < nexp
                if nxt:
                    pcs = w_issue(ex + 1)
                    G(ex + 1)
                F1(ex)
                if nxt:
                    w_cast(pcs)
                    TR(ex + 1)
                F2(ex)
            stg_all[0] = list(stg)
            stg_i[0] = 0
            kb.release(m3)
            g2 = []
            for row in (0, 1):
                g_ = kb.sb(f'g2_{row}', [128, D], F32)
                kb.dma('sp', g_[:], modbuf[l, row, 5 * D:6 * D].partition_broadcast(128), w=[f'g2_{row}'])
                g2.append(g_)
            xt = [kb.sb(f'mxt{i}', [128, D], F32) for i in range(2)]
            ya = [kb.sb(f'mya{i}', [128, D], F32) for i in range(2)]
            yb = [kb.sb(f'myb{i}', [128, D], F32) for i in range(2)]
            for t in tiles:
                i = t % 2; row = 0 if t < 2 else 1
                kb.dma('sp', xt[i][:], xbuf[t * 128:(t + 1) * 128, :], r=[('x', t)], w=[('mxt', i)])
                kb.dma('sp', ya[i][:], AB[t * 128:(t + 1) * 128, :], w=[('mya', i)])
                kb.dma('sp', yb[i][:], AB[T + t * 128:T + (t + 1) * 128, :], w=[('myb', i)])
                kb.op('pool', lambda e: e.tensor_tensor(out=ya[i][:], in0=ya[i][:], in1=yb[i][:], op=ALU.add), r=[('mya', i), ('myb', i)], w=[('mya', i)])
                kb.op('dve', lambda e: e.tensor_tensor(out=ya[i][:], in0=ya[i][:], in1=g2[row][:], op=ALU.mult), r=[('mya', i), f'g2_{row}'], w=[('mya', i)])
                kb.op('pool', lambda e: e.tensor_tensor(out=xt[i][:], in0=xt[i][:], in1=ya[i][:], op=ALU.add), r=[('mya', i), ('mxt', i)], w=[('mxt', i)])
                kb.dma('sp', xbuf[t * 128:(t + 1) * 128, :], xt[i][:], r=[('mxt', i)], w=[('x', t)])
            kb.release(m)

        def phase_final():
            m = kb.mark()
            nwb = kb.sb('fnw', [128, D], F32)
            kb.dma('sp', nwb[:], norm_final.partition_broadcast(128), w=['fnw'])
            xt = [kb.sb(f'fxt{i}', [128, D], F32) for i in range(2)]
            yt = [kb.sb(f'fyt{i}', [128, D], F32) for i in range(2)]
            ssq = [kb.sb(f'fss{i}', [128, 1], F32) for i in range(2)]
            junk = kb.sb('fjunk', [128, D], F32)
            for t in range(2, NT):
                i = t % 2
                kb.dma('sp', xt[i][:], xbuf[t * 128:(t + 1) * 128, :], r=[('x', t)], w=[('fxt', i)])
                kb.op('act', lambda e: e.activation(out=junk[:], in_=xt[i][:], func=AF.Square, accum_out=ssq[i][:]), r=[('fxt', i)], w=['fjunk', ('fss', i)])
                kb.op('act', lambda e: e.activation(out=ssq[i][:], in_=ssq[i][:], func=AF.Sqrt, scale=1.0 / D, bias=epsN), r=[('fss', i)], w=[('fss', i)])
                kb.op('dve', lambda e: e.reciprocal(out=ssq[i][:], in_=ssq[i][:]), r=[('fss', i)], w=[('fss', i)])
                kb.op('dve', lambda e: e.scalar_tensor_tensor(out=yt[i][:], in0=xt[i][:], scalar=ssq[i][:, 0:1], in1=nwb[:], op0=ALU.mult, op1=ALU.mult),
                      r=[('fxt', i), ('fss', i), 'fnw'], w=[('fyt', i)])
                kb.dma('sp', out[(t - 2) * 128:(t - 1) * 128, :], yt[i][:], r=[('fyt', i)], w=[('out', t)])
            kb.release(m)

        def finish_dbg(hT_=True, oT=None, mT=None):
            kb.barrier()
            for t in range(NT):
                pass
            kb.dma('sp', dbg['d_x'], xbuf[:, :], w=['d_x'])
            if hT_:
                kb.dma('sp', dbg['d_hT'], hT[:], w=['d_hT'])
            if oT is not None:
                kb.dma('sp', dbg['d_oT'], oT[:], w=['d_oT'])
            if mT is not None:
                kb.dma('sp', dbg['d_mT'], mT[:], w=['d_mT'])
            kb.finish()
            return nc

        kb.dma('sp', xbuf[0:L, :], ctx_in[:, :], w=['xinit0'])
        kb.dma('sp', xbuf[L:T, :], x_in[:, :], w=['xinit1'])
        for l in range(nlayers):
            phase_ada(l)
        kb.barrier()
        if stop == 'ada':
            kb.dma('sp', dbg['d_mod'], modbuf[0], w=['d_mod'])
            return finish_dbg()
        for l in range(nlayers):
            phase_norm(l, 1)
            if stop == f'norm{l}':
                return finish_dbg()
            mm = kb.mark()
            oT = kb.sb('oT', [128, 4, T], BF16, top=True)
            phase_ret(l, oT)
            if stop == f'ret{l}':
                return finish_dbg(oT=oT)
            mT = kb.sb('mT', [128, 8, T], BF16)
            merge(l, 2048, w_br, oT, 'retT', mT, True)
            phase_attn(l, oT)
            if stop == f'attn{l}':
                return finish_dbg(oT=oT)
            merge(l, 0, w_ba, oT, 'oaT', mT, False)
            phase_four(l, oT)
            if stop == f'four{l}':
                return finish_dbg(oT=oT)
            merge(l, 1024, w_bf, oT, 'ofT', mT, False)
            if stop == f'merge{l}':
                return finish_dbg(mT=mT)
            phase_out(l, mT)
            kb.release(mm)
            if stop == f'mix{l}':
                return finish_dbg()
            if os.environ.get('MOE_DENSE'):
                phase_moe(l)
            else:
                phase_moe_sparse(l)
            if stop == f'moe{l}':
                return finish_dbg()
        phase_final()
        kb.finish()
    print("SBUF peak bytes", kb.peak, "instr counts", kb.cnt)
    return nc


def _consts():
    bf = ml_dtypes.bfloat16
    f32 = np.float32
    n = np.arange(N)
    inv16 = (10000.0 ** (-(np.arange(16, dtype=f32)) / f32(16))).astype(f32)
    row = (n // 64).astype(f32); col = (n % 64).astype(f32)
    ropeA = np.zeros((2, 128, N), f32)
    for p in range(128):
        d = p % 64
        pos = row if d < 32 else col
        dd = d % 32
        ang = (pos * inv16[dd % 16]).astype(f32)
        ropeA[0, p] = np.cos(ang); ropeA[1, p] = np.sin(ang) * (-1.0 if dd < 16 else 1.0)
    inv32 = (10000.0 ** (-(np.arange(32, dtype=f32)) / f32(32))).astype(f32)
    ropeR = np.zeros((2, 128, N), f32)
    for p in range(128):
        d = p % 64
        ang = (n.astype(f32) * inv32[d % 32]).astype(f32)
        ropeR[0, p] = np.cos(ang); ropeR[1, p] = np.sin(ang) * (-1.0 if d < 32 else 1.0)
    k = np.arange(N, dtype=np.int64)
    ph = (np.outer(k, k) % N).astype(np.float64) * (2 * np.pi / N)
    dftN = np.stack([np.cos(ph), -np.sin(ph)]) / np.sqrt(N)
    k2 = np.arange(256, dtype=np.int64)
    ph2 = (np.outer(k2, k2) % 256).astype(np.float64) * (2 * np.pi / 256)
    dft256 = np.stack([np.cos(ph2), -np.sin(ph2)]) / np.sqrt(256)
    k3 = np.arange(128, dtype=np.int64)
    ph3 = (np.outer(k3, k3) % 128).astype(np.float64) * (2 * np.pi / 128)
    dftC = np.concatenate([np.cos(ph3), np.sin(ph3)], axis=1) / np.sqrt(128)
    b = np.arange(128)[:, None]; a = np.arange(128)[None, :]
    amask = np.concatenate([(b >= a), (b <= a)], axis=1).astype(f32)
    j = b; i = a
    rconst = np.concatenate([np.maximum(i - j, 0), np.maximum(j - i, 0), (i >= j), (j > i), (i + 1) + 0 * j, (128 - i) + 0 * j,
                             127 - np.arange(128)[:, None], np.arange(128)[:, None]], axis=1).astype(f32)
    mconst = np.concatenate([np.tile((np.arange(32) * CAP)[None, :], (128, 1)), np.arange(128)[:, None] + 128 * np.arange(NT)[None, :]], axis=1).astype(f32)
    return dict(mconst=mconst, ropeA=ropeA, ropeR=ropeR, dftN=dftN.astype(bf), dft256=dft256.astype(bf), dftC=dftC.astype(bf),
                amask=amask.astype(bf), rconst=rconst)


def _wext_index():
    idx = []
    swa = np.array([d + 16 if (d % 32) < 16 else d - 16 for d in range(64)])
    swr = np.array([d + 32 if d < 32 else d - 32 for d in range(64)])
    base = 0
    qa = [np.concatenate([np.arange(g * 64, (g + 1) * 64), np.arange((4 + g) * 64, (5 + g) * 64)]) for g in range(4)]
    qas = [np.concatenate([g * 64 + swa, (4 + g) * 64 + swa]) for g in range(4)]
    idx += qa + qas
    idx += [512 + np.arange(128), 512 + np.concatenate([swa, 64 + swa])]
    idx += [640 + np.arange(128)]
    idx += [768 + np.arange(512), 768 + np.concatenate([h * 64 + swr for h in range(8)])]
    idx += [1280 + np.arange(512), 1280 + np.concatenate([h * 64 + swr for h in range(8)])]
    idx += [1792 + np.arange(512), 2304 + np.arange(512), 2816 + np.arange(512), 3328 + np.arange(3072)]
    idx = np.concatenate(idx)
    assert idx.shape[0] == WEXT
    return idx


_CACHE = {}


def kernel(x, c, ctx, c_ctx, norm_mix, norm_ffn, w_ada, b_ada, w_in, attn_sink, ret_decay_fwd, ret_decay_bwd,
           w_branch_attn, w_branch_fourier, w_branch_ret, w_out, w_router_group, b_router_group,
           w_router_expert, b_router_expert, w_exp_gate, w_exp_up, w_exp_down, norm_final):
    f = lambda a: np.ascontiguousarray(np.asarray(a, dtype=np.float32))
    if 'nc' not in _CACHE:
        _CACHE['nc'] = build()
        _CACHE['consts'] = _consts()
        _CACHE['idx'] = _wext_index()
    nc = _CACHE['nc']
    shared = dict(_CACHE['consts'])
    w_in = f(w_in)
    shared.update(
        c_ctx=f(c_ctx), norm_mix=f(norm_mix), norm_ffn=f(norm_ffn), w_ada=f(w_ada), b_ada=f(b_ada),
        w_ext=np.ascontiguousarray(w_in[:, :, _CACHE['idx']]), attn_sink=f(attn_sink),
        ret_decay_fwd=f(ret_decay_fwd), ret_decay_bwd=f(ret_decay_bwd), w_branch_attn=f(w_branch_attn),
        w_branch_fourier=f(w_branch_fourier), w_branch_ret=f(w_branch_ret), w_out=f(w_out),
        w_rt=np.ascontiguousarray(np.concatenate([f(w_router_group), f(w_router_expert)], axis=-1)),
        b_rt=np.ascontiguousarray(np.concatenate([f(b_router_group), f(b_router_expert)], axis=-1)),
        w_exp_gate=f(w_exp_gate), w_exp_up=f(w_exp_up), w_exp_down=f(w_exp_down), norm_final=f(norm_final))
    x = f(x); c = f(c); ctx = f(ctx)
    B = x.shape[0]
    in_maps = []
    for b in range(B):
        d = dict(shared)
        d.update(x=x[b], ctx=ctx[b], c=c[b])
        in_maps.append(d)
    res = run_bass_kernel_spmd(nc, in_maps, core_ids=list(range(B)))
    return np.stack([np.asarray(r["out"], dtype=np.float32) for r in res.results], axis=0)
```

```python
import contextlib, os
import numpy as np
import ml_dtypes
import concourse.bass as bass
import concourse.mybir as mybir
from concourse.bass_utils import run_bass_kernel_spmd

F32 = mybir.dt.float32
BF16 = mybir.dt.bfloat16
AF = mybir.ActivationFunctionType
ALU = mybir.AluOpType
AX = mybir.AxisListType

ARENA_BASE = 20736
SBUF_BYTES = 229376 - ARENA_BASE - 128
SAME_ENGINE_SYNC = bool(int(os.environ.get("SES", "1")))
NDMA_SEM = 12
EPOCH = 30000


def _dsize(dt):
    return 2 if dt == BF16 else 4


class KB:
    def __init__(self, nc):
        self.nc = nc
        self.es = contextlib.ExitStack()
        self.eng = {'pe': nc.tensor, 'act': nc.scalar, 'dve': nc.vector, 'pool': nc.gpsimd, 'sp': nc.sync}
        self.cnt = {e: 0 for e in self.eng}
        self.sems = {e: [] for e in self.eng}
        self.seen = {e: {} for e in self.eng}
        self.res = {}
        self.dma_sems = {}
        self.dma_i = {}
        self.bot = ARENA_BASE
        self.top = ARENA_BASE + SBUF_BYTES
        self.nps = 0
        self.n_sem = 0
        self.peak = 0
        self.off = {}

    def __enter__(self):
        self.es.__enter__()
        return self

    def __exit__(self, *a):
        return self.es.__exit__(*a)

    def sb(self, name, shape, dtype, top=False):
        n = 1
        for s in shape[1:]:
            n *= s
        nbytes = (n * _dsize(dtype) + 63) // 64 * 64
        if top:
            self.top -= nbytes
            off = self.top
        else:
            off = self.bot
            self.bot += nbytes
        assert self.bot <= self.top, f"SBUF overflow allocating {name}: bot={self.bot} top={self.top}"
        self.peak = max(self.peak, self.bot + SBUF_BYTES - self.top)
        self.nps += 1
        self.off[name] = off
        return self.nc.alloc_sbuf_tensor_at(f"{name}_{self.nps}", list(shape), dtype, offset=off)

    def alias(self, name, shape, dtype, off):
        self.nps += 1
        return self.nc.alloc_sbuf_tensor_at(f"{name}_{self.nps}", list(shape), dtype, offset=off)

    def mark(self):
        return (self.bot, self.top)

    def release(self, m):
        self.barrier()
        self.bot, self.top = m

    def ps(self, name, shape, dtype=F32):
        return self.nc.alloc_psum_tensor(name, list(shape), dtype)

    def _newsem(self, name):
        self.n_sem += 1
        return self.es.enter_context(self.nc.semaphore(f"{name}_{self.n_sem}"))

    def _tick(self, e):
        c = self.cnt[e]
        ep, v = divmod(c, EPOCH)
        while len(self.sems[e]) <= ep:
            self.sems[e].append(self._newsem(f"s_{e}"))
        self.cnt[e] = c + 1
        return (self.sems[e][ep], v + 1, e)

    def _wait(self, e, tick):
        sem, val, owner = tick
        if owner == e and (e == 'pe' or not SAME_ENGINE_SYNC):
            return
        k = sem.num
        if self.seen[e].get(k, 0) >= val:
            return
        self.eng[e].wait_ge(sem, val)
        self.seen[e][k] = val

    def _deps(self, e, r, w):
        ticks = []
        for k in r:
            rec = self.res.get(k)
            if rec and rec[0]:
                ticks.append(rec[0])
        for k in w:
            rec = self.res.get(k)
            if rec:
                if rec[0]:
                    ticks.append(rec[0])
                ticks.extend(rec[1])
        for t in ticks:
            self._wait(e, t)

    def _record(self, tick, r, w):
        for k in r:
            rec = self.res.setdefault(k, [None, []])
            rec[1] = [t for t in rec[1] if not (t[0] is tick[0])] + [tick]
        for k in w:
            self.res[k] = [tick, []]

    def op(self, e, fn, r=(), w=()):
        self._deps(e, r, w)
        tick = self._tick(e)
        fn(self.eng[e]).then_inc(tick[0], 1)
        self._record(tick, r, w)

    def dma(self, q, out, in_, r=(), w=(), fn=None, **kw):
        self._deps(q, r, w)
        pool = self.dma_sems.setdefault(q, [])
        i = self.dma_i.get(q, 0)
        self.dma_i[q] = i + 1
        slot = i % NDMA_SEM
        if len(pool) <= slot:
            pool.append([self._newsem(f"d_{q}"), 0])
        ent = pool[slot]
        if ent[1] > 0:
            self._wait(q, (ent[0], ent[1], 'dma'))
        ent[1] += 16
        tick = (ent[0], ent[1], 'dma')
        if fn is not None:
            fn(self.eng[q]).then_inc(ent[0], 16)
        else:
            self.eng[q].dma_start(out=out, in_=in_, **kw).then_inc(ent[0], 16)
        self._record(tick, r, w)

    def all_ticks(self):
        ticks = []
        for e in self.eng:
            c = self.cnt[e]
            if c > 0:
                ep, v = divmod(c - 1, EPOCH)
                ticks.append((self.sems[e][ep], v + 1, e))
        for q, pool in self.dma_sems.items():
            for ent in pool:
                if ent[1] > 0:
                    ticks.append((ent[0], ent[1], 'dma'))
        return ticks

    def barrier(self):
        ticks = self.all_ticks()
        for e in self.eng:
            for t in ticks:
                if t[2] == e:
                    continue
                self._wait(e, t)
        self.res = {}

    def finish(self):
        ticks = self.all_ticks()
        for t in ticks:
            if t[2] != 'sp':
                self._wait('sp', t)

    def make_ident(self, ident):
        nc = self.nc
        self.op('pool', lambda e: e.memset(ident[:], 1.0), w=['ident'])
        self.op('pool', lambda e: e.affine_select(out=ident[:], in_=ident[:], pattern=[[-1, 128]],
                                                  compare_op=ALU.is_equal, fill=0.0, base=0,
                                                  channel_multiplier=1), r=['ident'], w=['ident'])

D = 1024; L = 256; N = 2048; T = 2304; NT = 18
QA, QAS, KA, KAS, VA, QR, QRS, KR, KRS, VR, GR, FU, GT = 0, 512, 1024, 1152, 1280, 1408, 1920, 2432, 2944, 3456, 3968, 4480, 4992
WEXT = 8064
CAP = int(os.environ.get('MOE_CAP', '1024'))
NS = CAP // 128
I32 = mybir.dt.int32
TB = [(0, 256), (256, 512), (768, 512), (1280, 512), (1792, 512)]


def bc(ap, free):
    return bass.AP(ap.tensor, ap.offset, [list(ap.ap[0])] + [list(f) for f in free])


def build(stop=None, nlayers=2):
    nc = bass.Bass("TRN2", target_bir_lowering=False)

    def din(name, shape, dt=F32):
        return nc.dram_tensor(name, list(shape), dt, kind="ExternalInput").ap()
    x_in = din("x", [N, D]); ctx_in = din("ctx", [L, D]); c_in = din("c", [D]); cctx_in = din("c_ctx", [D])
    norm_mix = din("norm_mix", [2, D]); norm_ffn = din("norm_ffn", [2, D])
    w_ada = din("w_ada", [2, D, 6 * D]); b_ada = din("b_ada", [2, 6 * D])
    w_ext = din("w_ext", [2, D, WEXT]); sink_in = din("attn_sink", [2, 8])
    dec_f = din("ret_decay_fwd", [2, 8]); dec_b = din("ret_decay_bwd", [2, 8])
    w_ba = din("w_branch_attn", [2, 512, D]); w_bf = din("w_branch_fourier", [2, 512, D]); w_br = din("w_branch_ret", [2, 512, D])
    w_out = din("w_out", [2, D, D]); w_rt = din("w_rt", [2, D, 36]); b_rt = din("b_rt", [2, 36])
    if stop is None or stop.startswith('moe'):
        w_eg = din("w_exp_gate", [2, 32, D, 512]); w_eu = din("w_exp_up", [2, 32, D, 512]); w_ed = din("w_exp_down", [2, 32, 512, D])
    norm_final = din("norm_final", [D])
    ropeA = din("ropeA", [2, 128, N]); ropeR = din("ropeR", [2, 128, N])
    dftN = din("dftN", [2, N, N], BF16); dft256 = din("dft256", [2, 256, 256], BF16); dftC = din("dftC", [128, 256], BF16)
    amask = din("amask", [128, 256], BF16); rconst = din("rconst", [128, 770])
    out = nc.dram_tensor("out", [N, D], F32, kind="ExternalOutput").ap()
    xbuf = nc.dram_tensor("xbuf", [T, D], F32).ap()
    mconst = din("mconst", [128, 32 + NT])
    h2d = nc.dram_tensor("h2d", [T, D], BF16).ap()
    rec_d = nc.dram_tensor("rec_d", [32 * CAP, 4], F32).ap()
    AB = nc.dram_tensor("AB", [2 * T, D], F32).ap()
    modbuf = nc.dram_tensor("modbuf", [2, 2, 6 * D], F32).ap()
    dbg = {}
    if stop is not None:
        dbg['d_x'] = nc.dram_tensor("d_x", [T, D], F32, kind="ExternalOutput").ap()
        dbg['d_mod'] = nc.dram_tensor("d_mod", [2, 6 * D], F32, kind="ExternalOutput").ap()
        dbg['d_hT'] = nc.dram_tensor("d_hT", [128, 8, T], BF16, kind="ExternalOutput").ap()
        dbg['d_oT'] = nc.dram_tensor("d_oT", [128, 4, T], BF16, kind="ExternalOutput").ap()
        dbg['d_mT'] = nc.dram_tensor("d_mT", [128, 8, T], BF16, kind="ExternalOutput").ap()

    kb = KB(nc)
    with kb:
        PSA = kb.ps("psa", [128, 7, 512], F32)
        PST = kb.ps("pst", [128, 8, 128], BF16)
        ident = kb.sb("ident", [128, 128], BF16)
        ident32 = kb.sb("ident32", [128, 128], F32)
        kb.make_ident(ident)
        kb.op('pool', lambda e: e.memset(ident32[:], 1.0), w=['ident32'])
        kb.op('pool', lambda e: e.affine_select(out=ident32[:], in_=ident32[:], pattern=[[-1, 128]], compare_op=ALU.is_equal,
                                                fill=0.0, base=0, channel_multiplier=1), r=['ident32'], w=['ident32'])
        cst = kb.sb("cst", [128, 4], F32)
        kb.op('pool', lambda e: e.memset(cst[:, 0:1], 1e-6), w=['cst'])
        kb.op('pool', lambda e: e.memset(cst[:, 1:2], 1e-5), w=['cst'])
        kb.op('pool', lambda e: e.memset(cst[:, 2:3], 1.0), w=['cst'])
        epsN = cst[:, 0:1]; epsG = cst[:, 1:2]; one1 = cst[:, 2:3]
        stg = [kb.sb(f"stg{i}", [128, 2048], F32) for i in range(2)]
        stg_i = [0]; cast_rr = [0]
        hT = kb.sb("hT", [128, 8, T], BF16)
        kb.barrier()

        stg_all = [list(stg)]

        def load_w(dst, dkey, src2d, K, W, engs=('pool',)):
            kk = max(1, 2048 // W)
            stg = stg_all[0]
            for k0 in range(0, K, kk):
                k1 = min(K, k0 + kk)
                i = stg_i[0] % len(stg); stg_i[0] += 1
                sv = stg[i][:, 0:(k1 - k0) * W].rearrange("p (k c) -> p k c", c=W)
                kb.dma('sp', sv, src2d[k0 * 128:k1 * 128, :].rearrange("(k p) c -> p k c", p=128), w=[('stg', i)])
                en = engs[cast_rr[0] % len(engs)]; cast_rr[0] += 1
                if en == 'act':
                    kb.op('act', lambda e: e.copy(out=dst[:, k0:k1, :], in_=sv), r=[('stg', i)], w=[dkey])
                else:
                    kb.op(en, lambda e: e.tensor_copy(out=dst[:, k0:k1, :], in_=sv), r=[('stg', i)], w=[dkey])

        def proj_fm(wt, wkey, c0, tok0, ntok, bank):
            for k in range(8):
                kb.op('pe', lambda e: e.matmul(PSA[:, bank, 0:ntok], lhsT=wt[:, k, c0:c0 + 128], rhs=hT[:, k, tok0:tok0 + ntok],
                                               start=(k == 0), stop=(k == 7)), r=[wkey, 'hT'], w=[('ps', bank)])

        def dump(name, src, r):
            kb.dma('sp', dbg[name], src, r=r, w=[name])

        def xdump(name, t, shape, dt):
            if stop is None or not os.environ.get('XDUMP'):
                return
            kb.barrier()
            d_ = nc.dram_tensor("x_" + name, list(shape), dt, kind="ExternalOutput").ap()
            kb.dma('sp', d_, t[:], w=["x_" + name])

        def phase_ada(l):
            m = kb.mark()
            cT = kb.sb('cT', [128, 8, 2], F32); cTb = kb.sb('cTb', [128, 8, 2], BF16)
            bsb = kb.sb('bsb', [2, 6 * D], F32); modsb = kb.sb('modsb', [2, 6 * D], F32)
            wa = [kb.sb(f'wa{i}', [128, 8, 512], BF16) for i in range(2)]
            kb.dma('sp', cT[:, :, 0], cctx_in.rearrange("(k p) -> p k", p=128), w=['cT'], allow_slow_non_contiguous=True)
            kb.dma('sp', cT[:, :, 1], c_in.rearrange("(k p) -> p k", p=128), w=['cT'], allow_slow_non_contiguous=True)
            kb.dma('sp', bsb[:], b_ada[l].partition_broadcast(2), w=['bsb'])
            kb.op('act', lambda e: e.activation(out=cTb[:], in_=cT[:], func=AF.Silu), r=['cT'], w=['cTb'])
            for nb in range(12):
                i = nb % 2
                load_w(wa[i], ('wa', i), w_ada[l][:, nb * 512:(nb + 1) * 512], 8, 512, engs=('pool', 'act'))
                for k in range(8):
                    kb.op('pe', lambda e: e.matmul(PSA[0:2, i, :], lhsT=cTb[:, k, :], rhs=wa[i][:, k, :], start=(k == 0), stop=(k == 7)),
                          r=['cTb', ('wa', i)], w=[('ps', i)])
                kb.op('dve', lambda e: e.tensor_tensor(out=modsb[:, nb * 512:(nb + 1) * 512], in0=PSA[0:2, i, :],
                                                       in1=bsb[:, nb * 512:(nb + 1) * 512], op=ALU.add), r=[('ps', i), 'bsb'], w=['modsb'])
            kb.dma('sp', modbuf[l], modsb[:], r=['modsb'], w=[('mod', l)])
            if stop == f'ada{l}':
                dump('d_mod', modsb[:], ['modsb'])
            kb.release(m)
            kb.res[('mod', l)] = None
            kb.res.pop(('mod', l))

        def phase_norm(l, which, logits=None, sparse=False):
            m = kb.mark()
            nw = norm_mix if which == 1 else norm_ffn
            si, ci = (0, 1) if which == 1 else (3, 4)
            tiles = range(NT) if (which == 1 or l == 0) else range(2, NT)
            nwb = kb.sb('nwb', [128, D], F32)
            kb.dma('sp', nwb[:], nw[l].partition_broadcast(128), w=['nwb'])
            G = []; S = []
            for row in (0, 1):
                g_ = kb.sb(f'G{row}', [128, D], F32); s_ = kb.sb(f'S{row}', [128, D], F32)
                kb.dma('sp', g_[:], modbuf[l, row, ci * D:(ci + 1) * D].partition_broadcast(128), w=[f'G{row}'])
                kb.dma('sp', s_[:], modbuf[l, row, si * D:(si + 1) * D].partition_broadcast(128), w=[f'S{row}'])
                kb.op('dve', lambda e: e.scalar_tensor_tensor(out=g_[:], in0=g_[:], scalar=1.0, in1=nwb[:], op0=ALU.add, op1=ALU.mult),
                      r=[f'G{row}', 'nwb'], w=[f'G{row}'])
                G.append(g_); S.append(s_)
            xt = [kb.sb(f'xt{i}', [128, D], F32) for i in range(2)]
            tmp = [kb.sb(f'tmp{i}', [128, D], F32) for i in range(2)]
            hb = [kb.sb(f'hb{i}', [128, D], BF16 if which == 1 else F32) for i in range(2)]
            ssq = [kb.sb(f'ssq{i}', [128, 1], F32) for i in range(2)]
            junk = kb.sb('junk', [128, D], F32)
            if which == 2:
                hbb = [kb.sb(f'hbb{i}', [128, D], BF16) for i in range(2)]
                h32 = [kb.sb(f'h32{i}', [128, 8, 128], F32) for i in range(2)]
                wrt32 = kb.sb('wrt32', [128, 8, 36], F32); brt = kb.sb('brt', [128, 36], F32)
                kb.dma('sp', wrt32[:], w_rt[l].rearrange("(k p) c -> p k c", p=128), w=['wrt32'])
                kb.dma('sp', brt[:], b_rt[l].partition_broadcast(128), w=['brt'])
            for t in tiles:
                i = t % 2; row = 0 if t < 2 else 1
                kb.dma('sp', xt[i][:], xbuf[t * 128:(t + 1) * 128, :], r=[('x', t)], w=[('xt', i)])
                kb.op('act', lambda e: e.activation(out=junk[:], in_=xt[i][:], func=AF.Square, accum_out=ssq[i][:]),
                      r=[('xt', i)], w=['junk', ('ssq', i)])
                kb.op('act', lambda e: e.activation(out=ssq[i][:], in_=ssq[i][:], func=AF.Sqrt, scale=1.0 / D, bias=epsN),
                      r=[('ssq', i)], w=[('ssq', i)])
                kb.op('dve', lambda e: e.reciprocal(out=ssq[i][:], in_=ssq[i][:]), r=[('ssq', i)], w=[('ssq', i)])
                kb.op('dve', lambda e: e.scalar_tensor_tensor(out=tmp[i][:], in0=xt[i][:], scalar=ssq[i][:, 0:1], in1=G[row][:],
                                                              op0=ALU.mult, op1=ALU.mult), r=[('xt', i), ('ssq', i), f'G{row}'], w=[('tmp', i)])
                kb.op('pool', lambda e: e.tensor_tensor(out=hb[i][:], in0=tmp[i][:], in1=S[row][:], op=ALU.add),
                      r=[('tmp', i), f'S{row}'], w=[('hb', i)])
                if which == 1:
                    for k in range(8):
                        kb.op('pe', lambda e: e.transpose(out=PST[:, k, :], in_=hb[i][:, k * 128:(k + 1) * 128], identity=ident[:]),
                              r=[('hb', i), 'ident'], w=['pst'])
                    kb.op('act', lambda e: e.copy(out=hT[:, :, t * 128:(t + 1) * 128], in_=PST[:]), r=['pst'], w=['hT'])
                else:
                    for k in range(8):
                        kb.op('pe', lambda e: e.matmul(PSA[:, 5 + k // 4, (k % 4) * 128:(k % 4 + 1) * 128], lhsT=hb[i][:, k * 128:(k + 1) * 128],
                                                       rhs=ident32[:], start=True, stop=True), r=[('hb', i), 'ident32'], w=[('ps', 5 + k // 4)])
                    pv = PSA[:, 5:7, :].rearrange("p a (b c) -> p (a b) c", c=128)
                    kb.op('dve', lambda e: e.tensor_copy(out=h32[i][:], in_=pv), r=[('ps', 5), ('ps', 6)], w=[('h32', i)])
                    if sparse:
                        kb.op('act', lambda e: e.copy(out=hbb[i][:], in_=hb[i][:]), r=[('hb', i)], w=[('hbb', i)])
                        kb.dma('sp', h2d[t * 128:(t + 1) * 128, :], hbb[i][:], r=[('hbb', i)], w=[('h2d', t)])
                    else:
                        kb.op('act', lambda e: e.copy(out=hT[:, :, t * 128:(t + 1) * 128], in_=h32[i][:]), r=[('h32', i)], w=['hT'])
                    for k in range(8):
                        kb.op('pe', lambda e: e.matmul(PSA[:, 4, 0:36], lhsT=h32[i][:, k, :], rhs=wrt32[:, k, :], start=(k == 0), stop=(k == 7)),
                              r=[('h32', i), 'wrt32'], w=[('ps', 4)])
                    kb.op('dve', lambda e: e.tensor_tensor(out=logits[:, t, :], in0=PSA[:, 4, 0:36], in1=brt[:], op=ALU.add),
                          r=[('ps', 4), 'brt'], w=['logits'])
            kb.release(m)

        def merge(l, gate_off, wb_dram, oT, okey, mT, first):
            m = kb.mark()
            wg = [kb.sb(f'mwg{i}', [128, 8, 128], BF16) for i in range(2)]
            wb = [kb.sb(f'mwb{i}', [128, 4, 128], BF16) for i in range(2)]
            sig = [kb.sb(f'sig{i}', [128, 512], F32) for i in range(2)]
            mtmp = [kb.sb(f'mtmp{i}', [128, 512], F32) for i in range(2)]
            it = 0
            for fc in range(8):
                j = fc % 2
                load_w(wg[j], ('mwg', j), w_ext[l][:, GT + gate_off + fc * 128: GT + gate_off + (fc + 1) * 128], 8, 128)
                load_w(wb[j], ('mwb', j), wb_dram[l][:, fc * 128:(fc + 1) * 128], 4, 128)
                for (s0, n) in (TB if l == 0 else TB[1:]):
                    i = it % 2; it += 1
                    proj_fm(wg[j], ('mwg', j), 0, s0, n, i)
                    kb.op('act', lambda e: e.activation(out=sig[i][:, 0:n], in_=PSA[:, i, 0:n], func=AF.Sigmoid), r=[('ps', i)], w=[('sig', i)])
                    for k in range(4):
                        kb.op('pe', lambda e: e.matmul(PSA[:, 2 + i, 0:n], lhsT=wb[j][:, k, :], rhs=oT[:, k, s0:s0 + n], start=(k == 0), stop=(k == 3)),
                              r=[('mwb', j), okey], w=[('ps', 2 + i)])
                    if first:
                        kb.op('dve', lambda e: e.tensor_tensor(out=mT[:, fc, s0:s0 + n], in0=sig[i][:, 0:n], in1=PSA[:, 2 + i, 0:n], op=ALU.mult),
                              r=[('sig', i), ('ps', 2 + i)], w=['mT'])
                    else:
                        kb.op('dve', lambda e: e.tensor_tensor(out=mtmp[i][:, 0:n], in0=sig[i][:, 0:n], in1=PSA[:, 2 + i, 0:n], op=ALU.mult),
                              r=[('sig', i), ('ps', 2 + i)], w=[('mtmp', i)])
                        kb.op('pool', lambda e: e.tensor_tensor(out=mT[:, fc, s0:s0 + n], in0=mT[:, fc, s0:s0 + n], in1=mtmp[i][:, 0:n], op=ALU.add),
                              r=[('mtmp', i), 'mT'], w=['mT'])
            kb.release(m)

        def rope_evac(bA, bB, dst, dkey, tC, tS, n, t1, t2, j):
            kb.op('dve', lambda e: e.tensor_tensor(out=t1[:, 0:n], in0=PSA[:, bA, 0:n], in1=tC[:, 0:n], op=ALU.mult), r=[('ps', bA), ('rc', j)], w=[('t1', j)])
            kb.op('dve', lambda e: e.tensor_tensor(out=t2[:, 0:n], in0=PSA[:, bB, 0:n], in1=tS[:, 0:n], op=ALU.mult), r=[('ps', bB), ('rs', j)], w=[('t2', j)])
            kb.op('pool', lambda e: e.tensor_tensor(out=dst, in0=t1[:, 0:n], in1=t2[:, 0:n], op=ALU.add), r=[('t1', j), ('t2', j)], w=[dkey])

        def phase_ret(l, retT):
            need_ctx = (l == 0)
            m = kb.mark()
            RC = kb.sb('RC', [128, 770], F32)
            kb.dma('sp', RC[:], rconst[:, :], w=['RC'])
            dpos = RC[:, 0:128]; dneg = RC[:, 128:256]; mge = RC[:, 256:384]; mlt = RC[:, 384:512]
            io1 = RC[:, 512:640]; iob = RC[:, 640:768]; pc127 = RC[:, 768:769]; pcol = RC[:, 769:770]
            lg = kb.sb('lg', [128, 16], F32)
            kb.dma('sp', lg[:, 0:8], dec_f[l].partition_broadcast(128), w=['lg'])
            kb.dma('sp', lg[:, 8:16], dec_b[l].partition_broadcast(128), w=['lg'])
            kb.op('act', lambda e: e.activation(out=lg[:], in_=lg[:], func=AF.Exp, scale=-1.0), r=['lg'], w=['lg'])
            kb.op('act', lambda e: e.activation(out=lg[:], in_=lg[:], func=AF.Ln, bias=one1), r=['lg', 'cst'], w=['lg'])
            kb.op('dve', lambda e: e.tensor_scalar(out=lg[:], in0=lg[:], scalar1=-1.0, scalar2=None, op0=ALU.mult), r=['lg'], w=['lg'])
            lgp = kb.sb('lgp', [128, 8], F32)
            for r in range(4):
                for hh in range(2):
                    for d in range(2):
                        kb.op('pool', lambda e: e.tensor_copy(out=lgp[hh * 64:(hh + 1) * 64, d * 4 + r:d * 4 + r + 1],
                                                              in_=lg[hh * 64:(hh + 1) * 64, d * 8 + 2 * r + hh:d * 8 + 2 * r + hh + 1]), r=['lg'], w=['lgp'])
            g128 = kb.sb('g128', [128, 8], F32)
            kb.op('act', lambda e: e.activation(out=g128[:], in_=lgp[:], func=AF.Exp, scale=128.0), r=['lgp'], w=['g128'])
            Z = kb.sb('Z', [128, 16], F32)
            kb.op('act', lambda e: e.activation(out=Z[:, 0:8], in_=lg[:, 0:8], func=AF.Exp, scale=pc127), r=['lg', 'RC'], w=['Z'])
            kb.op('act', lambda e: e.activation(out=Z[:, 8:16], in_=lg[:, 8:16], func=AF.Exp, scale=pcol), r=['lg', 'RC'], w=['Z'])
            kb.op('dve', lambda e: e.tensor_scalar(out=Z[:], in0=Z[:], scalar1=0.125, scalar2=None, op0=ALU.mult), r=['Z'], w=['Z'])
            DecT = kb.sb('DecT', [128, 8, 128], F32)
            d1 = kb.sb('d1', [128, 128], F32); d2 = kb.sb('d2', [128, 128], F32)
            for h in range(8):
                kb.op('act', lambda e: e.activation(out=d1[:], in_=dpos, func=AF.Exp, scale=lg[:, h:h + 1]), r=['lg', 'RC'], w=['d1'])
                kb.op('pool', lambda e: e.tensor_tensor(out=d1[:], in0=d1[:], in1=mge, op=ALU.mult), r=['d1', 'RC'], w=['d1'])
                kb.op('act', lambda e: e.activation(out=d2[:], in_=dneg, func=AF.Exp, scale=lg[:, 8 + h:9 + h]), r=['lg', 'RC'], w=['d2'])
                kb.op('pool', lambda e: e.tensor_tensor(out=d2[:], in0=d2[:], in1=mlt, op=ALU.mult), r=['d2', 'RC'], w=['d2'])
                kb.op('pool', lambda e: e.tensor_tensor(out=d1[:], in0=d1[:], in1=d2[:], op=ALU.add), r=['d1', 'd2'], w=['d1'])
                kb.op('dve', lambda e: e.tensor_scalar(out=DecT[:, h, :], in0=d1[:], scalar1=0.125, scalar2=None, op0=ALU.mult), r=['d1'], w=['DecT'])
            X = kb.sb('X', [128, 8, 128], F32)
            for r in range(4):
                kb.op('act', lambda e: e.activation(out=X[:, r, :], in_=io1, func=AF.Exp, scale=lgp[:, r:r + 1]), r=['lgp', 'RC'], w=['X'])
                kb.op('act', lambda e: e.activation(out=X[:, 4 + r, :], in_=iob, func=AF.Exp, scale=lgp[:, 4 + r:5 + r]), r=['lgp', 'RC'], w=['X'])
            if os.environ.get('RET_CUT') == '1':
                kb.release(m); return
            qT = kb.sb('qT', [128, T], BF16); kT = kb.sb('kT', [128, T], BF16)
            ktok = kb.sb('ktok', [128, NT, 128], BF16); vtok = kb.sb('vtok', [128, NT, 128], BF16)
            vf = kb.sb('vf', [128, NT, 128], BF16); vb = kb.sb('vb', [128, NT, 128], BF16)
            sg = kb.sb('sg', [128, NT, 128], F32)
            Sf = kb.sb('Sf', [128, NT, 128], BF16); Rb = kb.sb('Rb', [128, NT, 128], BF16)
            Srun = kb.sb('Srun', [128, 128], F32); Rrun = kb.sb('Rrun', [128, 128], F32)
            ws = {nm: kb.sb('w' + nm, [128, 8, 128], BF16) for nm in ('q', 'qs', 'k', 'ks', 'v', 'g')}
            rc = [kb.sb(f'rc{i}', [128, 512], F32) for i in range(2)]; rs = [kb.sb(f'rs{i}', [128, 512], F32) for i in range(2)]
            t1 = [kb.sb(f't1{i}', [128, 512], F32) for i in range(2)]; t2 = [kb.sb(f't2{i}', [128, 512], F32) for i in range(2)]
            AT = [kb.sb(f'AT{i}', [128, 2, 128], BF16) for i in range(2)]
            qxf = [kb.sb(f'qxf{i}', [128, 128], BF16) for i in range(2)]; qxb = [kb.sb(f'qxb{i}', [128, 128], BF16) for i in range(2)]
            oc = [kb.sb(f'oc{i}', [128, 128], F32) for i in range(2)]; sq = [kb.sb(f'sq{i}', [128, 128], F32) for i in range(2)]
            st = [kb.sb(f'st{i}', [128, 4], F32) for i in range(2)]
            rtok = [kb.sb(f'rtok{i}', [128, 128], BF16) for i in range(2)]
            o_all = kb.sb('o_all', [128, NT, 128], F32); sq_all = kb.sb('sq_all', [128, NT, 128], F32); rt_all = kb.sb('rt_all', [128, NT, 128], BF16)
            mu_ = kb.sb('mu_', [128, 2 * NT], F32); va_ = kb.sb('va_', [128, 2 * NT], F32)
            for r in range(4):
                for nm, c0 in (('q', QR), ('qs', QRS), ('k', KR), ('ks', KRS), ('v', VR), ('g', GR)):
                    load_w(ws[nm], 'w' + nm, w_ext[l][:, c0 + r * 128:c0 + (r + 1) * 128], 8, 128)
                for bi, (s0, n) in enumerate(TB):
                    if s0 == 0:
                        proj_fm(ws['q'], 'wq', 0, s0, n, 0)
                        kb.op('act', lambda e: e.copy(out=qT[:, s0:s0 + n], in_=PSA[:, 0, 0:n]), r=[('ps', 0)], w=['qT'])
                        proj_fm(ws['k'], 'wk', 0, s0, n, 1)
                        kb.op('act', lambda e: e.copy(out=kT[:, s0:s0 + n], in_=PSA[:, 1, 0:n]), r=[('ps', 1)], w=['kT'])
                    else:
                        j = bi % 2; p0 = s0 - 256
                        kb.dma('sp', rc[j][:, 0:n], ropeR[0, :, p0:p0 + n], w=[('rc', j)])
                        kb.dma('sp', rs[j][:, 0:n], ropeR[1, :, p0:p0 + n], w=[('rs', j)])
                        proj_fm(ws['q'], 'wq', 0, s0, n, 0); proj_fm(ws['qs'], 'wqs', 0, s0, n, 1)
                        rope_evac(0, 1, qT[:, s0:s0 + n], 'qT', rc[j], rs[j], n, t1[j], t2[j], j)
                        proj_fm(ws['k'], 'wk', 0, s0, n, 2); proj_fm(ws['ks'], 'wks', 0, s0, n, 3)
                        rope_evac(2, 3, kT[:, s0:s0 + n], 'kT', rc[j], rs[j], n, t1[j], t2[j], j)
                    for t in range(s0 // 128, (s0 + n) // 128):
                        bank = 4 + (t % 2)
                        for k in range(8):
                            kb.op('pe', lambda e: e.matmul(PSA[:, bank, 0:128], lhsT=hT[:, k, t * 128:(t + 1) * 128], rhs=ws['v'][:, k, :],
                                                           start=(k == 0), stop=(k == 7)), r=['hT', 'wv'], w=[('ps', bank)])
                        for k in range(8):
                            kb.op('pe', lambda e: e.matmul(PSA[:, bank, 128:256], lhsT=hT[:, k, t * 128:(t + 1) * 128], rhs=ws['g'][:, k, :],
                                                           start=(k == 0), stop=(k == 7)), r=['hT', 'wg'], w=[('ps', bank)])
                        kb.op('act', lambda e: e.copy(out=vtok[:, t, :], in_=PSA[:, bank, 0:128]), r=[('ps', bank)], w=['vtok'])
                        kb.op('act', lambda e: e.activation(out=sg[:, t, :], in_=PSA[:, bank, 128:256], func=AF.Silu), r=[('ps', bank)], w=['sg'])
                if os.environ.get('RET_CUT') == '2':
                    kb.release(m); return
                for t in range(NT):
                    kb.op('pe', lambda e: e.transpose(out=PST[:, t % 8, :], in_=kT[:, t * 128:(t + 1) * 128], identity=ident[:]), r=['kT', 'ident'], w=['pst'])
                    if t % 8 == 7 or t == NT - 1:
                        n8 = t % 8 + 1; t0 = t - n8 + 1
                        kb.op('act', lambda e: e.copy(out=ktok[:, t0:t + 1, :], in_=PST[:, 0:n8, :]), r=['pst'], w=['ktok'])
                v4 = lambda a: a[:].rearrange("p c (h e) -> p c h e", h=2)
                kb.op('pool', lambda e: e.tensor_tensor(out=v4(vf), in0=v4(vtok), in1=bc(Z[:, 2 * r:2 * r + 2], [[0, NT], [1, 2], [0, 64]]), op=ALU.mult),
                      r=['vtok', 'Z'], w=['vf'])
                kb.op('pool', lambda e: e.tensor_tensor(out=v4(vb), in0=v4(vtok), in1=bc(Z[:, 8 + 2 * r:8 + 2 * r + 2], [[0, NT], [1, 2], [0, 64]]), op=ALU.mult),
                      r=['vtok', 'Z'], w=['vb'])
                if os.environ.get('RET_CUT') == '3':
                    kb.release(m); return
                kb.op('pool', lambda e: e.memset(Srun[:], 0.0), w=['Srun'])
                kb.op('pool', lambda e: e.memset(Sf[:, 0, :], 0.0), w=['Sf'])
                for c in range(NT - 1):
                    bank = 4 + c % 2
                    kb.op('pe', lambda e: e.matmul(PSA[:, bank, 0:128], lhsT=ktok[:, c, :], rhs=vf[:, c, :], start=True, stop=True),
                          r=['ktok', 'vf'], w=[('ps', bank)])
                    kb.op('dve', lambda e: e.scalar_tensor_tensor(out=Srun[:], in0=Srun[:], scalar=g128[:, r:r + 1], in1=PSA[:, bank, 0:128],
                                                                  op0=ALU.mult, op1=ALU.add), r=['Srun', 'g128', ('ps', bank)], w=['Srun'])
                    kb.op('act', lambda e: e.copy(out=Sf[:, c + 1, :], in_=Srun[:]), r=['Srun'], w=['Sf'])
                kb.op('pool', lambda e: e.memset(Rrun[:], 0.0), w=['Rrun'])
                kb.op('pool', lambda e: e.memset(Rb[:, 1, :], 0.0), w=['Rb'])
                order = [1, 0] + list(range(17, 2, -1)); dest = [0, 17] + list(range(16, 1, -1))
                for ii, (c, dd) in enumerate(zip(order, dest)):
                    bank = 4 + ii % 2
                    kb.op('pe', lambda e: e.matmul(PSA[:, bank, 0:128], lhsT=ktok[:, c, :], rhs=vb[:, c, :], start=True, stop=True),
                          r=['ktok', 'vb'], w=[('ps', bank)])
                    kb.op('dve', lambda e: e.scalar_tensor_tensor(out=Rrun[:], in0=Rrun[:], scalar=g128[:, 4 + r:5 + r], in1=PSA[:, bank, 0:128],
                                                                  op0=ALU.mult, op1=ALU.add), r=['Rrun', 'g128', ('ps', bank)], w=['Rrun'])
                    kb.op('act', lambda e: e.copy(out=Rb[:, dd, :], in_=Rrun[:]), r=['Rrun'], w=['Rb'])
                if os.environ.get('RET_CUT') == '4':
                    kb.release(m); return
                for c in (range(NT) if need_ctx else range(2, NT)):
                    i = c % 2; cs = slice(c * 128, (c + 1) * 128)
                    SK = os.environ.get('RET_SKIP', '')
                    for hh in range(2):
                        if 'I' in SK:
                            break
                        ps_ = slice(hh * 64, (hh + 1) * 64)
                        kb.op('pe', lambda e: e.matmul(PSA[:, i + 4 * hh, 0:128], lhsT=kT[ps_, cs], rhs=qT[ps_, cs], start=True, stop=True),
                              r=['kT', 'qT'], w=[('ps', i + 4 * hh)])
                    if 'A' not in SK:
                        for hh in range(2):
                            kb.op('dve', lambda e: e.tensor_tensor(out=AT[i][:, hh, :], in0=PSA[:, i + 4 * hh, 0:128],
                                                                   in1=DecT[:, 2 * r + hh, :], op=ALU.mult), r=[('ps', i + 4 * hh), 'DecT'], w=[('AT', i)])
                    if 'Q' not in SK:
                        kb.op('pool', lambda e: e.tensor_tensor(out=qxf[i][:], in0=qT[:, cs], in1=X[:, r, :], op=ALU.mult), r=['qT', 'X'], w=[('qxf', i)])
                        kb.op('pool', lambda e: e.tensor_tensor(out=qxb[i][:], in0=qT[:, cs], in1=X[:, 4 + r, :], op=ALU.mult), r=['qT', 'X'], w=[('qxb', i)])
                    for hh in range(2):
                        if 'O' in SK:
                            break
                        ps_ = slice(hh * 64, (hh + 1) * 64)
                        o_ = PSA[:, 2 + i, hh * 64:(hh + 1) * 64]
                        if os.environ.get('RET_X') == 'A':
                            kb.op('pe', lambda e: e.matmul(o_, lhsT=AT[i][:, hh, :], rhs=vtok[:, c, hh * 64:(hh + 1) * 64], start=True, stop=True),
                                  r=[('AT', i), 'vtok'], w=[('ps', 2 + i)])
                            continue
                        kb.op('pe', lambda e: e.matmul(o_, lhsT=AT[i][:, hh, :], rhs=vtok[:, c, hh * 64:(hh + 1) * 64], start=True, stop=False),
                              r=[('AT', i), 'vtok'], w=[('ps', 2 + i)])
                        kb.op('pe', lambda e: e.matmul(o_, lhsT=qxf[i][ps_, :], rhs=Sf[ps_, c, hh * 64:(hh + 1) * 64], start=False, stop=False),
                              r=[('qxf', i), 'Sf'], w=[('ps', 2 + i)])
                        kb.op('pe', lambda e: e.matmul(o_, lhsT=qxb[i][ps_, :], rhs=Rb[ps_, c, hh * 64:(hh + 1) * 64], start=False, stop=True),
                              r=[('qxb', i), 'Rb'], w=[('ps', 2 + i)])
                    if os.environ.get('RET_CUT') == '6':
                        continue
                    kb.op('act', lambda e: e.copy(out=o_all[:, c, :], in_=PSA[:, 2 + i, 0:128]), r=[('ps', 2 + i)], w=['o_all'])
                c0_ = 0 if need_ctx else 2
                G_ = (NT - c0_) * 2
                og = o_all[:, c0_:NT, :].rearrange("p c (h e) -> p (c h) e", h=2)
                sqg = sq_all[:, c0_:NT, :].rearrange("p c (h e) -> p (c h) e", h=2)
                kb.op('dve', lambda e: e.reduce_sum(out=mu_[:, 0:G_], in_=og, axis=AX.X), r=['o_all'], w=['mu_'])
                kb.op('dve', lambda e: e.tensor_scalar(out=mu_[:, 0:G_], in0=mu_[:, 0:G_], scalar1=-1.0 / 64, scalar2=None, op0=ALU.mult), r=['mu_'], w=['mu_'])
                kb.op('pool', lambda e: e.tensor_tensor(out=og, in0=og, in1=bc(mu_[:, 0:G_], [[1, G_], [0, 64]]), op=ALU.add), r=['o_all', 'mu_'], w=['o_all'])
                kb.op('pool', lambda e: e.tensor_tensor(out=sqg, in0=og, in1=og, op=ALU.mult), r=['o_all'], w=['sq_all'])
                kb.op('dve', lambda e: e.reduce_sum(out=va_[:, 0:G_], in_=sqg, axis=AX.X), r=['sq_all'], w=['va_'])
                kb.op('act', lambda e: e.activation(out=va_[:, 0:G_], in_=va_[:, 0:G_], func=AF.Sqrt, scale=1.0 / 64, bias=epsG), r=['va_'], w=['va_'])
                kb.op('dve', lambda e: e.reciprocal(out=va_[:, 0:G_], in_=va_[:, 0:G_]), r=['va_'], w=['va_'])
                kb.op('pool', lambda e: e.tensor_tensor(out=og, in0=og, in1=bc(va_[:, 0:G_], [[1, G_], [0, 64]]), op=ALU.mult), r=['o_all', 'va_'], w=['o_all'])
                kb.op('pool', lambda e: e.tensor_tensor(out=rt_all[:, c0_:NT, :], in0=o_all[:, c0_:NT, :], in1=sg[:, c0_:NT, :], op=ALU.mult),
                      r=['o_all', 'sg'], w=['rt_all'])
                cl_ = list(range(c0_, NT))
                for n0 in range(0, len(cl_), 8):
                    grp_ = cl_[n0:n0 + 8]
                    for ii_, c in enumerate(grp_):
                        kb.op('pe', lambda e: e.transpose(out=PST[:, ii_, :], in_=rt_all[:, c, :], identity=ident[:]), r=['rt_all', 'ident'], w=['pst'])
                    kb.op('act', lambda e: e.copy(out=retT[:, r, grp_[0] * 128:(grp_[-1] + 1) * 128].rearrange("p (a b) -> p a b", b=128),
                                                  in_=PST[:, 0:len(grp_), :]), r=['pst'], w=['retT'])
                if os.environ.get('RET_CUT') in ('5', '6', '7'):
                    kb.release(m); return
                if r == 3:
                    for nm_, t_, sh_, dt_ in (('sg', sg, [128, NT, 128], F32), ('DecT', DecT, [128, 8, 128], F32), ('X', X, [128, 8, 128], F32),
                                              ('Z', Z, [128, 16], F32), ('g128', g128, [128, 8], F32), ('lg', lg, [128, 16], F32),
                                              ('Sf', Sf, [128, NT, 128], BF16), ('Rb', Rb, [128, NT, 128], BF16), ('qT', qT, [128, T], BF16),
                                              ('kT', kT, [128, T], BF16), ('vtok', vtok, [128, NT, 128], BF16), ('ktok', ktok, [128, NT, 128], BF16),
                                              ('vf', vf, [128, NT, 128], BF16), ('AT1', AT[1], [128, 2, 128], BF16), ('qxf1', qxf[1], [128, 128], BF16),
                                              ('qxb1', qxb[1], [128, 128], BF16)):
                        xdump(nm_, t_, sh_, dt_)
            kb.release(m)

        def phase_attn(l, oaT):
            need_ctx = (l == 0)
            m = kb.mark()
            qT = kb.sb('aqT', [128, 4, T], BF16); kT = kb.sb('akT', [128, T], BF16)
            Va = kb.sb('Va', [128, NT, 2, 66], BF16)
            msk = kb.sb('msk', [128, 256], BF16)
            kb.dma('sp', msk[:], amask[:, :], w=['msk'])
            snk = kb.sb('snk', [128, 8], F32)
            kb.dma('sp', snk[:], sink_in[l].partition_broadcast(128), w=['snk'])
            kb.op('act', lambda e: e.activation(out=snk[:], in_=snk[:], func=AF.Exp), r=['snk'], w=['snk'])
            kb.op('pool', lambda e: e.memset(Va[:, :, :, 64:66], 1.0), w=['Va'])
            wq = kb.sb('awq', [128, 8, 1024], BF16); wk = kb.sb('awk', [128, 8, 256], BF16); wv = kb.sb('awv', [128, 8, 128], BF16)
            load_w(wq, 'awq', w_ext[l][:, QA:QA + 1024], 8, 1024)
            load_w(wk, 'awk', w_ext[l][:, KA:KA + 256], 8, 256)
            load_w(wv, 'awv', w_ext[l][:, VA:VA + 128], 8, 128)
            rc = [kb.sb(f'arc{i}', [128, 512], F32) for i in range(2)]; rs = [kb.sb(f'ars{i}', [128, 512], F32) for i in range(2)]
            t1 = [kb.sb(f'at1{i}', [128, 512], F32) for i in range(2)]; t2 = [kb.sb(f'at2{i}', [128, 512], F32) for i in range(2)]
            for bi, (s0, n) in enumerate(TB):
                if s0 == 0:
                    for g in range(4):
                        proj_fm(wq, 'awq', g * 128, s0, n, g % 2)
                        kb.op('act', lambda e: e.copy(out=qT[:, g, s0:s0 + n], in_=PSA[:, g % 2, 0:n]), r=[('ps', g % 2)], w=['aqT'])
                    proj_fm(wk, 'awk', 0, s0, n, 2)
                    kb.op('act', lambda e: e.copy(out=kT[:, s0:s0 + n], in_=PSA[:, 2, 0:n]), r=[('ps', 2)], w=['akT'])
                else:
                    j = bi % 2; p0 = s0 - 256
                    kb.dma('sp', rc[j][:, 0:n], ropeA[0, :, p0:p0 + n], w=[('rc', j)])
                    kb.dma('sp', rs[j][:, 0:n], ropeA[1, :, p0:p0 + n], w=[('rs', j)])
                    for g in range(4):
                        b0 = 2 * (g % 2)
                        proj_fm(wq, 'awq', g * 128, s0, n, b0); proj_fm(wq, 'awq', 512 + g * 128, s0, n, b0 + 1)
                        rope_evac(b0, b0 + 1, qT[:, g, s0:s0 + n], 'aqT', rc[j], rs[j], n, t1[j], t2[j], j)
                    proj_fm(wk, 'awk', 0, s0, n, 4); proj_fm(wk, 'awk', 128, s0, n, 5)
                    rope_evac(4, 5, kT[:, s0:s0 + n], 'akT', rc[j], rs[j], n, t1[j], t2[j], j)
                for t in range(s0 // 128, (s0 + n) // 128):
                    for k in range(8):
                        kb.op('pe', lambda e: e.matmul(PSA[:, 6, 0:128], lhsT=hT[:, k, t * 128:(t + 1) * 128], rhs=wv[:, k, :],
                                                       start=(k == 0), stop=(k == 7)), r=['hT', 'awv'], w=[('ps', 6)])
                    kb.op('act', lambda e: e.copy(out=Va[:, t, :, 0:64], in_=PSA[:, 6, 0:128].rearrange("p (h e) -> p h e", h=2)),
                          r=[('ps', 6)], w=['Va'])
            PT = [[kb.sb(f'PT{i}_{j}', [128, 4, 128], BF16) for j in range(5)] for i in range(2)]
            oat = [kb.sb(f'oat{i}', [128, 8, 64], BF16) for i in range(2)]
            den = [kb.sb(f'den{i}', [128, 4], F32) for i in range(2)]
            it = 0
            for t in (range(NT) if need_ctx else range(2, NT)):
                if t < 2:
                    keys = [(0, None), (1, None)]
                else:
                    keys = []
                    if t > 2: keys.append((t - 1, 0))
                    keys.append((t, None))
                    if t < NT - 1: keys.append((t + 1, 1))
                    keys += [(0, None), (1, None)]
                ti = t % 2
                for h2 in range(2):
                    i = it % 2; it += 1
                    ps_ = slice(h2 * 64, (h2 + 1) * 64)
                    for ki, (kt, mk) in enumerate(keys):
                        bank = ki % 3
                        kb.op('pe', lambda e: e.matmul(PSA[:, bank, :].rearrange("p (g q) -> p g q", g=4), lhsT=kT[ps_, kt * 128:(kt + 1) * 128],
                                                       rhs=qT[ps_, :, t * 128:(t + 1) * 128], start=True, stop=True), r=['akT', 'aqT'], w=[('ps', bank)])
                        kb.op('act', lambda e: e.activation(out=PT[i][ki][:], in_=PSA[:, bank, :].rearrange("p (g q) -> p g q", g=4), func=AF.Exp, scale=0.125),
                              r=[('ps', bank)], w=[('PT', i, ki)])
                        if mk is not None:
                            kb.op('pool', lambda e: e.tensor_tensor(out=PT[i][ki][:], in0=PT[i][ki][:], in1=bc(msk[:, mk * 128:(mk + 1) * 128], [[0, 4], [1, 128]]),
                                                                    op=ALU.mult), r=[('PT', i, ki), 'msk'], w=[('PT', i, ki)])
                    ob = 3 + i
                    for g in range(4):
                        for ki, (kt, mk) in enumerate(keys):
                            kb.op('pe', lambda e: e.matmul(PSA[:, ob, g * 66:g * 66 + 65], lhsT=PT[i][ki][:, g, :], rhs=Va[:, kt, h2, 0:65],
                                                           start=(ki == 0), stop=(ki == len(keys) - 1)), r=[('PT', i, ki), 'Va'], w=[('ps', ob)])
                    ov = PSA[:, ob, 0:264].rearrange("p (g e) -> p g e", g=4)
                    kb.op('dve', lambda e: e.tensor_tensor(out=den[i][:], in0=ov[:, :, 64], in1=snk[:, h2 * 4:(h2 + 1) * 4], op=ALU.add),
                          r=[('ps', ob), 'snk'], w=[('den', i)])
                    kb.op('dve', lambda e: e.reciprocal(out=den[i][:], in_=den[i][:]), r=[('den', i)], w=[('den', i)])
                    kb.op('dve', lambda e: e.tensor_tensor(out=oat[ti][:, h2 * 4:(h2 + 1) * 4, :], in0=ov[:, :, 0:64], in1=bc(den[i][:, 0:4], [[1, 4], [0, 64]]),
                                                           op=ALU.mult), r=[('ps', ob), ('den', i)], w=[('oat', ti)])
                for k in range(4):
                    kb.op('pe', lambda e: e.transpose(out=PST[:, k, :], in_=oat[ti][:, 2 * k:2 * k + 2, :].rearrange("p h e -> p (h e)"), identity=ident[:]),
                          r=[('oat', ti), 'ident'], w=['pst'])
                kb.op('act', lambda e: e.copy(out=oaT[:, :, t * 128:(t + 1) * 128], in_=PST[:, 0:4, :]), r=['pst'], w=['oaT'])
            kb.release(m)

        def phase_four(l, ofT):
            need_ctx = (l == 0)
            m = kb.mark()
            wfu = kb.sb('wfu', [128, 8, 512], BF16)
            load_w(wfu, 'wfu', w_ext[l][:, FU:FU + 512], 8, 512)
            dC = kb.sb('dC', [128, 256], BF16)
            kb.dma('sp', dC[:], dftC[:, :], w=['dC'])
            W = kb.sb('W', [128, NT, 4, 256], BF16)
            uT = [kb.sb(f'uT{i}', [128, T], BF16) for i in range(2)]
            for g in range(4):
                i = g % 2
                for bi, (s0, n) in enumerate(TB):
                    proj_fm(wfu, 'wfu', g * 128, s0, n, bi % 2)
                    kb.op('act', lambda e: e.copy(out=uT[i][:, s0:s0 + n], in_=PSA[:, bi % 2, 0:n]), r=[('ps', bi % 2)], w=[('uT', i)])
                for t in range(NT):
                    bank = 2 + t % 2
                    kb.op('pe', lambda e: e.matmul(PSA[:, bank, 0:256], lhsT=uT[i][:, t * 128:(t + 1) * 128], rhs=dC[:], start=True, stop=True),
                          r=[('uT', i), 'dC'], w=[('ps', bank)])
                    kb.op('dve', lambda e: e.tensor_copy(out=W[:, t, g, :], in_=PSA[:, bank, 0:256]), r=[('ps', bank)], w=['W'])
            Cb = [kb.sb(f'Cb{i}', [128, 16, 256], BF16) for i in range(2)]
            Nb = [kb.sb(f'Nb{i}', [128, 16, 256], BF16) for i in range(2)]
            it = 0
            for nb in range(8):
                i = nb % 2
                kb.dma('sp', Cb[i][:], dftN[0, :, nb * 256:(nb + 1) * 256].rearrange("(t p) c -> p t c", p=128), w=[('Cb', i)])
                kb.dma('sp', Nb[i][:], dftN[1, :, nb * 256:(nb + 1) * 256].rearrange("(t p) c -> p t c", p=128), w=[('Nb', i)])
                for g in range(4):
                    bank = 4 + it % 2; it += 1
                    for t in range(16):
                        kb.op('pe', lambda e: e.matmul(PSA[:, bank, 0:256], lhsT=W[:, 2 + t, g, 0:128], rhs=Cb[i][:, t, :], start=(t == 0), stop=False),
                              r=['W', ('Cb', i)], w=[('ps', bank)])
                        kb.op('pe', lambda e: e.matmul(PSA[:, bank, 0:256], lhsT=W[:, 2 + t, g, 128:256], rhs=Nb[i][:, t, :], start=False, stop=(t == 15)),
                              r=['W', ('Nb', i)], w=[('ps', bank)])
                    kb.op('act', lambda e: e.copy(out=ofT[:, g, 256 + nb * 256:256 + (nb + 1) * 256], in_=PSA[:, bank, 0:256]), r=[('ps', bank)], w=['ofT'])
            if need_ctx:
                kb.dma('sp', Cb[0][:, 0:2, :], dft256[0].rearrange("(t p) c -> p t c", p=128), w=[('Cb', 0)])
                kb.dma('sp', Nb[0][:, 0:2, :], dft256[1].rearrange("(t p) c -> p t c", p=128), w=[('Nb', 0)])
                for g in range(4):
                    bank = 4 + g % 2
                    for t in range(2):
                        kb.op('pe', lambda e: e.matmul(PSA[:, bank, 0:256], lhsT=W[:, t, g, 0:128], rhs=Cb[0][:, t, :], start=(t == 0), stop=False),
                              r=['W', ('Cb', 0)], w=[('ps', bank)])
                        kb.op('pe', lambda e: e.matmul(PSA[:, bank, 0:256], lhsT=W[:, t, g, 128:256], rhs=Nb[0][:, t, :], start=False, stop=(t == 1)),
                              r=['W', ('Nb', 0)], w=[('ps', bank)])
                    kb.op('act', lambda e: e.copy(out=ofT[:, g, 0:256], in_=PSA[:, bank, 0:256]), r=[('ps', bank)], w=['ofT'])
            kb.release(m)

        def resid_update(l, gi, tiles, src_fn, src_keys):
            pass

        def phase_out(l, mT):
            m = kb.mark()
            wo = kb.sb('wo', [128, 8, D], BF16)
            load_w(wo, 'wo', w_out[l][:, :], 8, D)
            g1 = []
            for row in (0, 1):
                g_ = kb.sb(f'g1_{row}', [128, D], F32)
                kb.dma('sp', g_[:], modbuf[l, row, 2 * D:3 * D].partition_broadcast(128), w=[f'g1_{row}'])
                g1.append(g_)
            xt = [kb.sb(f'oxt{i}', [128, D], F32) for i in range(2)]
            yt = [kb.sb(f'oyt{i}', [128, D], F32) for i in range(2)]
            for t in (range(NT) if l == 0 else range(2, NT)):
                i = t % 2; row = 0 if t < 2 else 1
                kb.dma('sp', xt[i][:], xbuf[t * 128:(t + 1) * 128, :], r=[('x', t)], w=[('oxt', i)])
                for hf in range(2):
                    bank = 2 * i + hf
                    for fc in range(8):
                        kb.op('pe', lambda e: e.matmul(PSA[:, bank, :], lhsT=mT[:, fc, t * 128:(t + 1) * 128], rhs=wo[:, fc, hf * 512:(hf + 1) * 512],
                                                       start=(fc == 0), stop=(fc == 7)), r=['mT', 'wo'], w=[('ps', bank)])
                    kb.op('dve', lambda e: e.tensor_tensor(out=yt[i][:, hf * 512:(hf + 1) * 512], in0=PSA[:, bank, :], in1=g1[row][:, hf * 512:(hf + 1) * 512],
                                                           op=ALU.mult), r=[('ps', bank), f'g1_{row}'], w=[('oyt', i)])
                kb.op('pool', lambda e: e.tensor_tensor(out=yt[i][:], in0=yt[i][:], in1=xt[i][:], op=ALU.add), r=[('oyt', i), ('oxt', i)], w=[('oyt', i)])
                kb.dma('sp', xbuf[t * 128:(t + 1) * 128, :], yt[i][:], r=[('oyt', i)], w=[('x', t)])
            kb.release(m)

        def phase_moe(l):
            m = kb.mark()
            tiles = list(range(NT) if l == 0 else range(2, NT))
            blocks = TB if l == 0 else TB[1:]
            logits = kb.sb('logits', [128, NT, 36], F32)
            Wt = kb.sb('Wt', [128, NT, 32], F32)
            kb.op('pool', lambda e: e.memset(logits[:], 0.0), w=['logits'])
            phase_norm(l, 2, logits)
            if os.environ.get('MOE_CUT') == '1':
                kb.release(m); return
            m2 = kb.mark()
            lgG = logits[:, :, 0:4]; lgE = logits[:, :, 4:36]
            gmax = kb.sb('gmax', [128, NT], F32); ohg = kb.sb('ohg', [128, NT, 4], F32); eg = kb.sb('eg', [128, NT, 4], F32)
            pg = kb.sb('pg', [128, NT], F32); me = kb.sb('me', [128, NT, 32], F32); oh1 = kb.sb('oh1', [128, NT, 32], F32)
            oh2 = kb.sb('oh2', [128, NT, 32], F32); m1 = kb.sb('m1', [128, NT], F32); m2_ = kb.sb('m2', [128, NT], F32)
            w1 = kb.sb('w1', [128, NT], F32); w2 = kb.sb('w2', [128, NT], F32)
            b1 = lambda a, n_: bc(a, [[1, NT], [0, n_]])
            kb.op('dve', lambda e: e.reduce_max(out=gmax[:], in_=lgG, axis=AX.X), r=['logits'], w=['gmax'])
            kb.op('dve', lambda e: e.tensor_tensor(out=ohg[:], in0=lgG, in1=b1(gmax[:, 0:NT], 4), op=ALU.is_equal), r=['logits', 'gmax'], w=['ohg'])
            kb.op('dve', lambda e: e.tensor_tensor(out=eg[:], in0=lgG, in1=b1(gmax[:, 0:NT], 4), op=ALU.subtract), r=['logits', 'gmax'], w=['eg'])
            kb.op('act', lambda e: e.activation(out=eg[:], in_=eg[:], func=AF.Exp), r=['eg'], w=['eg'])
            kb.op('dve', lambda e: e.reduce_sum(out=pg[:], in_=eg[:], axis=AX.X), r=['eg'], w=['pg'])
            kb.op('dve', lambda e: e.reciprocal(out=pg[:], in_=pg[:]), r=['pg'], w=['pg'])
            kb.op('dve', lambda e: e.tensor_scalar(out=ohg[:], in0=ohg[:], scalar1=-1.0, scalar2=1e30, op0=ALU.add, op1=ALU.mult), r=['ohg'], w=['ohg'])
            kb.op('dve', lambda e: e.tensor_tensor(out=me[:].rearrange("p t (g x) -> p t g x", g=4), in0=lgE.rearrange("p t (g x) -> p t g x", g=4),
                                                   in1=bc(ohg[:, 0:NT, :], [[4, NT], [1, 4], [0, 8]]), op=ALU.add), r=['logits', 'ohg'], w=['me'])
            kb.op('dve', lambda e: e.reduce_max(out=m1[:], in_=me[:], axis=AX.X), r=['me'], w=['m1'])
            kb.op('dve', lambda e: e.tensor_tensor(out=oh1[:], in0=me[:], in1=b1(m1[:, 0:NT], 32), op=ALU.is_equal), r=['me', 'm1'], w=['oh1'])
            kb.op('dve', lambda e: e.scalar_tensor_tensor(out=me[:], in0=oh1[:], scalar=-1e30, in1=me[:], op0=ALU.mult, op1=ALU.add), r=['oh1', 'me'], w=['me'])
            kb.op('dve', lambda e: e.reduce_max(out=m2_[:], in_=me[:], axis=AX.X), r=['me'], w=['m2'])
            kb.op('dve', lambda e: e.tensor_tensor(out=oh2[:], in0=me[:], in1=b1(m2_[:, 0:NT], 32), op=ALU.is_equal), r=['me', 'm2'], w=['oh2'])
            kb.op('dve', lambda e: e.tensor_tensor(out=w1[:], in0=m1[:], in1=m2_[:], op=ALU.subtract), r=['m1', 'm2'], w=['w1'])
            kb.op('act', lambda e: e.activation(out=w2[:], in_=w1[:], func=AF.Sigmoid, scale=-1.0), r=['w1'], w=['w2'])
            kb.op('act', lambda e: e.activation(out=w1[:], in_=w1[:], func=AF.Sigmoid), r=['w1'], w=['w1'])
            kb.op('dve', lambda e: e.tensor_tensor(out=w1[:], in0=w1[:], in1=pg[:], op=ALU.mult), r=['w1', 'pg'], w=['w1'])
            kb.op('dve', lambda e: e.tensor_tensor(out=w2[:], in0=w2[:], in1=pg[:], op=ALU.mult), r=['w2', 'pg'], w=['w2'])
            kb.op('dve', lambda e: e.tensor_tensor(out=oh1[:], in0=oh1[:], in1=b1(w1[:, 0:NT], 32), op=ALU.mult), r=['oh1', 'w1'], w=['oh1'])
            kb.op('dve', lambda e: e.tensor_tensor(out=oh2[:], in0=oh2[:], in1=b1(w2[:, 0:NT], 32), op=ALU.mult), r=['oh2', 'w2'], w=['oh2'])
            kb.op('dve', lambda e: e.tensor_tensor(out=Wt[:], in0=oh1[:], in1=oh2[:], op=ALU.add), r=['oh1', 'oh2'], w=['Wt'])
            kb.release(m2)
            if os.environ.get('MOE_CUT') == '2':
                kb.release(m); return
            acc = kb.sb('acc', [128, NT, D], F32)
            m3 = kb.mark()
            wg = [kb.sb(f'ewg{i}', [128, 8, 512], BF16) for i in range(2)]
            wu = [kb.sb(f'ewu{i}', [128, 8, 512], BF16) for i in range(2)]
            wd = [kb.sb(f'ewd{i}', [128, 4, D], BF16) for i in range(2)]
            aT = [kb.sb(f'aT{i}', [128, 4, 512], BF16) for i in range(2)]
            sgt = [kb.sb(f'sgt{i}', [128, 512], F32) for i in range(2)]
            it = 0; ih = 0
            for ex in range(int(os.environ.get('MOE_NEXP', '32'))):
                j = ex % 2
                load_w(wg[j], ('ewg', j), w_eg[l, ex], 8, 512, engs=('pool', 'act'))
                load_w(wu[j], ('ewu', j), w_eu[l, ex], 8, 512, engs=('pool', 'act'))
                load_w(wd[j], ('ewd', j), w_ed[l, ex], 4, D, engs=('pool', 'act'))
                for (s0, n) in blocks:
                    i = it % 2; it += 1
                    for hc in range(4):
                        ii = ih % 2; ih += 1
                        for k in range(8):
                            kb.op('pe', lambda e: e.matmul(PSA[:, ii, 0:n], lhsT=wg[j][:, k, hc * 128:(hc + 1) * 128], rhs=hT[:, k, s0:s0 + n],
                                                           start=(k == 0), stop=(k == 7)), r=[('ewg', j), 'hT'], w=[('ps', ii)])
                        for k in range(8):
                            kb.op('pe', lambda e: e.matmul(PSA[:, 2 + ii, 0:n], lhsT=wu[j][:, k, hc * 128:(hc + 1) * 128], rhs=hT[:, k, s0:s0 + n],
                                                           start=(k == 0), stop=(k == 7)), r=[('ewu', j), 'hT'], w=[('ps', 2 + ii)])
                        kb.op('act', lambda e: e.activation(out=sgt[ii][:, 0:n], in_=PSA[:, ii, 0:n], func=AF.Silu), r=[('ps', ii)], w=[('sgt', ii)])
                        kb.op('dve', lambda e: e.tensor_tensor(out=aT[i][:, hc, 0:n], in0=sgt[ii][:, 0:n], in1=PSA[:, 2 + ii, 0:n], op=ALU.mult),
                              r=[('sgt', ii), ('ps', 2 + ii)], w=[('aT', i)])
                    for t in range(s0 // 128, (s0 + n) // 128):
                        tl = t * 128 - s0
                        for hf in range(2):
                            bank = 4 + (2 * t + hf) % 3
                            for hc in range(4):
                                kb.op('pe', lambda e: e.matmul(PSA[:, bank, :], lhsT=aT[i][:, hc, tl:tl + 128], rhs=wd[j][:, hc, hf * 512:(hf + 1) * 512],
                                                               start=(hc == 0), stop=(hc == 3)), r=[('aT', i), ('ewd', j)], w=[('ps', bank)])
                            a_ = acc[:, t, hf * 512:(hf + 1) * 512]
                            if ex == 0:
                                kb.op('dve', lambda e: e.tensor_scalar(out=a_, in0=PSA[:, bank, :], scalar1=Wt[:, t, ex:ex + 1], scalar2=None, op0=ALU.mult),
                                      r=[('ps', bank), 'Wt'], w=[('acc', t)])
                            else:
                                kb.op('dve', lambda e: e.scalar_tensor_tensor(out=a_, in0=PSA[:, bank, :], scalar=Wt[:, t, ex:ex + 1], in1=a_,
                                                                              op0=ALU.mult, op1=ALU.add), r=[('ps', bank), 'Wt', ('acc', t)], w=[('acc', t)])
            kb.release(m3)
            g2 = []
            for row in (0, 1):
                g_ = kb.sb(f'g2_{row}', [128, D], F32)
                kb.dma('sp', g_[:], modbuf[l, row, 5 * D:6 * D].partition_broadcast(128), w=[f'g2_{row}'])
                g2.append(g_)
            xt = [kb.sb(f'mxt{i}', [128, D], F32) for i in range(2)]
            for t in tiles:
                i = t % 2; row = 0 if t < 2 else 1
                kb.dma('sp', xt[i][:], xbuf[t * 128:(t + 1) * 128, :], r=[('x', t)], w=[('mxt', i)])
                kb.op('dve', lambda e: e.tensor_tensor(out=acc[:, t, :], in0=acc[:, t, :], in1=g2[row][:], op=ALU.mult), r=[('acc', t), f'g2_{row}'], w=[('acc', t)])
                kb.op('pool', lambda e: e.tensor_tensor(out=xt[i][:], in0=xt[i][:], in1=acc[:, t, :], op=ALU.add), r=[('acc', t), ('mxt', i)], w=[('mxt', i)])
                kb.dma('sp', xbuf[t * 128:(t + 1) * 128, :], xt[i][:], r=[('mxt', i)], w=[('x', t)])
            kb.release(m)

        moe_state = {}

        def phase_moe_sparse(l):
            IOA = bass.IndirectOffsetOnAxis
            ABv = AB.rearrange("r (h c) -> (r h) c", h=2)
            if 'bregs' not in moe_state:
                regs = {}
                for nm_, v_ in (('rec', 32 * CAP - 1), ('tok', T - 1), ('ab', 2 * T - 1)):
                    rg = nc.gpsimd.alloc_register('bnd_' + nm_)
                    nc.gpsimd.reg_mov(rg, v_)
                    regs[nm_] = rg
                moe_state['bregs'] = regs
            BR = moe_state['bregs']
            m = kb.mark()
            t0 = 0 if l == 0 else 2
            tiles = list(range(t0, NT))
            logits = kb.sb('logits', [128, NT, 36], F32)
            kb.op('pool', lambda e: e.memset(logits[:], 0.0), w=['logits'])
            phase_norm(l, 2, logits, sparse=True)
            MC = kb.sb('MC', [128, 32 + NT], F32)
            kb.dma('sp', MC[:], mconst[:, :], w=['MC'])
            eC = MC[:, 0:32]; tokid = MC[:, 32:32 + NT]
            zt = kb.sb('zt', [128, D], F32)
            kb.op('pool', lambda e: e.memset(zt[:], 0.0), w=['zt'])
            for q in range(2 * NT):
                kb.dma('sp', AB[q * 128:(q + 1) * 128, :], zt[:], r=['zt'], w=[('ABz', q)])
            ri_ = kb.sb('recinit', [128, (32 * CAP) // 128, 4], F32)
            kb.op('pool', lambda e: e.memset(ri_[:], 1.0e6), w=['recinit'])
            kb.op('pool', lambda e: e.memset(ri_[:, :, 2:3], 0.0), r=['recinit'], w=['recinit'])
            kb.dma('sp', rec_d.rearrange("(p s) c -> p s c", p=128), ri_[:], r=['recinit'], w=['rec_d'])
            lgG = logits[:, :, 0:4]; lgE = logits[:, :, 4:36]
            gmax = kb.sb('gmax', [128, NT], F32); ohg = kb.sb('ohg', [128, NT, 4], F32); eg = kb.sb('eg', [128, NT, 4], F32)
            pg = kb.sb('pg', [128, NT], F32); me = kb.sb('me', [128, NT, 32], F32); oh1 = kb.sb('oh1', [128, NT, 32], F32)
            oh2 = kb.sb('oh2', [128, NT, 32], F32); m1 = kb.sb('m1', [128, NT], F32); m2_ = kb.sb('m2', [128, NT], F32)
            w1 = kb.sb('w1', [128, NT], F32); w2 = kb.sb('w2', [128, NT], F32)
            b1 = lambda a, n_: bc(a, [[1, NT], [0, n_]])
            kb.op('dve', lambda e: e.reduce_max(out=gmax[:], in_=lgG, axis=AX.X), r=['logits'], w=['gmax'])
            kb.op('dve', lambda e: e.tensor_tensor(out=ohg[:], in0=lgG, in1=b1(gmax[:, 0:NT], 4), op=ALU.is_equal), r=['logits', 'gmax'], w=['ohg'])
            kb.op('dve', lambda e: e.tensor_tensor(out=eg[:], in0=lgG, in1=b1(gmax[:, 0:NT], 4), op=ALU.subtract), r=['logits', 'gmax'], w=['eg'])
            kb.op('act', lambda e: e.activation(out=eg[:], in_=eg[:], func=AF.Exp), r=['eg'], w=['eg'])
            kb.op('dve', lambda e: e.reduce_sum(out=pg[:], in_=eg[:], axis=AX.X), r=['eg'], w=['pg'])
            kb.op('dve', lambda e: e.reciprocal(out=pg[:], in_=pg[:]), r=['pg'], w=['pg'])
            kb.op('dve', lambda e: e.tensor_scalar(out=ohg[:], in0=ohg[:], scalar1=-1.0, scalar2=1e30, op0=ALU.add, op1=ALU.mult), r=['ohg'], w=['ohg'])
            kb.op('dve', lambda e: e.tensor_tensor(out=me[:].rearrange("p t (g x) -> p t g x", g=4), in0=lgE.rearrange("p t (g x) -> p t g x", g=4),
                                                   in1=bc(ohg[:, 0:NT, :], [[4, NT], [1, 4], [0, 8]]), op=ALU.add), r=['logits', 'ohg'], w=['me'])
            kb.op('dve', lambda e: e.reduce_max(out=m1[:], in_=me[:], axis=AX.X), r=['me'], w=['m1'])
            kb.op('dve', lambda e: e.tensor_tensor(out=oh1[:], in0=me[:], in1=b1(m1[:, 0:NT], 32), op=ALU.is_equal), r=['me', 'm1'], w=['oh1'])
            kb.op('dve', lambda e: e.scalar_tensor_tensor(out=me[:], in0=oh1[:], scalar=-1e30, in1=me[:], op0=ALU.mult, op1=ALU.add), r=['oh1', 'me'], w=['me'])
            kb.op('dve', lambda e: e.reduce_max(out=m2_[:], in_=me[:], axis=AX.X), r=['me'], w=['m2'])
            kb.op('dve', lambda e: e.tensor_tensor(out=oh2[:], in0=me[:], in1=b1(m2_[:, 0:NT], 32), op=ALU.is_equal), r=['me', 'm2'], w=['oh2'])
            kb.op('dve', lambda e: e.tensor_tensor(out=w1[:], in0=m1[:], in1=m2_[:], op=ALU.subtract), r=['m1', 'm2'], w=['w1'])
            kb.op('act', lambda e: e.activation(out=w2[:], in_=w1[:], func=AF.Sigmoid, scale=-1.0), r=['w1'], w=['w2'])
            kb.op('act', lambda e: e.activation(out=w1[:], in_=w1[:], func=AF.Sigmoid), r=['w1'], w=['w1'])
            kb.op('dve', lambda e: e.tensor_tensor(out=w1[:], in0=w1[:], in1=pg[:], op=ALU.mult), r=['w1', 'pg'], w=['w1'])
            kb.op('dve', lambda e: e.tensor_tensor(out=w2[:], in0=w2[:], in1=pg[:], op=ALU.mult), r=['w2', 'pg'], w=['w2'])
            if t0 > 0:
                kb.op('pool', lambda e: e.memset(oh1[:, 0:t0, :], 0.0), r=['oh1'], w=['oh1'])
                kb.op('pool', lambda e: e.memset(oh2[:, 0:t0, :], 0.0), r=['oh2'], w=['oh2'])
            selb = kb.sb('selb', [128, NT * 32], BF16)
            kb.op('pool', lambda e: e.tensor_tensor(out=selb[:], in0=oh1[:].rearrange("p t e -> p (t e)"), in1=oh2[:].rearrange("p t e -> p (t e)"), op=ALU.add),
                  r=['oh1', 'oh2'], w=['selb'])
            LT = kb.sb('LT', [128, 128], BF16); ones = kb.sb('ones', [128, 128], BF16)
            kb.op('pool', lambda e: e.memset(ones[:], 1.0), w=['ones'])
            kb.op('pool', lambda e: e.memset(LT[:], 1.0), w=['LT'])
            kb.op('pool', lambda e: e.affine_select(out=LT[:], in_=LT[:], pattern=[[1, 128]], compare_op=ALU.is_gt, fill=0.0, base=0,
                                                    channel_multiplier=-1), r=['LT'], w=['LT'])
            slot = kb.sb('slot', [128, NT, 32], F32); tot = kb.sb('tot', [128, NT, 32], F32); cum = kb.sb('cum', [128, NT, 32], F32)
            sl2 = slot[:].rearrange("p t e -> p (t e)"); to2 = tot[:].rearrange("p t e -> p (t e)")
            for (c0, c1, bank) in ((0, 512, 0), (512, NT * 32, 1)):
                kb.op('pe', lambda e: e.matmul(PSA[:, bank, 0:c1 - c0], lhsT=LT[:], rhs=selb[:, c0:c1], start=True, stop=True), r=['LT', 'selb'], w=[('ps', bank)])
                kb.op('dve', lambda e: e.tensor_copy(out=sl2[:, c0:c1], in_=PSA[:, bank, 0:c1 - c0]), r=[('ps', bank)], w=['slot'])
                kb.op('pe', lambda e: e.matmul(PSA[:, 2 + bank, 0:c1 - c0], lhsT=ones[:], rhs=selb[:, c0:c1], start=True, stop=True), r=['ones', 'selb'], w=[('ps', 2 + bank)])
                kb.op('dve', lambda e: e.tensor_copy(out=to2[:, c0:c1], in_=PSA[:, 2 + bank, 0:c1 - c0]), r=[('ps', 2 + bank)], w=['tot'])
            kb.op('pool', lambda e: e.memset(cum[:, 0, :], 0.0), w=['cum'])
            for t in range(1, NT):
                kb.op('dve', lambda e: e.tensor_tensor(out=cum[:, t, :], in0=cum[:, t - 1, :], in1=tot[:, t - 1, :], op=ALU.add), r=['cum', 'tot'], w=['cum'])
            kb.op('dve', lambda e: e.tensor_tensor(out=slot[:], in0=slot[:], in1=cum[:], op=ALU.add), r=['slot', 'cum'], w=['slot'])
            rec = kb.sb('rec', [128, NT, 2, 4], F32)
            rr = kb.sb('rr', [128, NT, 2], F32); rri = kb.sb('rri', [128, NT, 2], I32)
            s_ = kb.sb('s_', [128, NT], F32); e_ = kb.sb('e_', [128, NT], F32)
            kb.op('pool', lambda e: e.memset(rec[:], 0.0), w=['rec'])
            for k, (oh, wk) in enumerate(((oh1, w1), (oh2, w2))):
                kb.op('dve', lambda e: e.tensor_tensor(out=me[:], in0=oh[:], in1=slot[:], op=ALU.mult), r=['oh1', 'oh2', 'slot', 'me'], w=['me'])
                kb.op('dve', lambda e: e.reduce_sum(out=s_[:], in_=me[:], axis=AX.X), r=['me'], w=['s_'])
                kb.op('dve', lambda e: e.tensor_tensor(out=me[:], in0=oh[:], in1=bc(eC, [[0, NT], [1, 32]]), op=ALU.mult), r=['oh1', 'oh2', 'MC', 'me'], w=['me'])
                kb.op('dve', lambda e: e.reduce_sum(out=e_[:], in_=me[:], axis=AX.X), r=['me'], w=['e_'])
                kb.op('dve', lambda e: e.tensor_tensor(out=e_[:], in0=e_[:], in1=s_[:], op=ALU.add), r=['e_', 's_'], w=['e_'])
                kb.op('dve', lambda e: e.tensor_scalar(out=s_[:], in0=s_[:], scalar1=float(CAP), scalar2=1.0e6, op0=ALU.is_ge, op1=ALU.mult), r=['s_'], w=['s_'])
                kb.op('dve', lambda e: e.tensor_tensor(out=rr[:, :, k], in0=e_[:], in1=s_[:], op=ALU.add), r=['e_', 's_'], w=['rr'])
                kb.op('pool', lambda e: e.tensor_copy(out=rec[:, :, k, 0], in_=tokid), r=['MC', 'rec'], w=['rec'])
                kb.op('pool', lambda e: e.tensor_scalar(out=rec[:, :, k, 1], in0=tokid, scalar1=float(k * T), scalar2=None, op0=ALU.add), r=['MC', 'rec'], w=['rec'])
                kb.op('pool', lambda e: e.tensor_scalar(out=rec[:, :, k, 3], in0=tokid, scalar1=2.0, scalar2=float(2 * k * T + 1), op0=ALU.mult, op1=ALU.add), r=['MC', 'rec'], w=['rec'])
                kb.op('pool', lambda e: e.tensor_copy(out=rec[:, :, k, 2], in_=wk[:]), r=['w1', 'w2', 'rec'], w=['rec'])
            kb.op('dve', lambda e: e.tensor_copy(out=rri[:], in_=rr[:]), r=['rr'], w=['rri'])
            if stop == f'moe{l}' and os.environ.get('XDUMP'):
                xdump('rr', rr, [128, NT, 2], F32); xdump('rri', rri, [128, NT, 2], I32); xdump('rec', rec, [128, NT, 2, 4], F32)
                xdump('slot', slot, [128, NT, 32], F32)
            kb.barrier()
            for t in tiles:
                for k in range(2):
                    kb.dma('pool', None, None, r=['rec', 'rri'], w=[('recs', t, k)],
                           fn=lambda g: g.indirect_dma_start(out=rec_d[:, :], out_offset=IOA(ap=rri[:, t, k:k + 1], axis=0), in_=rec[:, t, k, :],
                                                             in_offset=None, bounds_check=BR['rec'], oob_is_err=False))
            kb.barrier()
            if os.environ.get('MOE_CUT') == '3':
                kb.release(m); return
            m3 = kb.mark()
            stg_all[0] = list(stg) + [kb.alias(f'stgx{i}', [128, 2048], F32, kb.off['hT'] + i * 8192) for i in range(4)]
            wg = [kb.sb(f'ewg{i}', [128, 8, 512], BF16) for i in range(2)]
            wu = [kb.sb(f'ewu{i}', [128, 8, 512], BF16) for i in range(2)]
            wd = [kb.sb(f'ewd{i}', [128, 4, D], BF16) for i in range(2)]
            XT = [kb.sb(f'XT{i}', [128, 8, CAP], BF16) for i in range(2)]
            aT = kb.sb('aT', [128, 4, CAP], BF16)
            sgt = [kb.sb(f'sgt{i}', [128, 512], F32) for i in range(2)]
            rsb = [kb.sb(f'rsb{i}', [128, NS, 4], F32) for i in range(2)]
            gi = [kb.sb(f'gi{i}', [128, NS, 4], I32) for i in range(2)]
            xg = [kb.sb(f'xg{i}', [128, D], BF16) for i in range(NS)]
            yw = [kb.sb(f'yw{i}', [128, D], F32) for i in range(3)]
            for i in range(NS):
                kb.op('pool', lambda e: e.memset(xg[i][:], 0.0), w=[('xg', i)])
            PST2 = PSA[:, 6, :].bitcast(BF16).rearrange("p (k c) -> p k c", c=128)
            stgs = stg_all[0]
            nexp = int(os.environ.get('MOE_NEXP', '32'))
            cnt = {'iy': 0, 'ih': 0}

            def w_issue(ex):
                j = ex % 2; pieces = []
                for (dst, dkey, src, K, W) in ((wg[j], ('ewg', j), w_eg[l, ex], 8, 512), (wu[j], ('ewu', j), w_eu[l, ex], 8, 512), (wd[j], ('ewd', j), w_ed[l, ex], 4, D)):
                    kk = 2048 // W
                    for k0 in range(0, K, kk):
                        i = len(pieces)
                        sv = stgs[i][:, 0:kk * W].rearrange("p (k c) -> p k c", c=W)
                        kb.dma('sp', sv, src[k0 * 128:(k0 + kk) * 128, :].rearrange("(k p) c -> p k c", p=128), w=[('stg', i)])
                        pieces.append((dst, dkey, k0, k0 + kk, sv, i))
                return pieces

            def w_cast(pieces):
                for n_, (dst, dkey, k0, k1, sv, i) in enumerate(pieces):
                    if n_ % 2 == 0:
                        kb.op('act', lambda e: e.copy(out=dst[:, k0:k1, :], in_=sv), r=[('stg', i)], w=[dkey])
                    else:
                        kb.op('dve', lambda e: e.tensor_copy(out=dst[:, k0:k1, :], in_=sv), r=[('stg', i)], w=[dkey])

            def G(ex):
                j = ex % 2
                kb.dma('sp', rsb[j][:], rec_d[ex * CAP:(ex + 1) * CAP, :].rearrange("(s p) c -> p s c", p=128), w=[('rsb', j)])
                kb.op('dve', lambda e: e.tensor_copy(out=gi[j][:], in_=rsb[j][:]), r=[('rsb', j)], w=[('gi', j)])
                for s_i in range(NS):
                    kb.dma('pool', None, None, r=[('gi', j)], w=[('xg', s_i)],
                           fn=lambda g: g.indirect_dma_start(out=xg[s_i][:], out_offset=None, in_=h2d[:, :],
                                                             in_offset=IOA(ap=gi[j][:, s_i, 0:1], axis=0), bounds_check=BR['tok'], oob_is_err=False))

            def TR(ex):
                j = ex % 2
                for s_i in range(NS):
                    pt_, pk_ = (PST, 'pst') if s_i % 2 == 0 else (PST2, ('ps', 6))
                    for k in range(8):
                        kb.op('pe', lambda e: e.transpose(out=pt_[:, k, :], in_=xg[s_i][:, k * 128:(k + 1) * 128], identity=ident[:]),
                              r=[('xg', s_i), 'ident'], w=[pk_])
                    kb.op('act', lambda e: e.copy(out=XT[j][:, :, s_i * 128:(s_i + 1) * 128], in_=pt_[:]), r=[pk_], w=[('XT', j)])

            def F1(ex):
                j = ex % 2
                for c0 in range(0, CAP, 512):
                    n = min(512, CAP - c0)
                    for hc in range(4):
                        ii = cnt['ih'] % 2; cnt['ih'] += 1
                        for k in range(8):
                            kb.op('pe', lambda e: e.matmul(PSA[:, ii, 0:n], lhsT=wg[j][:, k, hc * 128:(hc + 1) * 128], rhs=XT[j][:, k, c0:c0 + n],
                                                           start=(k == 0), stop=(k == 7)), r=[('ewg', j), ('XT', j)], w=[('ps', ii)])
                        for k in range(8):
                            kb.op('pe', lambda e: e.matmul(PSA[:, 2 + ii, 0:n], lhsT=wu[j][:, k, hc * 128:(hc + 1) * 128], rhs=XT[j][:, k, c0:c0 + n],
                                                           start=(k == 0), stop=(k == 7)), r=[('ewu', j), ('XT', j)], w=[('ps', 2 + ii)])
                        kb.op('act', lambda e: e.activation(out=sgt[ii][:, 0:n], in_=PSA[:, ii, 0:n], func=AF.Silu), r=[('ps', ii)], w=[('sgt', ii)])
                        kb.op('dve', lambda e: e.tensor_tensor(out=aT[:, hc, c0:c0 + n], in0=sgt[ii][:, 0:n], in1=PSA[:, 2 + ii, 0:n], op=ALU.mult),
                              r=[('sgt', ii), ('ps', 2 + ii)], w=['aT'])

            def F2(ex):
                j = ex % 2
                for s_i in range(NS):
                    q = cnt['iy'] % 3; cnt['iy'] += 1
                    for hf in range(2):
                        bank = 4 + hf
                        for hc in range(4):
                            kb.op('pe', lambda e: e.matmul(PSA[:, bank, :], lhsT=aT[:, hc, s_i * 128:(s_i + 1) * 128], rhs=wd[j][:, hc, hf * 512:(hf + 1) * 512],
                                                           start=(hc == 0), stop=(hc == 3)), r=['aT', ('ewd', j)], w=[('ps', bank)])
                        kb.op('dve', lambda e: e.tensor_scalar(out=yw[q][:, hf * 512:(hf + 1) * 512], in0=PSA[:, bank, :], scalar1=rsb[j][:, s_i, 2:3], scalar2=None,
                                                               op0=ALU.mult), r=[('ps', bank), ('rsb', j)], w=[('yw', q)])
                    kb.dma('pool', None, None, r=[('yw', q), ('gi', j)], w=[('ABs', ex, s_i)],
                           fn=lambda g: g.indirect_dma_start(out=AB[:, :], out_offset=IOA(ap=gi[j][:, s_i, 1:2], axis=0),
                                                             in_=yw[q][:], in_offset=None, bounds_check=BR['ab'], oob_is_err=False))

            w_cast(w_issue(0)); G(0); TR(0)
            for ex in range(nexp):
                nxt = ex + 1 < nexp
                if nxt:
                    pcs = w_issue(ex + 1)
                    G(ex + 1)
                F1(ex)
                if nxt:
                    w_cast(pcs)
                    TR(ex + 1)
                F2(ex)
            stg_all[0] = list(stg)
            stg_i[0] = 0
            kb.release(m3)
            g2 = []
            for row in (0, 1):
                g_ = kb.sb(f'g2_{row}', [128, D], F32)
                kb.dma('sp', g_[:], modbuf[l, row, 5 * D:6 * D].partition_broadcast(128), w=[f'g2_{row}'])
                g2.append(g_)
            xt = [kb.sb(f'mxt{i}', [128, D], F32) for i in range(2)]
            ya = [kb.sb(f'mya{i}', [128, D], F32) for i in range(2)]
            yb = [kb.sb(f'myb{i}', [128, D], F32) for i in range(2)]
            for t in tiles:
                i = t % 2; row = 0 if t < 2 else 1
                kb.dma('sp', xt[i][:], xbuf[t * 128:(t + 1) * 128, :], r=[('x', t)], w=[('mxt', i)])
                kb.dma('sp', ya[i][:], AB[t * 128:(t + 1) * 128, :], w=[('mya', i)])
                kb.dma('sp', yb[i][:], AB[T + t * 128:T + (t + 1) * 128, :], w=[('myb', i)])
                kb.op('pool', lambda e: e.tensor_tensor(out=ya[i][:], in0=ya[i][:], in1=yb[i][:], op=ALU.add), r=[('mya', i), ('myb', i)], w=[('mya', i)])
                kb.op('dve', lambda e: e.tensor_tensor(out=ya[i][:], in0=ya[i][:], in1=g2[row][:], op=ALU.mult), r=[('mya', i), f'g2_{row}'], w=[('mya', i)])
                kb.op('pool', lambda e: e.tensor_tensor(out=xt[i][:], in0=xt[i][:], in1=ya[i][:], op=ALU.add), r=[('mya', i), ('mxt', i)], w=[('mxt', i)])
                kb.dma('sp', xbuf[t * 128:(t + 1) * 128, :], xt[i][:], r=[('mxt', i)], w=[('x', t)])
            kb.release(m)

        def phase_final():
            m = kb.mark()
            nwb = kb.sb('fnw', [128, D], F32)
            kb.dma('sp', nwb[:], norm_final.partition_broadcast(128), w=['fnw'])
            xt = [kb.sb(f'fxt{i}', [128, D], F32) for i in range(2)]
            yt = [kb.sb(f'fyt{i}', [128, D], F32) for i in range(2)]
            ssq = [kb.sb(f'fss{i}', [128, 1], F32) for i in range(2)]
            junk = kb.sb('fjunk', [128, D], F32)
            for t in range(2, NT):
                i = t % 2
                kb.dma('sp', xt[i][:], xbuf[t * 128:(t + 1) * 128, :], r=[('x', t)], w=[('fxt', i)])
                kb.op('act', lambda e: e.activation(out=junk[:], in_=xt[i][:], func=AF.Square, accum_out=ssq[i][:]), r=[('fxt', i)], w=['fjunk', ('fss', i)])
                kb.op('act', lambda e: e.activation(out=ssq[i][:], in_=ssq[i][:], func=AF.Sqrt, scale=1.0 / D, bias=epsN), r=[('fss', i)], w=[('fss', i)])
                kb.op('dve', lambda e: e.reciprocal(out=ssq[i][:], in_=ssq[i][:]), r=[('fss', i)], w=[('fss', i)])
                kb.op('dve', lambda e: e.scalar_tensor_tensor(out=yt[i][:], in0=xt[i][:], scalar=ssq[i][:, 0:1], in1=nwb[:], op0=ALU.mult, op1=ALU.mult),
                      r=[('fxt', i), ('fss', i), 'fnw'], w=[('fyt', i)])
                kb.dma('sp', out[(t - 2) * 128:(t - 1) * 128, :], yt[i][:], r=[('fyt', i)], w=[('out', t)])
            kb.release(m)

        def finish_dbg(hT_=True, oT=None, mT=None):
            kb.barrier()
            for t in range(NT):
                pass
            kb.dma('sp', dbg['d_x'], xbuf[:, :], w=['d_x'])
            if hT_:
                kb.dma('sp', dbg['d_hT'], hT[:], w=['d_hT'])
            if oT is not None:
                kb.dma('sp', dbg['d_oT'], oT[:], w=['d_oT'])
            if mT is not None:
                kb.dma('sp', dbg['d_mT'], mT[:], w=['d_mT'])
            kb.finish()
            return nc

        kb.dma('sp', xbuf[0:L, :], ctx_in[:, :], w=['xinit0'])
        kb.dma('sp', xbuf[L:T, :], x_in[:, :], w=['xinit1'])
        for l in range(nlayers):
            phase_ada(l)
        kb.barrier()
        if stop == 'ada':
            kb.dma('sp', dbg['d_mod'], modbuf[0], w=['d_mod'])
            return finish_dbg()
        for l in range(nlayers):
            phase_norm(l, 1)
            if stop == f'norm{l}':
                return finish_dbg()
            mm = kb.mark()
            oT = kb.sb('oT', [128, 4, T], BF16, top=True)
            phase_ret(l, oT)
            if stop == f'ret{l}':
                return finish_dbg(oT=oT)
            mT = kb.sb('mT', [128, 8, T], BF16)
            merge(l, 2048, w_br, oT, 'retT', mT, True)
            phase_attn(l, oT)
            if stop == f'attn{l}':
                return finish_dbg(oT=oT)
            merge(l, 0, w_ba, oT, 'oaT', mT, False)
            phase_four(l, oT)
            if stop == f'four{l}':
                return finish_dbg(oT=oT)
            merge(l, 1024, w_bf, oT, 'ofT', mT, False)
            if stop == f'merge{l}':
                return finish_dbg(mT=mT)
            phase_out(l, mT)
            kb.release(mm)
            if stop == f'mix{l}':
                return finish_dbg()
            if os.environ.get('MOE_DENSE'):
                phase_moe(l)
            else:
                phase_moe_sparse(l)
            if stop == f'moe{l}':
                return finish_dbg()
        phase_final()
        kb.finish()
    print("SBUF peak bytes", kb.peak, "instr counts", kb.cnt)
    return nc


def _consts():
    bf = ml_dtypes.bfloat16
    f32 = np.float32
    n = np.arange(N)
    inv16 = (10000.0 ** (-(np.arange(16, dtype=f32)) / f32(16))).astype(f32)
    row = (n // 64).astype(f32); col = (n % 64).astype(f32)
    ropeA = np.zeros((2, 128, N), f32)
    for p in range(128):
        d = p % 64
        pos = row if d < 32 else col
        dd = d % 32
        ang = (pos * inv16[dd % 16]).astype(f32)
        ropeA[0, p] = np.cos(ang); ropeA[1, p] = np.sin(ang) * (-1.0 if dd < 16 else 1.0)
    inv32 = (10000.0 ** (-(np.arange(32, dtype=f32)) / f32(32))).astype(f32)
    ropeR = np.zeros((2, 128, N), f32)
    for p in range(128):
        d = p % 64
        ang = (n.astype(f32) * inv32[d % 32]).astype(f32)
        ropeR[0, p] = np.cos(ang); ropeR[1, p] = np.sin(ang) * (-1.0 if d < 32 else 1.0)
    k = np.arange(N, dtype=np.int64)
    ph = (np.outer(k, k) % N).astype(np.float64) * (2 * np.pi / N)
    dftN = np.stack([np.cos(ph), -np.sin(ph)]) / np.sqrt(N)
    k2 = np.arange(256, dtype=np.int64)
    ph2 = (np.outer(k2, k2) % 256).astype(np.float64) * (2 * np.pi / 256)
    dft256 = np.stack([np.cos(ph2), -np.sin(ph2)]) / np.sqrt(256)
    k3 = np.arange(128, dtype=np.int64)
    ph3 = (np.outer(k3, k3) % 128).astype(np.float64) * (2 * np.pi / 128)
    dftC = np.concatenate([np.cos(ph3), np.sin(ph3)], axis=1) / np.sqrt(128)
    b = np.arange(128)[:, None]; a = np.arange(128)[None, :]
    amask = np.concatenate([(b >= a), (b <= a)], axis=1).astype(f32)
    j = b; i = a
    rconst = np.concatenate([np.maximum(i - j, 0), np.maximum(j - i, 0), (i >= j), (j > i), (i + 1) + 0 * j, (128 - i) + 0 * j,
                             127 - np.arange(128)[:, None], np.arange(128)[:, None]], axis=1).astype(f32)
    mconst = np.concatenate([np.tile((np.arange(32) * CAP)[None, :], (128, 1)), np.arange(128)[:, None] + 128 * np.arange(NT)[None, :]], axis=1).astype(f32)
    return dict(mconst=mconst, ropeA=ropeA, ropeR=ropeR, dftN=dftN.astype(bf), dft256=dft256.astype(bf), dftC=dftC.astype(bf),
                amask=amask.astype(bf), rconst=rconst)


def _wext_index():
    idx = []
    swa = np.array([d + 16 if (d % 32) < 16 else d - 16 for d in range(64)])
    swr = np.array([d + 32 if d < 32 else d - 32 for d in range(64)])
    base = 0
    qa = [np.concatenate([np.arange(g * 64, (g + 1) * 64), np.arange((4 + g) * 64, (5 + g) * 64)]) for g in range(4)]
    qas = [np.concatenate([g * 64 + swa, (4 + g) * 64 + swa]) for g in range(4)]
    idx += qa + qas
    idx += [512 + np.arange(128), 512 + np.concatenate([swa, 64 + swa])]
    idx += [640 + np.arange(128)]
    idx += [768 + np.arange(512), 768 + np.concatenate([h * 64 + swr for h in range(8)])]
    idx += [1280 + np.arange(512), 1280 + np.concatenate([h * 64 + swr for h in range(8)])]
    idx += [1792 + np.arange(512), 2304 + np.arange(512), 2816 + np.arange(512), 3328 + np.arange(3072)]
    idx = np.concatenate(idx)
    assert idx.shape[0] == WEXT
    return idx


_CACHE = {}


def kernel(x, c, ctx, c_ctx, norm_mix, norm_ffn, w_ada, b_ada, w_in, attn_sink, ret_decay_fwd, ret_decay_bwd,
           w_branch_attn, w_branch_fourier, w_branch_ret, w_out, w_router_group, b_router_group,
           w_router_expert, b_router_expert, w_exp_gate, w_exp_up, w_exp_down, norm_final):
    f = lambda a: np.ascontiguousarray(np.asarray(a, dtype=np.float32))
    if 'nc' not in _CACHE:
        _CACHE['nc'] = build()
        _CACHE['consts'] = _consts()
        _CACHE['idx'] = _wext_index()
    nc = _CACHE['nc']
    shared = dict(_CACHE['consts'])
    w_in = f(w_in)
    shared.update(
        c_ctx=f(c_ctx), norm_mix=f(norm_mix), norm_ffn=f(norm_ffn), w_ada=f(w_ada), b_ada=f(b_ada),
        w_ext=np.ascontiguousarray(w_in[:, :, _CACHE['idx']]), attn_sink=f(attn_sink),
        ret_decay_fwd=f(ret_decay_fwd), ret_decay_bwd=f(ret_decay_bwd), w_branch_attn=f(w_branch_attn),
        w_branch_fourier=f(w_branch_fourier), w_branch_ret=f(w_branch_ret), w_out=f(w_out),
        w_rt=np.ascontiguousarray(np.concatenate([f(w_router_group), f(w_router_expert)], axis=-1)),
        b_rt=np.ascontiguousarray(np.concatenate([f(b_router_group), f(b_router_expert)], axis=-1)),
        w_exp_gate=f(w_exp_gate), w_exp_up=f(w_exp_up), w_exp_down=f(w_exp_down), norm_final=f(norm_final))
    x = f(x); c = f(c); ctx = f(ctx)
    B = x.shape[0]
    in_maps = []
    for b in range(B):
        d = dict(shared)
        d.update(x=x[b], ctx=ctx[b], c=c[b])
        in_maps.append(d)
    res = run_bass_kernel_spmd(nc, in_maps, core_ids=list(range(B)))
    return np.stack([np.asarray(r["out"], dtype=np.float32) for r in res.results], axis=0)
```

```python
import contextlib, os
import numpy as np
import ml_dtypes
import concourse.bass as bass
import concourse.mybir as mybir
from concourse.bass_utils import run_bass_kernel_spmd

F32 = mybir.dt.float32
BF16 = mybir.dt.bfloat16
AF = mybir.ActivationFunctionType
ALU = mybir.AluOpType
AX = mybir.AxisListType

ARENA_BASE = 20736
SBUF_BYTES = 229376 - ARENA_BASE - 128
SAME_ENGINE_SYNC = bool(int(os.environ.get("SES", "1")))
NDMA_SEM = 12
EPOCH = 30000


def _dsize(dt):
    return 2 if dt == BF16 else 4


class KB:
    def __init__(self, nc):
        self.nc = nc
        self.es = contextlib.ExitStack()
        self.eng = {'pe': nc.tensor, 'act': nc.scalar, 'dve': nc.vector, 'pool': nc.gpsimd, 'sp': nc.sync}
        self.cnt = {e: 0 for e in self.eng}
        self.sems = {e: [] for e in self.eng}
        self.seen = {e: {} for e in self.eng}
        self.res = {}
        self.dma_sems = {}
        self.dma_i = {}
        self.bot = ARENA_BASE
        self.top = ARENA_BASE + SBUF_BYTES
        self.nps = 0
        self.n_sem = 0
        self.peak = 0
        self.off = {}

    def __enter__(self):
        self.es.__enter__()
        return self

    def __exit__(self, *a):
        return self.es.__exit__(*a)

    def sb(self, name, shape, dtype, top=False):
        n = 1
        for s in shape[1:]:
            n *= s
        nbytes = (n * _dsize(dtype) + 63) // 64 * 64
        if top:
            self.top -= nbytes
            off = self.top
        else:
            off = self.bot
            self.bot += nbytes
        assert self.bot <= self.top, f"SBUF overflow allocating {name}: bot={self.bot} top={self.top}"
        self.peak = max(self.peak, self.bot + SBUF_BYTES - self.top)
        self.nps += 1
        self.off[name] = off
        return self.nc.alloc_sbuf_tensor_at(f"{name}_{self.nps}", list(shape), dtype, offset=off)

    def alias(self, name, shape, dtype, off):
        self.nps += 1
        return self.nc.alloc_sbuf_tensor_at(f"{name}_{self.nps}", list(shape), dtype, offset=off)

    def mark(self):
        return (self.bot, self.top)

    def release(self, m):
        self.barrier()
        self.bot, self.top = m

    def ps(self, name, shape, dtype=F32):
        return self.nc.alloc_psum_tensor(name, list(shape), dtype)

    def _newsem(self, name):
        self.n_sem += 1
        return self.es.enter_context(self.nc.semaphore(f"{name}_{self.n_sem}"))

    def _tick(self, e):
        c = self.cnt[e]
        ep, v = divmod(c, EPOCH)
        while len(self.sems[e]) <= ep:
            self.sems[e].append(self._newsem(f"s_{e}"))
        self.cnt[e] = c + 1
        return (self.sems[e][ep], v + 1, e)

    def _wait(self, e, tick):
        sem, val, owner = tick
        if owner == e and (e == 'pe' or not SAME_ENGINE_SYNC):
            return
        k = sem.num
        if self.seen[e].get(k, 0) >= val:
            return
        self.eng[e].wait_ge(sem, val)
        self.seen[e][k] = val

    def _deps(self, e, r, w):
        ticks = []
        for k in r:
            rec = self.res.get(k)
            if rec and rec[0]:
                ticks.append(rec[0])
        for k in w:
            rec = self.res.get(k)
            if rec:
                if rec[0]:
                    ticks.append(rec[0])
                ticks.extend(rec[1])
        for t in ticks:
            self._wait(e, t)

    def _record(self, tick, r, w):
        for k in r:
            rec = self.res.setdefault(k, [None, []])
            rec[1] = [t for t in rec[1] if not (t[0] is tick[0])] + [tick]
        for k in w:
            self.res[k] = [tick, []]

    def op(self, e, fn, r=(), w=()):
        self._deps(e, r, w)
        tick = self._tick(e)
        fn(self.eng[e]).then_inc(tick[0], 1)
        self._record(tick, r, w)

    def dma(self, q, out, in_, r=(), w=(), fn=None, **kw):
        self._deps(q, r, w)
        pool = self.dma_sems.setdefault(q, [])
        i = self.dma_i.get(q, 0)
        self.dma_i[q] = i + 1
        slot = i % NDMA_SEM
        if len(pool) <= slot:
            pool.append([self._newsem(f"d_{q}"), 0])
        ent = pool[slot]
        if ent[1] > 0:
            self._wait(q, (ent[0], ent[1], 'dma'))
        ent[1] += 16
        tick = (ent[0], ent[1], 'dma')
        if fn is not None:
            fn(self.eng[q]).then_inc(ent[0], 16)
        else:
            self.eng[q].dma_start(out=out, in_=in_, **kw).then_inc(ent[0], 16)
        self._record(tick, r, w)

    def all_ticks(self):
        ticks = []
        for e in self.eng:
            c = self.cnt[e]
            if c > 0:
                ep, v = divmod(c - 1, EPOCH)
                ticks.append((self.sems[e][ep], v + 1, e))
        for q, pool in self.dma_sems.items():
            for ent in pool:
                if ent[1] > 0:
                    ticks.append((ent[0], ent[1], 'dma'))
        return ticks

    def barrier(self):
        ticks = self.all_ticks()
        for e in self.eng:
            for t in ticks:
                if t[2] == e:
                    continue
                self._wait(e, t)
        self.res = {}

    def finish(self):
        ticks = self.all_ticks()
        for t in ticks:
            if t[2] != 'sp':
                self._wait('sp', t)

    def make_ident(self, ident):
        nc = self.nc
        self.op('pool', lambda e: e.memset(ident[:], 1.0), w=['ident'])
        self.op('pool', lambda e: e.affine_select(out=ident[:], in_=ident[:], pattern=[[-1, 128]],
                                                  compare_op=ALU.is_equal, fill=0.0, base=0,
                                                  channel_multiplier=1), r=['ident'], w=['ident'])

D = 1024; L = 256; N = 2048; T = 2304; NT = 18
QA, QAS, KA, KAS, VA, QR, QRS, KR, KRS, VR, GR, FU, GT = 0, 512, 1024, 1152, 1280, 1408, 1920, 2432, 2944, 3456, 3968, 4480, 4992
WEXT = 8064
CAP = int(os.environ.get('MOE_CAP', '1024'))
NS = CAP // 128
I32 = mybir.dt.int32
TB = [(0, 256), (256, 512), (768, 512), (1280, 512), (1792, 512)]


def bc(ap, free):
    return bass.AP(ap.tensor, ap.offset, [list(ap.ap[0])] + [list(f) for f in free])


def build(stop=None, nlayers=2):
    nc = bass.Bass("TRN2", target_bir_lowering=False)

    def din(name, shape, dt=F32):
        return nc.dram_tensor(name, list(shape), dt, kind="ExternalInput").ap()
    x_in = din("x", [N, D]); ctx_in = din("ctx", [L, D]); c_in = din("c", [D]); cctx_in = din("c_ctx", [D])
    norm_mix = din("norm_mix", [2, D]); norm_ffn = din("norm_ffn", [2, D])
    w_ada = din("w_ada", [2, D, 6 * D]); b_ada = din("b_ada", [2, 6 * D])
    w_ext = din("w_ext", [2, D, WEXT]); sink_in = din("attn_sink", [2, 8])
    dec_f = din("ret_decay_fwd", [2, 8]); dec_b = din("ret_decay_bwd", [2, 8])
    w_ba = din("w_branch_attn", [2, 512, D]); w_bf = din("w_branch_fourier", [2, 512, D]); w_br = din("w_branch_ret", [2, 512, D])
    w_out = din("w_out", [2, D, D]); w_rt = din("w_rt", [2, D, 36]); b_rt = din("b_rt", [2, 36])
    if stop is None or stop.startswith('moe'):
        w_eg = din("w_exp_gate", [2, 32, D, 512]); w_eu = din("w_exp_up", [2, 32, D, 512]); w_ed = din("w_exp_down", [2, 32, 512, D])
    norm_final = din("norm_final", [D])
    ropeA = din("ropeA", [2, 128, N]); ropeR = din("ropeR", [2, 128, N])
    dftN = din("dftN", [2, N, N], BF16); dft256 = din("dft256", [2, 256, 256], BF16); dftC = din("dftC", [128, 256], BF16)
    amask = din("amask", [128, 256], BF16); rconst = din("rconst", [128, 770])
    out = nc.dram_tensor("out", [N, D], F32, kind="ExternalOutput").ap()
    xbuf = nc.dram_tensor("xbuf", [T, D], F32).ap()
    mconst = din("mconst", [128, 32 + NT])
    h2d = nc.dram_tensor("h2d", [T, D], BF16).ap()
    rec_d = nc.dram_tensor("rec_d", [32 * CAP, 4], F32).ap()
    AB = nc.dram_tensor("AB", [2 * T, D], F32).ap()
    modbuf = nc.dram_tensor("modbuf", [2, 2, 6 * D], F32).ap()
    dbg = {}
    if stop is not None:
        dbg['d_x'] = nc.dram_tensor("d_x", [T, D], F32, kind="ExternalOutput").ap()
        dbg['d_mod'] = nc.dram_tensor("d_mod", [2, 6 * D], F32, kind="ExternalOutput").ap()
        dbg['d_hT'] = nc.dram_tensor("d_hT", [128, 8, T], BF16, kind="ExternalOutput").ap()
        dbg['d_oT'] = nc.dram_tensor("d_oT", [128, 4, T], BF16, kind="ExternalOutput").ap()
        dbg['d_mT'] = nc.dram_tensor("d_mT", [128, 8, T], BF16, kind="ExternalOutput").ap()

    kb = KB(nc)
    with kb:
        PSA = kb.ps("psa", [128, 7, 512], F32)
        PST = kb.ps("pst", [128, 8, 128], BF16)
        ident = kb.sb("ident", [128, 128], BF16)
        ident32 = kb.sb("ident32", [128, 128], F32)
        kb.make_ident(ident)
        kb.op('pool', lambda e: e.memset(ident32[:], 1.0), w=['ident32'])
        kb.op('pool', lambda e: e.affine_select(out=ident32[:], in_=ident32[:], pattern=[[-1, 128]], compare_op=ALU.is_equal,
                                                fill=0.0, base=0, channel_multiplier=1), r=['ident32'], w=['ident32'])
        cst = kb.sb("cst", [128, 4], F32)
        kb.op('pool', lambda e: e.memset(cst[:, 0:1], 1e-6), w=['cst'])
        kb.op('pool', lambda e: e.memset(cst[:, 1:2], 1e-5), w=['cst'])
        kb.op('pool', lambda e: e.memset(cst[:, 2:3], 1.0), w=['cst'])
        epsN = cst[:, 0:1]; epsG = cst[:, 1:2]; one1 = cst[:, 2:3]
        stg = [kb.sb(f"stg{i}", [128, 2048], F32) for i in range(2)]
        stg_i = [0]; cast_rr = [0]
        hT = kb.sb("hT", [128, 8, T], BF16)
        kb.barrier()

        stg_all = [list(stg)]

        def load_w(dst, dkey, src2d, K, W, engs=('pool',)):
            kk = max(1, 2048 // W)
            stg = stg_all[0]
            for k0 in range(0, K, kk):
                k1 = min(K, k0 + kk)
                i = stg_i[0] % len(stg); stg_i[0] += 1
                sv = stg[i][:, 0:(k1 - k0) * W].rearrange("p (k c) -> p k c", c=W)
                kb.dma('sp', sv, src2d[k0 * 128:k1 * 128, :].rearrange("(k p) c -> p k c", p=128), w=[('stg', i)])
                en = engs[cast_rr[0] % len(engs)]; cast_rr[0] += 1
                if en == 'act':
                    kb.op('act', lambda e: e.copy(out=dst[:, k0:k1, :], in_=sv), r=[('stg', i)], w=[dkey])
                else:
                    kb.op(en, lambda e: e.tensor_copy(out=dst[:, k0:k1, :], in_=sv), r=[('stg', i)], w=[dkey])

        def proj_fm(wt, wkey, c0, tok0, ntok, bank):
            for k in range(8):
                kb.op('pe', lambda e: e.matmul(PSA[:, bank, 0:ntok], lhsT=wt[:, k, c0:c0 + 128], rhs=hT[:, k, tok0:tok0 + ntok],
                                               start=(k == 0), stop=(k == 7)), r=[wkey, 'hT'], w=[('ps', bank)])

        def dump(name, src, r):
            kb.dma('sp', dbg[name], src, r=r, w=[name])

        def xdump(name, t, shape, dt):
            if stop is None or not os.environ.get('XDUMP'):
                return
            kb.barrier()
            d_ = nc.dram_tensor("x_" + name, list(shape), dt, kind="ExternalOutput").ap()
            kb.dma('sp', d_, t[:], w=["x_" + name])

        def phase_ada(l):
            m = kb.mark()
            cT = kb.sb('cT', [128, 8, 2], F32); cTb = kb.sb('cTb', [128, 8, 2], BF16)
            bsb = kb.sb('bsb', [2, 6 * D], F32); modsb = kb.sb('modsb', [2, 6 * D], F32)
            wa = [kb.sb(f'wa{i}', [128, 8, 512], BF16) for i in range(2)]
            kb.dma('sp', cT[:, :, 0], cctx_in.rearrange("(k p) -> p k", p=128), w=['cT'], allow_slow_non_contiguous=True)
            kb.dma('sp', cT[:, :, 1], c_in.rearrange("(k p) -> p k", p=128), w=['cT'], allow_slow_non_contiguous=True)
            kb.dma('sp', bsb[:], b_ada[l].partition_broadcast(2), w=['bsb'])
            kb.op('act', lambda e: e.activation(out=cTb[:], in_=cT[:], func=AF.Silu), r=['cT'], w=['cTb'])
            for nb in range(12):
                i = nb % 2
                load_w(wa[i], ('wa', i), w_ada[l][:, nb * 512:(nb + 1) * 512], 8, 512, engs=('pool', 'act'))
                for k in range(8):
                    kb.op('pe', lambda e: e.matmul(PSA[0:2, i, :], lhsT=cTb[:, k, :], rhs=wa[i][:, k, :], start=(k == 0), stop=(k == 7)),
                          r=['cTb', ('wa', i)], w=[('ps', i)])
                kb.op('dve', lambda e: e.tensor_tensor(out=modsb[:, nb * 512:(nb + 1) * 512], in0=PSA[0:2, i, :],
                                                       in1=bsb[:, nb * 512:(nb + 1) * 512], op=ALU.add), r=[('ps', i), 'bsb'], w=['modsb'])
            kb.dma('sp', modbuf[l], modsb[:], r=['modsb'], w=[('mod', l)])
            if stop == f'ada{l}':
                dump('d_mod', modsb[:], ['modsb'])
            kb.release(m)
            kb.res[('mod', l)] = None
            kb.res.pop(('mod', l))

        def phase_norm(l, which, logits=None, sparse=False):
            m = kb.mark()
            nw = norm_mix if which == 1 else norm_ffn
            si, ci = (0, 1) if which == 1 else (3, 4)
            tiles = range(NT) if (which == 1 or l == 0) else range(2, NT)
            nwb = kb.sb('nwb', [128, D], F32)
            kb.dma('sp', nwb[:], nw[l].partition_broadcast(128), w=['nwb'])
            G = []; S = []
            for row in (0, 1):
                g_ = kb.sb(f'G{row}', [128, D], F32); s_ = kb.sb(f'S{row}', [128, D], F32)
                kb.dma('sp', g_[:], modbuf[l, row, ci * D:(ci + 1) * D].partition_broadcast(128), w=[f'G{row}'])
                kb.dma('sp', s_[:], modbuf[l, row, si * D:(si + 1) * D].partition_broadcast(128), w=[f'S{row}'])
                kb.op('dve', lambda e: e.scalar_tensor_tensor(out=g_[:], in0=g_[:], scalar=1.0, in1=nwb[:], op0=ALU.add, op1=ALU.mult),
                      r=[f'G{row}', 'nwb'], w=[f'G{row}'])
                G.append(g_); S.append(s_)
            xt = [kb.sb(f'xt{i}', [128, D], F32) for i in range(2)]
            tmp = [kb.sb(f'tmp{i}', [128, D], F32) for i in range(2)]
            hb = [kb.sb(f'hb{i}', [128, D], BF16 if which == 1 else F32) for i in range(2)]
            ssq = [kb.sb(f'ssq{i}', [128, 1], F32) for i in range(2)]
            junk = kb.sb('junk', [128, D], F32)
            if which == 2:
                hbb = [kb.sb(f'hbb{i}', [128, D], BF16) for i in range(2)]
                h32 = [kb.sb(f'h32{i}', [128, 8, 128], F32) for i in range(2)]
                wrt32 = kb.sb('wrt32', [128, 8, 36], F32); brt = kb.sb('brt', [128, 36], F32)
                kb.dma('sp', wrt32[:], w_rt[l].rearrange("(k p) c -> p k c", p=128), w=['wrt32'])
                kb.dma('sp', brt[:], b_rt[l].partition_broadcast(128), w=['brt'])
            for t in tiles:
                i = t % 2; row = 0 if t < 2 else 1
                kb.dma('sp', xt[i][:], xbuf[t * 128:(t + 1) * 128, :], r=[('x', t)], w=[('xt', i)])
                kb.op('act', lambda e: e.activation(out=junk[:], in_=xt[i][:], func=AF.Square, accum_out=ssq[i][:]),
                      r=[('xt', i)], w=['junk', ('ssq', i)])
                kb.op('act', lambda e: e.activation(out=ssq[i][:], in_=ssq[i][:], func=AF.Sqrt, scale=1.0 / D, bias=epsN),
                      r=[('ssq', i)], w=[('ssq', i)])
                kb.op('dve', lambda e: e.reciprocal(out=ssq[i][:], in_=ssq[i][:]), r=[('ssq', i)], w=[('ssq', i)])
                kb.op('dve', lambda e: e.scalar_tensor_tensor(out=tmp[i][:], in0=xt[i][:], scalar=ssq[i][:, 0:1], in1=G[row][:],
                                                              op0=ALU.mult, op1=ALU.mult), r=[('xt', i), ('ssq', i), f'G{row}'], w=[('tmp', i)])
                kb.op('pool', lambda e: e.tensor_tensor(out=hb[i][:], in0=tmp[i][:], in1=S[row][:], op=ALU.add),
                      r=[('tmp', i), f'S{row}'], w=[('hb', i)])
                if which == 1:
                    for k in range(8):
                        kb.op('pe', lambda e: e.transpose(out=PST[:, k, :], in_=hb[i][:, k * 128:(k + 1) * 128], identity=ident[:]),
                              r=[('hb', i), 'ident'], w=['pst'])
                    kb.op('act', lambda e: e.copy(out=hT[:, :, t * 128:(t + 1) * 128], in_=PST[:]), r=['pst'], w=['hT'])
                else:
                    for k in range(8):
                        kb.op('pe', lambda e: e.matmul(PSA[:, 5 + k // 4, (k % 4) * 128:(k % 4 + 1) * 128], lhsT=hb[i][:, k * 128:(k + 1) * 128],
                                                       rhs=ident32[:], start=True, stop=True), r=[('hb', i), 'ident32'], w=[('ps', 5 + k // 4)])
                    pv = PSA[:, 5:7, :].rearrange("p a (b c) -> p (a b) c", c=128)
                    kb.op('dve', lambda e: e.tensor_copy(out=h32[i][:], in_=pv), r=[('ps', 5), ('ps', 6)], w=[('h32', i)])
                    if sparse:
                        kb.op('act', lambda e: e.copy(out=hbb[i][:], in_=hb[i][:]), r=[('hb', i)], w=[('hbb', i)])
                        kb.dma('sp', h2d[t * 128:(t + 1) * 128, :], hbb[i][:], r=[('hbb', i)], w=[('h2d', t)])
                    else:
                        kb.op('act', lambda e: e.copy(out=hT[:, :, t * 128:(t + 1) * 128], in_=h32[i][:]), r=[('h32', i)], w=['hT'])
                    for k in range(8):
                        kb.op('pe', lambda e: e.matmul(PSA[:, 4, 0:36], lhsT=h32[i][:, k, :], rhs=wrt32[:, k, :], start=(k == 0), stop=(k == 7)),
                              r=[('h32', i), 'wrt32'], w=[('ps', 4)])
                    kb.op('dve', lambda e: e.tensor_tensor(out=logits[:, t, :], in0=PSA[:, 4, 0:36], in1=brt[:], op=ALU.add),
                          r=[('ps', 4), 'brt'], w=['logits'])
            kb.release(m)

        def merge(l, gate_off, wb_dram, oT, okey, mT, first):
            m = kb.mark()
            wg = [kb.sb(f'mwg{i}', [128, 8, 128], BF16) for i in range(2)]
            wb = [kb.sb(f'mwb{i}', [128, 4, 128], BF16) for i in range(2)]
            sig = [kb.sb(f'sig{i}', [128, 512], F32) for i in range(2)]
            mtmp = [kb.sb(f'mtmp{i}', [128, 512], F32) for i in range(2)]
            it = 0
            for fc in range(8):
                j = fc % 2
                load_w(wg[j], ('mwg', j), w_ext[l][:, GT + gate_off + fc * 128: GT + gate_off + (fc + 1) * 128], 8, 128)
                load_w(wb[j], ('mwb', j), wb_dram[l][:, fc * 128:(fc + 1) * 128], 4, 128)
                for (s0, n) in (TB if l == 0 else TB[1:]):
                    i = it % 2; it += 1
                    proj_fm(wg[j], ('mwg', j), 0, s0, n, i)
                    kb.op('act', lambda e: e.activation(out=sig[i][:, 0:n], in_=PSA[:, i, 0:n], func=AF.Sigmoid), r=[('ps', i)], w=[('sig', i)])
                    for k in range(4):
                        kb.op('pe', lambda e: e.matmul(PSA[:, 2 + i, 0:n], lhsT=wb[j][:, k, :], rhs=oT[:, k, s0:s0 + n], start=(k == 0), stop=(k == 3)),
                              r=[('mwb', j), okey], w=[('ps', 2 + i)])
                    if first:
                        kb.op('dve', lambda e: e.tensor_tensor(out=mT[:, fc, s0:s0 + n], in0=sig[i][:, 0:n], in1=PSA[:, 2 + i, 0:n], op=ALU.mult),
                              r=[('sig', i), ('ps', 2 + i)], w=['mT'])
                    else:
                        kb.op('dve', lambda e: e.tensor_tensor(out=mtmp[i][:, 0:n], in0=sig[i][:, 0:n], in1=PSA[:, 2 + i, 0:n], op=ALU.mult),
                              r=[('sig', i), ('ps', 2 + i)], w=[('mtmp', i)])
                        kb.op('pool', lambda e: e.tensor_tensor(out=mT[:, fc, s0:s0 + n], in0=mT[:, fc, s0:s0 + n], in1=mtmp[i][:, 0:n], op=ALU.add),
                              r=[('mtmp', i), 'mT'], w=['mT'])
            kb.release(m)

        def rope_evac(bA, bB, dst, dkey, tC, tS, n, t1, t2, j):
            kb.op('dve', lambda e: e.tensor_tensor(out=t1[:, 0:n], in0=PSA[:, bA, 0:n], in1=tC[:, 0:n], op=ALU.mult), r=[('ps', bA), ('rc', j)], w=[('t1', j)])
            kb.op('dve', lambda e: e.tensor_tensor(out=t2[:, 0:n], in0=PSA[:, bB, 0:n], in1=tS[:, 0:n], op=ALU.mult), r=[('ps', bB), ('rs', j)], w=[('t2', j)])
            kb.op('pool', lambda e: e.tensor_tensor(out=dst, in0=t1[:, 0:n], in1=t2[:, 0:n], op=ALU.add), r=[('t1', j), ('t2', j)], w=[dkey])

        def phase_ret(l, retT):
            need_ctx = (l == 0)
            m = kb.mark()
            RC = kb.sb('RC', [128, 770], F32)
            kb.dma('sp', RC[:], rconst[:, :], w=['RC'])
            dpos = RC[:, 0:128]; dneg = RC[:, 128:256]; mge = RC[:, 256:384]; mlt = RC[:, 384:512]
            io1 = RC[:, 512:640]; iob = RC[:, 640:768]; pc127 = RC[:, 768:769]; pcol = RC[:, 769:770]
            lg = kb.sb('lg', [128, 16], F32)
            kb.dma('sp', lg[:, 0:8], dec_f[l].partition_broadcast(128), w=['lg'])
            kb.dma('sp', lg[:, 8:16], dec_b[l].partition_broadcast(128), w=['lg'])
            kb.op('act', lambda e: e.activation(out=lg[:], in_=lg[:], func=AF.Exp, scale=-1.0), r=['lg'], w=['lg'])
            kb.op('act', lambda e: e.activation(out=lg[:], in_=lg[:], func=AF.Ln, bias=one1), r=['lg', 'cst'], w=['lg'])
            kb.op('dve', lambda e: e.tensor_scalar(out=lg[:], in0=lg[:], scalar1=-1.0, scalar2=None, op0=ALU.mult), r=['lg'], w=['lg'])
            lgp = kb.sb('lgp', [128, 8], F32)
            for r in range(4):
                for hh in range(2):
                    for d in range(2):
                        kb.op('pool', lambda e: e.tensor_copy(out=lgp[hh * 64:(hh + 1) * 64, d * 4 + r:d * 4 + r + 1],
                                                              in_=lg[hh * 64:(hh + 1) * 64, d * 8 + 2 * r + hh:d * 8 + 2 * r + hh + 1]), r=['lg'], w=['lgp'])
            g128 = kb.sb('g128', [128, 8], F32)
            kb.op('act', lambda e: e.activation(out=g128[:], in_=lgp[:], func=AF.Exp, scale=128.0), r=['lgp'], w=['g128'])
            Z = kb.sb('Z', [128, 16], F32)
            kb.op('act', lambda e: e.activation(out=Z[:, 0:8], in_=lg[:, 0:8], func=AF.Exp, scale=pc127), r=['lg', 'RC'], w=['Z'])
            kb.op('act', lambda e: e.activation(out=Z[:, 8:16], in_=lg[:, 8:16], func=AF.Exp, scale=pcol), r=['lg', 'RC'], w=['Z'])
            kb.op('dve', lambda e: e.tensor_scalar(out=Z[:], in0=Z[:], scalar1=0.125, scalar2=None, op0=ALU.mult), r=['Z'], w=['Z'])
            DecT = kb.sb('DecT', [128, 8, 128], F32)
            d1 = kb.sb('d1', [128, 128], F32); d2 = kb.sb('d2', [128, 128], F32)
            for h in range(8):
                kb.op('act', lambda e: e.activation(out=d1[:], in_=dpos, func=AF.Exp, scale=lg[:, h:h + 1]), r=['lg', 'RC'], w=['d1'])
                kb.op('pool', lambda e: e.tensor_tensor(out=d1[:], in0=d1[:], in1=mge, op=ALU.mult), r=['d1', 'RC'], w=['d1'])
                kb.op('act', lambda e: e.activation(out=d2[:], in_=dneg, func=AF.Exp, scale=lg[:, 8 + h:9 + h]), r=['lg', 'RC'], w=['d2'])
                kb.op('pool', lambda e: e.tensor_tensor(out=d2[:], in0=d2[:], in1=mlt, op=ALU.mult), r=['d2', 'RC'], w=['d2'])
                kb.op('pool', lambda e: e.tensor_tensor(out=d1[:], in0=d1[:], in1=d2[:], op=ALU.add), r=['d1', 'd2'], w=['d1'])
                kb.op('dve', lambda e: e.tensor_scalar(out=DecT[:, h, :], in0=d1[:], scalar1=0.125, scalar2=None, op0=ALU.mult), r=['d1'], w=['DecT'])
            X = kb.sb('X', [128, 8, 128], F32)
            for r in range(4):
                kb.op('act', lambda e: e.activation(out=X[:, r, :], in_=io1, func=AF.Exp, scale=lgp[:, r:r + 1]), r=['lgp', 'RC'], w=['X'])
                kb.op('act', lambda e: e.activation(out=X[:, 4 + r, :], in_=iob, func=AF.Exp, scale=lgp[:, 4 + r:5 + r]), r=['lgp', 'RC'], w=['X'])
            if os.environ.get('RET_CUT') == '1':
                kb.release(m); return
            qT = kb.sb('qT', [128, T], BF16); kT = kb.sb('kT', [128, T], BF16)
            ktok = kb.sb('ktok', [128, NT, 128], BF16); vtok = kb.sb('vtok', [128, NT, 128], BF16)
            vf = kb.sb('vf', [128, NT, 128], BF16); vb = kb.sb('vb', [128, NT, 128], BF16)
            sg = kb.sb('sg', [128, NT, 128], F32)
            Sf = kb.sb('Sf', [128, NT, 128], BF16); Rb = kb.sb('Rb', [128, NT, 128], BF16)
            Srun = kb.sb('Srun', [128, 128], F32); Rrun = kb.sb('Rrun', [128, 128], F32)
            ws = {nm: kb.sb('w' + nm, [128, 8, 128], BF16) for nm in ('q', 'qs', 'k', 'ks', 'v', 'g')}
            rc = [kb.sb(f'rc{i}', [128, 512], F32) for i in range(2)]; rs = [kb.sb(f'rs{i}', [128, 512], F32) for i in range(2)]
            t1 = [kb.sb(f't1{i}', [128, 512], F32) for i in range(2)]; t2 = [kb.sb(f't2{i}', [128, 512], F32) for i in range(2)]
            AT = [kb.sb(f'AT{i}', [128, 2, 128], BF16) for i in range(2)]
            qxf = [kb.sb(f'qxf{i}', [128, 128], BF16) for i in range(2)]; qxb = [kb.sb(f'qxb{i}', [128, 128], BF16) for i in range(2)]
            oc = [kb.sb(f'oc{i}', [128, 128], F32) for i in range(2)]; sq = [kb.sb(f'sq{i}', [128, 128], F32) for i in range(2)]
            st = [kb.sb(f'st{i}', [128, 4], F32) for i in range(2)]
            rtok = [kb.sb(f'rtok{i}', [128, 128], BF16) for i in range(2)]
            o_all = kb.sb('o_all', [128, NT, 128], F32); sq_all = kb.sb('sq_all', [128, NT, 128], F32); rt_all = kb.sb('rt_all', [128, NT, 128], BF16)
            mu_ = kb.sb('mu_', [128, 2 * NT], F32); va_ = kb.sb('va_', [128, 2 * NT], F32)
            for r in range(4):
                for nm, c0 in (('q', QR), ('qs', QRS), ('k', KR), ('ks', KRS), ('v', VR), ('g', GR)):
                    load_w(ws[nm], 'w' + nm, w_ext[l][:, c0 + r * 128:c0 + (r + 1) * 128], 8, 128)
                for bi, (s0, n) in enumerate(TB):
                    if s0 == 0:
                        proj_fm(ws['q'], 'wq', 0, s0, n, 0)
                        kb.op('act', lambda e: e.copy(out=qT[:, s0:s0 + n], in_=PSA[:, 0, 0:n]), r=[('ps', 0)], w=['qT'])
                        proj_fm(ws['k'], 'wk', 0, s0, n, 1)
                        kb.op('act', lambda e: e.copy(out=kT[:, s0:s0 + n], in_=PSA[:, 1, 0:n]), r=[('ps', 1)], w=['kT'])
                    else:
                        j = bi % 2; p0 = s0 - 256
                        kb.dma('sp', rc[j][:, 0:n], ropeR[0, :, p0:p0 + n], w=[('rc', j)])
                        kb.dma('sp', rs[j][:, 0:n], ropeR[1, :, p0:p0 + n], w=[('rs', j)])
                        proj_fm(ws['q'], 'wq', 0, s0, n, 0); proj_fm(ws['qs'], 'wqs', 0, s0, n, 1)
                        rope_evac(0, 1, qT[:, s0:s0 + n], 'qT', rc[j], rs[j], n, t1[j], t2[j], j)
                        proj_fm(ws['k'], 'wk', 0, s0, n, 2); proj_fm(ws['ks'], 'wks', 0, s0, n, 3)
                        rope_evac(2, 3, kT[:, s0:s0 + n], 'kT', rc[j], rs[j], n, t1[j], t2[j], j)
                    for t in range(s0 // 128, (s0 + n) // 128):
                        bank = 4 + (t % 2)
                        for k in range(8):
                            kb.op('pe', lambda e: e.matmul(PSA[:, bank, 0:128], lhsT=hT[:, k, t * 128:(t + 1) * 128], rhs=ws['v'][:, k, :],
                                                           start=(k == 0), stop=(k == 7)), r=['hT', 'wv'], w=[('ps', bank)])
                        for k in range(8):
                            kb.op('pe', lambda e: e.matmul(PSA[:, bank, 128:256], lhsT=hT[:, k, t * 128:(t + 1) * 128], rhs=ws['g'][:, k, :],
                                                           start=(k == 0), stop=(k == 7)), r=['hT', 'wg'], w=[('ps', bank)])
                        kb.op('act', lambda e: e.copy(out=vtok[:, t, :], in_=PSA[:, bank, 0:128]), r=[('ps', bank)], w=['vtok'])
                        kb.op('act', lambda e: e.activation(out=sg[:, t, :], in_=PSA[:, bank, 128:256], func=AF.Silu), r=[('ps', bank)], w=['sg'])
                if os.environ.get('RET_CUT') == '2':
                    kb.release(m); return
                for t in range(NT):
                    kb.op('pe', lambda e: e.transpose(out=PST[:, t % 8, :], in_=kT[:, t * 128:(t + 1) * 128], identity=ident[:]), r=['kT', 'ident'], w=['pst'])
                    if t % 8 == 7 or t == NT - 1:
                        n8 = t % 8 + 1; t0 = t - n8 + 1
                        kb.op('act', lambda e: e.copy(out=ktok[:, t0:t + 1, :], in_=PST[:, 0:n8, :]), r=['pst'], w=['ktok'])
                v4 = lambda a: a[:].rearrange("p c (h e) -> p c h e", h=2)
                kb.op('pool', lambda e: e.tensor_tensor(out=v4(vf), in0=v4(vtok), in1=bc(Z[:, 2 * r:2 * r + 2], [[0, NT], [1, 2], [0, 64]]), op=ALU.mult),
                      r=['vtok', 'Z'], w=['vf'])
                kb.op('pool', lambda e: e.tensor_tensor(out=v4(vb), in0=v4(vtok), in1=bc(Z[:, 8 + 2 * r:8 + 2 * r + 2], [[0, NT], [1, 2], [0, 64]]), op=ALU.mult),
                      r=['vtok', 'Z'], w=['vb'])
                if os.environ.get('RET_CUT') == '3':
                    kb.release(m); return
                kb.op('pool', lambda e: e.memset(Srun[:], 0.0), w=['Srun'])
                kb.op('pool', lambda e: e.memset(Sf[:, 0, :], 0.0), w=['Sf'])
                for c in range(NT - 1):
                    bank = 4 + c % 2
                    kb.op('pe', lambda e: e.matmul(PSA[:, bank, 0:128], lhsT=ktok[:, c, :], rhs=vf[:, c, :], start=True, stop=True),
                          r=['ktok', 'vf'], w=[('ps', bank)])
                    kb.op('dve', lambda e: e.scalar_tensor_tensor(out=Srun[:], in0=Srun[:], scalar=g128[:, r:r + 1], in1=PSA[:, bank, 0:128],
                                                                  op0=ALU.mult, op1=ALU.add), r=['Srun', 'g128', ('ps', bank)], w=['Srun'])
                    kb.op('act', lambda e: e.copy(out=Sf[:, c + 1, :], in_=Srun[:]), r=['Srun'], w=['Sf'])
                kb.op('pool', lambda e: e.memset(Rrun[:], 0.0), w=['Rrun'])
                kb.op('pool', lambda e: e.memset(Rb[:, 1, :], 0.0), w=['Rb'])
                order = [1, 0] + list(range(17, 2, -1)); dest = [0, 17] + list(range(16, 1, -1))
                for ii, (c, dd) in enumerate(zip(order, dest)):
                    bank = 4 + ii % 2
                    kb.op('pe', lambda e: e.matmul(PSA[:, bank, 0:128], lhsT=ktok[:, c, :], rhs=vb[:, c, :], start=True, stop=True),
                          r=['ktok', 'vb'], w=[('ps', bank)])
                    kb.op('dve', lambda e: e.scalar_tensor_tensor(out=Rrun[:], in0=Rrun[:], scalar=g128[:, 4 + r:5 + r], in1=PSA[:, bank, 0:128],
                                                                  op0=ALU.mult, op1=ALU.add), r=['Rrun', 'g128', ('ps', bank)], w=['Rrun'])
                    kb.op('act', lambda e: e.copy(out=Rb[:, dd, :], in_=Rrun[:]), r=['Rrun'], w=['Rb'])
                if os.environ.get('RET_CUT') == '4':
                    kb.release(m); return
                chunks_ = list(range(NT) if need_ctx else range(2, NT))

                def inner_(c):
                    i = c % 2; cs = slice(c * 128, (c + 1) * 128)
                    for hh in range(2):
                        ps_ = slice(hh * 64, (hh + 1) * 64)
                        kb.op('pe', lambda e: e.matmul(PSA[:, i + 4 * hh, 0:128], lhsT=kT[ps_, cs], rhs=qT[ps_, cs], start=True, stop=True),
                              r=['kT', 'qT'], w=[('ps', i + 4 * hh)])

                def rest_(c):
                    i = c % 2; cs = slice(c * 128, (c + 1) * 128)
                    for hh in range(2):
                        kb.op('dve', lambda e: e.tensor_tensor(out=AT[i][:, hh, :], in0=PSA[:, i + 4 * hh, 0:128],
                                                               in1=DecT[:, 2 * r + hh, :], op=ALU.mult), r=[('ps', i + 4 * hh), 'DecT'], w=[('AT', i)])
                    kb.op('pool', lambda e: e.tensor_tensor(out=qxf[i][:], in0=qT[:, cs], in1=X[:, r, :], op=ALU.mult), r=['qT', 'X'], w=[('qxf', i)])
                    kb.op('pool', lambda e: e.tensor_tensor(out=qxb[i][:], in0=qT[:, cs], in1=X[:, 4 + r, :], op=ALU.mult), r=['qT', 'X'], w=[('qxb', i)])
                    for hh in range(2):
                        ps_ = slice(hh * 64, (hh + 1) * 64)
                        o_ = PSA[:, 2 + i, hh * 64:(hh + 1) * 64]
                        kb.op('pe', lambda e: e.matmul(o_, lhsT=AT[i][:, hh, :], rhs=vtok[:, c, hh * 64:(hh + 1) * 64], start=True, stop=False),
                              r=[('AT', i), 'vtok'], w=[('ps', 2 + i)])
                        kb.op('pe', lambda e: e.matmul(o_, lhsT=qxf[i][ps_, :], rhs=Sf[ps_, c, hh * 64:(hh + 1) * 64], start=False, stop=False),
                              r=[('qxf', i), 'Sf'], w=[('ps', 2 + i)])
                        kb.op('pe', lambda e: e.matmul(o_, lhsT=qxb[i][ps_, :], rhs=Rb[ps_, c, hh * 64:(hh + 1) * 64], start=False, stop=True),
                              r=[('qxb', i), 'Rb'], w=[('ps', 2 + i)])
                    kb.op('act', lambda e: e.copy(out=o_all[:, c, :], in_=PSA[:, 2 + i, 0:128]), r=[('ps', 2 + i)], w=['o_all'])

                inner_(chunks_[0])
                for n_, c in enumerate(chunks_):
                    if n_ + 1 < len(chunks_):
                        inner_(chunks_[n_ + 1])
                    rest_(c)
                c0_ = 0 if need_ctx else 2
                G_ = (NT - c0_) * 2
                og = o_all[:, c0_:NT, :].rearrange("p c (h e) -> p (c h) e", h=2)
                sqg = sq_all[:, c0_:NT, :].rearrange("p c (h e) -> p (c h) e", h=2)
                kb.op('dve', lambda e: e.reduce_sum(out=mu_[:, 0:G_], in_=og, axis=AX.X), r=['o_all'], w=['mu_'])
                kb.op('dve', lambda e: e.tensor_scalar(out=mu_[:, 0:G_], in0=mu_[:, 0:G_], scalar1=-1.0 / 64, scalar2=None, op0=ALU.mult), r=['mu_'], w=['mu_'])
                kb.op('pool', lambda e: e.tensor_tensor(out=og, in0=og, in1=bc(mu_[:, 0:G_], [[1, G_], [0, 64]]), op=ALU.add), r=['o_all', 'mu_'], w=['o_all'])
                kb.op('pool', lambda e: e.tensor_tensor(out=sqg, in0=og, in1=og, op=ALU.mult), r=['o_all'], w=['sq_all'])
                kb.op('dve', lambda e: e.reduce_sum(out=va_[:, 0:G_], in_=sqg, axis=AX.X), r=['sq_all'], w=['va_'])
                kb.op('act', lambda e: e.activation(out=va_[:, 0:G_], in_=va_[:, 0:G_], func=AF.Sqrt, scale=1.0 / 64, bias=epsG), r=['va_'], w=['va_'])
                kb.op('dve', lambda e: e.reciprocal(out=va_[:, 0:G_], in_=va_[:, 0:G_]), r=['va_'], w=['va_'])
                kb.op('pool', lambda e: e.tensor_tensor(out=og, in0=og, in1=bc(va_[:, 0:G_], [[1, G_], [0, 64]]), op=ALU.mult), r=['o_all', 'va_'], w=['o_all'])
                kb.op('pool', lambda e: e.tensor_tensor(out=rt_all[:, c0_:NT, :], in0=o_all[:, c0_:NT, :], in1=sg[:, c0_:NT, :], op=ALU.mult),
                      r=['o_all', 'sg'], w=['rt_all'])
                cl_ = list(range(c0_, NT))
                for n0 in range(0, len(cl_), 8):
                    grp_ = cl_[n0:n0 + 8]
                    for ii_, c in enumerate(grp_):
                        kb.op('pe', lambda e: e.transpose(out=PST[:, ii_, :], in_=rt_all[:, c, :], identity=ident[:]), r=['rt_all', 'ident'], w=['pst'])
                    kb.op('act', lambda e: e.copy(out=retT[:, r, grp_[0] * 128:(grp_[-1] + 1) * 128].rearrange("p (a b) -> p a b", b=128),
                                                  in_=PST[:, 0:len(grp_), :]), r=['pst'], w=['retT'])
                if os.environ.get('RET_CUT') in ('5', '6', '7'):
                    kb.release(m); return
                if r == 3:
                    for nm_, t_, sh_, dt_ in (('sg', sg, [128, NT, 128], F32), ('DecT', DecT, [128, 8, 128], F32), ('X', X, [128, 8, 128], F32),
                                              ('Z', Z, [128, 16], F32), ('g128', g128, [128, 8], F32), ('lg', lg, [128, 16], F32),
                                              ('Sf', Sf, [128, NT, 128], BF16), ('Rb', Rb, [128, NT, 128], BF16), ('qT', qT, [128, T], BF16),
                                              ('kT', kT, [128, T], BF16), ('vtok', vtok, [128, NT, 128], BF16), ('ktok', ktok, [128, NT, 128], BF16),
                                              ('vf', vf, [128, NT, 128], BF16), ('AT1', AT[1], [128, 2, 128], BF16), ('qxf1', qxf[1], [128, 128], BF16),
                                              ('qxb1', qxb[1], [128, 128], BF16)):
                        xdump(nm_, t_, sh_, dt_)
            kb.release(m)

        def phase_attn(l, oaT):
            need_ctx = (l == 0)
            m = kb.mark()
            qT = kb.sb('aqT', [128, 4, T], BF16); kT = kb.sb('akT', [128, T], BF16)
            Va = kb.sb('Va', [128, NT, 2, 66], BF16)
            msk = kb.sb('msk', [128, 256], BF16)
            kb.dma('sp', msk[:], amask[:, :], w=['msk'])
            snk = kb.sb('snk', [128, 8], F32)
            kb.dma('sp', snk[:], sink_in[l].partition_broadcast(128), w=['snk'])
            kb.op('act', lambda e: e.activation(out=snk[:], in_=snk[:], func=AF.Exp), r=['snk'], w=['snk'])
            kb.op('pool', lambda e: e.memset(Va[:, :, :, 64:66], 1.0), w=['Va'])
            wq = kb.sb('awq', [128, 8, 1024], BF16); wk = kb.sb('awk', [128, 8, 256], BF16); wv = kb.sb('awv', [128, 8, 128], BF16)
            load_w(wq, 'awq', w_ext[l][:, QA:QA + 1024], 8, 1024)
            load_w(wk, 'awk', w_ext[l][:, KA:KA + 256], 8, 256)
            load_w(wv, 'awv', w_ext[l][:, VA:VA + 128], 8, 128)
            rc = [kb.sb(f'arc{i}', [128, 512], F32) for i in range(2)]; rs = [kb.sb(f'ars{i}', [128, 512], F32) for i in range(2)]
            t1 = [kb.sb(f'at1{i}', [128, 512], F32) for i in range(2)]; t2 = [kb.sb(f'at2{i}', [128, 512], F32) for i in range(2)]
            for bi, (s0, n) in enumerate(TB):
                if s0 == 0:
                    for g in range(4):
                        proj_fm(wq, 'awq', g * 128, s0, n, g % 2)
                        kb.op('act', lambda e: e.copy(out=qT[:, g, s0:s0 + n], in_=PSA[:, g % 2, 0:n]), r=[('ps', g % 2)], w=['aqT'])
                    proj_fm(wk, 'awk', 0, s0, n, 2)
                    kb.op('act', lambda e: e.copy(out=kT[:, s0:s0 + n], in_=PSA[:, 2, 0:n]), r=[('ps', 2)], w=['akT'])
                else:
                    j = bi % 2; p0 = s0 - 256
                    kb.dma('sp', rc[j][:, 0:n], ropeA[0, :, p0:p0 + n], w=[('rc', j)])
                    kb.dma('sp', rs[j][:, 0:n], ropeA[1, :, p0:p0 + n], w=[('rs', j)])
                    for g in range(4):
                        b0 = 2 * (g % 2)
                        proj_fm(wq, 'awq', g * 128, s0, n, b0); proj_fm(wq, 'awq', 512 + g * 128, s0, n, b0 + 1)
                        rope_evac(b0, b0 + 1, qT[:, g, s0:s0 + n], 'aqT', rc[j], rs[j], n, t1[j], t2[j], j)
                    proj_fm(wk, 'awk', 0, s0, n, 4); proj_fm(wk, 'awk', 128, s0, n, 5)
                    rope_evac(4, 5, kT[:, s0:s0 + n], 'akT', rc[j], rs[j], n, t1[j], t2[j], j)
                for t in range(s0 // 128, (s0 + n) // 128):
                    for k in range(8):
                        kb.op('pe', lambda e: e.matmul(PSA[:, 6, 0:128], lhsT=hT[:, k, t * 128:(t + 1) * 128], rhs=wv[:, k, :],
                                                       start=(k == 0), stop=(k == 7)), r=['hT', 'awv'], w=[('ps', 6)])
                    kb.op('act', lambda e: e.copy(out=Va[:, t, :, 0:64], in_=PSA[:, 6, 0:128].rearrange("p (h e) -> p h e", h=2)),
                          r=[('ps', 6)], w=['Va'])
            PT = [[kb.sb(f'PT{i}_{j}', [128, 4, 128], BF16) for j in range(5)] for i in range(2)]
            oat = [kb.sb(f'oat{i}', [128, 8, 64], BF16) for i in range(2)]
            den = [kb.sb(f'den{i}', [128, 4], F32) for i in range(2)]
            def keys_of(t):
                if t < 2:
                    return [(0, None), (1, None)]
                keys = []
                if t > 2: keys.append((t - 1, 0))
                keys.append((t, None))
                if t < NT - 1: keys.append((t + 1, 1))
                return keys + [(0, None), (1, None)]

            groups = [(t, h2) for t in (range(NT) if need_ctx else range(2, NT)) for h2 in range(2)]
            SB = [0, 1, 2, 5, 6]
            sbc = [0]

            def SE(n):
                t, h2 = groups[n]; i = n % 2
                ps_ = slice(h2 * 64, (h2 + 1) * 64)
                for ki, (kt, mk) in enumerate(keys_of(t)):
                    bank = SB[sbc[0] % 5]; sbc[0] += 1
                    kb.op('pe', lambda e: e.matmul(PSA[:, bank, :].rearrange("p (g q) -> p g q", g=4), lhsT=kT[ps_, kt * 128:(kt + 1) * 128],
                                                   rhs=qT[ps_, :, t * 128:(t + 1) * 128], start=True, stop=True), r=['akT', 'aqT'], w=[('ps', bank)])
                    kb.op('act', lambda e: e.activation(out=PT[i][ki][:], in_=PSA[:, bank, :].rearrange("p (g q) -> p g q", g=4), func=AF.Exp, scale=0.125),
                          r=[('ps', bank)], w=[('PT', i, ki)])
                    if mk is not None:
                        kb.op('pool', lambda e: e.tensor_tensor(out=PT[i][ki][:], in0=PT[i][ki][:], in1=bc(msk[:, mk * 128:(mk + 1) * 128], [[0, 4], [1, 128]]),
                                                                op=ALU.mult), r=[('PT', i, ki), 'msk'], w=[('PT', i, ki)])

            def PVN(n):
                t, h2 = groups[n]; i = n % 2; ti = t % 2
                keys = keys_of(t)
                ob = 3 + i
                for g in range(4):
                    for ki, (kt, mk) in enumerate(keys):
                        kb.op('pe', lambda e: e.matmul(PSA[:, ob, g * 66:g * 66 + 65], lhsT=PT[i][ki][:, g, :], rhs=Va[:, kt, h2, 0:65],
                                                       start=(ki == 0), stop=(ki == len(keys) - 1)), r=[('PT', i, ki), 'Va'], w=[('ps', ob)])
                ov = PSA[:, ob, 0:264].rearrange("p (g e) -> p g e", g=4)
                kb.op('dve', lambda e: e.tensor_tensor(out=den[i][:], in0=ov[:, :, 64], in1=snk[:, h2 * 4:(h2 + 1) * 4], op=ALU.add),
                      r=[('ps', ob), 'snk'], w=[('den', i)])
                kb.op('dve', lambda e: e.reciprocal(out=den[i][:], in_=den[i][:]), r=[('den', i)], w=[('den', i)])
                kb.op('dve', lambda e: e.tensor_tensor(out=oat[ti][:, h2 * 4:(h2 + 1) * 4, :], in0=ov[:, :, 0:64], in1=bc(den[i][:, 0:4], [[1, 4], [0, 64]]),
                                                       op=ALU.mult), r=[('ps', ob), ('den', i)], w=[('oat', ti)])
                if h2 == 1:
                    for k in range(4):
                        kb.op('pe', lambda e: e.transpose(out=PST[:, k, :], in_=oat[ti][:, 2 * k:2 * k + 2, :].rearrange("p h e -> p (h e)"), identity=ident[:]),
                              r=[('oat', ti), 'ident'], w=['pst'])
                    kb.op('act', lambda e: e.copy(out=oaT[:, :, t * 128:(t + 1) * 128], in_=PST[:, 0:4, :]), r=['pst'], w=['oaT'])

            SE(0)
            for n in range(len(groups)):
                if n + 1 < len(groups):
                    SE(n + 1)
                PVN(n)
            kb.release(m)

        def phase_four(l, ofT):
            need_ctx = (l == 0)
            m = kb.mark()
            wfu = kb.sb('wfu', [128, 8, 512], BF16)
            load_w(wfu, 'wfu', w_ext[l][:, FU:FU + 512], 8, 512)
            dC = kb.sb('dC', [128, 256], BF16)
            kb.dma('sp', dC[:], dftC[:, :], w=['dC'])
            W = kb.sb('W', [128, NT, 4, 256], BF16)
            uT = [kb.sb(f'uT{i}', [128, T], BF16) for i in range(2)]
            for g in range(4):
                i = g % 2
                for bi, (s0, n) in enumerate(TB):
                    proj_fm(wfu, 'wfu', g * 128, s0, n, bi % 2)
                    kb.op('act', lambda e: e.copy(out=uT[i][:, s0:s0 + n], in_=PSA[:, bi % 2, 0:n]), r=[('ps', bi % 2)], w=[('uT', i)])
                for t in range(NT):
                    bank = 2 + t % 2
                    kb.op('pe', lambda e: e.matmul(PSA[:, bank, 0:256], lhsT=uT[i][:, t * 128:(t + 1) * 128], rhs=dC[:], start=True, stop=True),
                          r=[('uT', i), 'dC'], w=[('ps', bank)])
                    kb.op('dve', lambda e: e.tensor_copy(out=W[:, t, g, :], in_=PSA[:, bank, 0:256]), r=[('ps', bank)], w=['W'])
            Cb = [kb.sb(f'Cb{i}', [128, 16, 256], BF16) for i in range(2)]
            Nb = [kb.sb(f'Nb{i}', [128, 16, 256], BF16) for i in range(2)]
            it = 0
            for nb in range(8):
                i = nb % 2
                kb.dma('sp', Cb[i][:], dftN[0, :, nb * 256:(nb + 1) * 256].rearrange("(t p) c -> p t c", p=128), w=[('Cb', i)])
                kb.dma('sp', Nb[i][:], dftN[1, :, nb * 256:(nb + 1) * 256].rearrange("(t p) c -> p t c", p=128), w=[('Nb', i)])
                for g in range(4):
                    bank = 4 + it % 2; it += 1
                    for t in range(16):
                        kb.op('pe', lambda e: e.matmul(PSA[:, bank, 0:256], lhsT=W[:, 2 + t, g, 0:128], rhs=Cb[i][:, t, :], start=(t == 0), stop=False),
                              r=['W', ('Cb', i)], w=[('ps', bank)])
                        kb.op('pe', lambda e: e.matmul(PSA[:, bank, 0:256], lhsT=W[:, 2 + t, g, 128:256], rhs=Nb[i][:, t, :], start=False, stop=(t == 15)),
                              r=['W', ('Nb', i)], w=[('ps', bank)])
                    kb.op('act', lambda e: e.copy(out=ofT[:, g, 256 + nb * 256:256 + (nb + 1) * 256], in_=PSA[:, bank, 0:256]), r=[('ps', bank)], w=['ofT'])
            if need_ctx:
                kb.dma('sp', Cb[0][:, 0:2, :], dft256[0].rearrange("(t p) c -> p t c", p=128), w=[('Cb', 0)])
                kb.dma('sp', Nb[0][:, 0:2, :], dft256[1].rearrange("(t p) c -> p t c", p=128), w=[('Nb', 0)])
                for g in range(4):
                    bank = 4 + g % 2
                    for t in range(2):
                        kb.op('pe', lambda e: e.matmul(PSA[:, bank, 0:256], lhsT=W[:, t, g, 0:128], rhs=Cb[0][:, t, :], start=(t == 0), stop=False),
                              r=['W', ('Cb', 0)], w=[('ps', bank)])
                        kb.op('pe', lambda e: e.matmul(PSA[:, bank, 0:256], lhsT=W[:, t, g, 128:256], rhs=Nb[0][:, t, :], start=False, stop=(t == 1)),
                              r=['W', ('Nb', 0)], w=[('ps', bank)])
                    kb.op('act', lambda e: e.copy(out=ofT[:, g, 0:256], in_=PSA[:, bank, 0:256]), r=[('ps', bank)], w=['ofT'])
            kb.release(m)

        def resid_update(l, gi, tiles, src_fn, src_keys):
            pass

        def phase_out(l, mT):
            m = kb.mark()
            wo = kb.sb('wo', [128, 8, D], BF16)
            load_w(wo, 'wo', w_out[l][:, :], 8, D)
            g1 = []
            for row in (0, 1):
                g_ = kb.sb(f'g1_{row}', [128, D], F32)
                kb.dma('sp', g_[:], modbuf[l, row, 2 * D:3 * D].partition_broadcast(128), w=[f'g1_{row}'])
                g1.append(g_)
            xt = [kb.sb(f'oxt{i}', [128, D], F32) for i in range(2)]
            yt = [kb.sb(f'oyt{i}', [128, D], F32) for i in range(2)]
            for t in (range(NT) if l == 0 else range(2, NT)):
                i = t % 2; row = 0 if t < 2 else 1
                kb.dma('sp', xt[i][:], xbuf[t * 128:(t + 1) * 128, :], r=[('x', t)], w=[('oxt', i)])
                for hf in range(2):
                    bank = 2 * i + hf
                    for fc in range(8):
                        kb.op('pe', lambda e: e.matmul(PSA[:, bank, :], lhsT=mT[:, fc, t * 128:(t + 1) * 128], rhs=wo[:, fc, hf * 512:(hf + 1) * 512],
                                                       start=(fc == 0), stop=(fc == 7)), r=['mT', 'wo'], w=[('ps', bank)])
                    kb.op('dve', lambda e: e.tensor_tensor(out=yt[i][:, hf * 512:(hf + 1) * 512], in0=PSA[:, bank, :], in1=g1[row][:, hf * 512:(hf + 1) * 512],
                                                           op=ALU.mult), r=[('ps', bank), f'g1_{row}'], w=[('oyt', i)])
                kb.op('pool', lambda e: e.tensor_tensor(out=yt[i][:], in0=yt[i][:], in1=xt[i][:], op=ALU.add), r=[('oyt', i), ('oxt', i)], w=[('oyt', i)])
                kb.dma('sp', xbuf[t * 128:(t + 1) * 128, :], yt[i][:], r=[('oyt', i)], w=[('x', t)])
            kb.release(m)

        def phase_moe(l):
            m = kb.mark()
            tiles = list(range(NT) if l == 0 else range(2, NT))
            blocks = TB if l == 0 else TB[1:]
            logits = kb.sb('logits', [128, NT, 36], F32)
            Wt = kb.sb('Wt', [128, NT, 32], F32)
            kb.op('pool', lambda e: e.memset(logits[:], 0.0), w=['logits'])
            phase_norm(l, 2, logits)
            if os.environ.get('MOE_CUT') == '1':
                kb.release(m); return
            m2 = kb.mark()
            lgG = logits[:, :, 0:4]; lgE = logits[:, :, 4:36]
            gmax = kb.sb('gmax', [128, NT], F32); ohg = kb.sb('ohg', [128, NT, 4], F32); eg = kb.sb('eg', [128, NT, 4], F32)
            pg = kb.sb('pg', [128, NT], F32); me = kb.sb('me', [128, NT, 32], F32); oh1 = kb.sb('oh1', [128, NT, 32], F32)
            oh2 = kb.sb('oh2', [128, NT, 32], F32); m1 = kb.sb('m1', [128, NT], F32); m2_ = kb.sb('m2', [128, NT], F32)
            w1 = kb.sb('w1', [128, NT], F32); w2 = kb.sb('w2', [128, NT], F32)
            b1 = lambda a, n_: bc(a, [[1, NT], [0, n_]])
            kb.op('dve', lambda e: e.reduce_max(out=gmax[:], in_=lgG, axis=AX.X), r=['logits'], w=['gmax'])
            kb.op('dve', lambda e: e.tensor_tensor(out=ohg[:], in0=lgG, in1=b1(gmax[:, 0:NT], 4), op=ALU.is_equal), r=['logits', 'gmax'], w=['ohg'])
            kb.op('dve', lambda e: e.tensor_tensor(out=eg[:], in0=lgG, in1=b1(gmax[:, 0:NT], 4), op=ALU.subtract), r=['logits', 'gmax'], w=['eg'])
            kb.op('act', lambda e: e.activation(out=eg[:], in_=eg[:], func=AF.Exp), r=['eg'], w=['eg'])
            kb.op('dve', lambda e: e.reduce_sum(out=pg[:], in_=eg[:], axis=AX.X), r=['eg'], w=['pg'])
            kb.op('dve', lambda e: e.reciprocal(out=pg[:], in_=pg[:]), r=['pg'], w=['pg'])
            kb.op('dve', lambda e: e.tensor_scalar(out=ohg[:], in0=ohg[:], scalar1=-1.0, scalar2=1e30, op0=ALU.add, op1=ALU.mult), r=['ohg'], w=['ohg'])
            kb.op('dve', lambda e: e.tensor_tensor(out=me[:].rearrange("p t (g x) -> p t g x", g=4), in0=lgE.rearrange("p t (g x) -> p t g x", g=4),
                                                   in1=bc(ohg[:, 0:NT, :], [[4, NT], [1, 4], [0, 8]]), op=ALU.add), r=['logits', 'ohg'], w=['me'])
            kb.op('dve', lambda e: e.reduce_max(out=m1[:], in_=me[:], axis=AX.X), r=['me'], w=['m1'])
            kb.op('dve', lambda e: e.tensor_tensor(out=oh1[:], in0=me[:], in1=b1(m1[:, 0:NT], 32), op=ALU.is_equal), r=['me', 'm1'], w=['oh1'])
            kb.op('dve', lambda e: e.scalar_tensor_tensor(out=me[:], in0=oh1[:], scalar=-1e30, in1=me[:], op0=ALU.mult, op1=ALU.add), r=['oh1', 'me'], w=['me'])
            kb.op('dve', lambda e: e.reduce_max(out=m2_[:], in_=me[:], axis=AX.X), r=['me'], w=['m2'])
            kb.op('dve', lambda e: e.tensor_tensor(out=oh2[:], in0=me[:], in1=b1(m2_[:, 0:NT], 32), op=ALU.is_equal), r=['me', 'm2'], w=['oh2'])
            kb.op('dve', lambda e: e.tensor_tensor(out=w1[:], in0=m1[:], in1=m2_[:], op=ALU.subtract), r=['m1', 'm2'], w=['w1'])
            kb.op('act', lambda e: e.activation(out=w2[:], in_=w1[:], func=AF.Sigmoid, scale=-1.0), r=['w1'], w=['w2'])
            kb.op('act', lambda e: e.activation(out=w1[:], in_=w1[:], func=AF.Sigmoid), r=['w1'], w=['w1'])
            kb.op('dve', lambda e: e.tensor_tensor(out=w1[:], in0=w1[:], in1=pg[:], op=ALU.mult), r=['w1', 'pg'], w=['w1'])
            kb.op('dve', lambda e: e.tensor_tensor(out=w2[:], in0=w2[:], in1=pg[:], op=ALU.mult), r=['w2', 'pg'], w=['w2'])
            kb.op('dve', lambda e: e.tensor_tensor(out=oh1[:], in0=oh1[:], in1=b1(w1[:, 0:NT], 32), op=ALU.mult), r=['oh1', 'w1'], w=['oh1'])
            kb.op('dve', lambda e: e.tensor_tensor(out=oh2[:], in0=oh2[:], in1=b1(w2[:, 0:NT], 32), op=ALU.mult), r=['oh2', 'w2'], w=['oh2'])
            kb.op('dve', lambda e: e.tensor_tensor(out=Wt[:], in0=oh1[:], in1=oh2[:], op=ALU.add), r=['oh1', 'oh2'], w=['Wt'])
            kb.release(m2)
            if os.environ.get('MOE_CUT') == '2':
                kb.release(m); return
            acc = kb.sb('acc', [128, NT, D], F32)
            m3 = kb.mark()
            wg = [kb.sb(f'ewg{i}', [128, 8, 512], BF16) for i in range(2)]
            wu = [kb.sb(f'ewu{i}', [128, 8, 512], BF16) for i in range(2)]
            wd = [kb.sb(f'ewd{i}', [128, 4, D], BF16) for i in range(2)]
            aT = [kb.sb(f'aT{i}', [128, 4, 512], BF16) for i in range(2)]
            sgt = [kb.sb(f'sgt{i}', [128, 512], F32) for i in range(2)]
            it = 0; ih = 0
            for ex in range(int(os.environ.get('MOE_NEXP', '32'))):
                j = ex % 2
                load_w(wg[j], ('ewg', j), w_eg[l, ex], 8, 512, engs=('pool', 'act'))
                load_w(wu[j], ('ewu', j), w_eu[l, ex], 8, 512, engs=('pool', 'act'))
                load_w(wd[j], ('ewd', j), w_ed[l, ex], 4, D, engs=('pool', 'act'))
                for (s0, n) in blocks:
                    i = it % 2; it += 1
                    for hc in range(4):
                        ii = ih % 2; ih += 1
                        for k in range(8):
                            kb.op('pe', lambda e: e.matmul(PSA[:, ii, 0:n], lhsT=wg[j][:, k, hc * 128:(hc + 1) * 128], rhs=hT[:, k, s0:s0 + n],
                                                           start=(k == 0), stop=(k == 7)), r=[('ewg', j), 'hT'], w=[('ps', ii)])
                        for k in range(8):
                            kb.op('pe', lambda e: e.matmul(PSA[:, 2 + ii, 0:n], lhsT=wu[j][:, k, hc * 128:(hc + 1) * 128], rhs=hT[:, k, s0:s0 + n],
                                                           start=(k == 0), stop=(k == 7)), r=[('ewu', j), 'hT'], w=[('ps', 2 + ii)])
                        kb.op('act', lambda e: e.activation(out=sgt[ii][:, 0:n], in_=PSA[:, ii, 0:n], func=AF.Silu), r=[('ps', ii)], w=[('sgt', ii)])
                        kb.op('dve', lambda e: e.tensor_tensor(out=aT[i][:, hc, 0:n], in0=sgt[ii][:, 0:n], in1=PSA[:, 2 + ii, 0:n], op=ALU.mult),
                              r=[('sgt', ii), ('ps', 2 + ii)], w=[('aT', i)])
                    for t in range(s0 // 128, (s0 + n) // 128):
                        tl = t * 128 - s0
                        for hf in range(2):
                            bank = 4 + (2 * t + hf) % 3
                            for hc in range(4):
                                kb.op('pe', lambda e: e.matmul(PSA[:, bank, :], lhsT=aT[i][:, hc, tl:tl + 128], rhs=wd[j][:, hc, hf * 512:(hf + 1) * 512],
                                                               start=(hc == 0), stop=(hc == 3)), r=[('aT', i), ('ewd', j)], w=[('ps', bank)])
                            a_ = acc[:, t, hf * 512:(hf + 1) * 512]
                            if ex == 0:
                                kb.op('dve', lambda e: e.tensor_scalar(out=a_, in0=PSA[:, bank, :], scalar1=Wt[:, t, ex:ex + 1], scalar2=None, op0=ALU.mult),
                                      r=[('ps', bank), 'Wt'], w=[('acc', t)])
                            else:
                                kb.op('dve', lambda e: e.scalar_tensor_tensor(out=a_, in0=PSA[:, bank, :], scalar=Wt[:, t, ex:ex + 1], in1=a_,
                                                                              op0=ALU.mult, op1=ALU.add), r=[('ps', bank), 'Wt', ('acc', t)], w=[('acc', t)])
            kb.release(m3)
            g2 = []
            for row in (0, 1):
                g_ = kb.sb(f'g2_{row}', [128, D], F32)
                kb.dma('sp', g_[:], modbuf[l, row, 5 * D:6 * D].partition_broadcast(128), w=[f'g2_{row}'])
                g2.append(g_)
            xt = [kb.sb(f'mxt{i}', [128, D], F32) for i in range(2)]
            for t in tiles:
                i = t % 2; row = 0 if t < 2 else 1
                kb.dma('sp', xt[i][:], xbuf[t * 128:(t + 1) * 128, :], r=[('x', t)], w=[('mxt', i)])
                kb.op('dve', lambda e: e.tensor_tensor(out=acc[:, t, :], in0=acc[:, t, :], in1=g2[row][:], op=ALU.mult), r=[('acc', t), f'g2_{row}'], w=[('acc', t)])
                kb.op('pool', lambda e: e.tensor_tensor(out=xt[i][:], in0=xt[i][:], in1=acc[:, t, :], op=ALU.add), r=[('acc', t), ('mxt', i)], w=[('mxt', i)])
                kb.dma('sp', xbuf[t * 128:(t + 1) * 128, :], xt[i][:], r=[('mxt', i)], w=[('x', t)])
            kb.release(m)

        moe_state = {}

        def phase_moe_sparse(l):
            IOA = bass.IndirectOffsetOnAxis
            ABv = AB.rearrange("r (h c) -> (r h) c", h=2)
            if 'bregs' not in moe_state:
                regs = {}
                for nm_, v_ in (('rec', 32 * CAP - 1), ('tok', T - 1), ('ab', 2 * T - 1)):
                    rg = nc.gpsimd.alloc_register('bnd_' + nm_)
                    nc.gpsimd.reg_mov(rg, v_)
                    regs[nm_] = rg
                moe_state['bregs'] = regs
            BR = moe_state['bregs']
            m = kb.mark()
            t0 = 0 if l == 0 else 2
            tiles = list(range(t0, NT))
            logits = kb.sb('logits', [128, NT, 36], F32)
            kb.op('pool', lambda e: e.memset(logits[:], 0.0), w=['logits'])
            phase_norm(l, 2, logits, sparse=True)
            MC = kb.sb('MC', [128, 32 + NT], F32)
            kb.dma('sp', MC[:], mconst[:, :], w=['MC'])
            eC = MC[:, 0:32]; tokid = MC[:, 32:32 + NT]
            zt = kb.sb('zt', [128, D], F32)
            kb.op('pool', lambda e: e.memset(zt[:], 0.0), w=['zt'])
            for q in range(2 * NT):
                kb.dma('sp', AB[q * 128:(q + 1) * 128, :], zt[:], r=['zt'], w=[('ABz', q)])
            ri_ = kb.sb('recinit', [128, (32 * CAP) // 128, 4], F32)
            kb.op('pool', lambda e: e.memset(ri_[:], 1.0e6), w=['recinit'])
            kb.op('pool', lambda e: e.memset(ri_[:, :, 2:3], 0.0), r=['recinit'], w=['recinit'])
            kb.dma('sp', rec_d.rearrange("(p s) c -> p s c", p=128), ri_[:], r=['recinit'], w=['rec_d'])
            lgG = logits[:, :, 0:4]; lgE = logits[:, :, 4:36]
            gmax = kb.sb('gmax', [128, NT], F32); ohg = kb.sb('ohg', [128, NT, 4], F32); eg = kb.sb('eg', [128, NT, 4], F32)
            pg = kb.sb('pg', [128, NT], F32); me = kb.sb('me', [128, NT, 32], F32); oh1 = kb.sb('oh1', [128, NT, 32], F32)
            oh2 = kb.sb('oh2', [128, NT, 32], F32); m1 = kb.sb('m1', [128, NT], F32); m2_ = kb.sb('m2', [128, NT], F32)
            w1 = kb.sb('w1', [128, NT], F32); w2 = kb.sb('w2', [128, NT], F32)
            b1 = lambda a, n_: bc(a, [[1, NT], [0, n_]])
            kb.op('dve', lambda e: e.reduce_max(out=gmax[:], in_=lgG, axis=AX.X), r=['logits'], w=['gmax'])
            kb.op('dve', lambda e: e.tensor_tensor(out=ohg[:], in0=lgG, in1=b1(gmax[:, 0:NT], 4), op=ALU.is_equal), r=['logits', 'gmax'], w=['ohg'])
            kb.op('dve', lambda e: e.tensor_tensor(out=eg[:], in0=lgG, in1=b1(gmax[:, 0:NT], 4), op=ALU.subtract), r=['logits', 'gmax'], w=['eg'])
            kb.op('act', lambda e: e.activation(out=eg[:], in_=eg[:], func=AF.Exp), r=['eg'], w=['eg'])
            kb.op('dve', lambda e: e.reduce_sum(out=pg[:], in_=eg[:], axis=AX.X), r=['eg'], w=['pg'])
            kb.op('dve', lambda e: e.reciprocal(out=pg[:], in_=pg[:]), r=['pg'], w=['pg'])
            kb.op('dve', lambda e: e.tensor_scalar(out=ohg[:], in0=ohg[:], scalar1=-1.0, scalar2=1e30, op0=ALU.add, op1=ALU.mult), r=['ohg'], w=['ohg'])
            kb.op('dve', lambda e: e.tensor_tensor(out=me[:].rearrange("p t (g x) -> p t g x", g=4), in0=lgE.rearrange("p t (g x) -> p t g x", g=4),
                                                   in1=bc(ohg[:, 0:NT, :], [[4, NT], [1, 4], [0, 8]]), op=ALU.add), r=['logits', 'ohg'], w=['me'])
            kb.op('dve', lambda e: e.reduce_max(out=m1[:], in_=me[:], axis=AX.X), r=['me'], w=['m1'])
            kb.op('dve', lambda e: e.tensor_tensor(out=oh1[:], in0=me[:], in1=b1(m1[:, 0:NT], 32), op=ALU.is_equal), r=['me', 'm1'], w=['oh1'])
            kb.op('dve', lambda e: e.scalar_tensor_tensor(out=me[:], in0=oh1[:], scalar=-1e30, in1=me[:], op0=ALU.mult, op1=ALU.add), r=['oh1', 'me'], w=['me'])
            kb.op('dve', lambda e: e.reduce_max(out=m2_[:], in_=me[:], axis=AX.X), r=['me'], w=['m2'])
            kb.op('dve', lambda e: e.tensor_tensor(out=oh2[:], in0=me[:], in1=b1(m2_[:, 0:NT], 32), op=ALU.is_equal), r=['me', 'm2'], w=['oh2'])
            kb.op('dve', lambda e: e.tensor_tensor(out=w1[:], in0=m1[:], in1=m2_[:], op=ALU.subtract), r=['m1', 'm2'], w=['w1'])
            kb.op('act', lambda e: e.activation(out=w2[:], in_=w1[:], func=AF.Sigmoid, scale=-1.0), r=['w1'], w=['w2'])
            kb.op('act', lambda e: e.activation(out=w1[:], in_=w1[:], func=AF.Sigmoid), r=['w1'], w=['w1'])
            kb.op('dve', lambda e: e.tensor_tensor(out=w1[:], in0=w1[:], in1=pg[:], op=ALU.mult), r=['w1', 'pg'], w=['w1'])
            kb.op('dve', lambda e: e.tensor_tensor(out=w2[:], in0=w2[:], in1=pg[:], op=ALU.mult), r=['w2', 'pg'], w=['w2'])
            if t0 > 0:
                kb.op('pool', lambda e: e.memset(oh1[:, 0:t0, :], 0.0), r=['oh1'], w=['oh1'])
                kb.op('pool', lambda e: e.memset(oh2[:, 0:t0, :], 0.0), r=['oh2'], w=['oh2'])
            selb = kb.sb('selb', [128, NT * 32], BF16)
            kb.op('pool', lambda e: e.tensor_tensor(out=selb[:], in0=oh1[:].rearrange("p t e -> p (t e)"), in1=oh2[:].rearrange("p t e -> p (t e)"), op=ALU.add),
                  r=['oh1', 'oh2'], w=['selb'])
            LT = kb.sb('LT', [128, 128], BF16); ones = kb.sb('ones', [128, 128], BF16)
            kb.op('pool', lambda e: e.memset(ones[:], 1.0), w=['ones'])
            kb.op('pool', lambda e: e.memset(LT[:], 1.0), w=['LT'])
            kb.op('pool', lambda e: e.affine_select(out=LT[:], in_=LT[:], pattern=[[1, 128]], compare_op=ALU.is_gt, fill=0.0, base=0,
                                                    channel_multiplier=-1), r=['LT'], w=['LT'])
            slot = kb.sb('slot', [128, NT, 32], F32); tot = kb.sb('tot', [128, NT, 32], F32); cum = kb.sb('cum', [128, NT, 32], F32)
            sl2 = slot[:].rearrange("p t e -> p (t e)"); to2 = tot[:].rearrange("p t e -> p (t e)")
            for (c0, c1, bank) in ((0, 512, 0), (512, NT * 32, 1)):
                kb.op('pe', lambda e: e.matmul(PSA[:, bank, 0:c1 - c0], lhsT=LT[:], rhs=selb[:, c0:c1], start=True, stop=True), r=['LT', 'selb'], w=[('ps', bank)])
                kb.op('dve', lambda e: e.tensor_copy(out=sl2[:, c0:c1], in_=PSA[:, bank, 0:c1 - c0]), r=[('ps', bank)], w=['slot'])
                kb.op('pe', lambda e: e.matmul(PSA[:, 2 + bank, 0:c1 - c0], lhsT=ones[:], rhs=selb[:, c0:c1], start=True, stop=True), r=['ones', 'selb'], w=[('ps', 2 + bank)])
                kb.op('dve', lambda e: e.tensor_copy(out=to2[:, c0:c1], in_=PSA[:, 2 + bank, 0:c1 - c0]), r=[('ps', 2 + bank)], w=['tot'])
            kb.op('pool', lambda e: e.memset(cum[:, 0, :], 0.0), w=['cum'])
            for t in range(1, NT):
                kb.op('dve', lambda e: e.tensor_tensor(out=cum[:, t, :], in0=cum[:, t - 1, :], in1=tot[:, t - 1, :], op=ALU.add), r=['cum', 'tot'], w=['cum'])
            kb.op('dve', lambda e: e.tensor_tensor(out=slot[:], in0=slot[:], in1=cum[:], op=ALU.add), r=['slot', 'cum'], w=['slot'])
            rec = kb.sb('rec', [128, NT, 2, 4], F32)
            rr = kb.sb('rr', [128, NT, 2], F32); rri = kb.sb('rri', [128, NT, 2], I32)
            s_ = kb.sb('s_', [128, NT], F32); e_ = kb.sb('e_', [128, NT], F32)
            kb.op('pool', lambda e: e.memset(rec[:], 0.0), w=['rec'])
            for k, (oh, wk) in enumerate(((oh1, w1), (oh2, w2))):
                kb.op('dve', lambda e: e.tensor_tensor(out=me[:], in0=oh[:], in1=slot[:], op=ALU.mult), r=['oh1', 'oh2', 'slot', 'me'], w=['me'])
                kb.op('dve', lambda e: e.reduce_sum(out=s_[:], in_=me[:], axis=AX.X), r=['me'], w=['s_'])
                kb.op('dve', lambda e: e.tensor_tensor(out=me[:], in0=oh[:], in1=bc(eC, [[0, NT], [1, 32]]), op=ALU.mult), r=['oh1', 'oh2', 'MC', 'me'], w=['me'])
                kb.op('dve', lambda e: e.reduce_sum(out=e_[:], in_=me[:], axis=AX.X), r=['me'], w=['e_'])
                kb.op('dve', lambda e: e.tensor_tensor(out=e_[:], in0=e_[:], in1=s_[:], op=ALU.add), r=['e_', 's_'], w=['e_'])
                kb.op('dve', lambda e: e.tensor_scalar(out=s_[:], in0=s_[:], scalar1=float(CAP), scalar2=1.0e6, op0=ALU.is_ge, op1=ALU.mult), r=['s_'], w=['s_'])
                kb.op('dve', lambda e: e.tensor_tensor(out=rr[:, :, k], in0=e_[:], in1=s_[:], op=ALU.add), r=['e_', 's_'], w=['rr'])
                kb.op('pool', lambda e: e.tensor_copy(out=rec[:, :, k, 0], in_=tokid), r=['MC', 'rec'], w=['rec'])
                kb.op('pool', lambda e: e.tensor_scalar(out=rec[:, :, k, 1], in0=tokid, scalar1=float(k * T), scalar2=None, op0=ALU.add), r=['MC', 'rec'], w=['rec'])
                kb.op('pool', lambda e: e.tensor_scalar(out=rec[:, :, k, 3], in0=tokid, scalar1=2.0, scalar2=float(2 * k * T + 1), op0=ALU.mult, op1=ALU.add), r=['MC', 'rec'], w=['rec'])
                kb.op('pool', lambda e: e.tensor_copy(out=rec[:, :, k, 2], in_=wk[:]), r=['w1', 'w2', 'rec'], w=['rec'])
            kb.op('dve', lambda e: e.tensor_copy(out=rri[:], in_=rr[:]), r=['rr'], w=['rri'])
            if stop == f'moe{l}' and os.environ.get('XDUMP'):
                xdump('rr', rr, [128, NT, 2], F32); xdump('rri', rri, [128, NT, 2], I32); xdump('rec', rec, [128, NT, 2, 4], F32)
                xdump('slot', slot, [128, NT, 32], F32)
            kb.barrier()
            for t in tiles:
                for k in range(2):
                    kb.dma('pool', None, None, r=['rec', 'rri'], w=[('recs', t, k)],
                           fn=lambda g: g.indirect_dma_start(out=rec_d[:, :], out_offset=IOA(ap=rri[:, t, k:k + 1], axis=0), in_=rec[:, t, k, :],
                                                             in_offset=None, bounds_check=BR['rec'], oob_is_err=False))
            kb.barrier()
            if os.environ.get('MOE_CUT') == '3':
                kb.release(m); return
            m3 = kb.mark()
            stg_all[0] = list(stg) + [kb.alias(f'stgx{i}', [128, 2048], F32, kb.off['hT'] + i * 8192) for i in range(4)]
            wg = [kb.sb(f'ewg{i}', [128, 8, 512], BF16) for i in range(2)]
            wu = [kb.sb(f'ewu{i}', [128, 8, 512], BF16) for i in range(2)]
            wd = [kb.sb(f'ewd{i}', [128, 4, D], BF16) for i in range(2)]
            XT = [kb.sb(f'XT{i}', [128, 8, CAP], BF16) for i in range(2)]
            aT = kb.sb('aT', [128, 4, CAP], BF16)
            sgt = [kb.sb(f'sgt{i}', [128, 512], F32) for i in range(2)]
            rsb = [kb.sb(f'rsb{i}', [128, NS, 4], F32) for i in range(2)]
            gi = [kb.sb(f'gi{i}', [128, NS, 4], I32) for i in range(2)]
            xg = [kb.sb(f'xg{i}', [128, D], BF16) for i in range(NS)]
            yw = [kb.sb(f'yw{i}', [128, D], F32) for i in range(3)]
            for i in range(NS):
                kb.op('pool', lambda e: e.memset(xg[i][:], 0.0), w=[('xg', i)])
            PST2 = PSA[:, 6, :].bitcast(BF16).rearrange("p (k c) -> p k c", c=128)
            stgs = stg_all[0]
            nexp = int(os.environ.get('MOE_NEXP', '32'))
            cnt = {'iy': 0, 'ih': 0}

            def w_issue(ex):
                j = ex % 2; pieces = []
                for (dst, dkey, src, K, W) in ((wg[j], ('ewg', j), w_eg[l, ex], 8, 512), (wu[j], ('ewu', j), w_eu[l, ex], 8, 512), (wd[j], ('ewd', j), w_ed[l, ex], 4, D)):
                    kk = 2048 // W
                    for k0 in range(0, K, kk):
                        i = len(pieces)
                        sv = stgs[i][:, 0:kk * W].rearrange("p (k c) -> p k c", c=W)
                        kb.dma('sp', sv, src[k0 * 128:(k0 + kk) * 128, :].rearrange("(k p) c -> p k c", p=128), w=[('stg', i)])
                        pieces.append((dst, dkey, k0, k0 + kk, sv, i))
                return pieces

            def w_cast(pieces):
                for n_, (dst, dkey, k0, k1, sv, i) in enumerate(pieces):
                    if n_ % 2 == 0:
                        kb.op('act', lambda e: e.copy(out=dst[:, k0:k1, :], in_=sv), r=[('stg', i)], w=[dkey])
                    else:
                        kb.op('dve', lambda e: e.tensor_copy(out=dst[:, k0:k1, :], in_=sv), r=[('stg', i)], w=[dkey])

            def G(ex):
                j = ex % 2
                kb.dma('sp', rsb[j][:], rec_d[ex * CAP:(ex + 1) * CAP, :].rearrange("(s p) c -> p s c", p=128), w=[('rsb', j)])
                kb.op('dve', lambda e: e.tensor_copy(out=gi[j][:], in_=rsb[j][:]), r=[('rsb', j)], w=[('gi', j)])
                for s_i in range(NS):
                    kb.dma('pool', None, None, r=[('gi', j)], w=[('xg', s_i)],
                           fn=lambda g: g.indirect_dma_start(out=xg[s_i][:], out_offset=None, in_=h2d[:, :],
                                                             in_offset=IOA(ap=gi[j][:, s_i, 0:1], axis=0), bounds_check=BR['tok'], oob_is_err=False))

            def TR(ex):
                j = ex % 2
                for s_i in range(NS):
                    pt_, pk_ = (PST, 'pst') if s_i % 2 == 0 else (PST2, ('ps', 6))
                    for k in range(8):
                        kb.op('pe', lambda e: e.transpose(out=pt_[:, k, :], in_=xg[s_i][:, k * 128:(k + 1) * 128], identity=ident[:]),
                              r=[('xg', s_i), 'ident'], w=[pk_])
                    kb.op('act', lambda e: e.copy(out=XT[j][:, :, s_i * 128:(s_i + 1) * 128], in_=pt_[:]), r=[pk_], w=[('XT', j)])

            def F1(ex):
                j = ex % 2
                for c0 in range(0, CAP, 512):
                    n = min(512, CAP - c0)
                    for hc in range(4):
                        ii = cnt['ih'] % 2; cnt['ih'] += 1
                        for k in range(8):
                            kb.op('pe', lambda e: e.matmul(PSA[:, ii, 0:n], lhsT=wg[j][:, k, hc * 128:(hc + 1) * 128], rhs=XT[j][:, k, c0:c0 + n],
                                                           start=(k == 0), stop=(k == 7)), r=[('ewg', j), ('XT', j)], w=[('ps', ii)])
                        for k in range(8):
                            kb.op('pe', lambda e: e.matmul(PSA[:, 2 + ii, 0:n], lhsT=wu[j][:, k, hc * 128:(hc + 1) * 128], rhs=XT[j][:, k, c0:c0 + n],
                                                           start=(k == 0), stop=(k == 7)), r=[('ewu', j), ('XT', j)], w=[('ps', 2 + ii)])
                        kb.op('act', lambda e: e.activation(out=sgt[ii][:, 0:n], in_=PSA[:, ii, 0:n], func=AF.Silu), r=[('ps', ii)], w=[('sgt', ii)])
                        kb.op('dve', lambda e: e.tensor_tensor(out=aT[:, hc, c0:c0 + n], in0=sgt[ii][:, 0:n], in1=PSA[:, 2 + ii, 0:n], op=ALU.mult),
                              r=[('sgt', ii), ('ps', 2 + ii)], w=['aT'])

            def F2(ex):
                j = ex % 2
                for s_i in range(NS):
                    q = cnt['iy'] % 3; cnt['iy'] += 1
                    for hf in range(2):
                        bank = 4 + hf
                        for hc in range(4):
                            kb.op('pe', lambda e: e.matmul(PSA[:, bank, :], lhsT=aT[:, hc, s_i * 128:(s_i + 1) * 128], rhs=wd[j][:, hc, hf * 512:(hf + 1) * 512],
                                                           start=(hc == 0), stop=(hc == 3)), r=['aT', ('ewd', j)], w=[('ps', bank)])
                        kb.op('dve', lambda e: e.tensor_scalar(out=yw[q][:, hf * 512:(hf + 1) * 512], in0=PSA[:, bank, :], scalar1=rsb[j][:, s_i, 2:3], scalar2=None,
                                                               op0=ALU.mult), r=[('ps', bank), ('rsb', j)], w=[('yw', q)])
                    kb.dma('pool', None, None, r=[('yw', q), ('gi', j)], w=[('ABs', ex, s_i)],
                           fn=lambda g: g.indirect_dma_start(out=AB[:, :], out_offset=IOA(ap=gi[j][:, s_i, 1:2], axis=0),
                                                             in_=yw[q][:], in_offset=None, bounds_check=BR['ab'], oob_is_err=False))

            w_cast(w_issue(0)); G(0); TR(0)
            for ex in range(nexp):
                nxt = ex + 1 < nexp
                if nxt:
                    pcs = w_issue(ex + 1)
                    G(ex + 1)
                F1(ex)
                if nxt:
                    w_cast(pcs)
                    TR(ex + 1)
                F2(ex)
            stg_all[0] = list(stg)
            stg_i[0] = 0
            kb.release(m3)
            g2 = []
            for row in (0, 1):
                g_ = kb.sb(f'g2_{row}', [128, D], F32)
                kb.dma('sp', g_[:], modbuf[l, row, 5 * D:6 * D].partition_broadcast(128), w=[f'g2_{row}'])
                g2.append(g_)
            xt = [kb.sb(f'mxt{i}', [128, D], F32) for i in range(2)]
            ya = [kb.sb(f'mya{i}', [128, D], F32) for i in range(2)]
            yb = [kb.sb(f'myb{i}', [128, D], F32) for i in range(2)]
            for t in tiles:
                i = t % 2; row = 0 if t < 2 else 1
                kb.dma('sp', xt[i][:], xbuf[t * 128:(t + 1) * 128, :], r=[('x', t)], w=[('mxt', i)])
                kb.dma('sp', ya[i][:], AB[t * 128:(t + 1) * 128, :], w=[('mya', i)])
                kb.dma('sp', yb[i][:], AB[T + t * 128:T + (t + 1) * 128, :], w=[('myb', i)])
                kb.op('pool', lambda e: e.tensor_tensor(out=ya[i][:], in0=ya[i][:], in1=yb[i][:], op=ALU.add), r=[('mya', i), ('myb', i)], w=[('mya', i)])
                kb.op('dve', lambda e: e.tensor_tensor(out=ya[i][:], in0=ya[i][:], in1=g2[row][:], op=ALU.mult), r=[('mya', i), f'g2_{row}'], w=[('mya', i)])
                kb.op('pool', lambda e: e.tensor_tensor(out=xt[i][:], in0=xt[i][:], in1=ya[i][:], op=ALU.add), r=[('mya', i), ('mxt', i)], w=[('mxt', i)])
                kb.dma('sp', xbuf[t * 128:(t + 1) * 128, :], xt[i][:], r=[('mxt', i)], w=[('x', t)])
            kb.release(m)

        def phase_final():
            m = kb.mark()
            nwb = kb.sb('fnw', [128, D], F32)
            kb.dma('sp', nwb[:], norm_final.partition_broadcast(128), w=['fnw'])
            xt = [kb.sb(f'fxt{i}', [128, D], F32) for i in range(2)]
            yt = [kb.sb(f'fyt{i}', [128, D], F32) for i in range(2)]
            ssq = [kb.sb(f'fss{i}', [128, 1], F32) for i in range(2)]
            junk = kb.sb('fjunk', [128, D], F32)
            for t in range(2, NT):
                i = t % 2
                kb.dma('sp', xt[i][:], xbuf[t * 128:(t + 1) * 128, :], r=[('x', t)], w=[('fxt', i)])
                kb.op('act', lambda e: e.activation(out=junk[:], in_=xt[i][:], func=AF.Square, accum_out=ssq[i][:]), r=[('fxt', i)], w=['fjunk', ('fss', i)])
                kb.op('act', lambda e: e.activation(out=ssq[i][:], in_=ssq[i][:], func=AF.Sqrt, scale=1.0 / D, bias=epsN), r=[('fss', i)], w=[('fss', i)])
                kb.op('dve', lambda e: e.reciprocal(out=ssq[i][:], in_=ssq[i][:]), r=[('fss', i)], w=[('fss', i)])
                kb.op('dve', lambda e: e.scalar_tensor_tensor(out=yt[i][:], in0=xt[i][:], scalar=ssq[i][:, 0:1], in1=nwb[:], op0=ALU.mult, op1=ALU.mult),
                      r=[('fxt', i), ('fss', i), 'fnw'], w=[('fyt', i)])
                kb.dma('sp', out[(t - 2) * 128:(t - 1) * 128, :], yt[i][:], r=[('fyt', i)], w=[('out', t)])
            kb.release(m)

        def finish_dbg(hT_=True, oT=None, mT=None):
            kb.barrier()
            for t in range(NT):
                pass
            kb.dma('sp', dbg['d_x'], xbuf[:, :], w=['d_x'])
            if hT_:
                kb.dma('sp', dbg['d_hT'], hT[:], w=['d_hT'])
            if oT is not None:
                kb.dma('sp', dbg['d_oT'], oT[:], w=['d_oT'])
            if mT is not None:
                kb.dma('sp', dbg['d_mT'], mT[:], w=['d_mT'])
            kb.finish()
            return nc

        kb.dma('sp', xbuf[0:L, :], ctx_in[:, :], w=['xinit0'])
        kb.dma('sp', xbuf[L:T, :], x_in[:, :], w=['xinit1'])
        for l in range(nlayers):
            phase_ada(l)
        kb.barrier()
        if stop == 'ada':
            kb.dma('sp', dbg['d_mod'], modbuf[0], w=['d_mod'])
            return finish_dbg()
        for l in range(nlayers):
            phase_norm(l, 1)
            if stop == f'norm{l}':
                return finish_dbg()
            mm = kb.mark()
            oT = kb.sb('oT', [128, 4, T], BF16, top=True)
            phase_ret(l, oT)
            if stop == f'ret{l}':
                return finish_dbg(oT=oT)
            mT = kb.sb('mT', [128, 8, T], BF16)
            merge(l, 2048, w_br, oT, 'retT', mT, True)
            phase_attn(l, oT)
            if stop == f'attn{l}':
                return finish_dbg(oT=oT)
            merge(l, 0, w_ba, oT, 'oaT', mT, False)
            phase_four(l, oT)
            if stop == f'four{l}':
                return finish_dbg(oT=oT)
            merge(l, 1024, w_bf, oT, 'ofT', mT, False)
            if stop == f'merge{l}':
                return finish_dbg(mT=mT)
            phase_out(l, mT)
            kb.release(mm)
            if stop == f'mix{l}':
                return finish_dbg()
            if os.environ.get('MOE_DENSE'):
                phase_moe(l)
            else:
                phase_moe_sparse(l)
            if stop == f'moe{l}':
                return finish_dbg()
        phase_final()
        kb.finish()
    print("SBUF peak bytes", kb.peak, "instr counts", kb.cnt)
    return nc


def _consts():
    bf = ml_dtypes.bfloat16
    f32 = np.float32
    n = np.arange(N)
    inv16 = (10000.0 ** (-(np.arange(16, dtype=f32)) / f32(16))).astype(f32)
    row = (n // 64).astype(f32); col = (n % 64).astype(f32)
    ropeA = np.zeros((2, 128, N), f32)
    for p in range(128):
        d = p % 64
        pos = row if d < 32 else col
        dd = d % 32
        ang = (pos * inv16[dd % 16]).astype(f32)
        ropeA[0, p] = np.cos(ang); ropeA[1, p] = np.sin(ang) * (-1.0 if dd < 16 else 1.0)
    inv32 = (10000.0 ** (-(np.arange(32, dtype=f32)) / f32(32))).astype(f32)
    ropeR = np.zeros((2, 128, N), f32)
    for p in range(128):
        d = p % 64
        ang = (n.astype(f32) * inv32[d % 32]).astype(f32)
        ropeR[0, p] = np.cos(ang); ropeR[1, p] = np.sin(ang) * (-1.0 if d < 32 else 1.0)
    k = np.arange(N, dtype=np.int64)
    ph = (np.outer(k, k) % N).astype(np.float64) * (2 * np.pi / N)
    dftN = np.stack([np.cos(ph), -np.sin(ph)]) / np.sqrt(N)
    k2 = np.arange(256, dtype=np.int64)
    ph2 = (np.outer(k2, k2) % 256).astype(np.float64) * (2 * np.pi / 256)
    dft256 = np.stack([np.cos(ph2), -np.sin(ph2)]) / np.sqrt(256)
    k3 = np.arange(128, dtype=np.int64)
    ph3 = (np.outer(k3, k3) % 128).astype(np.float64) * (2 * np.pi / 128)
    dftC = np.concatenate([np.cos(ph3), np.sin(ph3)], axis=1) / np.sqrt(128)
    b = np.arange(128)[:, None]; a = np.arange(128)[None, :]
    amask = np.concatenate([(b >= a), (b <= a)], axis=1).astype(f32)
    j = b; i = a
    rconst = np.concatenate([np.maximum(i - j, 0), np.maximum(j - i, 0), (i >= j), (j > i), (i + 1) + 0 * j, (128 - i) + 0 * j,
                             127 - np.arange(128)[:, None], np.arange(128)[:, None]], axis=1).astype(f32)
    mconst = np.concatenate([np.tile((np.arange(32) * CAP)[None, :], (128, 1)), np.arange(128)[:, None] + 128 * np.arange(NT)[None, :]], axis=1).astype(f32)
    return dict(mconst=mconst, ropeA=ropeA, ropeR=ropeR, dftN=dftN.astype(bf), dft256=dft256.astype(bf), dftC=dftC.astype(bf),
                amask=amask.astype(bf), rconst=rconst)


def _wext_index():
    idx = []
    swa = np.array([d + 16 if (d % 32) < 16 else d - 16 for d in range(64)])
    swr = np.array([d + 32 if d < 32 else d - 32 for d in range(64)])
    base = 0
    qa = [np.concatenate([np.arange(g * 64, (g + 1) * 64), np.arange((4 + g) * 64, (5 + g) * 64)]) for g in range(4)]
    qas = [np.concatenate([g * 64 + swa, (4 + g) * 64 + swa]) for g in range(4)]
    idx += qa + qas
    idx += [512 + np.arange(128), 512 + np.concatenate([swa, 64 + swa])]
    idx += [640 + np.arange(128)]
    idx += [768 + np.arange(512), 768 + np.concatenate([h * 64 + swr for h in range(8)])]
    idx += [1280 + np.arange(512), 1280 + np.concatenate([h * 64 + swr for h in range(8)])]
    idx += [1792 + np.arange(512), 2304 + np.arange(512), 2816 + np.arange(512), 3328 + np.arange(3072)]
    idx = np.concatenate(idx)
    assert idx.shape[0] == WEXT
    return idx


_CACHE = {}


def kernel(x, c, ctx, c_ctx, norm_mix, norm_ffn, w_ada, b_ada, w_in, attn_sink, ret_decay_fwd, ret_decay_bwd,
           w_branch_attn, w_branch_fourier, w_branch_ret, w_out, w_router_group, b_router_group,
           w_router_expert, b_router_expert, w_exp_gate, w_exp_up, w_exp_down, norm_final):
    f = lambda a: np.ascontiguousarray(np.asarray(a, dtype=np.float32))
    if 'nc' not in _CACHE:
        _CACHE['nc'] = build()
        _CACHE['consts'] = _consts()
        _CACHE['idx'] = _wext_index()
    nc = _CACHE['nc']
    shared = dict(_CACHE['consts'])
    w_in = f(w_in)
    shared.update(
        c_ctx=f(c_ctx), norm_mix=f(norm_mix), norm_ffn=f(norm_ffn), w_ada=f(w_ada), b_ada=f(b_ada),
        w_ext=np.ascontiguousarray(w_in[:, :, _CACHE['idx']]), attn_sink=f(attn_sink),
        ret_decay_fwd=f(ret_decay_fwd), ret_decay_bwd=f(ret_decay_bwd), w_branch_attn=f(w_branch_attn),
        w_branch_fourier=f(w_branch_fourier), w_branch_ret=f(w_branch_ret), w_out=f(w_out),
        w_rt=np.ascontiguousarray(np.concatenate([f(w_router_group), f(w_router_expert)], axis=-1)),
        b_rt=np.ascontiguousarray(np.concatenate([f(b_router_group), f(b_router_expert)], axis=-1)),
        w_exp_gate=f(w_exp_gate), w_exp_up=f(w_exp_up), w_exp_down=f(w_exp_down), norm_final=f(norm_final))
    x = f(x); c = f(c); ctx = f(ctx)
    B = x.shape[0]
    in_maps = []
    for b in range(B):
        d = dict(shared)
        d.update(x=x[b], ctx=ctx[b], c=c[b])
        in_maps.append(d)
    res = run_bass_kernel_spmd(nc, in_maps, core_ids=list(range(B)))
    return np.stack([np.asarray(r["out"], dtype=np.float32) for r in res.results], axis=0)
```

```python
import contextlib, os
import numpy as np
import ml_dtypes
import concourse.bass as bass
import concourse.mybir as mybir
from concourse.bass_utils import run_bass_kernel_spmd

F32 = mybir.dt.float32
BF16 = mybir.dt.bfloat16
AF = mybir.ActivationFunctionType
ALU = mybir.AluOpType
AX = mybir.AxisListType

ARENA_BASE = 20736
SBUF_BYTES = 229376 - ARENA_BASE - 128
SAME_ENGINE_SYNC = bool(int(os.environ.get("SES", "1")))
NDMA_SEM = 12
EPOCH = 30000


def _dsize(dt):
    return 2 if dt == BF16 else 4


class KB:
    def __init__(self, nc):
        self.nc = nc
        self.es = contextlib.ExitStack()
        self.eng = {'pe': nc.tensor, 'act': nc.scalar, 'dve': nc.vector, 'pool': nc.gpsimd, 'sp': nc.sync}
        self.cnt = {e: 0 for e in self.eng}
        self.sems = {e: [] for e in self.eng}
        self.seen = {e: {} for e in self.eng}
        self.res = {}
        self.dma_sems = {}
        self.dma_i = {}
        self.bot = ARENA_BASE
        self.top = ARENA_BASE + SBUF_BYTES
        self.nps = 0
        self.n_sem = 0
        self.peak = 0
        self.off = {}

    def __enter__(self):
        self.es.__enter__()
        return self

    def __exit__(self, *a):
        return self.es.__exit__(*a)

    def sb(self, name, shape, dtype, top=False):
        n = 1
        for s in shape[1:]:
            n *= s
        nbytes = (n * _dsize(dtype) + 63) // 64 * 64
        if top:
            self.top -= nbytes
            off = self.top
        else:
            off = self.bot
            self.bot += nbytes
        assert self.bot <= self.top, f"SBUF overflow allocating {name}: bot={self.bot} top={self.top}"
        self.peak = max(self.peak, self.bot + SBUF_BYTES - self.top)
        self.nps += 1
        self.off[name] = off
        return self.nc.alloc_sbuf_tensor_at(f"{name}_{self.nps}", list(shape), dtype, offset=off)

    def alias(self, name, shape, dtype, off):
        self.nps += 1
        return self.nc.alloc_sbuf_tensor_at(f"{name}_{self.nps}", list(shape), dtype, offset=off)

    def mark(self):
        return (self.bot, self.top)

    def release(self, m):
        self.barrier()
        self.bot, self.top = m

    def ps(self, name, shape, dtype=F32):
        return self.nc.alloc_psum_tensor(name, list(shape), dtype)

    def _newsem(self, name):
        self.n_sem += 1
        return self.es.enter_context(self.nc.semaphore(f"{name}_{self.n_sem}"))

    def _tick(self, e):
        c = self.cnt[e]
        ep, v = divmod(c, EPOCH)
        while len(self.sems[e]) <= ep:
            self.sems[e].append(self._newsem(f"s_{e}"))
        self.cnt[e] = c + 1
        return (self.sems[e][ep], v + 1, e)

    def _wait(self, e, tick):
        sem, val, owner = tick
        if owner == e and (e == 'pe' or not SAME_ENGINE_SYNC):
            return
        k = sem.num
        if self.seen[e].get(k, 0) >= val:
            return
        self.eng[e].wait_ge(sem, val)
        self.seen[e][k] = val

    def _deps(self, e, r, w):
        ticks = []
        for k in r:
            rec = self.res.get(k)
            if rec and rec[0]:
                ticks.append(rec[0])
        for k in w:
            rec = self.res.get(k)
            if rec:
                if rec[0]:
                    ticks.append(rec[0])
                ticks.extend(rec[1])
        for t in ticks:
            self._wait(e, t)

    def _record(self, tick, r, w):
        for k in r:
            rec = self.res.setdefault(k, [None, []])
            rec[1] = [t for t in rec[1] if not (t[0] is tick[0])] + [tick]
        for k in w:
            self.res[k] = [tick, []]

    def op(self, e, fn, r=(), w=()):
        self._deps(e, r, w)
        tick = self._tick(e)
        fn(self.eng[e]).then_inc(tick[0], 1)
        self._record(tick, r, w)

    def dma(self, q, out, in_, r=(), w=(), fn=None, **kw):
        self._deps(q, r, w)
        pool = self.dma_sems.setdefault(q, [])
        i = self.dma_i.get(q, 0)
        self.dma_i[q] = i + 1
        slot = i % NDMA_SEM
        if len(pool) <= slot:
            pool.append([self._newsem(f"d_{q}"), 0])
        ent = pool[slot]
        if ent[1] > 0:
            self._wait(q, (ent[0], ent[1], 'dma'))
        ent[1] += 16
        tick = (ent[0], ent[1], 'dma')
        if fn is not None:
            fn(self.eng[q]).then_inc(ent[0], 16)
        else:
            self.eng[q].dma_start(out=out, in_=in_, **kw).then_inc(ent[0], 16)
        self._record(tick, r, w)

    def all_ticks(self):
        ticks = []
        for e in self.eng:
            c = self.cnt[e]
            if c > 0:
                ep, v = divmod(c - 1, EPOCH)
                ticks.append((self.sems[e][ep], v + 1, e))
        for q, pool in self.dma_sems.items():
            for ent in pool:
                if ent[1] > 0:
                    ticks.append((ent[0], ent[1], 'dma'))
        return ticks

    def barrier(self):
        ticks = self.all_ticks()
        for e in self.eng:
            for t in ticks:
                if t[2] == e:
                    continue
                self._wait(e, t)
        self.res = {}

    def finish(self):
        ticks = self.all_ticks()
        for t in ticks:
            if t[2] != 'sp':
                self._wait('sp', t)

    def make_ident(self, ident):
        nc = self.nc
        self.op('pool', lambda e: e.memset(ident[:], 1.0), w=['ident'])
        self.op('pool', lambda e: e.affine_select(out=ident[:], in_=ident[:], pattern=[[-1, 128]],
                                                  compare_op=ALU.is_equal, fill=0.0, base=0,
                                                  channel_multiplier=1), r=['ident'], w=['ident'])

D = 1024; L = 256; N = 2048; T = 2304; NT = 18
QA, QAS, KA, KAS, VA, QR, QRS, KR, KRS, VR, GR, FU, GT = 0, 512, 1024, 1152, 1280, 1408, 1920, 2432, 2944, 3456, 3968, 4480, 4992
WEXT = 8064
CAP = int(os.environ.get('MOE_CAP', '1024'))
NS = CAP // 128
I32 = mybir.dt.int32
TB = [(0, 256), (256, 512), (768, 512), (1280, 512), (1792, 512)]


def bc(ap, free):
    return bass.AP(ap.tensor, ap.offset, [list(ap.ap[0])] + [list(f) for f in free])


def build(stop=None, nlayers=2):
    nc = bass.Bass("TRN2", target_bir_lowering=False)

    def din(name, shape, dt=F32):
        return nc.dram_tensor(name, list(shape), dt, kind="ExternalInput").ap()
    x_in = din("x", [N, D]); ctx_in = din("ctx", [L, D]); c_in = din("c", [D]); cctx_in = din("c_ctx", [D])
    norm_mix = din("norm_mix", [2, D]); norm_ffn = din("norm_ffn", [2, D])
    w_ada = din("w_ada", [2, D, 6 * D]); b_ada = din("b_ada", [2, 6 * D])
    w_ext = din("w_ext", [2, D, WEXT]); sink_in = din("attn_sink", [2, 8])
    dec_f = din("ret_decay_fwd", [2, 8]); dec_b = din("ret_decay_bwd", [2, 8])
    w_ba = din("w_branch_attn", [2, 512, D]); w_bf = din("w_branch_fourier", [2, 512, D]); w_br = din("w_branch_ret", [2, 512, D])
    w_out = din("w_out", [2, D, D]); w_rt = din("w_rt", [2, D, 36]); b_rt = din("b_rt", [2, 36])
    if stop is None or stop.startswith('moe'):
        w_eg = din("w_exp_gate", [2, 32, D, 512]); w_eu = din("w_exp_up", [2, 32, D, 512]); w_ed = din("w_exp_down", [2, 32, 512, D])
    norm_final = din("norm_final", [D])
    ropeA = din("ropeA", [2, 128, N]); ropeR = din("ropeR", [2, 128, N])
    dftN = din("dftN", [2, N, N], BF16); dft256 = din("dft256", [2, 256, 256], BF16); dftC = din("dftC", [128, 256], BF16)
    amask = din("amask", [128, 256], BF16); rconst = din("rconst", [128, 770])
    out = nc.dram_tensor("out", [N, D], F32, kind="ExternalOutput").ap()
    xbuf = nc.dram_tensor("xbuf", [T, D], F32).ap()
    mconst = din("mconst", [128, 32 + NT])
    h2d = nc.dram_tensor("h2d", [T, D], BF16).ap()
    rec_d = nc.dram_tensor("rec_d", [32 * CAP, 4], F32).ap()
    AB = nc.dram_tensor("AB", [2 * T, D], F32).ap()
    modbuf = nc.dram_tensor("modbuf", [2, 2, 6 * D], F32).ap()
    dbg = {}
    if stop is not None:
        dbg['d_x'] = nc.dram_tensor("d_x", [T, D], F32, kind="ExternalOutput").ap()
        dbg['d_mod'] = nc.dram_tensor("d_mod", [2, 6 * D], F32, kind="ExternalOutput").ap()
        dbg['d_hT'] = nc.dram_tensor("d_hT", [128, 8, T], BF16, kind="ExternalOutput").ap()
        dbg['d_oT'] = nc.dram_tensor("d_oT", [128, 4, T], BF16, kind="ExternalOutput").ap()
        dbg['d_mT'] = nc.dram_tensor("d_mT", [128, 8, T], BF16, kind="ExternalOutput").ap()

    kb = KB(nc)
    with kb:
        PSA = kb.ps("psa", [128, 7, 512], F32)
        PST = kb.ps("pst", [128, 8, 128], BF16)
        ident = kb.sb("ident", [128, 128], BF16)
        ident32 = kb.sb("ident32", [128, 128], F32)
        kb.make_ident(ident)
        kb.op('pool', lambda e: e.memset(ident32[:], 1.0), w=['ident32'])
        kb.op('pool', lambda e: e.affine_select(out=ident32[:], in_=ident32[:], pattern=[[-1, 128]], compare_op=ALU.is_equal,
                                                fill=0.0, base=0, channel_multiplier=1), r=['ident32'], w=['ident32'])
        cst = kb.sb("cst", [128, 4], F32)
        kb.op('pool', lambda e: e.memset(cst[:, 0:1], 1e-6), w=['cst'])
        kb.op('pool', lambda e: e.memset(cst[:, 1:2], 1e-5), w=['cst'])
        kb.op('pool', lambda e: e.memset(cst[:, 2:3], 1.0), w=['cst'])
        epsN = cst[:, 0:1]; epsG = cst[:, 1:2]; one1 = cst[:, 2:3]
        stg = [kb.sb(f"stg{i}", [128, 2048], F32) for i in range(2)]
        stg_i = [0]; cast_rr = [0]
        hT = kb.sb("hT", [128, 8, T], BF16)
        kb.barrier()

        stg_all = [list(stg)]

        def load_w(dst, dkey, src2d, K, W, engs=('pool',)):
            kk = max(1, 2048 // W)
            stg = stg_all[0]
            for k0 in range(0, K, kk):
                k1 = min(K, k0 + kk)
                i = stg_i[0] % len(stg); stg_i[0] += 1
                sv = stg[i][:, 0:(k1 - k0) * W].rearrange("p (k c) -> p k c", c=W)
                kb.dma('sp', sv, src2d[k0 * 128:k1 * 128, :].rearrange("(k p) c -> p k c", p=128), w=[('stg', i)])
                en = engs[cast_rr[0] % len(engs)]; cast_rr[0] += 1
                if en == 'act':
                    kb.op('act', lambda e: e.copy(out=dst[:, k0:k1, :], in_=sv), r=[('stg', i)], w=[dkey])
                else:
                    kb.op(en, lambda e: e.tensor_copy(out=dst[:, k0:k1, :], in_=sv), r=[('stg', i)], w=[dkey])

        def proj_fm(wt, wkey, c0, tok0, ntok, bank):
            for k in range(8):
                kb.op('pe', lambda e: e.matmul(PSA[:, bank, 0:ntok], lhsT=wt[:, k, c0:c0 + 128], rhs=hT[:, k, tok0:tok0 + ntok],
                                               start=(k == 0), stop=(k == 7)), r=[wkey, 'hT'], w=[('ps', bank)])

        def dump(name, src, r):
            kb.dma('sp', dbg[name], src, r=r, w=[name])

        def xdump(name, t, shape, dt):
            if stop is None or not os.environ.get('XDUMP'):
                return
            kb.barrier()
            d_ = nc.dram_tensor("x_" + name, list(shape), dt, kind="ExternalOutput").ap()
            kb.dma('sp', d_, t[:], w=["x_" + name])

        def phase_ada(l):
            m = kb.mark()
            cT = kb.sb('cT', [128, 8, 2], F32); cTb = kb.sb('cTb', [128, 8, 2], BF16)
            bsb = kb.sb('bsb', [2, 6 * D], F32); modsb = kb.sb('modsb', [2, 6 * D], F32)
            wa = [kb.sb(f'wa{i}', [128, 8, 512], BF16) for i in range(2)]
            kb.dma('sp', cT[:, :, 0], cctx_in.rearrange("(k p) -> p k", p=128), w=['cT'], allow_slow_non_contiguous=True)
            kb.dma('sp', cT[:, :, 1], c_in.rearrange("(k p) -> p k", p=128), w=['cT'], allow_slow_non_contiguous=True)
            kb.dma('sp', bsb[:], b_ada[l].partition_broadcast(2), w=['bsb'])
            kb.op('act', lambda e: e.activation(out=cTb[:], in_=cT[:], func=AF.Silu), r=['cT'], w=['cTb'])
            for nb in range(12):
                i = nb % 2
                load_w(wa[i], ('wa', i), w_ada[l][:, nb * 512:(nb + 1) * 512], 8, 512, engs=('pool', 'act'))
                for k in range(8):
                    kb.op('pe', lambda e: e.matmul(PSA[0:2, i, :], lhsT=cTb[:, k, :], rhs=wa[i][:, k, :], start=(k == 0), stop=(k == 7)),
                          r=['cTb', ('wa', i)], w=[('ps', i)])
                kb.op('dve', lambda e: e.tensor_tensor(out=modsb[:, nb * 512:(nb + 1) * 512], in0=PSA[0:2, i, :],
                                                       in1=bsb[:, nb * 512:(nb + 1) * 512], op=ALU.add), r=[('ps', i), 'bsb'], w=['modsb'])
            kb.dma('sp', modbuf[l], modsb[:], r=['modsb'], w=[('mod', l)])
            if stop == f'ada{l}':
                dump('d_mod', modsb[:], ['modsb'])
            kb.release(m)
            kb.res[('mod', l)] = None
            kb.res.pop(('mod', l))

        def phase_norm(l, which, logits=None, sparse=False):
            m = kb.mark()
            nw = norm_mix if which == 1 else norm_ffn
            si, ci = (0, 1) if which == 1 else (3, 4)
            tiles = range(NT) if (which == 1 or l == 0) else range(2, NT)
            nwb = kb.sb('nwb', [128, D], F32)
            kb.dma('sp', nwb[:], nw[l].partition_broadcast(128), w=['nwb'])
            G = []; S = []
            for row in (0, 1):
                g_ = kb.sb(f'G{row}', [128, D], F32); s_ = kb.sb(f'S{row}', [128, D], F32)
                kb.dma('sp', g_[:], modbuf[l, row, ci * D:(ci + 1) * D].partition_broadcast(128), w=[f'G{row}'])
                kb.dma('sp', s_[:], modbuf[l, row, si * D:(si + 1) * D].partition_broadcast(128), w=[f'S{row}'])
                kb.op('dve', lambda e: e.scalar_tensor_tensor(out=g_[:], in0=g_[:], scalar=1.0, in1=nwb[:], op0=ALU.add, op1=ALU.mult),
                      r=[f'G{row}', 'nwb'], w=[f'G{row}'])
                G.append(g_); S.append(s_)
            xt = [kb.sb(f'xt{i}', [128, D], F32) for i in range(3)]
            tmp = [kb.sb(f'tmp{i}', [128, D], F32) for i in range(3)]
            hb = [kb.sb(f'hb{i}', [128, D], BF16 if which == 1 else F32) for i in range(3)]
            ssq = [kb.sb(f'ssq{i}', [128, 1], F32) for i in range(3)]
            junk = kb.sb('junk', [128, D], F32)
            if which == 2:
                hbb = [kb.sb(f'hbb{i}', [128, D], BF16) for i in range(3)]
                h32 = [kb.sb(f'h32{i}', [128, 8, 128], F32) for i in range(3)]
                wrt32 = kb.sb('wrt32', [128, 8, 36], F32); brt = kb.sb('brt', [128, 36], F32)
                kb.dma('sp', wrt32[:], w_rt[l].rearrange("(k p) c -> p k c", p=128), w=['wrt32'])
                kb.dma('sp', brt[:], b_rt[l].partition_broadcast(128), w=['brt'])
            def stA(t):
                i = t % 3; row = 0 if t < 2 else 1
                kb.dma('sp', xt[i][:], xbuf[t * 128:(t + 1) * 128, :], r=[('x', t)], w=[('xt', i)])
                kb.op('act', lambda e: e.activation(out=junk[:], in_=xt[i][:], func=AF.Square, accum_out=ssq[i][:]),
                      r=[('xt', i)], w=['junk', ('ssq', i)])
                kb.op('act', lambda e: e.activation(out=ssq[i][:], in_=ssq[i][:], func=AF.Sqrt, scale=1.0 / D, bias=epsN),
                      r=[('ssq', i)], w=[('ssq', i)])
                kb.op('dve', lambda e: e.reciprocal(out=ssq[i][:], in_=ssq[i][:]), r=[('ssq', i)], w=[('ssq', i)])
                kb.op('dve', lambda e: e.scalar_tensor_tensor(out=tmp[i][:], in0=xt[i][:], scalar=ssq[i][:, 0:1], in1=G[row][:],
                                                              op0=ALU.mult, op1=ALU.mult), r=[('xt', i), ('ssq', i), f'G{row}'], w=[('tmp', i)])
                kb.op('pool', lambda e: e.tensor_tensor(out=hb[i][:], in0=tmp[i][:], in1=S[row][:], op=ALU.add),
                      r=[('tmp', i), f'S{row}'], w=[('hb', i)])

            def stB(t):
                i = t % 3; row = 0 if t < 2 else 1
                if which == 1:
                    for k in range(8):
                        kb.op('pe', lambda e: e.transpose(out=PST[:, k, :], in_=hb[i][:, k * 128:(k + 1) * 128], identity=ident[:]),
                              r=[('hb', i), 'ident'], w=['pst'])
                    kb.op('act', lambda e: e.copy(out=hT[:, :, t * 128:(t + 1) * 128], in_=PST[:]), r=['pst'], w=['hT'])
                else:
                    for k in range(8):
                        kb.op('pe', lambda e: e.matmul(PSA[:, 5 + k // 4, (k % 4) * 128:(k % 4 + 1) * 128], lhsT=hb[i][:, k * 128:(k + 1) * 128],
                                                       rhs=ident32[:], start=True, stop=True), r=[('hb', i), 'ident32'], w=[('ps', 5 + k // 4)])
                    pv = PSA[:, 5:7, :].rearrange("p a (b c) -> p (a b) c", c=128)
                    kb.op('dve', lambda e: e.tensor_copy(out=h32[i][:], in_=pv), r=[('ps', 5), ('ps', 6)], w=[('h32', i)])
                    if sparse:
                        kb.op('act', lambda e: e.copy(out=hbb[i][:], in_=hb[i][:]), r=[('hb', i)], w=[('hbb', i)])
                        kb.dma('sp', h2d[t * 128:(t + 1) * 128, :], hbb[i][:], r=[('hbb', i)], w=[('h2d', t)])
                    else:
                        kb.op('act', lambda e: e.copy(out=hT[:, :, t * 128:(t + 1) * 128], in_=h32[i][:]), r=[('h32', i)], w=['hT'])
                    for k in range(8):
                        kb.op('pe', lambda e: e.matmul(PSA[:, 4, 0:36], lhsT=h32[i][:, k, :], rhs=wrt32[:, k, :], start=(k == 0), stop=(k == 7)),
                              r=[('h32', i), 'wrt32'], w=[('ps', 4)])
                    kb.op('dve', lambda e: e.tensor_tensor(out=logits[:, t, :], in0=PSA[:, 4, 0:36], in1=brt[:], op=ALU.add),
                          r=[('ps', 4), 'brt'], w=['logits'])

            tl_ = list(tiles)
            stA(tl_[0])
            for n_, t in enumerate(tl_):
                if n_ + 1 < len(tl_):
                    stA(tl_[n_ + 1])
                stB(t)
            kb.release(m)

        def merge(l, gate_off, wb_dram, oT, okey, mT, first):
            m = kb.mark()
            wg = [kb.sb(f'mwg{i}', [128, 8, 128], BF16) for i in range(2)]
            wb = [kb.sb(f'mwb{i}', [128, 4, 128], BF16) for i in range(2)]
            sig = [kb.sb(f'sig{i}', [128, 512], F32) for i in range(2)]
            mtmp = [kb.sb(f'mtmp{i}', [128, 512], F32) for i in range(2)]
            it = 0
            for fc in range(8):
                j = fc % 2
                load_w(wg[j], ('mwg', j), w_ext[l][:, GT + gate_off + fc * 128: GT + gate_off + (fc + 1) * 128], 8, 128)
                load_w(wb[j], ('mwb', j), wb_dram[l][:, fc * 128:(fc + 1) * 128], 4, 128)
                for (s0, n) in (TB if l == 0 else TB[1:]):
                    i = it % 2; it += 1
                    proj_fm(wg[j], ('mwg', j), 0, s0, n, i)
                    kb.op('act', lambda e: e.activation(out=sig[i][:, 0:n], in_=PSA[:, i, 0:n], func=AF.Sigmoid), r=[('ps', i)], w=[('sig', i)])
                    for k in range(4):
                        kb.op('pe', lambda e: e.matmul(PSA[:, 2 + i, 0:n], lhsT=wb[j][:, k, :], rhs=oT[:, k, s0:s0 + n], start=(k == 0), stop=(k == 3)),
                              r=[('mwb', j), okey], w=[('ps', 2 + i)])
                    if first:
                        kb.op('dve', lambda e: e.tensor_tensor(out=mT[:, fc, s0:s0 + n], in0=sig[i][:, 0:n], in1=PSA[:, 2 + i, 0:n], op=ALU.mult),
                              r=[('sig', i), ('ps', 2 + i)], w=['mT'])
                    else:
                        kb.op('dve', lambda e: e.tensor_tensor(out=mtmp[i][:, 0:n], in0=sig[i][:, 0:n], in1=PSA[:, 2 + i, 0:n], op=ALU.mult),
                              r=[('sig', i), ('ps', 2 + i)], w=[('mtmp', i)])
                        kb.op('pool', lambda e: e.tensor_tensor(out=mT[:, fc, s0:s0 + n], in0=mT[:, fc, s0:s0 + n], in1=mtmp[i][:, 0:n], op=ALU.add),
                              r=[('mtmp', i), 'mT'], w=['mT'])
            kb.release(m)

        def rope_evac(bA, bB, dst, dkey, tC, tS, n, t1, t2, j):
            kb.op('dve', lambda e: e.tensor_tensor(out=t1[:, 0:n], in0=PSA[:, bA, 0:n], in1=tC[:, 0:n], op=ALU.mult), r=[('ps', bA), ('rc', j)], w=[('t1', j)])
            kb.op('dve', lambda e: e.tensor_tensor(out=t2[:, 0:n], in0=PSA[:, bB, 0:n], in1=tS[:, 0:n], op=ALU.mult), r=[('ps', bB), ('rs', j)], w=[('t2', j)])
            kb.op('pool', lambda e: e.tensor_tensor(out=dst, in0=t1[:, 0:n], in1=t2[:, 0:n], op=ALU.add), r=[('t1', j), ('t2', j)], w=[dkey])

        def phase_ret(l, retT):
            need_ctx = (l == 0)
            m = kb.mark()
            RC = kb.sb('RC', [128, 770], F32)
            kb.dma('sp', RC[:], rconst[:, :], w=['RC'])
            dpos = RC[:, 0:128]; dneg = RC[:, 128:256]; mge = RC[:, 256:384]; mlt = RC[:, 384:512]
            io1 = RC[:, 512:640]; iob = RC[:, 640:768]; pc127 = RC[:, 768:769]; pcol = RC[:, 769:770]
            lg = kb.sb('lg', [128, 16], F32)
            kb.dma('sp', lg[:, 0:8], dec_f[l].partition_broadcast(128), w=['lg'])
            kb.dma('sp', lg[:, 8:16], dec_b[l].partition_broadcast(128), w=['lg'])
            kb.op('act', lambda e: e.activation(out=lg[:], in_=lg[:], func=AF.Exp, scale=-1.0), r=['lg'], w=['lg'])
            kb.op('act', lambda e: e.activation(out=lg[:], in_=lg[:], func=AF.Ln, bias=one1), r=['lg', 'cst'], w=['lg'])
            kb.op('dve', lambda e: e.tensor_scalar(out=lg[:], in0=lg[:], scalar1=-1.0, scalar2=None, op0=ALU.mult), r=['lg'], w=['lg'])
            lgp = kb.sb('lgp', [128, 8], F32)
            for r in range(4):
                for hh in range(2):
                    for d in range(2):
                        kb.op('pool', lambda e: e.tensor_copy(out=lgp[hh * 64:(hh + 1) * 64, d * 4 + r:d * 4 + r + 1],
                                                              in_=lg[hh * 64:(hh + 1) * 64, d * 8 + 2 * r + hh:d * 8 + 2 * r + hh + 1]), r=['lg'], w=['lgp'])
            g128 = kb.sb('g128', [128, 8], F32)
            kb.op('act', lambda e: e.activation(out=g128[:], in_=lgp[:], func=AF.Exp, scale=128.0), r=['lgp'], w=['g128'])
            Z = kb.sb('Z', [128, 16], F32)
            kb.op('act', lambda e: e.activation(out=Z[:, 0:8], in_=lg[:, 0:8], func=AF.Exp, scale=pc127), r=['lg', 'RC'], w=['Z'])
            kb.op('act', lambda e: e.activation(out=Z[:, 8:16], in_=lg[:, 8:16], func=AF.Exp, scale=pcol), r=['lg', 'RC'], w=['Z'])
            kb.op('dve', lambda e: e.tensor_scalar(out=Z[:], in0=Z[:], scalar1=0.125, scalar2=None, op0=ALU.mult), r=['Z'], w=['Z'])
            DecT = kb.sb('DecT', [128, 8, 128], F32)
            d1 = kb.sb('d1', [128, 128], F32); d2 = kb.sb('d2', [128, 128], F32)
            for h in range(8):
                kb.op('act', lambda e: e.activation(out=d1[:], in_=dpos, func=AF.Exp, scale=lg[:, h:h + 1]), r=['lg', 'RC'], w=['d1'])
                kb.op('pool', lambda e: e.tensor_tensor(out=d1[:], in0=d1[:], in1=mge, op=ALU.mult), r=['d1', 'RC'], w=['d1'])
                kb.op('act', lambda e: e.activation(out=d2[:], in_=dneg, func=AF.Exp, scale=lg[:, 8 + h:9 + h]), r=['lg', 'RC'], w=['d2'])
                kb.op('pool', lambda e: e.tensor_tensor(out=d2[:], in0=d2[:], in1=mlt, op=ALU.mult), r=['d2', 'RC'], w=['d2'])
                kb.op('pool', lambda e: e.tensor_tensor(out=d1[:], in0=d1[:], in1=d2[:], op=ALU.add), r=['d1', 'd2'], w=['d1'])
                kb.op('dve', lambda e: e.tensor_scalar(out=DecT[:, h, :], in0=d1[:], scalar1=0.125, scalar2=None, op0=ALU.mult), r=['d1'], w=['DecT'])
            X = kb.sb('X', [128, 8, 128], F32)
            for r in range(4):
                kb.op('act', lambda e: e.activation(out=X[:, r, :], in_=io1, func=AF.Exp, scale=lgp[:, r:r + 1]), r=['lgp', 'RC'], w=['X'])
                kb.op('act', lambda e: e.activation(out=X[:, 4 + r, :], in_=iob, func=AF.Exp, scale=lgp[:, 4 + r:5 + r]), r=['lgp', 'RC'], w=['X'])
            if os.environ.get('RET_CUT') == '1':
                kb.release(m); return
            qT = kb.sb('qT', [128, T], BF16); kT = kb.sb('kT', [128, T], BF16)
            ktok = kb.sb('ktok', [128, NT, 128], BF16); vtok = kb.sb('vtok', [128, NT, 128], BF16)
            vf = kb.sb('vf', [128, NT, 128], BF16); vb = kb.sb('vb', [128, NT, 128], BF16)
            sg = kb.sb('sg', [128, NT, 128], F32)
            Sf = kb.sb('Sf', [128, NT, 128], BF16); Rb = kb.sb('Rb', [128, NT, 128], BF16)
            Srun = kb.sb('Srun', [128, 128], F32); Rrun = kb.sb('Rrun', [128, 128], F32)
            ws = {nm: kb.sb('w' + nm, [128, 8, 128], BF16) for nm in ('q', 'qs', 'k', 'ks', 'v', 'g')}
            rc = [kb.sb(f'rc{i}', [128, 512], F32) for i in range(2)]; rs = [kb.sb(f'rs{i}', [128, 512], F32) for i in range(2)]
            t1 = [kb.sb(f't1{i}', [128, 512], F32) for i in range(2)]; t2 = [kb.sb(f't2{i}', [128, 512], F32) for i in range(2)]
            AT = [kb.sb(f'AT{i}', [128, 2, 128], BF16) for i in range(2)]
            qxf = [kb.sb(f'qxf{i}', [128, 128], BF16) for i in range(2)]; qxb = [kb.sb(f'qxb{i}', [128, 128], BF16) for i in range(2)]
            oc = [kb.sb(f'oc{i}', [128, 128], F32) for i in range(2)]; sq = [kb.sb(f'sq{i}', [128, 128], F32) for i in range(2)]
            st = [kb.sb(f'st{i}', [128, 4], F32) for i in range(2)]
            rtok = [kb.sb(f'rtok{i}', [128, 128], BF16) for i in range(2)]
            o_all = kb.sb('o_all', [128, NT, 128], F32); sq_all = kb.sb('sq_all', [128, NT, 128], F32); rt_all = kb.sb('rt_all', [128, NT, 128], BF16)
            mu_ = kb.sb('mu_', [128, 2 * NT], F32); va_ = kb.sb('va_', [128, 2 * NT], F32)
            for r in range(4):
                for nm, c0 in (('q', QR), ('qs', QRS), ('k', KR), ('ks', KRS), ('v', VR), ('g', GR)):
                    load_w(ws[nm], 'w' + nm, w_ext[l][:, c0 + r * 128:c0 + (r + 1) * 128], 8, 128)
                for bi, (s0, n) in enumerate(TB):
                    if s0 == 0:
                        proj_fm(ws['q'], 'wq', 0, s0, n, 0)
                        kb.op('act', lambda e: e.copy(out=qT[:, s0:s0 + n], in_=PSA[:, 0, 0:n]), r=[('ps', 0)], w=['qT'])
                        proj_fm(ws['k'], 'wk', 0, s0, n, 1)
                        kb.op('act', lambda e: e.copy(out=kT[:, s0:s0 + n], in_=PSA[:, 1, 0:n]), r=[('ps', 1)], w=['kT'])
                    else:
                        j = bi % 2; p0 = s0 - 256
                        kb.dma('sp', rc[j][:, 0:n], ropeR[0, :, p0:p0 + n], w=[('rc', j)])
                        kb.dma('sp', rs[j][:, 0:n], ropeR[1, :, p0:p0 + n], w=[('rs', j)])
                        proj_fm(ws['q'], 'wq', 0, s0, n, 0); proj_fm(ws['qs'], 'wqs', 0, s0, n, 1)
                        rope_evac(0, 1, qT[:, s0:s0 + n], 'qT', rc[j], rs[j], n, t1[j], t2[j], j)
                        proj_fm(ws['k'], 'wk', 0, s0, n, 2); proj_fm(ws['ks'], 'wks', 0, s0, n, 3)
                        rope_evac(2, 3, kT[:, s0:s0 + n], 'kT', rc[j], rs[j], n, t1[j], t2[j], j)
                    for t in range(s0 // 128, (s0 + n) // 128):
                        bank = 4 + (t % 2)
                        for k in range(8):
                            kb.op('pe', lambda e: e.matmul(PSA[:, bank, 0:128], lhsT=hT[:, k, t * 128:(t + 1) * 128], rhs=ws['v'][:, k, :],
                                                           start=(k == 0), stop=(k == 7)), r=['hT', 'wv'], w=[('ps', bank)])
                        for k in range(8):
                            kb.op('pe', lambda e: e.matmul(PSA[:, bank, 128:256], lhsT=hT[:, k, t * 128:(t + 1) * 128], rhs=ws['g'][:, k, :],
                                                           start=(k == 0), stop=(k == 7)), r=['hT', 'wg'], w=[('ps', bank)])
                        kb.op('act', lambda e: e.copy(out=vtok[:, t, :], in_=PSA[:, bank, 0:128]), r=[('ps', bank)], w=['vtok'])
                        kb.op('act', lambda e: e.activation(out=sg[:, t, :], in_=PSA[:, bank, 128:256], func=AF.Silu), r=[('ps', bank)], w=['sg'])
                if os.environ.get('RET_CUT') == '2':
                    kb.release(m); return
                for t in range(NT):
                    kb.op('pe', lambda e: e.transpose(out=PST[:, t % 8, :], in_=kT[:, t * 128:(t + 1) * 128], identity=ident[:]), r=['kT', 'ident'], w=['pst'])
                    if t % 8 == 7 or t == NT - 1:
                        n8 = t % 8 + 1; t0 = t - n8 + 1
                        kb.op('act', lambda e: e.copy(out=ktok[:, t0:t + 1, :], in_=PST[:, 0:n8, :]), r=['pst'], w=['ktok'])
                v4 = lambda a: a[:].rearrange("p c (h e) -> p c h e", h=2)
                kb.op('pool', lambda e: e.tensor_tensor(out=v4(vf), in0=v4(vtok), in1=bc(Z[:, 2 * r:2 * r + 2], [[0, NT], [1, 2], [0, 64]]), op=ALU.mult),
                      r=['vtok', 'Z'], w=['vf'])
                kb.op('pool', lambda e: e.tensor_tensor(out=v4(vb), in0=v4(vtok), in1=bc(Z[:, 8 + 2 * r:8 + 2 * r + 2], [[0, NT], [1, 2], [0, 64]]), op=ALU.mult),
                      r=['vtok', 'Z'], w=['vb'])
                if os.environ.get('RET_CUT') == '3':
                    kb.release(m); return
                kb.op('pool', lambda e: e.memset(Srun[:], 0.0), w=['Srun'])
                kb.op('pool', lambda e: e.memset(Sf[:, 0, :], 0.0), w=['Sf'])
                for c in range(NT - 1):
                    bank = 4 + c % 2
                    kb.op('pe', lambda e: e.matmul(PSA[:, bank, 0:128], lhsT=ktok[:, c, :], rhs=vf[:, c, :], start=True, stop=True),
                          r=['ktok', 'vf'], w=[('ps', bank)])
                    kb.op('dve', lambda e: e.scalar_tensor_tensor(out=Srun[:], in0=Srun[:], scalar=g128[:, r:r + 1], in1=PSA[:, bank, 0:128],
                                                                  op0=ALU.mult, op1=ALU.add), r=['Srun', 'g128', ('ps', bank)], w=['Srun'])
                    kb.op('act', lambda e: e.copy(out=Sf[:, c + 1, :], in_=Srun[:]), r=['Srun'], w=['Sf'])
                kb.op('pool', lambda e: e.memset(Rrun[:], 0.0), w=['Rrun'])
                kb.op('pool', lambda e: e.memset(Rb[:, 1, :], 0.0), w=['Rb'])
                order = [1, 0] + list(range(17, 2, -1)); dest = [0, 17] + list(range(16, 1, -1))
                for ii, (c, dd) in enumerate(zip(order, dest)):
                    bank = 4 + ii % 2
                    kb.op('pe', lambda e: e.matmul(PSA[:, bank, 0:128], lhsT=ktok[:, c, :], rhs=vb[:, c, :], start=True, stop=True),
                          r=['ktok', 'vb'], w=[('ps', bank)])
                    kb.op('dve', lambda e: e.scalar_tensor_tensor(out=Rrun[:], in0=Rrun[:], scalar=g128[:, 4 + r:5 + r], in1=PSA[:, bank, 0:128],
                                                                  op0=ALU.mult, op1=ALU.add), r=['Rrun', 'g128', ('ps', bank)], w=['Rrun'])
                    kb.op('act', lambda e: e.copy(out=Rb[:, dd, :], in_=Rrun[:]), r=['Rrun'], w=['Rb'])
                if os.environ.get('RET_CUT') == '4':
                    kb.release(m); return
                chunks_ = list(range(NT) if need_ctx else range(2, NT))

                def inner_(c):
                    i = c % 2; cs = slice(c * 128, (c + 1) * 128)
                    for hh in range(2):
                        ps_ = slice(hh * 64, (hh + 1) * 64)
                        kb.op('pe', lambda e: e.matmul(PSA[:, i + 4 * hh, 0:128], lhsT=kT[ps_, cs], rhs=qT[ps_, cs], start=True, stop=True),
                              r=['kT', 'qT'], w=[('ps', i + 4 * hh)])

                def rest_(c):
                    i = c % 2; cs = slice(c * 128, (c + 1) * 128)
                    for hh in range(2):
                        kb.op('dve', lambda e: e.tensor_tensor(out=AT[i][:, hh, :], in0=PSA[:, i + 4 * hh, 0:128],
                                                               in1=DecT[:, 2 * r + hh, :], op=ALU.mult), r=[('ps', i + 4 * hh), 'DecT'], w=[('AT', i)])
                    kb.op('pool', lambda e: e.tensor_tensor(out=qxf[i][:], in0=qT[:, cs], in1=X[:, r, :], op=ALU.mult), r=['qT', 'X'], w=[('qxf', i)])
                    kb.op('pool', lambda e: e.tensor_tensor(out=qxb[i][:], in0=qT[:, cs], in1=X[:, 4 + r, :], op=ALU.mult), r=['qT', 'X'], w=[('qxb', i)])
                    for hh in range(2):
                        ps_ = slice(hh * 64, (hh + 1) * 64)
                        o_ = PSA[:, 2 + i, hh * 64:(hh + 1) * 64]
                        kb.op('pe', lambda e: e.matmul(o_, lhsT=AT[i][:, hh, :], rhs=vtok[:, c, hh * 64:(hh + 1) * 64], start=True, stop=False),
                              r=[('AT', i), 'vtok'], w=[('ps', 2 + i)])
                        kb.op('pe', lambda e: e.matmul(o_, lhsT=qxf[i][ps_, :], rhs=Sf[ps_, c, hh * 64:(hh + 1) * 64], start=False, stop=False),
                              r=[('qxf', i), 'Sf'], w=[('ps', 2 + i)])
                        kb.op('pe', lambda e: e.matmul(o_, lhsT=qxb[i][ps_, :], rhs=Rb[ps_, c, hh * 64:(hh + 1) * 64], start=False, stop=True),
                              r=[('qxb', i), 'Rb'], w=[('ps', 2 + i)])
                    kb.op('act', lambda e: e.copy(out=o_all[:, c, :], in_=PSA[:, 2 + i, 0:128]), r=[('ps', 2 + i)], w=['o_all'])

                inner_(chunks_[0])
                for n_, c in enumerate(chunks_):
                    if n_ + 1 < len(chunks_):
                        inner_(chunks_[n_ + 1])
                    rest_(c)
                c0_ = 0 if need_ctx else 2
                G_ = (NT - c0_) * 2
                og = o_all[:, c0_:NT, :].rearrange("p c (h e) -> p (c h) e", h=2)
                sqg = sq_all[:, c0_:NT, :].rearrange("p c (h e) -> p (c h) e", h=2)
                kb.op('dve', lambda e: e.reduce_sum(out=mu_[:, 0:G_], in_=og, axis=AX.X), r=['o_all'], w=['mu_'])
                kb.op('dve', lambda e: e.tensor_scalar(out=mu_[:, 0:G_], in0=mu_[:, 0:G_], scalar1=-1.0 / 64, scalar2=None, op0=ALU.mult), r=['mu_'], w=['mu_'])
                kb.op('pool', lambda e: e.tensor_tensor(out=og, in0=og, in1=bc(mu_[:, 0:G_], [[1, G_], [0, 64]]), op=ALU.add), r=['o_all', 'mu_'], w=['o_all'])
                kb.op('pool', lambda e: e.tensor_tensor(out=sqg, in0=og, in1=og, op=ALU.mult), r=['o_all'], w=['sq_all'])
                kb.op('dve', lambda e: e.reduce_sum(out=va_[:, 0:G_], in_=sqg, axis=AX.X), r=['sq_all'], w=['va_'])
                kb.op('act', lambda e: e.activation(out=va_[:, 0:G_], in_=va_[:, 0:G_], func=AF.Sqrt, scale=1.0 / 64, bias=epsG), r=['va_'], w=['va_'])
                kb.op('dve', lambda e: e.reciprocal(out=va_[:, 0:G_], in_=va_[:, 0:G_]), r=['va_'], w=['va_'])
                kb.op('pool', lambda e: e.tensor_tensor(out=og, in0=og, in1=bc(va_[:, 0:G_], [[1, G_], [0, 64]]), op=ALU.mult), r=['o_all', 'va_'], w=['o_all'])
                kb.op('pool', lambda e: e.tensor_tensor(out=rt_all[:, c0_:NT, :], in0=o_all[:, c0_:NT, :], in1=sg[:, c0_:NT, :], op=ALU.mult),
                      r=['o_all', 'sg'], w=['rt_all'])
                cl_ = list(range(c0_, NT))
                for n0 in range(0, len(cl_), 8):
                    grp_ = cl_[n0:n0 + 8]
                    for ii_, c in enumerate(grp_):
                        kb.op('pe', lambda e: e.transpose(out=PST[:, ii_, :], in_=rt_all[:, c, :], identity=ident[:]), r=['rt_all', 'ident'], w=['pst'])
                    kb.op('act', lambda e: e.copy(out=retT[:, r, grp_[0] * 128:(grp_[-1] + 1) * 128].rearrange("p (a b) -> p a b", b=128),
                                                  in_=PST[:, 0:len(grp_), :]), r=['pst'], w=['retT'])
                if os.environ.get('RET_CUT') in ('5', '6', '7'):
                    kb.release(m); return
                if r == 3:
                    for nm_, t_, sh_, dt_ in (('sg', sg, [128, NT, 128], F32), ('DecT', DecT, [128, 8, 128], F32), ('X', X, [128, 8, 128], F32),
                                              ('Z', Z, [128, 16], F32), ('g128', g128, [128, 8], F32), ('lg', lg, [128, 16], F32),
                                              ('Sf', Sf, [128, NT, 128], BF16), ('Rb', Rb, [128, NT, 128], BF16), ('qT', qT, [128, T], BF16),
                                              ('kT', kT, [128, T], BF16), ('vtok', vtok, [128, NT, 128], BF16), ('ktok', ktok, [128, NT, 128], BF16),
                                              ('vf', vf, [128, NT, 128], BF16), ('AT1', AT[1], [128, 2, 128], BF16), ('qxf1', qxf[1], [128, 128], BF16),
                                              ('qxb1', qxb[1], [128, 128], BF16)):
                        xdump(nm_, t_, sh_, dt_)
            kb.release(m)

        def phase_attn(l, oaT):
            need_ctx = (l == 0)
            m = kb.mark()
            qT = kb.sb('aqT', [128, 4, T], BF16); kT = kb.sb('akT', [128, T], BF16)
            Va = kb.sb('Va', [128, NT, 2, 66], BF16)
            msk = kb.sb('msk', [128, 256], BF16)
            kb.dma('sp', msk[:], amask[:, :], w=['msk'])
            snk = kb.sb('snk', [128, 8], F32)
            kb.dma('sp', snk[:], sink_in[l].partition_broadcast(128), w=['snk'])
            kb.op('act', lambda e: e.activation(out=snk[:], in_=snk[:], func=AF.Exp), r=['snk'], w=['snk'])
            kb.op('pool', lambda e: e.memset(Va[:, :, :, 64:66], 1.0), w=['Va'])
            wq = kb.sb('awq', [128, 8, 1024], BF16); wk = kb.sb('awk', [128, 8, 256], BF16); wv = kb.sb('awv', [128, 8, 128], BF16)
            load_w(wq, 'awq', w_ext[l][:, QA:QA + 1024], 8, 1024)
            load_w(wk, 'awk', w_ext[l][:, KA:KA + 256], 8, 256)
            load_w(wv, 'awv', w_ext[l][:, VA:VA + 128], 8, 128)
            rc = [kb.sb(f'arc{i}', [128, 512], F32) for i in range(2)]; rs = [kb.sb(f'ars{i}', [128, 512], F32) for i in range(2)]
            t1 = [kb.sb(f'at1{i}', [128, 512], F32) for i in range(2)]; t2 = [kb.sb(f'at2{i}', [128, 512], F32) for i in range(2)]
            for bi, (s0, n) in enumerate(TB):
                if s0 == 0:
                    for g in range(4):
                        proj_fm(wq, 'awq', g * 128, s0, n, g % 2)
                        kb.op('act', lambda e: e.copy(out=qT[:, g, s0:s0 + n], in_=PSA[:, g % 2, 0:n]), r=[('ps', g % 2)], w=['aqT'])
                    proj_fm(wk, 'awk', 0, s0, n, 2)
                    kb.op('act', lambda e: e.copy(out=kT[:, s0:s0 + n], in_=PSA[:, 2, 0:n]), r=[('ps', 2)], w=['akT'])
                else:
                    j = bi % 2; p0 = s0 - 256
                    kb.dma('sp', rc[j][:, 0:n], ropeA[0, :, p0:p0 + n], w=[('rc', j)])
                    kb.dma('sp', rs[j][:, 0:n], ropeA[1, :, p0:p0 + n], w=[('rs', j)])
                    for g in range(4):
                        b0 = 2 * (g % 2)
                        proj_fm(wq, 'awq', g * 128, s0, n, b0); proj_fm(wq, 'awq', 512 + g * 128, s0, n, b0 + 1)
                        rope_evac(b0, b0 + 1, qT[:, g, s0:s0 + n], 'aqT', rc[j], rs[j], n, t1[j], t2[j], j)
                    proj_fm(wk, 'awk', 0, s0, n, 4); proj_fm(wk, 'awk', 128, s0, n, 5)
                    rope_evac(4, 5, kT[:, s0:s0 + n], 'akT', rc[j], rs[j], n, t1[j], t2[j], j)
                for t in range(s0 // 128, (s0 + n) // 128):
                    for k in range(8):
                        kb.op('pe', lambda e: e.matmul(PSA[:, 6, 0:128], lhsT=hT[:, k, t * 128:(t + 1) * 128], rhs=wv[:, k, :],
                                                       start=(k == 0), stop=(k == 7)), r=['hT', 'awv'], w=[('ps', 6)])
                    kb.op('act', lambda e: e.copy(out=Va[:, t, :, 0:64], in_=PSA[:, 6, 0:128].rearrange("p (h e) -> p h e", h=2)),
                          r=[('ps', 6)], w=['Va'])
            PT = [[kb.sb(f'PT{i}_{j}', [128, 4, 128], BF16) for j in range(5)] for i in range(2)]
            oat = [kb.sb(f'oat{i}', [128, 8, 64], BF16) for i in range(2)]
            den = [kb.sb(f'den{i}', [128, 4], F32) for i in range(2)]
            def keys_of(t):
                if t < 2:
                    return [(0, None), (1, None)]
                keys = []
                if t > 2: keys.append((t - 1, 0))
                keys.append((t, None))
                if t < NT - 1: keys.append((t + 1, 1))
                return keys + [(0, None), (1, None)]

            groups = [(t, h2) for t in (range(NT) if need_ctx else range(2, NT)) for h2 in range(2)]
            SB = [0, 1, 2, 5, 6]
            sbc = [0]

            def SE(n):
                t, h2 = groups[n]; i = n % 2
                ps_ = slice(h2 * 64, (h2 + 1) * 64)
                for ki, (kt, mk) in enumerate(keys_of(t)):
                    bank = SB[sbc[0] % 5]; sbc[0] += 1
                    kb.op('pe', lambda e: e.matmul(PSA[:, bank, :].rearrange("p (g q) -> p g q", g=4), lhsT=kT[ps_, kt * 128:(kt + 1) * 128],
                                                   rhs=qT[ps_, :, t * 128:(t + 1) * 128], start=True, stop=True), r=['akT', 'aqT'], w=[('ps', bank)])
                    kb.op('act', lambda e: e.activation(out=PT[i][ki][:], in_=PSA[:, bank, :].rearrange("p (g q) -> p g q", g=4), func=AF.Exp, scale=0.125),
                          r=[('ps', bank)], w=[('PT', i, ki)])
                    if mk is not None:
                        kb.op('pool', lambda e: e.tensor_tensor(out=PT[i][ki][:], in0=PT[i][ki][:], in1=bc(msk[:, mk * 128:(mk + 1) * 128], [[0, 4], [1, 128]]),
                                                                op=ALU.mult), r=[('PT', i, ki), 'msk'], w=[('PT', i, ki)])

            def PVN(n):
                t, h2 = groups[n]; i = n % 2; ti = t % 2
                keys = keys_of(t)
                ob = 3 + i
                for g in range(4):
                    for ki, (kt, mk) in enumerate(keys):
                        kb.op('pe', lambda e: e.matmul(PSA[:, ob, g * 66:g * 66 + 65], lhsT=PT[i][ki][:, g, :], rhs=Va[:, kt, h2, 0:65],
                                                       start=(ki == 0), stop=(ki == len(keys) - 1)), r=[('PT', i, ki), 'Va'], w=[('ps', ob)])
                ov = PSA[:, ob, 0:264].rearrange("p (g e) -> p g e", g=4)
                kb.op('dve', lambda e: e.tensor_tensor(out=den[i][:], in0=ov[:, :, 64], in1=snk[:, h2 * 4:(h2 + 1) * 4], op=ALU.add),
                      r=[('ps', ob), 'snk'], w=[('den', i)])
                kb.op('dve', lambda e: e.reciprocal(out=den[i][:], in_=den[i][:]), r=[('den', i)], w=[('den', i)])
                kb.op('dve', lambda e: e.tensor_tensor(out=oat[ti][:, h2 * 4:(h2 + 1) * 4, :], in0=ov[:, :, 0:64], in1=bc(den[i][:, 0:4], [[1, 4], [0, 64]]),
                                                       op=ALU.mult), r=[('ps', ob), ('den', i)], w=[('oat', ti)])
                if h2 == 1:
                    for k in range(4):
                        kb.op('pe', lambda e: e.transpose(out=PST[:, k, :], in_=oat[ti][:, 2 * k:2 * k + 2, :].rearrange("p h e -> p (h e)"), identity=ident[:]),
                              r=[('oat', ti), 'ident'], w=['pst'])
                    kb.op('act', lambda e: e.copy(out=oaT[:, :, t * 128:(t + 1) * 128], in_=PST[:, 0:4, :]), r=['pst'], w=['oaT'])

            SE(0)
            for n in range(len(groups)):
                if n + 1 < len(groups):
                    SE(n + 1)
                PVN(n)
            kb.release(m)

        def phase_four(l, ofT):
            need_ctx = (l == 0)
            m = kb.mark()
            wfu = kb.sb('wfu', [128, 8, 512], BF16)
            load_w(wfu, 'wfu', w_ext[l][:, FU:FU + 512], 8, 512)
            dC = kb.sb('dC', [128, 256], BF16)
            kb.dma('sp', dC[:], dftC[:, :], w=['dC'])
            W = kb.sb('W', [128, NT, 4, 256], BF16)
            uT = [kb.sb(f'uT{i}', [128, T], BF16) for i in range(2)]
            for g in range(4):
                i = g % 2
                for bi, (s0, n) in enumerate(TB):
                    proj_fm(wfu, 'wfu', g * 128, s0, n, bi % 2)
                    kb.op('act', lambda e: e.copy(out=uT[i][:, s0:s0 + n], in_=PSA[:, bi % 2, 0:n]), r=[('ps', bi % 2)], w=[('uT', i)])
                for t in range(NT):
                    bank = 2 + t % 2
                    kb.op('pe', lambda e: e.matmul(PSA[:, bank, 0:256], lhsT=uT[i][:, t * 128:(t + 1) * 128], rhs=dC[:], start=True, stop=True),
                          r=[('uT', i), 'dC'], w=[('ps', bank)])
                    kb.op('dve', lambda e: e.tensor_copy(out=W[:, t, g, :], in_=PSA[:, bank, 0:256]), r=[('ps', bank)], w=['W'])
            Cb = [kb.sb(f'Cb{i}', [128, 16, 256], BF16) for i in range(2)]
            Nb = [kb.sb(f'Nb{i}', [128, 16, 256], BF16) for i in range(2)]
            it = 0
            for nb in range(8):
                i = nb % 2
                kb.dma('sp', Cb[i][:], dftN[0, :, nb * 256:(nb + 1) * 256].rearrange("(t p) c -> p t c", p=128), w=[('Cb', i)])
                kb.dma('sp', Nb[i][:], dftN[1, :, nb * 256:(nb + 1) * 256].rearrange("(t p) c -> p t c", p=128), w=[('Nb', i)])
                for g in range(4):
                    bank = 4 + it % 2; it += 1
                    for t in range(16):
                        kb.op('pe', lambda e: e.matmul(PSA[:, bank, 0:256], lhsT=W[:, 2 + t, g, 0:128], rhs=Cb[i][:, t, :], start=(t == 0), stop=False),
                              r=['W', ('Cb', i)], w=[('ps', bank)])
                        kb.op('pe', lambda e: e.matmul(PSA[:, bank, 0:256], lhsT=W[:, 2 + t, g, 128:256], rhs=Nb[i][:, t, :], start=False, stop=(t == 15)),
                              r=['W', ('Nb', i)], w=[('ps', bank)])
                    kb.op('act', lambda e: e.copy(out=ofT[:, g, 256 + nb * 256:256 + (nb + 1) * 256], in_=PSA[:, bank, 0:256]), r=[('ps', bank)], w=['ofT'])
            if need_ctx:
                kb.dma('sp', Cb[0][:, 0:2, :], dft256[0].rearrange("(t p) c -> p t c", p=128), w=[('Cb', 0)])
                kb.dma('sp', Nb[0][:, 0:2, :], dft256[1].rearrange("(t p) c -> p t c", p=128), w=[('Nb', 0)])
                for g in range(4):
                    bank = 4 + g % 2
                    for t in range(2):
                        kb.op('pe', lambda e: e.matmul(PSA[:, bank, 0:256], lhsT=W[:, t, g, 0:128], rhs=Cb[0][:, t, :], start=(t == 0), stop=False),
                              r=['W', ('Cb', 0)], w=[('ps', bank)])
                        kb.op('pe', lambda e: e.matmul(PSA[:, bank, 0:256], lhsT=W[:, t, g, 128:256], rhs=Nb[0][:, t, :], start=False, stop=(t == 1)),
                              r=['W', ('Nb', 0)], w=[('ps', bank)])
                    kb.op('act', lambda e: e.copy(out=ofT[:, g, 0:256], in_=PSA[:, bank, 0:256]), r=[('ps', bank)], w=['ofT'])
            kb.release(m)

        def resid_update(l, gi, tiles, src_fn, src_keys):
            pass

        def phase_out(l, mT):
            m = kb.mark()
            wo = kb.sb('wo', [128, 8, D], BF16)
            load_w(wo, 'wo', w_out[l][:, :], 8, D)
            g1 = []
            for row in (0, 1):
                g_ = kb.sb(f'g1_{row}', [128, D], F32)
                kb.dma('sp', g_[:], modbuf[l, row, 2 * D:3 * D].partition_broadcast(128), w=[f'g1_{row}'])
                g1.append(g_)
            xt = [kb.sb(f'oxt{i}', [128, D], F32) for i in range(2)]
            yt = [kb.sb(f'oyt{i}', [128, D], F32) for i in range(2)]
            for t in (range(NT) if l == 0 else range(2, NT)):
                i = t % 2; row = 0 if t < 2 else 1
                kb.dma('sp', xt[i][:], xbuf[t * 128:(t + 1) * 128, :], r=[('x', t)], w=[('oxt', i)])
                for hf in range(2):
                    bank = 2 * i + hf
                    for fc in range(8):
                        kb.op('pe', lambda e: e.matmul(PSA[:, bank, :], lhsT=mT[:, fc, t * 128:(t + 1) * 128], rhs=wo[:, fc, hf * 512:(hf + 1) * 512],
                                                       start=(fc == 0), stop=(fc == 7)), r=['mT', 'wo'], w=[('ps', bank)])
                    kb.op('dve', lambda e: e.tensor_tensor(out=yt[i][:, hf * 512:(hf + 1) * 512], in0=PSA[:, bank, :], in1=g1[row][:, hf * 512:(hf + 1) * 512],
                                                           op=ALU.mult), r=[('ps', bank), f'g1_{row}'], w=[('oyt', i)])
                kb.op('pool', lambda e: e.tensor_tensor(out=yt[i][:], in0=yt[i][:], in1=xt[i][:], op=ALU.add), r=[('oyt', i), ('oxt', i)], w=[('oyt', i)])
                kb.dma('sp', xbuf[t * 128:(t + 1) * 128, :], yt[i][:], r=[('oyt', i)], w=[('x', t)])
            kb.release(m)

        def phase_moe(l):
            m = kb.mark()
            tiles = list(range(NT) if l == 0 else range(2, NT))
            blocks = TB if l == 0 else TB[1:]
            logits = kb.sb('logits', [128, NT, 36], F32)
            Wt = kb.sb('Wt', [128, NT, 32], F32)
            kb.op('pool', lambda e: e.memset(logits[:], 0.0), w=['logits'])
            phase_norm(l, 2, logits)
            if os.environ.get('MOE_CUT') == '1':
                kb.release(m); return
            m2 = kb.mark()
            lgG = logits[:, :, 0:4]; lgE = logits[:, :, 4:36]
            gmax = kb.sb('gmax', [128, NT], F32); ohg = kb.sb('ohg', [128, NT, 4], F32); eg = kb.sb('eg', [128, NT, 4], F32)
            pg = kb.sb('pg', [128, NT], F32); me = kb.sb('me', [128, NT, 32], F32); oh1 = kb.sb('oh1', [128, NT, 32], F32)
            oh2 = kb.sb('oh2', [128, NT, 32], F32); m1 = kb.sb('m1', [128, NT], F32); m2_ = kb.sb('m2', [128, NT], F32)
            w1 = kb.sb('w1', [128, NT], F32); w2 = kb.sb('w2', [128, NT], F32)
            b1 = lambda a, n_: bc(a, [[1, NT], [0, n_]])
            kb.op('dve', lambda e: e.reduce_max(out=gmax[:], in_=lgG, axis=AX.X), r=['logits'], w=['gmax'])
            kb.op('dve', lambda e: e.tensor_tensor(out=ohg[:], in0=lgG, in1=b1(gmax[:, 0:NT], 4), op=ALU.is_equal), r=['logits', 'gmax'], w=['ohg'])
            kb.op('dve', lambda e: e.tensor_tensor(out=eg[:], in0=lgG, in1=b1(gmax[:, 0:NT], 4), op=ALU.subtract), r=['logits', 'gmax'], w=['eg'])
            kb.op('act', lambda e: e.activation(out=eg[:], in_=eg[:], func=AF.Exp), r=['eg'], w=['eg'])
            kb.op('dve', lambda e: e.reduce_sum(out=pg[:], in_=eg[:], axis=AX.X), r=['eg'], w=['pg'])
            kb.op('dve', lambda e: e.reciprocal(out=pg[:], in_=pg[:]), r=['pg'], w=['pg'])
            kb.op('dve', lambda e: e.tensor_scalar(out=ohg[:], in0=ohg[:], scalar1=-1.0, scalar2=1e30, op0=ALU.add, op1=ALU.mult), r=['ohg'], w=['ohg'])
            kb.op('dve', lambda e: e.tensor_tensor(out=me[:].rearrange("p t (g x) -> p t g x", g=4), in0=lgE.rearrange("p t (g x) -> p t g x", g=4),
                                                   in1=bc(ohg[:, 0:NT, :], [[4, NT], [1, 4], [0, 8]]), op=ALU.add), r=['logits', 'ohg'], w=['me'])
            kb.op('dve', lambda e: e.reduce_max(out=m1[:], in_=me[:], axis=AX.X), r=['me'], w=['m1'])
            kb.op('dve', lambda e: e.tensor_tensor(out=oh1[:], in0=me[:], in1=b1(m1[:, 0:NT], 32), op=ALU.is_equal), r=['me', 'm1'], w=['oh1'])
            kb.op('dve', lambda e: e.scalar_tensor_tensor(out=me[:], in0=oh1[:], scalar=-1e30, in1=me[:], op0=ALU.mult, op1=ALU.add), r=['oh1', 'me'], w=['me'])
            kb.op('dve', lambda e: e.reduce_max(out=m2_[:], in_=me[:], axis=AX.X), r=['me'], w=['m2'])
            kb.op('dve', lambda e: e.tensor_tensor(out=oh2[:], in0=me[:], in1=b1(m2_[:, 0:NT], 32), op=ALU.is_equal), r=['me', 'm2'], w=['oh2'])
            kb.op('dve', lambda e: e.tensor_tensor(out=w1[:], in0=m1[:], in1=m2_[:], op=ALU.subtract), r=['m1', 'm2'], w=['w1'])
            kb.op('act', lambda e: e.activation(out=w2[:], in_=w1[:], func=AF.Sigmoid, scale=-1.0), r=['w1'], w=['w2'])
            kb.op('act', lambda e: e.activation(out=w1[:], in_=w1[:], func=AF.Sigmoid), r=['w1'], w=['w1'])
            kb.op('dve', lambda e: e.tensor_tensor(out=w1[:], in0=w1[:], in1=pg[:], op=ALU.mult), r=['w1', 'pg'], w=['w1'])
            kb.op('dve', lambda e: e.tensor_tensor(out=w2[:], in0=w2[:], in1=pg[:], op=ALU.mult), r=['w2', 'pg'], w=['w2'])
            kb.op('dve', lambda e: e.tensor_tensor(out=oh1[:], in0=oh1[:], in1=b1(w1[:, 0:NT], 32), op=ALU.mult), r=['oh1', 'w1'], w=['oh1'])
            kb.op('dve', lambda e: e.tensor_tensor(out=oh2[:], in0=oh2[:], in1=b1(w2[:, 0:NT], 32), op=ALU.mult), r=['oh2', 'w2'], w=['oh2'])
            kb.op('dve', lambda e: e.tensor_tensor(out=Wt[:], in0=oh1[:], in1=oh2[:], op=ALU.add), r=['oh1', 'oh2'], w=['Wt'])
            kb.release(m2)
            if os.environ.get('MOE_CUT') == '2':
                kb.release(m); return
            acc = kb.sb('acc', [128, NT, D], F32)
            m3 = kb.mark()
            wg = [kb.sb(f'ewg{i}', [128, 8, 512], BF16) for i in range(2)]
            wu = [kb.sb(f'ewu{i}', [128, 8, 512], BF16) for i in range(2)]
            wd = [kb.sb(f'ewd{i}', [128, 4, D], BF16) for i in range(2)]
            aT = [kb.sb(f'aT{i}', [128, 4, 512], BF16) for i in range(2)]
            sgt = [kb.sb(f'sgt{i}', [128, 512], F32) for i in range(2)]
            it = 0; ih = 0
            for ex in range(int(os.environ.get('MOE_NEXP', '32'))):
                j = ex % 2
                load_w(wg[j], ('ewg', j), w_eg[l, ex], 8, 512, engs=('pool', 'act'))
                load_w(wu[j], ('ewu', j), w_eu[l, ex], 8, 512, engs=('pool', 'act'))
                load_w(wd[j], ('ewd', j), w_ed[l, ex], 4, D, engs=('pool', 'act'))
                for (s0, n) in blocks:
                    i = it % 2; it += 1
                    for hc in range(4):
                        ii = ih % 2; ih += 1
                        for k in range(8):
                            kb.op('pe', lambda e: e.matmul(PSA[:, ii, 0:n], lhsT=wg[j][:, k, hc * 128:(hc + 1) * 128], rhs=hT[:, k, s0:s0 + n],
                                                           start=(k == 0), stop=(k == 7)), r=[('ewg', j), 'hT'], w=[('ps', ii)])
                        for k in range(8):
                            kb.op('pe', lambda e: e.matmul(PSA[:, 2 + ii, 0:n], lhsT=wu[j][:, k, hc * 128:(hc + 1) * 128], rhs=hT[:, k, s0:s0 + n],
                                                           start=(k == 0), stop=(k == 7)), r=[('ewu', j), 'hT'], w=[('ps', 2 + ii)])
                        kb.op('act', lambda e: e.activation(out=sgt[ii][:, 0:n], in_=PSA[:, ii, 0:n], func=AF.Silu), r=[('ps', ii)], w=[('sgt', ii)])
                        kb.op('dve', lambda e: e.tensor_tensor(out=aT[i][:, hc, 0:n], in0=sgt[ii][:, 0:n], in1=PSA[:, 2 + ii, 0:n], op=ALU.mult),
                              r=[('sgt', ii), ('ps', 2 + ii)], w=[('aT', i)])
                    for t in range(s0 // 128, (s0 + n) // 128):
                        tl = t * 128 - s0
                        for hf in range(2):
                            bank = 4 + (2 * t + hf) % 3
                            for hc in range(4):
                                kb.op('pe', lambda e: e.matmul(PSA[:, bank, :], lhsT=aT[i][:, hc, tl:tl + 128], rhs=wd[j][:, hc, hf * 512:(hf + 1) * 512],
                                                               start=(hc == 0), stop=(hc == 3)), r=[('aT', i), ('ewd', j)], w=[('ps', bank)])
                            a_ = acc[:, t, hf * 512:(hf + 1) * 512]
                            if ex == 0:
                                kb.op('dve', lambda e: e.tensor_scalar(out=a_, in0=PSA[:, bank, :], scalar1=Wt[:, t, ex:ex + 1], scalar2=None, op0=ALU.mult),
                                      r=[('ps', bank), 'Wt'], w=[('acc', t)])
                            else:
                                kb.op('dve', lambda e: e.scalar_tensor_tensor(out=a_, in0=PSA[:, bank, :], scalar=Wt[:, t, ex:ex + 1], in1=a_,
                                                                              op0=ALU.mult, op1=ALU.add), r=[('ps', bank), 'Wt', ('acc', t)], w=[('acc', t)])
            kb.release(m3)
            g2 = []
            for row in (0, 1):
                g_ = kb.sb(f'g2_{row}', [128, D], F32)
                kb.dma('sp', g_[:], modbuf[l, row, 5 * D:6 * D].partition_broadcast(128), w=[f'g2_{row}'])
                g2.append(g_)
            xt = [kb.sb(f'mxt{i}', [128, D], F32) for i in range(2)]
            for t in tiles:
                i = t % 2; row = 0 if t < 2 else 1
                kb.dma('sp', xt[i][:], xbuf[t * 128:(t + 1) * 128, :], r=[('x', t)], w=[('mxt', i)])
                kb.op('dve', lambda e: e.tensor_tensor(out=acc[:, t, :], in0=acc[:, t, :], in1=g2[row][:], op=ALU.mult), r=[('acc', t), f'g2_{row}'], w=[('acc', t)])
                kb.op('pool', lambda e: e.tensor_tensor(out=xt[i][:], in0=xt[i][:], in1=acc[:, t, :], op=ALU.add), r=[('acc', t), ('mxt', i)], w=[('mxt', i)])
                kb.dma('sp', xbuf[t * 128:(t + 1) * 128, :], xt[i][:], r=[('mxt', i)], w=[('x', t)])
            kb.release(m)

        moe_state = {}

        def phase_moe_sparse(l):
            IOA = bass.IndirectOffsetOnAxis
            ABv = AB.rearrange("r (h c) -> (r h) c", h=2)
            if 'bregs' not in moe_state:
                regs = {}
                for nm_, v_ in (('rec', 32 * CAP - 1), ('tok', T - 1), ('ab', 2 * T - 1)):
                    rg = nc.gpsimd.alloc_register('bnd_' + nm_)
                    nc.gpsimd.reg_mov(rg, v_)
                    regs[nm_] = rg
                moe_state['bregs'] = regs
            BR = moe_state['bregs']
            m = kb.mark()
            t0 = 0 if l == 0 else 2
            tiles = list(range(t0, NT))
            logits = kb.sb('logits', [128, NT, 36], F32)
            kb.op('pool', lambda e: e.memset(logits[:], 0.0), w=['logits'])
            zt = kb.sb('zt', [128, D], F32)
            kb.op('pool', lambda e: e.memset(zt[:], 0.0), w=['zt'])
            for q in range(2 * NT):
                kb.dma('act', AB[q * 128:(q + 1) * 128, :], zt[:], r=['zt'], w=[('ABz', q)])
            ri_ = kb.sb('recinit', [128, (32 * CAP) // 128, 4], F32)
            kb.op('pool', lambda e: e.memset(ri_[:], 1.0e6), w=['recinit'])
            kb.op('pool', lambda e: e.memset(ri_[:, :, 2:3], 0.0), r=['recinit'], w=['recinit'])
            kb.dma('act', rec_d.rearrange("(p s) c -> p s c", p=128), ri_[:], r=['recinit'], w=['rec_d'])
            phase_norm(l, 2, logits, sparse=True)
            MC = kb.sb('MC', [128, 32 + NT], F32)
            kb.dma('sp', MC[:], mconst[:, :], w=['MC'])
            eC = MC[:, 0:32]; tokid = MC[:, 32:32 + NT]
            lgG = logits[:, :, 0:4]; lgE = logits[:, :, 4:36]
            gmax = kb.sb('gmax', [128, NT], F32); ohg = kb.sb('ohg', [128, NT, 4], F32); eg = kb.sb('eg', [128, NT, 4], F32)
            pg = kb.sb('pg', [128, NT], F32); me = kb.sb('me', [128, NT, 32], F32); oh1 = kb.sb('oh1', [128, NT, 32], F32)
            oh2 = kb.sb('oh2', [128, NT, 32], F32); m1 = kb.sb('m1', [128, NT], F32); m2_ = kb.sb('m2', [128, NT], F32)
            w1 = kb.sb('w1', [128, NT], F32); w2 = kb.sb('w2', [128, NT], F32)
            b1 = lambda a, n_: bc(a, [[1, NT], [0, n_]])
            kb.op('dve', lambda e: e.reduce_max(out=gmax[:], in_=lgG, axis=AX.X), r=['logits'], w=['gmax'])
            kb.op('dve', lambda e: e.tensor_tensor(out=ohg[:], in0=lgG, in1=b1(gmax[:, 0:NT], 4), op=ALU.is_equal), r=['logits', 'gmax'], w=['ohg'])
            kb.op('dve', lambda e: e.tensor_tensor(out=eg[:], in0=lgG, in1=b1(gmax[:, 0:NT], 4), op=ALU.subtract), r=['logits', 'gmax'], w=['eg'])
            kb.op('act', lambda e: e.activation(out=eg[:], in_=eg[:], func=AF.Exp), r=['eg'], w=['eg'])
            kb.op('dve', lambda e: e.reduce_sum(out=pg[:], in_=eg[:], axis=AX.X), r=['eg'], w=['pg'])
            kb.op('dve', lambda e: e.reciprocal(out=pg[:], in_=pg[:]), r=['pg'], w=['pg'])
            kb.op('dve', lambda e: e.tensor_scalar(out=ohg[:], in0=ohg[:], scalar1=-1.0, scalar2=1e30, op0=ALU.add, op1=ALU.mult), r=['ohg'], w=['ohg'])
            kb.op('dve', lambda e: e.tensor_tensor(out=me[:].rearrange("p t (g x) -> p t g x", g=4), in0=lgE.rearrange("p t (g x) -> p t g x", g=4),
                                                   in1=bc(ohg[:, 0:NT, :], [[4, NT], [1, 4], [0, 8]]), op=ALU.add), r=['logits', 'ohg'], w=['me'])
            kb.op('dve', lambda e: e.reduce_max(out=m1[:], in_=me[:], axis=AX.X), r=['me'], w=['m1'])
            kb.op('dve', lambda e: e.tensor_tensor(out=oh1[:], in0=me[:], in1=b1(m1[:, 0:NT], 32), op=ALU.is_equal), r=['me', 'm1'], w=['oh1'])
            kb.op('dve', lambda e: e.scalar_tensor_tensor(out=me[:], in0=oh1[:], scalar=-1e30, in1=me[:], op0=ALU.mult, op1=ALU.add), r=['oh1', 'me'], w=['me'])
            kb.op('dve', lambda e: e.reduce_max(out=m2_[:], in_=me[:], axis=AX.X), r=['me'], w=['m2'])
            kb.op('dve', lambda e: e.tensor_tensor(out=oh2[:], in0=me[:], in1=b1(m2_[:, 0:NT], 32), op=ALU.is_equal), r=['me', 'm2'], w=['oh2'])
            kb.op('dve', lambda e: e.tensor_tensor(out=w1[:], in0=m1[:], in1=m2_[:], op=ALU.subtract), r=['m1', 'm2'], w=['w1'])
            kb.op('act', lambda e: e.activation(out=w2[:], in_=w1[:], func=AF.Sigmoid, scale=-1.0), r=['w1'], w=['w2'])
            kb.op('act', lambda e: e.activation(out=w1[:], in_=w1[:], func=AF.Sigmoid), r=['w1'], w=['w1'])
            kb.op('dve', lambda e: e.tensor_tensor(out=w1[:], in0=w1[:], in1=pg[:], op=ALU.mult), r=['w1', 'pg'], w=['w1'])
            kb.op('dve', lambda e: e.tensor_tensor(out=w2[:], in0=w2[:], in1=pg[:], op=ALU.mult), r=['w2', 'pg'], w=['w2'])
            if t0 > 0:
                kb.op('pool', lambda e: e.memset(oh1[:, 0:t0, :], 0.0), r=['oh1'], w=['oh1'])
                kb.op('pool', lambda e: e.memset(oh2[:, 0:t0, :], 0.0), r=['oh2'], w=['oh2'])
            selb = kb.sb('selb', [128, NT * 32], BF16)
            kb.op('pool', lambda e: e.tensor_tensor(out=selb[:], in0=oh1[:].rearrange("p t e -> p (t e)"), in1=oh2[:].rearrange("p t e -> p (t e)"), op=ALU.add),
                  r=['oh1', 'oh2'], w=['selb'])
            LT = kb.sb('LT', [128, 128], BF16); ones = kb.sb('ones', [128, 128], BF16)
            kb.op('pool', lambda e: e.memset(ones[:], 1.0), w=['ones'])
            kb.op('pool', lambda e: e.memset(LT[:], 1.0), w=['LT'])
            kb.op('pool', lambda e: e.affine_select(out=LT[:], in_=LT[:], pattern=[[1, 128]], compare_op=ALU.is_gt, fill=0.0, base=0,
                                                    channel_multiplier=-1), r=['LT'], w=['LT'])
            slot = kb.sb('slot', [128, NT, 32], F32); tot = kb.sb('tot', [128, NT, 32], F32); cum = kb.sb('cum', [128, NT, 32], F32)
            sl2 = slot[:].rearrange("p t e -> p (t e)"); to2 = tot[:].rearrange("p t e -> p (t e)")
            for (c0, c1, bank) in ((0, 512, 0), (512, NT * 32, 1)):
                kb.op('pe', lambda e: e.matmul(PSA[:, bank, 0:c1 - c0], lhsT=LT[:], rhs=selb[:, c0:c1], start=True, stop=True), r=['LT', 'selb'], w=[('ps', bank)])
                kb.op('dve', lambda e: e.tensor_copy(out=sl2[:, c0:c1], in_=PSA[:, bank, 0:c1 - c0]), r=[('ps', bank)], w=['slot'])
                kb.op('pe', lambda e: e.matmul(PSA[:, 2 + bank, 0:c1 - c0], lhsT=ones[:], rhs=selb[:, c0:c1], start=True, stop=True), r=['ones', 'selb'], w=[('ps', 2 + bank)])
                kb.op('dve', lambda e: e.tensor_copy(out=to2[:, c0:c1], in_=PSA[:, 2 + bank, 0:c1 - c0]), r=[('ps', 2 + bank)], w=['tot'])
            kb.op('pool', lambda e: e.memset(cum[:, 0, :], 0.0), w=['cum'])
            for t in range(1, NT):
                kb.op('dve', lambda e: e.tensor_tensor(out=cum[:, t, :], in0=cum[:, t - 1, :], in1=tot[:, t - 1, :], op=ALU.add), r=['cum', 'tot'], w=['cum'])
            kb.op('dve', lambda e: e.tensor_tensor(out=slot[:], in0=slot[:], in1=cum[:], op=ALU.add), r=['slot', 'cum'], w=['slot'])
            rec = kb.sb('rec', [128, NT, 2, 4], F32)
            rr = kb.sb('rr', [128, NT, 2], F32); rri = kb.sb('rri', [128, NT, 2], I32)
            s_ = kb.sb('s_', [128, NT], F32); e_ = kb.sb('e_', [128, NT], F32)
            kb.op('pool', lambda e: e.memset(rec[:], 0.0), w=['rec'])
            for k, (oh, wk) in enumerate(((oh1, w1), (oh2, w2))):
                kb.op('dve', lambda e: e.tensor_tensor(out=me[:], in0=oh[:], in1=slot[:], op=ALU.mult), r=['oh1', 'oh2', 'slot', 'me'], w=['me'])
                kb.op('dve', lambda e: e.reduce_sum(out=s_[:], in_=me[:], axis=AX.X), r=['me'], w=['s_'])
                kb.op('dve', lambda e: e.tensor_tensor(out=me[:], in0=oh[:], in1=bc(eC, [[0, NT], [1, 32]]), op=ALU.mult), r=['oh1', 'oh2', 'MC', 'me'], w=['me'])
                kb.op('dve', lambda e: e.reduce_sum(out=e_[:], in_=me[:], axis=AX.X), r=['me'], w=['e_'])
                kb.op('dve', lambda e: e.tensor_tensor(out=e_[:], in0=e_[:], in1=s_[:], op=ALU.add), r=['e_', 's_'], w=['e_'])
                kb.op('dve', lambda e: e.tensor_scalar(out=s_[:], in0=s_[:], scalar1=float(CAP), scalar2=1.0e6, op0=ALU.is_ge, op1=ALU.mult), r=['s_'], w=['s_'])
                kb.op('dve', lambda e: e.tensor_tensor(out=rr[:, :, k], in0=e_[:], in1=s_[:], op=ALU.add), r=['e_', 's_'], w=['rr'])
                kb.op('pool', lambda e: e.tensor_copy(out=rec[:, :, k, 0], in_=tokid), r=['MC', 'rec'], w=['rec'])
                kb.op('pool', lambda e: e.tensor_scalar(out=rec[:, :, k, 1], in0=tokid, scalar1=float(k * T), scalar2=None, op0=ALU.add), r=['MC', 'rec'], w=['rec'])
                kb.op('pool', lambda e: e.tensor_scalar(out=rec[:, :, k, 3], in0=tokid, scalar1=2.0, scalar2=float(2 * k * T + 1), op0=ALU.mult, op1=ALU.add), r=['MC', 'rec'], w=['rec'])
                kb.op('pool', lambda e: e.tensor_copy(out=rec[:, :, k, 2], in_=wk[:]), r=['w1', 'w2', 'rec'], w=['rec'])
            kb.op('dve', lambda e: e.tensor_copy(out=rri[:], in_=rr[:]), r=['rr'], w=['rri'])
            if stop == f'moe{l}' and os.environ.get('XDUMP'):
                xdump('rr', rr, [128, NT, 2], F32); xdump('rri', rri, [128, NT, 2], I32); xdump('rec', rec, [128, NT, 2, 4], F32)
                xdump('slot', slot, [128, NT, 32], F32)
            kb.barrier()
            for t in tiles:
                for k in range(2):
                    kb.dma('pool', None, None, r=['rec', 'rri'], w=[('recs', t, k)],
                           fn=lambda g: g.indirect_dma_start(out=rec_d[:, :], out_offset=IOA(ap=rri[:, t, k:k + 1], axis=0), in_=rec[:, t, k, :],
                                                             in_offset=None, bounds_check=BR['rec'], oob_is_err=False))
            kb.barrier()
            if os.environ.get('MOE_CUT') == '3':
                kb.release(m); return
            m3 = kb.mark()
            stg_all[0] = list(stg) + [kb.alias(f'stgx{i}', [128, 2048], F32, kb.off['hT'] + i * 8192) for i in range(4)]
            wg = [kb.sb(f'ewg{i}', [128, 8, 512], BF16) for i in range(2)]
            wu = [kb.sb(f'ewu{i}', [128, 8, 512], BF16) for i in range(2)]
            wd = [kb.sb(f'ewd{i}', [128, 4, D], BF16) for i in range(2)]
            XT = [kb.sb(f'XT{i}', [128, 8, CAP], BF16) for i in range(2)]
            aT = kb.sb('aT', [128, 4, CAP], BF16)
            sgt = [kb.sb(f'sgt{i}', [128, 512], F32) for i in range(2)]
            rsb = [kb.sb(f'rsb{i}', [128, NS, 4], F32) for i in range(2)]
            gi = [kb.sb(f'gi{i}', [128, NS, 4], I32) for i in range(2)]
            xg = [kb.sb(f'xg{i}', [128, D], BF16) for i in range(NS)]
            yw = [kb.sb(f'yw{i}', [128, D], F32) for i in range(3)]
            for i in range(NS):
                kb.op('pool', lambda e: e.memset(xg[i][:], 0.0), w=[('xg', i)])
            PST2 = PSA[:, 6, :].bitcast(BF16).rearrange("p (k c) -> p k c", c=128)
            stgs = stg_all[0]
            nexp = int(os.environ.get('MOE_NEXP', '32'))
            cnt = {'iy': 0, 'ih': 0}

            def w_issue(ex):
                j = ex % 2; pieces = []
                for (dst, dkey, src, K, W) in ((wg[j], ('ewg', j), w_eg[l, ex], 8, 512), (wu[j], ('ewu', j), w_eu[l, ex], 8, 512), (wd[j], ('ewd', j), w_ed[l, ex], 4, D)):
                    kk = 2048 // W
                    for k0 in range(0, K, kk):
                        i = len(pieces)
                        sv = stgs[i][:, 0:kk * W].rearrange("p (k c) -> p k c", c=W)
                        kb.dma('sp', sv, src[k0 * 128:(k0 + kk) * 128, :].rearrange("(k p) c -> p k c", p=128), w=[('stg', i)])
                        pieces.append((dst, dkey, k0, k0 + kk, sv, i))
                return pieces

            def w_cast(pieces):
                for n_, (dst, dkey, k0, k1, sv, i) in enumerate(pieces):
                    if n_ % 2 == 0:
                        kb.op('act', lambda e: e.copy(out=dst[:, k0:k1, :], in_=sv), r=[('stg', i)], w=[dkey])
                    else:
                        kb.op('dve', lambda e: e.tensor_copy(out=dst[:, k0:k1, :], in_=sv), r=[('stg', i)], w=[dkey])

            def G(ex):
                j = ex % 2
                kb.dma('sp', rsb[j][:], rec_d[ex * CAP:(ex + 1) * CAP, :].rearrange("(s p) c -> p s c", p=128), w=[('rsb', j)])
                kb.op('dve', lambda e: e.tensor_copy(out=gi[j][:], in_=rsb[j][:]), r=[('rsb', j)], w=[('gi', j)])
                for s_i in range(NS):
                    kb.dma('pool', None, None, r=[('gi', j)], w=[('xg', s_i)],
                           fn=lambda g: g.indirect_dma_start(out=xg[s_i][:], out_offset=None, in_=h2d[:, :],
                                                             in_offset=IOA(ap=gi[j][:, s_i, 0:1], axis=0), bounds_check=BR['tok'], oob_is_err=False))

            def TR(ex):
                j = ex % 2
                for s_i in range(NS):
                    pt_, pk_ = (PST, 'pst') if s_i % 2 == 0 else (PST2, ('ps', 6))
                    for k in range(8):
                        kb.op('pe', lambda e: e.transpose(out=pt_[:, k, :], in_=xg[s_i][:, k * 128:(k + 1) * 128], identity=ident[:]),
                              r=[('xg', s_i), 'ident'], w=[pk_])
                    kb.op('act', lambda e: e.copy(out=XT[j][:, :, s_i * 128:(s_i + 1) * 128], in_=pt_[:]), r=[pk_], w=[('XT', j)])

            def F1(ex):
                j = ex % 2
                for c0 in range(0, CAP, 512):
                    n = min(512, CAP - c0)
                    for hc in range(4):
                        ii = cnt['ih'] % 2; cnt['ih'] += 1
                        for k in range(8):
                            kb.op('pe', lambda e: e.matmul(PSA[:, ii, 0:n], lhsT=wg[j][:, k, hc * 128:(hc + 1) * 128], rhs=XT[j][:, k, c0:c0 + n],
                                                           start=(k == 0), stop=(k == 7)), r=[('ewg', j), ('XT', j)], w=[('ps', ii)])
                        for k in range(8):
                            kb.op('pe', lambda e: e.matmul(PSA[:, 2 + ii, 0:n], lhsT=wu[j][:, k, hc * 128:(hc + 1) * 128], rhs=XT[j][:, k, c0:c0 + n],
                                                           start=(k == 0), stop=(k == 7)), r=[('ewu', j), ('XT', j)], w=[('ps', 2 + ii)])
                        kb.op('act', lambda e: e.activation(out=sgt[ii][:, 0:n], in_=PSA[:, ii, 0:n], func=AF.Silu), r=[('ps', ii)], w=[('sgt', ii)])
                        kb.op('dve', lambda e: e.tensor_tensor(out=aT[:, hc, c0:c0 + n], in0=sgt[ii][:, 0:n], in1=PSA[:, 2 + ii, 0:n], op=ALU.mult),
                              r=[('sgt', ii), ('ps', 2 + ii)], w=['aT'])

            def F2(ex):
                j = ex % 2
                for s_i in range(NS):
                    q = cnt['iy'] % 3; cnt['iy'] += 1
                    for hf in range(2):
                        bank = 4 + hf
                        for hc in range(4):
                            kb.op('pe', lambda e: e.matmul(PSA[:, bank, :], lhsT=aT[:, hc, s_i * 128:(s_i + 1) * 128], rhs=wd[j][:, hc, hf * 512:(hf + 1) * 512],
                                                           start=(hc == 0), stop=(hc == 3)), r=['aT', ('ewd', j)], w=[('ps', bank)])
                        kb.op('dve', lambda e: e.tensor_scalar(out=yw[q][:, hf * 512:(hf + 1) * 512], in0=PSA[:, bank, :], scalar1=rsb[j][:, s_i, 2:3], scalar2=None,
                                                               op0=ALU.mult), r=[('ps', bank), ('rsb', j)], w=[('yw', q)])
                    kb.dma('pool', None, None, r=[('yw', q), ('gi', j)], w=[('ABs', ex, s_i)],
                           fn=lambda g: g.indirect_dma_start(out=AB[:, :], out_offset=IOA(ap=gi[j][:, s_i, 1:2], axis=0),
                                                             in_=yw[q][:], in_offset=None, bounds_check=BR['ab'], oob_is_err=False))

            w_cast(w_issue(0)); G(0); TR(0)
            for ex in range(nexp):
                nxt = ex + 1 < nexp
                if nxt:
                    pcs = w_issue(ex + 1)
                    G(ex + 1)
                F1(ex)
                if nxt:
                    w_cast(pcs)
                    TR(ex + 1)
                F2(ex)
            stg_all[0] = list(stg)
            stg_i[0] = 0
            kb.release(m3)
            g2 = []
            for row in (0, 1):
                g_ = kb.sb(f'g2_{row}', [128, D], F32)
                kb.dma('sp', g_[:], modbuf[l, row, 5 * D:6 * D].partition_broadcast(128), w=[f'g2_{row}'])
                g2.append(g_)
            xt = [kb.sb(f'mxt{i}', [128, D], F32) for i in range(2)]
            ya = [kb.sb(f'mya{i}', [128, D], F32) for i in range(2)]
            yb = [kb.sb(f'myb{i}', [128, D], F32) for i in range(2)]
            for t in tiles:
                i = t % 2; row = 0 if t < 2 else 1
                kb.dma('sp', xt[i][:], xbuf[t * 128:(t + 1) * 128, :], r=[('x', t)], w=[('mxt', i)])
                kb.dma('sp', ya[i][:], AB[t * 128:(t + 1) * 128, :], w=[('mya', i)])
                kb.dma('sp', yb[i][:], AB[T + t * 128:T + (t + 1) * 128, :], w=[('myb', i)])
                kb.op('pool', lambda e: e.tensor_tensor(out=ya[i][:], in0=ya[i][:], in1=yb[i][:], op=ALU.add), r=[('mya', i), ('myb', i)], w=[('mya', i)])
                kb.op('dve', lambda e: e.tensor_tensor(out=ya[i][:], in0=ya[i][:], in1=g2[row][:], op=ALU.mult), r=[('mya', i), f'g2_{row}'], w=[('mya', i)])
                kb.op('pool', lambda e: e.tensor_tensor(out=xt[i][:], in0=xt[i][:], in1=ya[i][:], op=ALU.add), r=[('mya', i), ('mxt', i)], w=[('mxt', i)])
                kb.dma('sp', xbuf[t * 128:(t + 1) * 128, :], xt[i][:], r=[('mxt', i)], w=[('x', t)])
            kb.release(m)

        def phase_final():
            m = kb.mark()
            nwb = kb.sb('fnw', [128, D], F32)
            kb.dma('sp', nwb[:], norm_final.partition_broadcast(128), w=['fnw'])
            xt = [kb.sb(f'fxt{i}', [128, D], F32) for i in range(2)]
            yt = [kb.sb(f'fyt{i}', [128, D], F32) for i in range(2)]
            ssq = [kb.sb(f'fss{i}', [128, 1], F32) for i in range(2)]
            junk = kb.sb('fjunk', [128, D], F32)
            for t in range(2, NT):
                i = t % 2
                kb.dma('sp', xt[i][:], xbuf[t * 128:(t + 1) * 128, :], r=[('x', t)], w=[('fxt', i)])
                kb.op('act', lambda e: e.activation(out=junk[:], in_=xt[i][:], func=AF.Square, accum_out=ssq[i][:]), r=[('fxt', i)], w=['fjunk', ('fss', i)])
                kb.op('act', lambda e: e.activation(out=ssq[i][:], in_=ssq[i][:], func=AF.Sqrt, scale=1.0 / D, bias=epsN), r=[('fss', i)], w=[('fss', i)])
                kb.op('dve', lambda e: e.reciprocal(out=ssq[i][:], in_=ssq[i][:]), r=[('fss', i)], w=[('fss', i)])
                kb.op('dve', lambda e: e.scalar_tensor_tensor(out=yt[i][:], in0=xt[i][:], scalar=ssq[i][:, 0:1], in1=nwb[:], op0=ALU.mult, op1=ALU.mult),
                      r=[('fxt', i), ('fss', i), 'fnw'], w=[('fyt', i)])
                kb.dma('sp', out[(t - 2) * 128:(t - 1) * 128, :], yt[i][:], r=[('fyt', i)], w=[('out', t)])
            kb.release(m)

        def finish_dbg(hT_=True, oT=None, mT=None):
            kb.barrier()
            for t in range(NT):
                pass
            kb.dma('sp', dbg['d_x'], xbuf[:, :], w=['d_x'])
            if hT_:
                kb.dma('sp', dbg['d_hT'], hT[:], w=['d_hT'])
            if oT is not None:
                kb.dma('sp', dbg['d_oT'], oT[:], w=['d_oT'])
            if mT is not None:
                kb.dma('sp', dbg['d_mT'], mT[:], w=['d_mT'])
            kb.finish()
            return nc

        kb.dma('sp', xbuf[0:L, :], ctx_in[:, :], w=['xinit0'])
        kb.dma('sp', xbuf[L:T, :], x_in[:, :], w=['xinit1'])
        for l in range(nlayers):
            phase_ada(l)
        kb.barrier()
        if stop == 'ada':
            kb.dma('sp', dbg['d_mod'], modbuf[0], w=['d_mod'])
            return finish_dbg()
        for l in range(nlayers):
            phase_norm(l, 1)
            if stop == f'norm{l}':
                return finish_dbg()
            mm = kb.mark()
            oT = kb.sb('oT', [128, 4, T], BF16, top=True)
            phase_ret(l, oT)
            if stop == f'ret{l}':
                return finish_dbg(oT=oT)
            mT = kb.sb('mT', [128, 8, T], BF16)
            merge(l, 2048, w_br, oT, 'retT', mT, True)
            phase_attn(l, oT)
            if stop == f'attn{l}':
                return finish_dbg(oT=oT)
            merge(l, 0, w_ba, oT, 'oaT', mT, False)
            phase_four(l, oT)
            if stop == f'four{l}':
                return finish_dbg(oT=oT)
            merge(l, 1024, w_bf, oT, 'ofT', mT, False)
            if stop == f'merge{l}':
                return finish_dbg(mT=mT)
            phase_out(l, mT)
            kb.release(mm)
            if stop == f'mix{l}':
                return finish_dbg()
            if os.environ.get('MOE_DENSE'):
                phase_moe(l)
            else:
                phase_moe_sparse(l)
            if stop == f'moe{l}':
                return finish_dbg()
        phase_final()
        kb.finish()
    print("SBUF peak bytes", kb.peak, "instr counts", kb.cnt)
    return nc


def _consts():
    bf = ml_dtypes.bfloat16
    f32 = np.float32
    n = np.arange(N)
    inv16 = (10000.0 ** (-(np.arange(16, dtype=f32)) / f32(16))).astype(f32)
    row = (n // 64).astype(f32); col = (n % 64).astype(f32)
    ropeA = np.zeros((2, 128, N), f32)
    for p in range(128):
        d = p % 64
        pos = row if d < 32 else col
        dd = d % 32
        ang = (pos * inv16[dd % 16]).astype(f32)
        ropeA[0, p] = np.cos(ang); ropeA[1, p] = np.sin(ang) * (-1.0 if dd < 16 else 1.0)
    inv32 = (10000.0 ** (-(np.arange(32, dtype=f32)) / f32(32))).astype(f32)
    ropeR = np.zeros((2, 128, N), f32)
    for p in range(128):
        d = p % 64
        ang = (n.astype(f32) * inv32[d % 32]).astype(f32)
        ropeR[0, p] = np.cos(ang); ropeR[1, p] = np.sin(ang) * (-1.0 if d < 32 else 1.0)
    k = np.arange(N, dtype=np.int64)
    ph = (np.outer(k, k) % N).astype(np.float64) * (2 * np.pi / N)
    dftN = np.stack([np.cos(ph), -np.sin(ph)]) / np.sqrt(N)
    k2 = np.arange(256, dtype=np.int64)
    ph2 = (np.outer(k2, k2) % 256).astype(np.float64) * (2 * np.pi / 256)
    dft256 = np.stack([np.cos(ph2), -np.sin(ph2)]) / np.sqrt(256)
    k3 = np.arange(128, dtype=np.int64)
    ph3 = (np.outer(k3, k3) % 128).astype(np.float64) * (2 * np.pi / 128)
    dftC = np.concatenate([np.cos(ph3), np.sin(ph3)], axis=1) / np.sqrt(128)
    b = np.arange(128)[:, None]; a = np.arange(128)[None, :]
    amask = np.concatenate([(b >= a), (b <= a)], axis=1).astype(f32)
    j = b; i = a
    rconst = np.concatenate([np.maximum(i - j, 0), np.maximum(j - i, 0), (i >= j), (j > i), (i + 1) + 0 * j, (128 - i) + 0 * j,
                             127 - np.arange(128)[:, None], np.arange(128)[:, None]], axis=1).astype(f32)
    mconst = np.concatenate([np.tile((np.arange(32) * CAP)[None, :], (128, 1)), np.arange(128)[:, None] + 128 * np.arange(NT)[None, :]], axis=1).astype(f32)
    return dict(mconst=mconst, ropeA=ropeA, ropeR=ropeR, dftN=dftN.astype(bf), dft256=dft256.astype(bf), dftC=dftC.astype(bf),
                amask=amask.astype(bf), rconst=rconst)


def _wext_index():
    idx = []
    swa = np.array([d + 16 if (d % 32) < 16 else d - 16 for d in range(64)])
    swr = np.array([d + 32 if d < 32 else d - 32 for d in range(64)])
    base = 0
    qa = [np.concatenate([np.arange(g * 64, (g + 1) * 64), np.arange((4 + g) * 64, (5 + g) * 64)]) for g in range(4)]
    qas = [np.concatenate([g * 64 + swa, (4 + g) * 64 + swa]) for g in range(4)]
    idx += qa + qas
    idx += [512 + np.arange(128), 512 + np.concatenate([swa, 64 + swa])]
    idx += [640 + np.arange(128)]
    idx += [768 + np.arange(512), 768 + np.concatenate([h * 64 + swr for h in range(8)])]
    idx += [1280 + np.arange(512), 1280 + np.concatenate([h * 64 + swr for h in range(8)])]
    idx += [1792 + np.arange(512), 2304 + np.arange(512), 2816 + np.arange(512), 3328 + np.arange(3072)]
    idx = np.concatenate(idx)
    assert idx.shape[0] == WEXT
    return idx


_CACHE = {}


def kernel(x, c, ctx, c_ctx, norm_mix, norm_ffn, w_ada, b_ada, w_in, attn_sink, ret_decay_fwd, ret_decay_bwd,
           w_branch_attn, w_branch_fourier, w_branch_ret, w_out, w_router_group, b_router_group,
           w_router_expert, b_router_expert, w_exp_gate, w_exp_up, w_exp_down, norm_final):
    f = lambda a: np.ascontiguousarray(np.asarray(a, dtype=np.float32))
    if 'nc' not in _CACHE:
        _CACHE['nc'] = build()
        _CACHE['consts'] = _consts()
        _CACHE['idx'] = _wext_index()
    nc = _CACHE['nc']
    shared = dict(_CACHE['consts'])
    w_in = f(w_in)
    shared.update(
        c_ctx=f(c_ctx), norm_mix=f(norm_mix), norm_ffn=f(norm_ffn), w_ada=f(w_ada), b_ada=f(b_ada),
        w_ext=np.ascontiguousarray(w_in[:, :, _CACHE['idx']]), attn_sink=f(attn_sink),
        ret_decay_fwd=f(ret_decay_fwd), ret_decay_bwd=f(ret_decay_bwd), w_branch_attn=f(w_branch_attn),
        w_branch_fourier=f(w_branch_fourier), w_branch_ret=f(w_branch_ret), w_out=f(w_out),
        w_rt=np.ascontiguousarray(np.concatenate([f(w_router_group), f(w_router_expert)], axis=-1)),
        b_rt=np.ascontiguousarray(np.concatenate([f(b_router_group), f(b_router_expert)], axis=-1)),
        w_exp_gate=f(w_exp_gate), w_exp_up=f(w_exp_up), w_exp_down=f(w_exp_down), norm_final=f(norm_final))
    x = f(x); c = f(c); ctx = f(ctx)
    B = x.shape[0]
    in_maps = []
    for b in range(B):
        d = dict(shared)
        d.update(x=x[b], ctx=ctx[b], c=c[b])
        in_maps.append(d)
    res = run_bass_kernel_spmd(nc, in_maps, core_ids=list(range(B)))
    return np.stack([np.asarray(r["out"], dtype=np.float32) for r in res.results], axis=0)
```

```python
import contextlib, os
import numpy as np
import ml_dtypes
import concourse.bass as bass
import concourse.mybir as mybir
from concourse.bass_utils import run_bass_kernel_spmd

F32 = mybir.dt.float32
BF16 = mybir.dt.bfloat16
AF = mybir.ActivationFunctionType
ALU = mybir.AluOpType
AX = mybir.AxisListType

ARENA_BASE = 20736
SBUF_BYTES = 229376 - ARENA_BASE - 128
SAME_ENGINE_SYNC = bool(int(os.environ.get("SES", "1")))
NDMA_SEM = 12
EPOCH = 30000


def _dsize(dt):
    return 2 if dt == BF16 else 4


class KB:
    def __init__(self, nc):
        self.nc = nc
        self.es = contextlib.ExitStack()
        self.eng = {'pe': nc.tensor, 'act': nc.scalar, 'dve': nc.vector, 'pool': nc.gpsimd, 'sp': nc.sync}
        self.cnt = {e: 0 for e in self.eng}
        self.sems = {e: [] for e in self.eng}
        self.seen = {e: {} for e in self.eng}
        self.res = {}
        self.dma_sems = {}
        self.dma_i = {}
        self.bot = ARENA_BASE
        self.top = ARENA_BASE + SBUF_BYTES
        self.nps = 0
        self.n_sem = 0
        self.peak = 0
        self.off = {}

    def __enter__(self):
        self.es.__enter__()
        return self

    def __exit__(self, *a):
        return self.es.__exit__(*a)

    def sb(self, name, shape, dtype, top=False):
        n = 1
        for s in shape[1:]:
            n *= s
        nbytes = (n * _dsize(dtype) + 63) // 64 * 64
        if top:
            self.top -= nbytes
            off = self.top
        else:
            off = self.bot
            self.bot += nbytes
        assert self.bot <= self.top, f"SBUF overflow allocating {name}: bot={self.bot} top={self.top}"
        self.peak = max(self.peak, self.bot + SBUF_BYTES - self.top)
        self.nps += 1
        self.off[name] = off
        return self.nc.alloc_sbuf_tensor_at(f"{name}_{self.nps}", list(shape), dtype, offset=off)

    def alias(self, name, shape, dtype, off):
        self.nps += 1
        return self.nc.alloc_sbuf_tensor_at(f"{name}_{self.nps}", list(shape), dtype, offset=off)

    def mark(self):
        return (self.bot, self.top)

    def release(self, m):
        self.barrier()
        self.bot, self.top = m

    def ps(self, name, shape, dtype=F32):
        return self.nc.alloc_psum_tensor(name, list(shape), dtype)

    def _newsem(self, name):
        self.n_sem += 1
        return self.es.enter_context(self.nc.semaphore(f"{name}_{self.n_sem}"))

    def _tick(self, e):
        c = self.cnt[e]
        ep, v = divmod(c, EPOCH)
        while len(self.sems[e]) <= ep:
            self.sems[e].append(self._newsem(f"s_{e}"))
        self.cnt[e] = c + 1
        return (self.sems[e][ep], v + 1, e)

    def _wait(self, e, tick):
        sem, val, owner = tick
        if owner == e and (e == 'pe' or not SAME_ENGINE_SYNC):
            return
        k = sem.num
        if self.seen[e].get(k, 0) >= val:
            return
        self.eng[e].wait_ge(sem, val)
        self.seen[e][k] = val

    def _deps(self, e, r, w):
        ticks = []
        for k in r:
            rec = self.res.get(k)
            if rec and rec[0]:
                ticks.append(rec[0])
        for k in w:
            rec = self.res.get(k)
            if rec:
                if rec[0]:
                    ticks.append(rec[0])
                ticks.extend(rec[1])
        for t in ticks:
            self._wait(e, t)

    def _record(self, tick, r, w):
        for k in r:
            rec = self.res.setdefault(k, [None, []])
            rec[1] = [t for t in rec[1] if not (t[0] is tick[0])] + [tick]
        for k in w:
            self.res[k] = [tick, []]

    def op(self, e, fn, r=(), w=()):
        self._deps(e, r, w)
        tick = self._tick(e)
        fn(self.eng[e]).then_inc(tick[0], 1)
        self._record(tick, r, w)

    def dma(self, q, out, in_, r=(), w=(), fn=None, **kw):
        self._deps(q, r, w)
        pool = self.dma_sems.setdefault(q, [])
        i = self.dma_i.get(q, 0)
        self.dma_i[q] = i + 1
        slot = i % NDMA_SEM
        if len(pool) <= slot:
            pool.append([self._newsem(f"d_{q}"), 0])
        ent = pool[slot]
        if ent[1] > 0:
            self._wait(q, (ent[0], ent[1], 'dma'))
        ent[1] += 16
        tick = (ent[0], ent[1], 'dma')
        if fn is not None:
            fn(self.eng[q]).then_inc(ent[0], 16)
        else:
            self.eng[q].dma_start(out=out, in_=in_, **kw).then_inc(ent[0], 16)
        self._record(tick, r, w)

    def all_ticks(self):
        ticks = []
        for e in self.eng:
            c = self.cnt[e]
            if c > 0:
                ep, v = divmod(c - 1, EPOCH)
                ticks.append((self.sems[e][ep], v + 1, e))
        for q, pool in self.dma_sems.items():
            for ent in pool:
                if ent[1] > 0:
                    ticks.append((ent[0], ent[1], 'dma'))
        return ticks

    def barrier(self):
        ticks = self.all_ticks()
        for e in self.eng:
            for t in ticks:
                self._wait(e, t)
        self.res = {}

    def finish(self):
        ticks = self.all_ticks()
        for t in ticks:
            if t[2] != 'sp':
                self._wait('sp', t)

    def make_ident(self, ident):
        nc = self.nc
        self.op('pool', lambda e: e.memset(ident[:], 1.0), w=['ident'])
        self.op('pool', lambda e: e.affine_select(out=ident[:], in_=ident[:], pattern=[[-1, 128]],
                                                  compare_op=ALU.is_equal, fill=0.0, base=0,
                                                  channel_multiplier=1), r=['ident'], w=['ident'])

D = 1024; L = 256; N = 2048; T = 2304; NT = 18
QA, QAS, KA, KAS, VA, QR, QRS, KR, KRS, VR, GR, FU, GT = 0, 512, 1024, 1152, 1280, 1408, 1920, 2432, 2944, 3456, 3968, 4480, 4992
WEXT = 8064
CAP = int(os.environ.get('MOE_CAP', '1024'))
NS = CAP // 128
I32 = mybir.dt.int32
TB = [(0, 256), (256, 512), (768, 512), (1280, 512), (1792, 512)]


def bc(ap, free):
    return bass.AP(ap.tensor, ap.offset, [list(ap.ap[0])] + [list(f) for f in free])


def build(stop=None, nlayers=2):
    nc = bass.Bass("TRN2", target_bir_lowering=False)

    def din(name, shape, dt=F32):
        return nc.dram_tensor(name, list(shape), dt, kind="ExternalInput").ap()
    x_in = din("x", [N, D]); ctx_in = din("ctx", [L, D]); c_in = din("c", [D]); cctx_in = din("c_ctx", [D])
    norm_mix = din("norm_mix", [2, D]); norm_ffn = din("norm_ffn", [2, D])
    w_ada = din("w_ada", [2, D, 6 * D]); b_ada = din("b_ada", [2, 6 * D])
    w_ext = din("w_ext", [2, D, WEXT]); sink_in = din("attn_sink", [2, 8])
    dec_f = din("ret_decay_fwd", [2, 8]); dec_b = din("ret_decay_bwd", [2, 8])
    w_ba = din("w_branch_attn", [2, 512, D]); w_bf = din("w_branch_fourier", [2, 512, D]); w_br = din("w_branch_ret", [2, 512, D])
    w_out = din("w_out", [2, D, D]); w_rt = din("w_rt", [2, D, 36]); b_rt = din("b_rt", [2, 36])
    if stop is None or stop.startswith('moe'):
        w_eg = din("w_exp_gate", [2, 32, D, 512]); w_eu = din("w_exp_up", [2, 32, D, 512]); w_ed = din("w_exp_down", [2, 32, 512, D])
    norm_final = din("norm_final", [D])
    ropeA = din("ropeA", [2, 128, N]); ropeR = din("ropeR", [2, 128, N])
    dftN = din("dftN", [2, N, N], BF16); dft256 = din("dft256", [2, 256, 256], BF16); dftC = din("dftC", [128, 256], BF16)
    amask = din("amask", [128, 256], BF16); rconst = din("rconst", [128, 770])
    out = nc.dram_tensor("out", [N, D], F32, kind="ExternalOutput").ap()
    xbuf = nc.dram_tensor("xbuf", [T, D], F32).ap()
    mconst = din("mconst", [128, 32 + NT])
    h2d = nc.dram_tensor("h2d", [T, D], BF16).ap()
    rec_d = nc.dram_tensor("rec_d", [32 * CAP, 4], F32).ap()
    AB = nc.dram_tensor("AB", [2 * T, D], F32).ap()
    modbuf = nc.dram_tensor("modbuf", [2, 2, 6 * D], F32).ap()
    dbg = {}
    if stop is not None:
        dbg['d_x'] = nc.dram_tensor("d_x", [T, D], F32, kind="ExternalOutput").ap()
        dbg['d_mod'] = nc.dram_tensor("d_mod", [2, 6 * D], F32, kind="ExternalOutput").ap()
        dbg['d_hT'] = nc.dram_tensor("d_hT", [128, 8, T], BF16, kind="ExternalOutput").ap()
        dbg['d_oT'] = nc.dram_tensor("d_oT", [128, 4, T], BF16, kind="ExternalOutput").ap()
        dbg['d_mT'] = nc.dram_tensor("d_mT", [128, 8, T], BF16, kind="ExternalOutput").ap()

    kb = KB(nc)
    with kb:
        PSA = kb.ps("psa", [128, 7, 512], F32)
        PST = kb.ps("pst", [128, 8, 128], BF16)
        ident = kb.sb("ident", [128, 128], BF16)
        ident32 = kb.sb("ident32", [128, 128], F32)
        kb.make_ident(ident)
        kb.op('pool', lambda e: e.memset(ident32[:], 1.0), w=['ident32'])
        kb.op('pool', lambda e: e.affine_select(out=ident32[:], in_=ident32[:], pattern=[[-1, 128]], compare_op=ALU.is_equal,
                                                fill=0.0, base=0, channel_multiplier=1), r=['ident32'], w=['ident32'])
        cst = kb.sb("cst", [128, 4], F32)
        kb.op('pool', lambda e: e.memset(cst[:, 0:1], 1e-6), w=['cst'])
        kb.op('pool', lambda e: e.memset(cst[:, 1:2], 1e-5), w=['cst'])
        kb.op('pool', lambda e: e.memset(cst[:, 2:3], 1.0), w=['cst'])
        epsN = cst[:, 0:1]; epsG = cst[:, 1:2]; one1 = cst[:, 2:3]
        stg = [kb.sb(f"stg{i}", [128, 2048], F32) for i in range(2)]
        stg_i = [0]; cast_rr = [0]
        hT = kb.sb("hT", [128, 8, T], BF16)
        stgx = [kb.alias(f'stgx{i}', [128, 2048], F32, kb.off['hT'] + i * 8192) for i in range(4)]
        kb.barrier()

        stg_all = [list(stg)]

        def load_w(dst, dkey, src2d, K, W, engs=('pool',)):
            kk = max(1, 2048 // W)
            stg = stg_all[0]
            for k0 in range(0, K, kk):
                k1 = min(K, k0 + kk)
                i = stg_i[0] % len(stg); stg_i[0] += 1
                sv = stg[i][:, 0:(k1 - k0) * W].rearrange("p (k c) -> p k c", c=W)
                kb.dma('sp', sv, src2d[k0 * 128:k1 * 128, :].rearrange("(k p) c -> p k c", p=128), w=[('stg', i)])
                en = engs[cast_rr[0] % len(engs)]; cast_rr[0] += 1
                if en == 'act':
                    kb.op('act', lambda e: e.copy(out=dst[:, k0:k1, :], in_=sv), r=[('stg', i)], w=[dkey])
                else:
                    kb.op(en, lambda e: e.tensor_copy(out=dst[:, k0:k1, :], in_=sv), r=[('stg', i)], w=[dkey])

        def proj_fm(wt, wkey, c0, tok0, ntok, bank):
            for k in range(8):
                kb.op('pe', lambda e: e.matmul(PSA[:, bank, 0:ntok], lhsT=wt[:, k, c0:c0 + 128], rhs=hT[:, k, tok0:tok0 + ntok],
                                               start=(k == 0), stop=(k == 7)), r=[wkey, 'hT'], w=[('ps', bank)])

        def dump(name, src, r):
            kb.dma('sp', dbg[name], src, r=r, w=[name])

        def xdump(name, t, shape, dt):
            if stop is None or not os.environ.get('XDUMP'):
                return
            kb.barrier()
            d_ = nc.dram_tensor("x_" + name, list(shape), dt, kind="ExternalOutput").ap()
            kb.dma('sp', d_, t[:], w=["x_" + name])

        def phase_ada(l):
            m = kb.mark()
            stg_all[0] = list(stg) + stgx
            stg_i[0] = 0
            cT = kb.sb('cT', [128, 8, 2], F32); cTb = kb.sb('cTb', [128, 8, 2], BF16)
            bsb = kb.sb('bsb', [2, 6 * D], F32); modsb = kb.sb('modsb', [2, 6 * D], F32)
            wa = [kb.sb(f'wa{i}', [128, 8, 512], BF16) for i in range(2)]
            kb.dma('sp', cT[:, :, 0], cctx_in.rearrange("(k p) -> p k", p=128), w=['cT'], allow_slow_non_contiguous=True)
            kb.dma('sp', cT[:, :, 1], c_in.rearrange("(k p) -> p k", p=128), w=['cT'], allow_slow_non_contiguous=True)
            kb.dma('sp', bsb[:], b_ada[l].partition_broadcast(2), w=['bsb'])
            kb.op('act', lambda e: e.activation(out=cTb[:], in_=cT[:], func=AF.Silu), r=['cT'], w=['cTb'])
            for nb in range(12):
                i = nb % 2
                load_w(wa[i], ('wa', i), w_ada[l][:, nb * 512:(nb + 1) * 512], 8, 512, engs=('pool', 'act'))
                for k in range(8):
                    kb.op('pe', lambda e: e.matmul(PSA[0:2, i, :], lhsT=cTb[:, k, :], rhs=wa[i][:, k, :], start=(k == 0), stop=(k == 7)),
                          r=['cTb', ('wa', i)], w=[('ps', i)])
                kb.op('dve', lambda e: e.tensor_tensor(out=modsb[:, nb * 512:(nb + 1) * 512], in0=PSA[0:2, i, :],
                                                       in1=bsb[:, nb * 512:(nb + 1) * 512], op=ALU.add), r=[('ps', i), 'bsb'], w=['modsb'])
            kb.dma('sp', modbuf[l], modsb[:], r=['modsb'], w=[('mod', l)])
            if stop == f'ada{l}':
                dump('d_mod', modsb[:], ['modsb'])
            stg_all[0] = list(stg)
            stg_i[0] = 0
            kb.release(m)
            kb.res[('mod', l)] = None
            kb.res.pop(('mod', l))

        def phase_norm(l, which, logits=None, sparse=False):
            m = kb.mark()
            nw = norm_mix if which == 1 else norm_ffn
            si, ci = (0, 1) if which == 1 else (3, 4)
            tiles = range(NT) if (which == 1 or l == 0) else range(2, NT)
            nwb = kb.sb('nwb', [128, D], F32)
            kb.dma('sp', nwb[:], nw[l].partition_broadcast(128), w=['nwb'])
            G = []; S = []
            for row in (0, 1):
                g_ = kb.sb(f'G{row}', [128, D], F32); s_ = kb.sb(f'S{row}', [128, D], F32)
                kb.dma('sp', g_[:], modbuf[l, row, ci * D:(ci + 1) * D].partition_broadcast(128), w=[f'G{row}'])
                kb.dma('sp', s_[:], modbuf[l, row, si * D:(si + 1) * D].partition_broadcast(128), w=[f'S{row}'])
                kb.op('dve', lambda e: e.scalar_tensor_tensor(out=g_[:], in0=g_[:], scalar=1.0, in1=nwb[:], op0=ALU.add, op1=ALU.mult),
                      r=[f'G{row}', 'nwb'], w=[f'G{row}'])
                G.append(g_); S.append(s_)
            xt = [kb.sb(f'xt{i}', [128, D], F32) for i in range(3)]
            tmp = [kb.sb(f'tmp{i}', [128, D], F32) for i in range(3)]
            hb = [kb.sb(f'hb{i}', [128, D], BF16 if which == 1 else F32) for i in range(3)]
            ssq = [kb.sb(f'ssq{i}', [128, 1], F32) for i in range(3)]
            junk = kb.sb('junk', [128, D], F32)
            if which == 2:
                hbb = [kb.sb(f'hbb{i}', [128, D], BF16) for i in range(3)]
                h32 = [kb.sb(f'h32{i}', [128, 8, 128], F32) for i in range(3)]
                wrt32 = kb.sb('wrt32', [128, 8, 36], F32); brt = kb.sb('brt', [128, 36], F32)
                kb.dma('sp', wrt32[:], w_rt[l].rearrange("(k p) c -> p k c", p=128), w=['wrt32'])
                kb.dma('sp', brt[:], b_rt[l].partition_broadcast(128), w=['brt'])
            def stA(t):
                i = t % 3; row = 0 if t < 2 else 1
                kb.dma('sp', xt[i][:], xbuf[t * 128:(t + 1) * 128, :], r=[('x', t)], w=[('xt', i)])
                kb.op('act', lambda e: e.activation(out=junk[:], in_=xt[i][:], func=AF.Square, accum_out=ssq[i][:]),
                      r=[('xt', i)], w=['junk', ('ssq', i)])
                kb.op('act', lambda e: e.activation(out=ssq[i][:], in_=ssq[i][:], func=AF.Sqrt, scale=1.0 / D, bias=epsN),
                      r=[('ssq', i)], w=[('ssq', i)])
                kb.op('dve', lambda e: e.reciprocal(out=ssq[i][:], in_=ssq[i][:]), r=[('ssq', i)], w=[('ssq', i)])
                kb.op('dve', lambda e: e.scalar_tensor_tensor(out=tmp[i][:], in0=xt[i][:], scalar=ssq[i][:, 0:1], in1=G[row][:],
                                                              op0=ALU.mult, op1=ALU.mult), r=[('xt', i), ('ssq', i), f'G{row}'], w=[('tmp', i)])
                kb.op('pool', lambda e: e.tensor_tensor(out=hb[i][:], in0=tmp[i][:], in1=S[row][:], op=ALU.add),
                      r=[('tmp', i), f'S{row}'], w=[('hb', i)])

            def stB(t):
                i = t % 3; row = 0 if t < 2 else 1
                if which == 1:
                    for k in range(8):
                        kb.op('pe', lambda e: e.transpose(out=PST[:, k, :], in_=hb[i][:, k * 128:(k + 1) * 128], identity=ident[:]),
                              r=[('hb', i), 'ident'], w=['pst'])
                    kb.op('act', lambda e: e.copy(out=hT[:, :, t * 128:(t + 1) * 128], in_=PST[:]), r=['pst'], w=['hT'])
                else:
                    for k in range(8):
                        kb.op('pe', lambda e: e.matmul(PSA[:, 5 + k // 4, (k % 4) * 128:(k % 4 + 1) * 128], lhsT=hb[i][:, k * 128:(k + 1) * 128],
                                                       rhs=ident32[:], start=True, stop=True), r=[('hb', i), 'ident32'], w=[('ps', 5 + k // 4)])
                    pv = PSA[:, 5:7, :].rearrange("p a (b c) -> p (a b) c", c=128)
                    kb.op('dve', lambda e: e.tensor_copy(out=h32[i][:], in_=pv), r=[('ps', 5), ('ps', 6)], w=[('h32', i)])
                    if sparse:
                        kb.op('act', lambda e: e.copy(out=hbb[i][:], in_=hb[i][:]), r=[('hb', i)], w=[('hbb', i)])
                        kb.dma('sp', h2d[t * 128:(t + 1) * 128, :], hbb[i][:], r=[('hbb', i)], w=[('h2d', t)])
                    else:
                        kb.op('act', lambda e: e.copy(out=hT[:, :, t * 128:(t + 1) * 128], in_=h32[i][:]), r=[('h32', i)], w=['hT'])
                    for k in range(8):
                        kb.op('pe', lambda e: e.matmul(PSA[:, 4, 0:36], lhsT=h32[i][:, k, :], rhs=wrt32[:, k, :], start=(k == 0), stop=(k == 7)),
                              r=[('h32', i), 'wrt32'], w=[('ps', 4)])
                    kb.op('dve', lambda e: e.tensor_tensor(out=logits[:, t, :], in0=PSA[:, 4, 0:36], in1=brt[:], op=ALU.add),
                          r=[('ps', 4), 'brt'], w=['logits'])

            tl_ = list(tiles)
            stA(tl_[0])
            for n_, t in enumerate(tl_):
                if n_ + 1 < len(tl_):
                    stA(tl_[n_ + 1])
                stB(t)
            kb.release(m)

        def merge(l, gate_off, wb_dram, oT, okey, mT, first):
            m = kb.mark()
            wg = [kb.sb(f'mwg{i}', [128, 8, 128], BF16) for i in range(2)]
            wb = [kb.sb(f'mwb{i}', [128, 4, 128], BF16) for i in range(2)]
            sig = [kb.sb(f'sig{i}', [128, 512], F32) for i in range(2)]
            mtmp = [kb.sb(f'mtmp{i}', [128, 512], F32) for i in range(2)]
            it = 0
            for fc in range(8):
                j = fc % 2
                load_w(wg[j], ('mwg', j), w_ext[l][:, GT + gate_off + fc * 128: GT + gate_off + (fc + 1) * 128], 8, 128)
                load_w(wb[j], ('mwb', j), wb_dram[l][:, fc * 128:(fc + 1) * 128], 4, 128)
                for (s0, n) in (TB if l == 0 else TB[1:]):
                    i = it % 2; it += 1
                    proj_fm(wg[j], ('mwg', j), 0, s0, n, i)
                    kb.op('act', lambda e: e.activation(out=sig[i][:, 0:n], in_=PSA[:, i, 0:n], func=AF.Sigmoid), r=[('ps', i)], w=[('sig', i)])
                    for k in range(4):
                        kb.op('pe', lambda e: e.matmul(PSA[:, 2 + i, 0:n], lhsT=wb[j][:, k, :], rhs=oT[:, k, s0:s0 + n], start=(k == 0), stop=(k == 3)),
                              r=[('mwb', j), okey], w=[('ps', 2 + i)])
                    if first:
                        kb.op('dve', lambda e: e.tensor_tensor(out=mT[:, fc, s0:s0 + n], in0=sig[i][:, 0:n], in1=PSA[:, 2 + i, 0:n], op=ALU.mult),
                              r=[('sig', i), ('ps', 2 + i)], w=['mT'])
                    else:
                        kb.op('dve', lambda e: e.tensor_tensor(out=mtmp[i][:, 0:n], in0=sig[i][:, 0:n], in1=PSA[:, 2 + i, 0:n], op=ALU.mult),
                              r=[('sig', i), ('ps', 2 + i)], w=[('mtmp', i)])
                        kb.op('pool', lambda e: e.tensor_tensor(out=mT[:, fc, s0:s0 + n], in0=mT[:, fc, s0:s0 + n], in1=mtmp[i][:, 0:n], op=ALU.add),
                              r=[('mtmp', i), 'mT'], w=['mT'])
            kb.release(m)

        def rope_evac(bA, bB, dst, dkey, tC, tS, n, t1, t2, j):
            kb.op('dve', lambda e: e.tensor_tensor(out=t1[:, 0:n], in0=PSA[:, bA, 0:n], in1=tC[:, 0:n], op=ALU.mult), r=[('ps', bA), ('rc', j)], w=[('t1', j)])
            kb.op('dve', lambda e: e.tensor_tensor(out=t2[:, 0:n], in0=PSA[:, bB, 0:n], in1=tS[:, 0:n], op=ALU.mult), r=[('ps', bB), ('rs', j)], w=[('t2', j)])
            kb.op('pool', lambda e: e.tensor_tensor(out=dst, in0=t1[:, 0:n], in1=t2[:, 0:n], op=ALU.add), r=[('t1', j), ('t2', j)], w=[dkey])

        def phase_ret(l, retT):
            need_ctx = (l == 0)
            m = kb.mark()
            RC = kb.sb('RC', [128, 770], F32)
            kb.dma('sp', RC[:], rconst[:, :], w=['RC'])
            dpos = RC[:, 0:128]; dneg = RC[:, 128:256]; mge = RC[:, 256:384]; mlt = RC[:, 384:512]
            io1 = RC[:, 512:640]; iob = RC[:, 640:768]; pc127 = RC[:, 768:769]; pcol = RC[:, 769:770]
            lg = kb.sb('lg', [128, 16], F32)
            kb.dma('sp', lg[:, 0:8], dec_f[l].partition_broadcast(128), w=['lg'])
            kb.dma('sp', lg[:, 8:16], dec_b[l].partition_broadcast(128), w=['lg'])
            kb.op('act', lambda e: e.activation(out=lg[:], in_=lg[:], func=AF.Exp, scale=-1.0), r=['lg'], w=['lg'])
            kb.op('act', lambda e: e.activation(out=lg[:], in_=lg[:], func=AF.Ln, bias=one1), r=['lg', 'cst'], w=['lg'])
            kb.op('dve', lambda e: e.tensor_scalar(out=lg[:], in0=lg[:], scalar1=-1.0, scalar2=None, op0=ALU.mult), r=['lg'], w=['lg'])
            lgp = kb.sb('lgp', [128, 8], F32)
            for r in range(4):
                for hh in range(2):
                    for d in range(2):
                        kb.op('pool', lambda e: e.tensor_copy(out=lgp[hh * 64:(hh + 1) * 64, d * 4 + r:d * 4 + r + 1],
                                                              in_=lg[hh * 64:(hh + 1) * 64, d * 8 + 2 * r + hh:d * 8 + 2 * r + hh + 1]), r=['lg'], w=['lgp'])
            g128 = kb.sb('g128', [128, 8], F32)
            kb.op('act', lambda e: e.activation(out=g128[:], in_=lgp[:], func=AF.Exp, scale=128.0), r=['lgp'], w=['g128'])
            Z = kb.sb('Z', [128, 16], F32)
            kb.op('act', lambda e: e.activation(out=Z[:, 0:8], in_=lg[:, 0:8], func=AF.Exp, scale=pc127), r=['lg', 'RC'], w=['Z'])
            kb.op('act', lambda e: e.activation(out=Z[:, 8:16], in_=lg[:, 8:16], func=AF.Exp, scale=pcol), r=['lg', 'RC'], w=['Z'])
            kb.op('dve', lambda e: e.tensor_scalar(out=Z[:], in0=Z[:], scalar1=0.125, scalar2=None, op0=ALU.mult), r=['Z'], w=['Z'])
            DecT = kb.sb('DecT', [128, 8, 128], F32)
            d1 = kb.sb('d1', [128, 128], F32); d2 = kb.sb('d2', [128, 128], F32)
            for h in range(8):
                kb.op('act', lambda e: e.activation(out=d1[:], in_=dpos, func=AF.Exp, scale=lg[:, h:h + 1]), r=['lg', 'RC'], w=['d1'])
                kb.op('pool', lambda e: e.tensor_tensor(out=d1[:], in0=d1[:], in1=mge, op=ALU.mult), r=['d1', 'RC'], w=['d1'])
                kb.op('act', lambda e: e.activation(out=d2[:], in_=dneg, func=AF.Exp, scale=lg[:, 8 + h:9 + h]), r=['lg', 'RC'], w=['d2'])
                kb.op('pool', lambda e: e.tensor_tensor(out=d2[:], in0=d2[:], in1=mlt, op=ALU.mult), r=['d2', 'RC'], w=['d2'])
                kb.op('pool', lambda e: e.tensor_tensor(out=d1[:], in0=d1[:], in1=d2[:], op=ALU.add), r=['d1', 'd2'], w=['d1'])
                kb.op('dve', lambda e: e.tensor_scalar(out=DecT[:, h, :], in0=d1[:], scalar1=0.125, scalar2=None, op0=ALU.mult), r=['d1'], w=['DecT'])
            X = kb.sb('X', [128, 8, 128], F32)
            for r in range(4):
                kb.op('act', lambda e: e.activation(out=X[:, r, :], in_=io1, func=AF.Exp, scale=lgp[:, r:r + 1]), r=['lgp', 'RC'], w=['X'])
                kb.op('act', lambda e: e.activation(out=X[:, 4 + r, :], in_=iob, func=AF.Exp, scale=lgp[:, 4 + r:5 + r]), r=['lgp', 'RC'], w=['X'])
            if os.environ.get('RET_CUT') == '1':
                kb.release(m); return
            qT = kb.sb('qT', [128, T], BF16); kT = kb.sb('kT', [128, T], BF16)
            ktok = kb.sb('ktok', [128, NT, 128], BF16); vtok = kb.sb('vtok', [128, NT, 128], BF16)
            vf = kb.sb('vf', [128, NT, 128], BF16); vb = kb.sb('vb', [128, NT, 128], BF16)
            sg = kb.sb('sg', [128, NT, 128], F32)
            Sf = kb.sb('Sf', [128, NT, 128], BF16); Rb = kb.sb('Rb', [128, NT, 128], BF16)
            Srun = kb.sb('Srun', [128, 128], F32); Rrun = kb.sb('Rrun', [128, 128], F32)
            ws = {nm: kb.sb('w' + nm, [128, 8, 128], BF16) for nm in ('q', 'qs', 'k', 'ks', 'v', 'g')}
            rc = [kb.sb(f'rc{i}', [128, 512], F32) for i in range(2)]; rs = [kb.sb(f'rs{i}', [128, 512], F32) for i in range(2)]
            t1 = [kb.sb(f't1{i}', [128, 512], F32) for i in range(2)]; t2 = [kb.sb(f't2{i}', [128, 512], F32) for i in range(2)]
            AT = [kb.sb(f'AT{i}', [128, 2, 128], BF16) for i in range(2)]
            qxf = [kb.sb(f'qxf{i}', [128, 128], BF16) for i in range(2)]; qxb = [kb.sb(f'qxb{i}', [128, 128], BF16) for i in range(2)]
            oc = [kb.sb(f'oc{i}', [128, 128], F32) for i in range(2)]; sq = [kb.sb(f'sq{i}', [128, 128], F32) for i in range(2)]
            st = [kb.sb(f'st{i}', [128, 4], F32) for i in range(2)]
            rtok = [kb.sb(f'rtok{i}', [128, 128], BF16) for i in range(2)]
            o_all = kb.sb('o_all', [128, NT, 128], F32); sq_all = kb.sb('sq_all', [128, NT, 128], F32); rt_all = kb.sb('rt_all', [128, NT, 128], BF16)
            mu_ = kb.sb('mu_', [128, 2 * NT], F32); va_ = kb.sb('va_', [128, 2 * NT], F32)
            for r in range(4):
                for nm, c0 in (('q', QR), ('qs', QRS), ('k', KR), ('ks', KRS), ('v', VR), ('g', GR)):
                    load_w(ws[nm], 'w' + nm, w_ext[l][:, c0 + r * 128:c0 + (r + 1) * 128], 8, 128)
                for bi, (s0, n) in enumerate(TB):
                    if s0 == 0:
                        proj_fm(ws['q'], 'wq', 0, s0, n, 0)
                        kb.op('act', lambda e: e.copy(out=qT[:, s0:s0 + n], in_=PSA[:, 0, 0:n]), r=[('ps', 0)], w=['qT'])
                        proj_fm(ws['k'], 'wk', 0, s0, n, 1)
                        kb.op('act', lambda e: e.copy(out=kT[:, s0:s0 + n], in_=PSA[:, 1, 0:n]), r=[('ps', 1)], w=['kT'])
                    else:
                        j = bi % 2; p0 = s0 - 256
                        kb.dma('sp', rc[j][:, 0:n], ropeR[0, :, p0:p0 + n], w=[('rc', j)])
                        kb.dma('sp', rs[j][:, 0:n], ropeR[1, :, p0:p0 + n], w=[('rs', j)])
                        proj_fm(ws['q'], 'wq', 0, s0, n, 0); proj_fm(ws['qs'], 'wqs', 0, s0, n, 1)
                        rope_evac(0, 1, qT[:, s0:s0 + n], 'qT', rc[j], rs[j], n, t1[j], t2[j], j)
                        proj_fm(ws['k'], 'wk', 0, s0, n, 2); proj_fm(ws['ks'], 'wks', 0, s0, n, 3)
                        rope_evac(2, 3, kT[:, s0:s0 + n], 'kT', rc[j], rs[j], n, t1[j], t2[j], j)
                    for t in range(s0 // 128, (s0 + n) // 128):
                        bank = 4 + (t % 2)
                        for k in range(8):
                            kb.op('pe', lambda e: e.matmul(PSA[:, bank, 0:128], lhsT=hT[:, k, t * 128:(t + 1) * 128], rhs=ws['v'][:, k, :],
                                                           start=(k == 0), stop=(k == 7)), r=['hT', 'wv'], w=[('ps', bank)])
                        for k in range(8):
                            kb.op('pe', lambda e: e.matmul(PSA[:, bank, 128:256], lhsT=hT[:, k, t * 128:(t + 1) * 128], rhs=ws['g'][:, k, :],
                                                           start=(k == 0), stop=(k == 7)), r=['hT', 'wg'], w=[('ps', bank)])
                        kb.op('act', lambda e: e.copy(out=vtok[:, t, :], in_=PSA[:, bank, 0:128]), r=[('ps', bank)], w=['vtok'])
                        kb.op('act', lambda e: e.activation(out=sg[:, t, :], in_=PSA[:, bank, 128:256], func=AF.Silu), r=[('ps', bank)], w=['sg'])
                if os.environ.get('RET_CUT') == '2':
                    kb.release(m); return
                for t in range(NT):
                    kb.op('pe', lambda e: e.transpose(out=PST[:, t % 8, :], in_=kT[:, t * 128:(t + 1) * 128], identity=ident[:]), r=['kT', 'ident'], w=['pst'])
                    if t % 8 == 7 or t == NT - 1:
                        n8 = t % 8 + 1; t0 = t - n8 + 1
                        kb.op('act', lambda e: e.copy(out=ktok[:, t0:t + 1, :], in_=PST[:, 0:n8, :]), r=['pst'], w=['ktok'])
                v4 = lambda a: a[:].rearrange("p c (h e) -> p c h e", h=2)
                kb.op('pool', lambda e: e.tensor_tensor(out=v4(vf), in0=v4(vtok), in1=bc(Z[:, 2 * r:2 * r + 2], [[0, NT], [1, 2], [0, 64]]), op=ALU.mult),
                      r=['vtok', 'Z'], w=['vf'])
                kb.op('pool', lambda e: e.tensor_tensor(out=v4(vb), in0=v4(vtok), in1=bc(Z[:, 8 + 2 * r:8 + 2 * r + 2], [[0, NT], [1, 2], [0, 64]]), op=ALU.mult),
                      r=['vtok', 'Z'], w=['vb'])
                if os.environ.get('RET_CUT') == '3':
                    kb.release(m); return
                kb.op('pool', lambda e: e.memset(Srun[:], 0.0), w=['Srun'])
                kb.op('pool', lambda e: e.memset(Sf[:, 0, :], 0.0), w=['Sf'])
                for c in range(NT - 1):
                    bank = 4 + c % 2
                    kb.op('pe', lambda e: e.matmul(PSA[:, bank, 0:128], lhsT=ktok[:, c, :], rhs=vf[:, c, :], start=True, stop=True),
                          r=['ktok', 'vf'], w=[('ps', bank)])
                    kb.op('dve', lambda e: e.scalar_tensor_tensor(out=Srun[:], in0=Srun[:], scalar=g128[:, r:r + 1], in1=PSA[:, bank, 0:128],
                                                                  op0=ALU.mult, op1=ALU.add), r=['Srun', 'g128', ('ps', bank)], w=['Srun'])
                    kb.op('act', lambda e: e.copy(out=Sf[:, c + 1, :], in_=Srun[:]), r=['Srun'], w=['Sf'])
                kb.op('pool', lambda e: e.memset(Rrun[:], 0.0), w=['Rrun'])
                kb.op('pool', lambda e: e.memset(Rb[:, 1, :], 0.0), w=['Rb'])
                order = [1, 0] + list(range(17, 2, -1)); dest = [0, 17] + list(range(16, 1, -1))
                for ii, (c, dd) in enumerate(zip(order, dest)):
                    bank = 4 + ii % 2
                    kb.op('pe', lambda e: e.matmul(PSA[:, bank, 0:128], lhsT=ktok[:, c, :], rhs=vb[:, c, :], start=True, stop=True),
                          r=['ktok', 'vb'], w=[('ps', bank)])
                    kb.op('dve', lambda e: e.scalar_tensor_tensor(out=Rrun[:], in0=Rrun[:], scalar=g128[:, 4 + r:5 + r], in1=PSA[:, bank, 0:128],
                                                                  op0=ALU.mult, op1=ALU.add), r=['Rrun', 'g128', ('ps', bank)], w=['Rrun'])
                    kb.op('act', lambda e: e.copy(out=Rb[:, dd, :], in_=Rrun[:]), r=['Rrun'], w=['Rb'])
                if os.environ.get('RET_CUT') == '4':
                    kb.release(m); return
                chunks_ = list(range(NT) if need_ctx else range(2, NT))

                def inner_(c):
                    i = c % 2; cs = slice(c * 128, (c + 1) * 128)
                    for hh in range(2):
                        ps_ = slice(hh * 64, (hh + 1) * 64)
                        kb.op('pe', lambda e: e.matmul(PSA[:, i + 4 * hh, 0:128], lhsT=kT[ps_, cs], rhs=qT[ps_, cs], start=True, stop=True),
                              r=['kT', 'qT'], w=[('ps', i + 4 * hh)])

                def rest_(c):
                    i = c % 2; cs = slice(c * 128, (c + 1) * 128)
                    for hh in range(2):
                        kb.op('dve', lambda e: e.tensor_tensor(out=AT[i][:, hh, :], in0=PSA[:, i + 4 * hh, 0:128],
                                                               in1=DecT[:, 2 * r + hh, :], op=ALU.mult), r=[('ps', i + 4 * hh), 'DecT'], w=[('AT', i)])
                    kb.op('pool', lambda e: e.tensor_tensor(out=qxf[i][:], in0=qT[:, cs], in1=X[:, r, :], op=ALU.mult), r=['qT', 'X'], w=[('qxf', i)])
                    kb.op('pool', lambda e: e.tensor_tensor(out=qxb[i][:], in0=qT[:, cs], in1=X[:, 4 + r, :], op=ALU.mult), r=['qT', 'X'], w=[('qxb', i)])
                    for hh in range(2):
                        ps_ = slice(hh * 64, (hh + 1) * 64)
                        o_ = PSA[:, 2 + i, hh * 64:(hh + 1) * 64]
                        kb.op('pe', lambda e: e.matmul(o_, lhsT=AT[i][:, hh, :], rhs=vtok[:, c, hh * 64:(hh + 1) * 64], start=True, stop=False),
                              r=[('AT', i), 'vtok'], w=[('ps', 2 + i)])
                        kb.op('pe', lambda e: e.matmul(o_, lhsT=qxf[i][ps_, :], rhs=Sf[ps_, c, hh * 64:(hh + 1) * 64], start=False, stop=False),
                              r=[('qxf', i), 'Sf'], w=[('ps', 2 + i)])
                        kb.op('pe', lambda e: e.matmul(o_, lhsT=qxb[i][ps_, :], rhs=Rb[ps_, c, hh * 64:(hh + 1) * 64], start=False, stop=True),
                              r=[('qxb', i), 'Rb'], w=[('ps', 2 + i)])
                    kb.op('act', lambda e: e.copy(out=o_all[:, c, :], in_=PSA[:, 2 + i, 0:128]), r=[('ps', 2 + i)], w=['o_all'])

                inner_(chunks_[0])
                for n_, c in enumerate(chunks_):
                    if n_ + 1 < len(chunks_):
                        inner_(chunks_[n_ + 1])
                    rest_(c)
                c0_ = 0 if need_ctx else 2
                G_ = (NT - c0_) * 2
                og = o_all[:, c0_:NT, :].rearrange("p c (h e) -> p (c h) e", h=2)
                sqg = sq_all[:, c0_:NT, :].rearrange("p c (h e) -> p (c h) e", h=2)
                kb.op('dve', lambda e: e.reduce_sum(out=mu_[:, 0:G_], in_=og, axis=AX.X), r=['o_all'], w=['mu_'])
                kb.op('dve', lambda e: e.tensor_scalar(out=mu_[:, 0:G_], in0=mu_[:, 0:G_], scalar1=-1.0 / 64, scalar2=None, op0=ALU.mult), r=['mu_'], w=['mu_'])
                kb.op('pool', lambda e: e.tensor_tensor(out=og, in0=og, in1=bc(mu_[:, 0:G_], [[1, G_], [0, 64]]), op=ALU.add), r=['o_all', 'mu_'], w=['o_all'])
                kb.op('pool', lambda e: e.tensor_tensor(out=sqg, in0=og, in1=og, op=ALU.mult), r=['o_all'], w=['sq_all'])
                kb.op('dve', lambda e: e.reduce_sum(out=va_[:, 0:G_], in_=sqg, axis=AX.X), r=['sq_all'], w=['va_'])
                kb.op('act', lambda e: e.activation(out=va_[:, 0:G_], in_=va_[:, 0:G_], func=AF.Sqrt, scale=1.0 / 64, bias=epsG), r=['va_'], w=['va_'])
                kb.op('dve', lambda e: e.reciprocal(out=va_[:, 0:G_], in_=va_[:, 0:G_]), r=['va_'], w=['va_'])
                kb.op('pool', lambda e: e.tensor_tensor(out=og, in0=og, in1=bc(va_[:, 0:G_], [[1, G_], [0, 64]]), op=ALU.mult), r=['o_all', 'va_'], w=['o_all'])
                kb.op('pool', lambda e: e.tensor_tensor(out=rt_all[:, c0_:NT, :], in0=o_all[:, c0_:NT, :], in1=sg[:, c0_:NT, :], op=ALU.mult),
                      r=['o_all', 'sg'], w=['rt_all'])
                cl_ = list(range(c0_, NT))
                for n0 in range(0, len(cl_), 8):
                    grp_ = cl_[n0:n0 + 8]
                    for ii_, c in enumerate(grp_):
                        kb.op('pe', lambda e: e.transpose(out=PST[:, ii_, :], in_=rt_all[:, c, :], identity=ident[:]), r=['rt_all', 'ident'], w=['pst'])
                    kb.op('act', lambda e: e.copy(out=retT[:, r, grp_[0] * 128:(grp_[-1] + 1) * 128].rearrange("p (a b) -> p a b", b=128),
                                                  in_=PST[:, 0:len(grp_), :]), r=['pst'], w=['retT'])
                if os.environ.get('RET_CUT') in ('5', '6', '7'):
                    kb.release(m); return
                if r == 3:
                    for nm_, t_, sh_, dt_ in (('sg', sg, [128, NT, 128], F32), ('DecT', DecT, [128, 8, 128], F32), ('X', X, [128, 8, 128], F32),
                                              ('Z', Z, [128, 16], F32), ('g128', g128, [128, 8], F32), ('lg', lg, [128, 16], F32),
                                              ('Sf', Sf, [128, NT, 128], BF16), ('Rb', Rb, [128, NT, 128], BF16), ('qT', qT, [128, T], BF16),
                                              ('kT', kT, [128, T], BF16), ('vtok', vtok, [128, NT, 128], BF16), ('ktok', ktok, [128, NT, 128], BF16),
                                              ('vf', vf, [128, NT, 128], BF16), ('AT1', AT[1], [128, 2, 128], BF16), ('qxf1', qxf[1], [128, 128], BF16),
                                              ('qxb1', qxb[1], [128, 128], BF16)):
                        xdump(nm_, t_, sh_, dt_)
            kb.release(m)

        def phase_attn(l, oaT):
            need_ctx = (l == 0)
            m = kb.mark()
            qT = kb.sb('aqT', [128, 4, T], BF16); kT = kb.sb('akT', [128, T], BF16)
            Va = kb.sb('Va', [128, NT, 2, 66], BF16)
            msk = kb.sb('msk', [128, 256], BF16)
            kb.dma('sp', msk[:], amask[:, :], w=['msk'])
            snk = kb.sb('snk', [128, 8], F32)
            kb.dma('sp', snk[:], sink_in[l].partition_broadcast(128), w=['snk'])
            kb.op('act', lambda e: e.activation(out=snk[:], in_=snk[:], func=AF.Exp), r=['snk'], w=['snk'])
            kb.op('pool', lambda e: e.memset(Va[:, :, :, 64:66], 1.0), w=['Va'])
            wq = kb.sb('awq', [128, 8, 1024], BF16); wk = kb.sb('awk', [128, 8, 256], BF16); wv = kb.sb('awv', [128, 8, 128], BF16)
            load_w(wq, 'awq', w_ext[l][:, QA:QA + 1024], 8, 1024)
            load_w(wk, 'awk', w_ext[l][:, KA:KA + 256], 8, 256)
            load_w(wv, 'awv', w_ext[l][:, VA:VA + 128], 8, 128)
            rc = [kb.sb(f'arc{i}', [128, 512], F32) for i in range(2)]; rs = [kb.sb(f'ars{i}', [128, 512], F32) for i in range(2)]
            t1 = [kb.sb(f'at1{i}', [128, 512], F32) for i in range(2)]; t2 = [kb.sb(f'at2{i}', [128, 512], F32) for i in range(2)]
            for bi, (s0, n) in enumerate(TB):
                if s0 == 0:
                    for g in range(4):
                        proj_fm(wq, 'awq', g * 128, s0, n, g % 2)
                        kb.op('act', lambda e: e.copy(out=qT[:, g, s0:s0 + n], in_=PSA[:, g % 2, 0:n]), r=[('ps', g % 2)], w=['aqT'])
                    proj_fm(wk, 'awk', 0, s0, n, 2)
                    kb.op('act', lambda e: e.copy(out=kT[:, s0:s0 + n], in_=PSA[:, 2, 0:n]), r=[('ps', 2)], w=['akT'])
                else:
                    j = bi % 2; p0 = s0 - 256
                    kb.dma('sp', rc[j][:, 0:n], ropeA[0, :, p0:p0 + n], w=[('rc', j)])
                    kb.dma('sp', rs[j][:, 0:n], ropeA[1, :, p0:p0 + n], w=[('rs', j)])
                    for g in range(4):
                        b0 = 2 * (g % 2)
                        proj_fm(wq, 'awq', g * 128, s0, n, b0); proj_fm(wq, 'awq', 512 + g * 128, s0, n, b0 + 1)
                        rope_evac(b0, b0 + 1, qT[:, g, s0:s0 + n], 'aqT', rc[j], rs[j], n, t1[j], t2[j], j)
                    proj_fm(wk, 'awk', 0, s0, n, 4); proj_fm(wk, 'awk', 128, s0, n, 5)
                    rope_evac(4, 5, kT[:, s0:s0 + n], 'akT', rc[j], rs[j], n, t1[j], t2[j], j)
                for t in range(s0 // 128, (s0 + n) // 128):
                    for k in range(8):
                        kb.op('pe', lambda e: e.matmul(PSA[:, 6, 0:128], lhsT=hT[:, k, t * 128:(t + 1) * 128], rhs=wv[:, k, :],
                                                       start=(k == 0), stop=(k == 7)), r=['hT', 'awv'], w=[('ps', 6)])
                    kb.op('act', lambda e: e.copy(out=Va[:, t, :, 0:64], in_=PSA[:, 6, 0:128].rearrange("p (h e) -> p h e", h=2)),
                          r=[('ps', 6)], w=['Va'])
            PT = [[kb.sb(f'PT{i}_{j}', [128, 4, 128], BF16) for j in range(5)] for i in range(2)]
            oat = [kb.sb(f'oat{i}', [128, 8, 64], BF16) for i in range(2)]
            den = [kb.sb(f'den{i}', [128, 4], F32) for i in range(2)]
            def keys_of(t):
                if t < 2:
                    return [(0, None), (1, None)]
                keys = []
                if t > 2: keys.append((t - 1, 0))
                keys.append((t, None))
                if t < NT - 1: keys.append((t + 1, 1))
                return keys + [(0, None), (1, None)]

            groups = [(t, h2) for t in (range(NT) if need_ctx else range(2, NT)) for h2 in range(2)]
            SB = [0, 1, 2, 5, 6]
            sbc = [0]

            def SE(n):
                t, h2 = groups[n]; i = n % 2
                ps_ = slice(h2 * 64, (h2 + 1) * 64)
                for ki, (kt, mk) in enumerate(keys_of(t)):
                    bank = SB[sbc[0] % 5]; sbc[0] += 1
                    kb.op('pe', lambda e: e.matmul(PSA[:, bank, :].rearrange("p (g q) -> p g q", g=4), lhsT=kT[ps_, kt * 128:(kt + 1) * 128],
                                                   rhs=qT[ps_, :, t * 128:(t + 1) * 128], start=True, stop=True), r=['akT', 'aqT'], w=[('ps', bank)])
                    kb.op('act', lambda e: e.activation(out=PT[i][ki][:], in_=PSA[:, bank, :].rearrange("p (g q) -> p g q", g=4), func=AF.Exp, scale=0.125),
                          r=[('ps', bank)], w=[('PT', i, ki)])
                    if mk is not None:
                        kb.op('pool', lambda e: e.tensor_tensor(out=PT[i][ki][:], in0=PT[i][ki][:], in1=bc(msk[:, mk * 128:(mk + 1) * 128], [[0, 4], [1, 128]]),
                                                                op=ALU.mult), r=[('PT', i, ki), 'msk'], w=[('PT', i, ki)])

            def PVN(n):
                t, h2 = groups[n]; i = n % 2; ti = t % 2
                keys = keys_of(t)
                ob = 3 + i
                for g in range(4):
                    for ki, (kt, mk) in enumerate(keys):
                        kb.op('pe', lambda e: e.matmul(PSA[:, ob, g * 66:g * 66 + 65], lhsT=PT[i][ki][:, g, :], rhs=Va[:, kt, h2, 0:65],
                                                       start=(ki == 0), stop=(ki == len(keys) - 1)), r=[('PT', i, ki), 'Va'], w=[('ps', ob)])
                ov = PSA[:, ob, 0:264].rearrange("p (g e) -> p g e", g=4)
                kb.op('dve', lambda e: e.tensor_tensor(out=den[i][:], in0=ov[:, :, 64], in1=snk[:, h2 * 4:(h2 + 1) * 4], op=ALU.add),
                      r=[('ps', ob), 'snk'], w=[('den', i)])
                kb.op('dve', lambda e: e.reciprocal(out=den[i][:], in_=den[i][:]), r=[('den', i)], w=[('den', i)])
                kb.op('dve', lambda e: e.tensor_tensor(out=oat[ti][:, h2 * 4:(h2 + 1) * 4, :], in0=ov[:, :, 0:64], in1=bc(den[i][:, 0:4], [[1, 4], [0, 64]]),
                                                       op=ALU.mult), r=[('ps', ob), ('den', i)], w=[('oat', ti)])
                if h2 == 1:
                    for k in range(4):
                        kb.op('pe', lambda e: e.transpose(out=PST[:, k, :], in_=oat[ti][:, 2 * k:2 * k + 2, :].rearrange("p h e -> p (h e)"), identity=ident[:]),
                              r=[('oat', ti), 'ident'], w=['pst'])
                    kb.op('act', lambda e: e.copy(out=oaT[:, :, t * 128:(t + 1) * 128], in_=PST[:, 0:4, :]), r=['pst'], w=['oaT'])

            SE(0)
            for n in range(len(groups)):
                if n + 1 < len(groups):
                    SE(n + 1)
                PVN(n)
            kb.release(m)

        def phase_four(l, ofT):
            need_ctx = (l == 0)
            m = kb.mark()
            wfu = kb.sb('wfu', [128, 8, 512], BF16)
            load_w(wfu, 'wfu', w_ext[l][:, FU:FU + 512], 8, 512)
            dC = kb.sb('dC', [128, 256], BF16)
            kb.dma('sp', dC[:], dftC[:, :], w=['dC'])
            W = kb.sb('W', [128, NT, 4, 256], BF16)
            uT = [kb.sb(f'uT{i}', [128, T], BF16) for i in range(2)]
            for g in range(4):
                i = g % 2
                for bi, (s0, n) in enumerate(TB):
                    proj_fm(wfu, 'wfu', g * 128, s0, n, bi % 2)
                    kb.op('act', lambda e: e.copy(out=uT[i][:, s0:s0 + n], in_=PSA[:, bi % 2, 0:n]), r=[('ps', bi % 2)], w=[('uT', i)])
                for t in range(NT):
                    bank = 2 + t % 2
                    kb.op('pe', lambda e: e.matmul(PSA[:, bank, 0:256], lhsT=uT[i][:, t * 128:(t + 1) * 128], rhs=dC[:], start=True, stop=True),
                          r=[('uT', i), 'dC'], w=[('ps', bank)])
                    kb.op('dve', lambda e: e.tensor_copy(out=W[:, t, g, :], in_=PSA[:, bank, 0:256]), r=[('ps', bank)], w=['W'])
            Cb = [kb.sb(f'Cb{i}', [128, 16, 256], BF16) for i in range(2)]
            Nb = [kb.sb(f'Nb{i}', [128, 16, 256], BF16) for i in range(2)]
            it = 0
            for nb in range(8):
                i = nb % 2
                kb.dma('sp', Cb[i][:], dftN[0, :, nb * 256:(nb + 1) * 256].rearrange("(t p) c -> p t c", p=128), w=[('Cb', i)])
                kb.dma('sp', Nb[i][:], dftN[1, :, nb * 256:(nb + 1) * 256].rearrange("(t p) c -> p t c", p=128), w=[('Nb', i)])
                for g in range(4):
                    bank = 4 + it % 2; it += 1
                    for t in range(16):
                        kb.op('pe', lambda e: e.matmul(PSA[:, bank, 0:256], lhsT=W[:, 2 + t, g, 0:128], rhs=Cb[i][:, t, :], start=(t == 0), stop=False),
                              r=['W', ('Cb', i)], w=[('ps', bank)])
                        kb.op('pe', lambda e: e.matmul(PSA[:, bank, 0:256], lhsT=W[:, 2 + t, g, 128:256], rhs=Nb[i][:, t, :], start=False, stop=(t == 15)),
                              r=['W', ('Nb', i)], w=[('ps', bank)])
                    kb.op('act', lambda e: e.copy(out=ofT[:, g, 256 + nb * 256:256 + (nb + 1) * 256], in_=PSA[:, bank, 0:256]), r=[('ps', bank)], w=['ofT'])
            if need_ctx:
                kb.dma('sp', Cb[0][:, 0:2, :], dft256[0].rearrange("(t p) c -> p t c", p=128), w=[('Cb', 0)])
                kb.dma('sp', Nb[0][:, 0:2, :], dft256[1].rearrange("(t p) c -> p t c", p=128), w=[('Nb', 0)])
                for g in range(4):
                    bank = 4 + g % 2
                    for t in range(2):
                        kb.op('pe', lambda e: e.matmul(PSA[:, bank, 0:256], lhsT=W[:, t, g, 0:128], rhs=Cb[0][:, t, :], start=(t == 0), stop=False),
                              r=['W', ('Cb', 0)], w=[('ps', bank)])
                        kb.op('pe', lambda e: e.matmul(PSA[:, bank, 0:256], lhsT=W[:, t, g, 128:256], rhs=Nb[0][:, t, :], start=False, stop=(t == 1)),
                              r=['W', ('Nb', 0)], w=[('ps', bank)])
                    kb.op('act', lambda e: e.copy(out=ofT[:, g, 0:256], in_=PSA[:, bank, 0:256]), r=[('ps', bank)], w=['ofT'])
            kb.release(m)

        def resid_update(l, gi, tiles, src_fn, src_keys):
            pass

        def phase_out(l, mT):
            m = kb.mark()
            wo = kb.sb('wo', [128, 8, D], BF16)
            load_w(wo, 'wo', w_out[l][:, :], 8, D)
            g1 = []
            for row in (0, 1):
                g_ = kb.sb(f'g1_{row}', [128, D], F32)
                kb.dma('sp', g_[:], modbuf[l, row, 2 * D:3 * D].partition_broadcast(128), w=[f'g1_{row}'])
                g1.append(g_)
            xt = [kb.sb(f'oxt{i}', [128, D], F32) for i in range(3)]
            yt = [kb.sb(f'oyt{i}', [128, D], F32) for i in range(3)]
            otl = list(range(NT) if l == 0 else range(2, NT))
            kb.dma('sp', xt[otl[0] % 3][:], xbuf[otl[0] * 128:(otl[0] + 1) * 128, :], r=[('x', otl[0])], w=[('oxt', otl[0] % 3)])
            for n_, t in enumerate(otl):
                i = t % 3; row = 0 if t < 2 else 1
                if n_ + 1 < len(otl):
                    t2 = otl[n_ + 1]
                    kb.dma('sp', xt[t2 % 3][:], xbuf[t2 * 128:(t2 + 1) * 128, :], r=[('x', t2)], w=[('oxt', t2 % 3)])
                for hf in range(2):
                    bank = 2 * i + hf
                    for fc in range(8):
                        kb.op('pe', lambda e: e.matmul(PSA[:, bank, :], lhsT=mT[:, fc, t * 128:(t + 1) * 128], rhs=wo[:, fc, hf * 512:(hf + 1) * 512],
                                                       start=(fc == 0), stop=(fc == 7)), r=['mT', 'wo'], w=[('ps', bank)])
                    kb.op('dve', lambda e: e.tensor_tensor(out=yt[i][:, hf * 512:(hf + 1) * 512], in0=PSA[:, bank, :], in1=g1[row][:, hf * 512:(hf + 1) * 512],
                                                           op=ALU.mult), r=[('ps', bank), f'g1_{row}'], w=[('oyt', i)])
                kb.op('pool', lambda e: e.tensor_tensor(out=yt[i][:], in0=yt[i][:], in1=xt[i][:], op=ALU.add), r=[('oyt', i), ('oxt', i)], w=[('oyt', i)])
                kb.dma('sp', xbuf[t * 128:(t + 1) * 128, :], yt[i][:], r=[('oyt', i)], w=[('x', t)])
            kb.release(m)

        def phase_moe(l):
            m = kb.mark()
            tiles = list(range(NT) if l == 0 else range(2, NT))
            blocks = TB if l == 0 else TB[1:]
            logits = kb.sb('logits', [128, NT, 36], F32)
            Wt = kb.sb('Wt', [128, NT, 32], F32)
            kb.op('pool', lambda e: e.memset(logits[:], 0.0), w=['logits'])
            phase_norm(l, 2, logits)
            if os.environ.get('MOE_CUT') == '1':
                kb.release(m); return
            m2 = kb.mark()
            lgG = logits[:, :, 0:4]; lgE = logits[:, :, 4:36]
            gmax = kb.sb('gmax', [128, NT], F32); ohg = kb.sb('ohg', [128, NT, 4], F32); eg = kb.sb('eg', [128, NT, 4], F32)
            pg = kb.sb('pg', [128, NT], F32); me = kb.sb('me', [128, NT, 32], F32); oh1 = kb.sb('oh1', [128, NT, 32], F32)
            oh2 = kb.sb('oh2', [128, NT, 32], F32); m1 = kb.sb('m1', [128, NT], F32); m2_ = kb.sb('m2', [128, NT], F32)
            w1 = kb.sb('w1', [128, NT], F32); w2 = kb.sb('w2', [128, NT], F32)
            b1 = lambda a, n_: bc(a, [[1, NT], [0, n_]])
            kb.op('dve', lambda e: e.reduce_max(out=gmax[:], in_=lgG, axis=AX.X), r=['logits'], w=['gmax'])
            kb.op('dve', lambda e: e.tensor_tensor(out=ohg[:], in0=lgG, in1=b1(gmax[:, 0:NT], 4), op=ALU.is_equal), r=['logits', 'gmax'], w=['ohg'])
            kb.op('dve', lambda e: e.tensor_tensor(out=eg[:], in0=lgG, in1=b1(gmax[:, 0:NT], 4), op=ALU.subtract), r=['logits', 'gmax'], w=['eg'])
            kb.op('act', lambda e: e.activation(out=eg[:], in_=eg[:], func=AF.Exp), r=['eg'], w=['eg'])
            kb.op('dve', lambda e: e.reduce_sum(out=pg[:], in_=eg[:], axis=AX.X), r=['eg'], w=['pg'])
            kb.op('dve', lambda e: e.reciprocal(out=pg[:], in_=pg[:]), r=['pg'], w=['pg'])
            kb.op('dve', lambda e: e.tensor_scalar(out=ohg[:], in0=ohg[:], scalar1=-1.0, scalar2=1e30, op0=ALU.add, op1=ALU.mult), r=['ohg'], w=['ohg'])
            kb.op('dve', lambda e: e.tensor_tensor(out=me[:].rearrange("p t (g x) -> p t g x", g=4), in0=lgE.rearrange("p t (g x) -> p t g x", g=4),
                                                   in1=bc(ohg[:, 0:NT, :], [[4, NT], [1, 4], [0, 8]]), op=ALU.add), r=['logits', 'ohg'], w=['me'])
            kb.op('dve', lambda e: e.reduce_max(out=m1[:], in_=me[:], axis=AX.X), r=['me'], w=['m1'])
            kb.op('dve', lambda e: e.tensor_tensor(out=oh1[:], in0=me[:], in1=b1(m1[:, 0:NT], 32), op=ALU.is_equal), r=['me', 'm1'], w=['oh1'])
            kb.op('dve', lambda e: e.scalar_tensor_tensor(out=me[:], in0=oh1[:], scalar=-1e30, in1=me[:], op0=ALU.mult, op1=ALU.add), r=['oh1', 'me'], w=['me'])
            kb.op('dve', lambda e: e.reduce_max(out=m2_[:], in_=me[:], axis=AX.X), r=['me'], w=['m2'])
            kb.op('dve', lambda e: e.tensor_tensor(out=oh2[:], in0=me[:], in1=b1(m2_[:, 0:NT], 32), op=ALU.is_equal), r=['me', 'm2'], w=['oh2'])
            kb.op('dve', lambda e: e.tensor_tensor(out=w1[:], in0=m1[:], in1=m2_[:], op=ALU.subtract), r=['m1', 'm2'], w=['w1'])
            kb.op('act', lambda e: e.activation(out=w2[:], in_=w1[:], func=AF.Sigmoid, scale=-1.0), r=['w1'], w=['w2'])
            kb.op('act', lambda e: e.activation(out=w1[:], in_=w1[:], func=AF.Sigmoid), r=['w1'], w=['w1'])
            kb.op('dve', lambda e: e.tensor_tensor(out=w1[:], in0=w1[:], in1=pg[:], op=ALU.mult), r=['w1', 'pg'], w=['w1'])
            kb.op('dve', lambda e: e.tensor_tensor(out=w2[:], in0=w2[:], in1=pg[:], op=ALU.mult), r=['w2', 'pg'], w=['w2'])
            kb.op('dve', lambda e: e.tensor_tensor(out=oh1[:], in0=oh1[:], in1=b1(w1[:, 0:NT], 32), op=ALU.mult), r=['oh1', 'w1'], w=['oh1'])
            kb.op('dve', lambda e: e.tensor_tensor(out=oh2[:], in0=oh2[:], in1=b1(w2[:, 0:NT], 32), op=ALU.mult), r=['oh2', 'w2'], w=['oh2'])
            kb.op('dve', lambda e: e.tensor_tensor(out=Wt[:], in0=oh1[:], in1=oh2[:], op=ALU.add), r=['oh1', 'oh2'], w=['Wt'])
            kb.release(m2)
            if os.environ.get('MOE_CUT') == '2':
                kb.release(m); return
            acc = kb.sb('acc', [128, NT, D], F32)
            m3 = kb.mark()
            wg = [kb.sb(f'ewg{i}', [128, 8, 512], BF16) for i in range(2)]
            wu = [kb.sb(f'ewu{i}', [128, 8, 512], BF16) for i in range(2)]
            wd = [kb.sb(f'ewd{i}', [128, 4, D], BF16) for i in range(2)]
            aT = [kb.sb(f'aT{i}', [128, 4, 512], BF16) for i in range(2)]
            sgt = [kb.sb(f'sgt{i}', [128, 512], F32) for i in range(2)]
            it = 0; ih = 0
            for ex in range(int(os.environ.get('MOE_NEXP', '32'))):
                j = ex % 2
                load_w(wg[j], ('ewg', j), w_eg[l, ex], 8, 512, engs=('pool', 'act'))
                load_w(wu[j], ('ewu', j), w_eu[l, ex], 8, 512, engs=('pool', 'act'))
                load_w(wd[j], ('ewd', j), w_ed[l, ex], 4, D, engs=('pool', 'act'))
                for (s0, n) in blocks:
                    i = it % 2; it += 1
                    for hc in range(4):
                        ii = ih % 2; ih += 1
                        for k in range(8):
                            kb.op('pe', lambda e: e.matmul(PSA[:, ii, 0:n], lhsT=wg[j][:, k, hc * 128:(hc + 1) * 128], rhs=hT[:, k, s0:s0 + n],
                                                           start=(k == 0), stop=(k == 7)), r=[('ewg', j), 'hT'], w=[('ps', ii)])
                        for k in range(8):
                            kb.op('pe', lambda e: e.matmul(PSA[:, 2 + ii, 0:n], lhsT=wu[j][:, k, hc * 128:(hc + 1) * 128], rhs=hT[:, k, s0:s0 + n],
                                                           start=(k == 0), stop=(k == 7)), r=[('ewu', j), 'hT'], w=[('ps', 2 + ii)])
                        kb.op('act', lambda e: e.activation(out=sgt[ii][:, 0:n], in_=PSA[:, ii, 0:n], func=AF.Silu), r=[('ps', ii)], w=[('sgt', ii)])
                        kb.op('dve', lambda e: e.tensor_tensor(out=aT[i][:, hc, 0:n], in0=sgt[ii][:, 0:n], in1=PSA[:, 2 + ii, 0:n], op=ALU.mult),
                              r=[('sgt', ii), ('ps', 2 + ii)], w=[('aT', i)])
                    for t in range(s0 // 128, (s0 + n) // 128):
                        tl = t * 128 - s0
                        for hf in range(2):
                            bank = 4 + (2 * t + hf) % 3
                            for hc in range(4):
                                kb.op('pe', lambda e: e.matmul(PSA[:, bank, :], lhsT=aT[i][:, hc, tl:tl + 128], rhs=wd[j][:, hc, hf * 512:(hf + 1) * 512],
                                                               start=(hc == 0), stop=(hc == 3)), r=[('aT', i), ('ewd', j)], w=[('ps', bank)])
                            a_ = acc[:, t, hf * 512:(hf + 1) * 512]
                            if ex == 0:
                                kb.op('dve', lambda e: e.tensor_scalar(out=a_, in0=PSA[:, bank, :], scalar1=Wt[:, t, ex:ex + 1], scalar2=None, op0=ALU.mult),
                                      r=[('ps', bank), 'Wt'], w=[('acc', t)])
                            else:
                                kb.op('dve', lambda e: e.scalar_tensor_tensor(out=a_, in0=PSA[:, bank, :], scalar=Wt[:, t, ex:ex + 1], in1=a_,
                                                                              op0=ALU.mult, op1=ALU.add), r=[('ps', bank), 'Wt', ('acc', t)], w=[('acc', t)])
            kb.release(m3)
            g2 = []
            for row in (0, 1):
                g_ = kb.sb(f'g2_{row}', [128, D], F32)
                kb.dma('sp', g_[:], modbuf[l, row, 5 * D:6 * D].partition_broadcast(128), w=[f'g2_{row}'])
                g2.append(g_)
            xt = [kb.sb(f'mxt{i}', [128, D], F32) for i in range(2)]
            for t in tiles:
                i = t % 2; row = 0 if t < 2 else 1
                kb.dma('sp', xt[i][:], xbuf[t * 128:(t + 1) * 128, :], r=[('x', t)], w=[('mxt', i)])
                kb.op('dve', lambda e: e.tensor_tensor(out=acc[:, t, :], in0=acc[:, t, :], in1=g2[row][:], op=ALU.mult), r=[('acc', t), f'g2_{row}'], w=[('acc', t)])
                kb.op('pool', lambda e: e.tensor_tensor(out=xt[i][:], in0=xt[i][:], in1=acc[:, t, :], op=ALU.add), r=[('acc', t), ('mxt', i)], w=[('mxt', i)])
                kb.dma('sp', xbuf[t * 128:(t + 1) * 128, :], xt[i][:], r=[('mxt', i)], w=[('x', t)])
            kb.release(m)

        moe_state = {}

        def phase_moe_sparse(l):
            IOA = bass.IndirectOffsetOnAxis
            ABv = AB.rearrange("r (h c) -> (r h) c", h=2)
            if 'bregs' not in moe_state:
                regs = {}
                for nm_, v_ in (('rec', 32 * CAP - 1), ('tok', T - 1), ('ab', 2 * T - 1)):
                    rg = nc.gpsimd.alloc_register('bnd_' + nm_)
                    nc.gpsimd.reg_mov(rg, v_)
                    regs[nm_] = rg
                moe_state['bregs'] = regs
            BR = moe_state['bregs']
            m = kb.mark()
            t0 = 0 if l == 0 else 2
            tiles = list(range(t0, NT))
            logits = kb.sb('logits', [128, NT, 36], F32)
            kb.op('pool', lambda e: e.memset(logits[:], 0.0), w=['logits'])
            zt = kb.sb('zt', [128, D], F32)
            kb.op('pool', lambda e: e.memset(zt[:], 0.0), w=['zt'])
            for q in range(2 * NT):
                kb.dma('act', AB[q * 128:(q + 1) * 128, :], zt[:], r=['zt'], w=[('ABz', q)])
            ri_ = kb.sb('recinit', [128, (32 * CAP) // 128, 4], F32)
            kb.op('pool', lambda e: e.memset(ri_[:], 1.0e6), w=['recinit'])
            kb.op('pool', lambda e: e.memset(ri_[:, :, 2:3], 0.0), r=['recinit'], w=['recinit'])
            kb.dma('act', rec_d.rearrange("(p s) c -> p s c", p=128), ri_[:], r=['recinit'], w=['rec_d'])
            phase_norm(l, 2, logits, sparse=True)
            MC = kb.sb('MC', [128, 32 + NT], F32)
            kb.dma('sp', MC[:], mconst[:, :], w=['MC'])
            eC = MC[:, 0:32]; tokid = MC[:, 32:32 + NT]
            lgG = logits[:, :, 0:4]; lgE = logits[:, :, 4:36]
            gmax = kb.sb('gmax', [128, NT], F32); ohg = kb.sb('ohg', [128, NT, 4], F32); eg = kb.sb('eg', [128, NT, 4], F32)
            pg = kb.sb('pg', [128, NT], F32); me = kb.sb('me', [128, NT, 32], F32); oh1 = kb.sb('oh1', [128, NT, 32], F32)
            oh2 = kb.sb('oh2', [128, NT, 32], F32); m1 = kb.sb('m1', [128, NT], F32); m2_ = kb.sb('m2', [128, NT], F32)
            w1 = kb.sb('w1', [128, NT], F32); w2 = kb.sb('w2', [128, NT], F32)
            b1 = lambda a, n_: bc(a, [[1, NT], [0, n_]])
            kb.op('dve', lambda e: e.reduce_max(out=gmax[:], in_=lgG, axis=AX.X), r=['logits'], w=['gmax'])
            kb.op('dve', lambda e: e.tensor_tensor(out=ohg[:], in0=lgG, in1=b1(gmax[:, 0:NT], 4), op=ALU.is_equal), r=['logits', 'gmax'], w=['ohg'])
            kb.op('dve', lambda e: e.tensor_tensor(out=eg[:], in0=lgG, in1=b1(gmax[:, 0:NT], 4), op=ALU.subtract), r=['logits', 'gmax'], w=['eg'])
            kb.op('act', lambda e: e.activation(out=eg[:], in_=eg[:], func=AF.Exp), r=['eg'], w=['eg'])
            kb.op('dve', lambda e: e.reduce_sum(out=pg[:], in_=eg[:], axis=AX.X), r=['eg'], w=['pg'])
            kb.op('dve', lambda e: e.reciprocal(out=pg[:], in_=pg[:]), r=['pg'], w=['pg'])
            kb.op('dve', lambda e: e.tensor_scalar(out=ohg[:], in0=ohg[:], scalar1=-1.0, scalar2=1e30, op0=ALU.add, op1=ALU.mult), r=['ohg'], w=['ohg'])
            kb.op('dve', lambda e: e.tensor_tensor(out=me[:].rearrange("p t (g x) -> p t g x", g=4), in0=lgE.rearrange("p t (g x) -> p t g x", g=4),
                                                   in1=bc(ohg[:, 0:NT, :], [[4, NT], [1, 4], [0, 8]]), op=ALU.add), r=['logits', 'ohg'], w=['me'])
            kb.op('dve', lambda e: e.reduce_max(out=m1[:], in_=me[:], axis=AX.X), r=['me'], w=['m1'])
            kb.op('dve', lambda e: e.tensor_tensor(out=oh1[:], in0=me[:], in1=b1(m1[:, 0:NT], 32), op=ALU.is_equal), r=['me', 'm1'], w=['oh1'])
            kb.op('dve', lambda e: e.scalar_tensor_tensor(out=me[:], in0=oh1[:], scalar=-1e30, in1=me[:], op0=ALU.mult, op1=ALU.add), r=['oh1', 'me'], w=['me'])
            kb.op('dve', lambda e: e.reduce_max(out=m2_[:], in_=me[:], axis=AX.X), r=['me'], w=['m2'])
            kb.op('dve', lambda e: e.tensor_tensor(out=oh2[:], in0=me[:], in1=b1(m2_[:, 0:NT], 32), op=ALU.is_equal), r=['me', 'm2'], w=['oh2'])
            kb.op('dve', lambda e: e.tensor_tensor(out=w1[:], in0=m1[:], in1=m2_[:], op=ALU.subtract), r=['m1', 'm2'], w=['w1'])
            kb.op('act', lambda e: e.activation(out=w2[:], in_=w1[:], func=AF.Sigmoid, scale=-1.0), r=['w1'], w=['w2'])
            kb.op('act', lambda e: e.activation(out=w1[:], in_=w1[:], func=AF.Sigmoid), r=['w1'], w=['w1'])
            kb.op('dve', lambda e: e.tensor_tensor(out=w1[:], in0=w1[:], in1=pg[:], op=ALU.mult), r=['w1', 'pg'], w=['w1'])
            kb.op('dve', lambda e: e.tensor_tensor(out=w2[:], in0=w2[:], in1=pg[:], op=ALU.mult), r=['w2', 'pg'], w=['w2'])
            if t0 > 0:
                kb.op('pool', lambda e: e.memset(oh1[:, 0:t0, :], 0.0), r=['oh1'], w=['oh1'])
                kb.op('pool', lambda e: e.memset(oh2[:, 0:t0, :], 0.0), r=['oh2'], w=['oh2'])
            selb = kb.sb('selb', [128, NT * 32], BF16)
            kb.op('pool', lambda e: e.tensor_tensor(out=selb[:], in0=oh1[:].rearrange("p t e -> p (t e)"), in1=oh2[:].rearrange("p t e -> p (t e)"), op=ALU.add),
                  r=['oh1', 'oh2'], w=['selb'])
            LT = kb.sb('LT', [128, 128], BF16); ones = kb.sb('ones', [128, 128], BF16)
            kb.op('pool', lambda e: e.memset(ones[:], 1.0), w=['ones'])
            kb.op('pool', lambda e: e.memset(LT[:], 1.0), w=['LT'])
            kb.op('pool', lambda e: e.affine_select(out=LT[:], in_=LT[:], pattern=[[1, 128]], compare_op=ALU.is_gt, fill=0.0, base=0,
                                                    channel_multiplier=-1), r=['LT'], w=['LT'])
            slot = kb.sb('slot', [128, NT, 32], F32); tot = kb.sb('tot', [128, NT, 32], F32); cum = kb.sb('cum', [128, NT, 32], F32)
            sl2 = slot[:].rearrange("p t e -> p (t e)"); to2 = tot[:].rearrange("p t e -> p (t e)")
            for (c0, c1, bank) in ((0, 512, 0), (512, NT * 32, 1)):
                kb.op('pe', lambda e: e.matmul(PSA[:, bank, 0:c1 - c0], lhsT=LT[:], rhs=selb[:, c0:c1], start=True, stop=True), r=['LT', 'selb'], w=[('ps', bank)])
                kb.op('dve', lambda e: e.tensor_copy(out=sl2[:, c0:c1], in_=PSA[:, bank, 0:c1 - c0]), r=[('ps', bank)], w=['slot'])
                kb.op('pe', lambda e: e.matmul(PSA[:, 2 + bank, 0:c1 - c0], lhsT=ones[:], rhs=selb[:, c0:c1], start=True, stop=True), r=['ones', 'selb'], w=[('ps', 2 + bank)])
                kb.op('dve', lambda e: e.tensor_copy(out=to2[:, c0:c1], in_=PSA[:, 2 + bank, 0:c1 - c0]), r=[('ps', 2 + bank)], w=['tot'])
            kb.op('pool', lambda e: e.memset(cum[:, 0, :], 0.0), w=['cum'])
            for t in range(1, NT):
                kb.op('dve', lambda e: e.tensor_tensor(out=cum[:, t, :], in0=cum[:, t - 1, :], in1=tot[:, t - 1, :], op=ALU.add), r=['cum', 'tot'], w=['cum'])
            kb.op('dve', lambda e: e.tensor_tensor(out=slot[:], in0=slot[:], in1=cum[:], op=ALU.add), r=['slot', 'cum'], w=['slot'])
            rec = kb.sb('rec', [128, NT, 2, 4], F32)
            rr = kb.sb('rr', [128, NT, 2], F32); rri = kb.sb('rri', [128, NT, 2], I32)
            s_ = kb.sb('s_', [128, NT], F32); e_ = kb.sb('e_', [128, NT], F32)
            kb.op('pool', lambda e: e.memset(rec[:], 0.0), w=['rec'])
            for k, (oh, wk) in enumerate(((oh1, w1), (oh2, w2))):
                kb.op('dve', lambda e: e.tensor_tensor(out=me[:], in0=oh[:], in1=slot[:], op=ALU.mult), r=['oh1', 'oh2', 'slot', 'me'], w=['me'])
                kb.op('dve', lambda e: e.reduce_sum(out=s_[:], in_=me[:], axis=AX.X), r=['me'], w=['s_'])
                kb.op('dve', lambda e: e.tensor_tensor(out=me[:], in0=oh[:], in1=bc(eC, [[0, NT], [1, 32]]), op=ALU.mult), r=['oh1', 'oh2', 'MC', 'me'], w=['me'])
                kb.op('dve', lambda e: e.reduce_sum(out=e_[:], in_=me[:], axis=AX.X), r=['me'], w=['e_'])
                kb.op('dve', lambda e: e.tensor_tensor(out=e_[:], in0=e_[:], in1=s_[:], op=ALU.add), r=['e_', 's_'], w=['e_'])
                kb.op('dve', lambda e: e.tensor_scalar(out=s_[:], in0=s_[:], scalar1=float(CAP), scalar2=1.0e6, op0=ALU.is_ge, op1=ALU.mult), r=['s_'], w=['s_'])
                kb.op('dve', lambda e: e.tensor_tensor(out=rr[:, :, k], in0=e_[:], in1=s_[:], op=ALU.add), r=['e_', 's_'], w=['rr'])
                kb.op('pool', lambda e: e.tensor_copy(out=rec[:, :, k, 0], in_=tokid), r=['MC', 'rec'], w=['rec'])
                kb.op('pool', lambda e: e.tensor_scalar(out=rec[:, :, k, 1], in0=tokid, scalar1=float(k * T), scalar2=None, op0=ALU.add), r=['MC', 'rec'], w=['rec'])
                kb.op('pool', lambda e: e.tensor_scalar(out=rec[:, :, k, 3], in0=tokid, scalar1=2.0, scalar2=float(2 * k * T + 1), op0=ALU.mult, op1=ALU.add), r=['MC', 'rec'], w=['rec'])
                kb.op('pool', lambda e: e.tensor_copy(out=rec[:, :, k, 2], in_=wk[:]), r=['w1', 'w2', 'rec'], w=['rec'])
            kb.op('dve', lambda e: e.tensor_copy(out=rri[:], in_=rr[:]), r=['rr'], w=['rri'])
            if stop == f'moe{l}' and os.environ.get('XDUMP'):
                xdump('rr', rr, [128, NT, 2], F32); xdump('rri', rri, [128, NT, 2], I32); xdump('rec', rec, [128, NT, 2, 4], F32)
                xdump('slot', slot, [128, NT, 32], F32)
            kb.barrier()
            for t in tiles:
                for k in range(2):
                    kb.dma('pool', None, None, r=['rec', 'rri'], w=[('recs', t, k)],
                           fn=lambda g: g.indirect_dma_start(out=rec_d[:, :], out_offset=IOA(ap=rri[:, t, k:k + 1], axis=0), in_=rec[:, t, k, :],
                                                             in_offset=None, bounds_check=BR['rec'], oob_is_err=False))
            kb.barrier()
            if os.environ.get('MOE_CUT') == '3':
                kb.release(m); return
            m3 = kb.mark()
            stg_all[0] = list(stg) + stgx
            wg = [kb.sb(f'ewg{i}', [128, 8, 512], BF16) for i in range(2)]
            wu = [kb.sb(f'ewu{i}', [128, 8, 512], BF16) for i in range(2)]
            wd = [kb.sb(f'ewd{i}', [128, 4, D], BF16) for i in range(2)]
            XT = [kb.sb(f'XT{i}', [128, 8, CAP], BF16) for i in range(2)]
            aT = kb.sb('aT', [128, 4, CAP], BF16)
            sgt = [kb.sb(f'sgt{i}', [128, 512], F32) for i in range(2)]
            rsb = [kb.sb(f'rsb{i}', [128, NS, 4], F32) for i in range(2)]
            gi = [kb.sb(f'gi{i}', [128, NS, 4], I32) for i in range(2)]
            xg = [kb.sb(f'xg{i}', [128, D], BF16) for i in range(NS)]
            yw = [kb.sb(f'yw{i}', [128, D], F32) for i in range(3)]
            for i in range(NS):
                kb.op('pool', lambda e: e.memset(xg[i][:], 0.0), w=[('xg', i)])
            PST2 = PSA[:, 6, :].bitcast(BF16).rearrange("p (k c) -> p k c", c=128)
            stgs = stg_all[0]
            nexp = int(os.environ.get('MOE_NEXP', '32'))
            cnt = {'iy': 0, 'ih': 0}

            def w_issue(ex):
                j = ex % 2; pieces = []
                for (dst, dkey, src, K, W) in ((wg[j], ('ewg', j), w_eg[l, ex], 8, 512), (wu[j], ('ewu', j), w_eu[l, ex], 8, 512), (wd[j], ('ewd', j), w_ed[l, ex], 4, D)):
                    kk = 2048 // W
                    for k0 in range(0, K, kk):
                        i = len(pieces)
                        sv = stgs[i][:, 0:kk * W].rearrange("p (k c) -> p k c", c=W)
                        kb.dma('sp', sv, src[k0 * 128:(k0 + kk) * 128, :].rearrange("(k p) c -> p k c", p=128), w=[('stg', i)])
                        pieces.append((dst, dkey, k0, k0 + kk, sv, i))
                return pieces

            def w_cast(pieces):
                for n_, (dst, dkey, k0, k1, sv, i) in enumerate(pieces):
                    if n_ % 2 == 0:
                        kb.op('act', lambda e: e.copy(out=dst[:, k0:k1, :], in_=sv), r=[('stg', i)], w=[dkey])
                    else:
                        kb.op('dve', lambda e: e.tensor_copy(out=dst[:, k0:k1, :], in_=sv), r=[('stg', i)], w=[dkey])

            def G(ex):
                j = ex % 2
                kb.dma('sp', rsb[j][:], rec_d[ex * CAP:(ex + 1) * CAP, :].rearrange("(s p) c -> p s c", p=128), w=[('rsb', j)])
                kb.op('dve', lambda e: e.tensor_copy(out=gi[j][:], in_=rsb[j][:]), r=[('rsb', j)], w=[('gi', j)])
                for s_i in range(NS):
                    kb.dma('pool', None, None, r=[('gi', j)], w=[('xg', s_i)],
                           fn=lambda g: g.indirect_dma_start(out=xg[s_i][:], out_offset=None, in_=h2d[:, :],
                                                             in_offset=IOA(ap=gi[j][:, s_i, 0:1], axis=0), bounds_check=BR['tok'], oob_is_err=False))

            def TR(ex):
                j = ex % 2
                for s_i in range(NS):
                    pt_, pk_ = (PST, 'pst') if s_i % 2 == 0 else (PST2, ('ps', 6))
                    for k in range(8):
                        kb.op('pe', lambda e: e.transpose(out=pt_[:, k, :], in_=xg[s_i][:, k * 128:(k + 1) * 128], identity=ident[:]),
                              r=[('xg', s_i), 'ident'], w=[pk_])
                    kb.op('act', lambda e: e.copy(out=XT[j][:, :, s_i * 128:(s_i + 1) * 128], in_=pt_[:]), r=[pk_], w=[('XT', j)])

            def F1(ex):
                j = ex % 2
                for c0 in range(0, CAP, 512):
                    n = min(512, CAP - c0)
                    for hc in range(4):
                        ii = cnt['ih'] % 2; cnt['ih'] += 1
                        for k in range(8):
                            kb.op('pe', lambda e: e.matmul(PSA[:, ii, 0:n], lhsT=wg[j][:, k, hc * 128:(hc + 1) * 128], rhs=XT[j][:, k, c0:c0 + n],
                                                           start=(k == 0), stop=(k == 7)), r=[('ewg', j), ('XT', j)], w=[('ps', ii)])
                        for k in range(8):
                            kb.op('pe', lambda e: e.matmul(PSA[:, 2 + ii, 0:n], lhsT=wu[j][:, k, hc * 128:(hc + 1) * 128], rhs=XT[j][:, k, c0:c0 + n],
                                                           start=(k == 0), stop=(k == 7)), r=[('ewu', j), ('XT', j)], w=[('ps', 2 + ii)])
                        kb.op('act', lambda e: e.activation(out=sgt[ii][:, 0:n], in_=PSA[:, ii, 0:n], func=AF.Silu), r=[('ps', ii)], w=[('sgt', ii)])
                        kb.op('dve', lambda e: e.tensor_tensor(out=aT[:, hc, c0:c0 + n], in0=sgt[ii][:, 0:n], in1=PSA[:, 2 + ii, 0:n], op=ALU.mult),
                              r=[('sgt', ii), ('ps', 2 + ii)], w=['aT'])

            def F2(ex):
                j = ex % 2
                for s_i in range(NS):
                    q = cnt['iy'] % 3; cnt['iy'] += 1
                    for hf in range(2):
                        bank = 4 + hf
                        for hc in range(4):
                            kb.op('pe', lambda e: e.matmul(PSA[:, bank, :], lhsT=aT[:, hc, s_i * 128:(s_i + 1) * 128], rhs=wd[j][:, hc, hf * 512:(hf + 1) * 512],
                                                           start=(hc == 0), stop=(hc == 3)), r=['aT', ('ewd', j)], w=[('ps', bank)])
                        kb.op('dve', lambda e: e.tensor_scalar(out=yw[q][:, hf * 512:(hf + 1) * 512], in0=PSA[:, bank, :], scalar1=rsb[j][:, s_i, 2:3], scalar2=None,
                                                               op0=ALU.mult), r=[('ps', bank), ('rsb', j)], w=[('yw', q)])
                    kb.dma('pool', None, None, r=[('yw', q), ('gi', j)], w=[('ABs', ex, s_i)],
                           fn=lambda g: g.indirect_dma_start(out=AB[:, :], out_offset=IOA(ap=gi[j][:, s_i, 1:2], axis=0),
                                                             in_=yw[q][:], in_offset=None, bounds_check=BR['ab'], oob_is_err=False))

            w_cast(w_issue(0)); G(0); TR(0)
            for ex in range(nexp):
                nxt = ex + 1 < nexp
                if nxt:
                    pcs = w_issue(ex + 1)
                    G(ex + 1)
                F1(ex)
                if nxt:
                    w_cast(pcs)
                    TR(ex + 1)
                F2(ex)
            stg_all[0] = list(stg)
            stg_i[0] = 0
            kb.release(m3)
            g2 = []
            for row in (0, 1):
                g_ = kb.sb(f'g2_{row}', [128, D], F32)
                kb.dma('sp', g_[:], modbuf[l, row, 5 * D:6 * D].partition_broadcast(128), w=[f'g2_{row}'])
                g2.append(g_)
            xt = [kb.sb(f'mxt{i}', [128, D], F32) for i in range(2)]
            ya = [kb.sb(f'mya{i}', [128, D], F32) for i in range(2)]
            yb = [kb.sb(f'myb{i}', [128, D], F32) for i in range(2)]
            def rload(t):
                i = t % 2
                kb.dma('sp', xt[i][:], xbuf[t * 128:(t + 1) * 128, :], r=[('x', t)], w=[('mxt', i)])
                kb.dma('sp', ya[i][:], AB[t * 128:(t + 1) * 128, :], w=[('mya', i)])
                kb.dma('sp', yb[i][:], AB[T + t * 128:T + (t + 1) * 128, :], w=[('myb', i)])
            rload(tiles[0])
            for n_, t in enumerate(tiles):
                i = t % 2; row = 0 if t < 2 else 1
                if n_ + 1 < len(tiles):
                    rload(tiles[n_ + 1])
                kb.op('pool', lambda e: e.tensor_tensor(out=ya[i][:], in0=ya[i][:], in1=yb[i][:], op=ALU.add), r=[('mya', i), ('myb', i)], w=[('mya', i)])
                kb.op('dve', lambda e: e.tensor_tensor(out=ya[i][:], in0=ya[i][:], in1=g2[row][:], op=ALU.mult), r=[('mya', i), f'g2_{row}'], w=[('mya', i)])
                kb.op('pool', lambda e: e.tensor_tensor(out=xt[i][:], in0=xt[i][:], in1=ya[i][:], op=ALU.add), r=[('mya', i), ('mxt', i)], w=[('mxt', i)])
                kb.dma('sp', xbuf[t * 128:(t + 1) * 128, :], xt[i][:], r=[('mxt', i)], w=[('x', t)])
            kb.release(m)

        def phase_final():
            m = kb.mark()
            nwb = kb.sb('fnw', [128, D], F32)
            kb.dma('sp', nwb[:], norm_final.partition_broadcast(128), w=['fnw'])
            xt = [kb.sb(f'fxt{i}', [128, D], F32) for i in range(2)]
            yt = [kb.sb(f'fyt{i}', [128, D], F32) for i in range(2)]
            ssq = [kb.sb(f'fss{i}', [128, 1], F32) for i in range(2)]
            junk = kb.sb('fjunk', [128, D], F32)
            for t in range(2, NT):
                i = t % 2
                kb.dma('sp', xt[i][:], xbuf[t * 128:(t + 1) * 128, :], r=[('x', t)], w=[('fxt', i)])
                kb.op('act', lambda e: e.activation(out=junk[:], in_=xt[i][:], func=AF.Square, accum_out=ssq[i][:]), r=[('fxt', i)], w=['fjunk', ('fss', i)])
                kb.op('act', lambda e: e.activation(out=ssq[i][:], in_=ssq[i][:], func=AF.Sqrt, scale=1.0 / D, bias=epsN), r=[('fss', i)], w=[('fss', i)])
                kb.op('dve', lambda e: e.reciprocal(out=ssq[i][:], in_=ssq[i][:]), r=[('fss', i)], w=[('fss', i)])
                kb.op('dve', lambda e: e.scalar_tensor_tensor(out=yt[i][:], in0=xt[i][:], scalar=ssq[i][:, 0:1], in1=nwb[:], op0=ALU.mult, op1=ALU.mult),
                      r=[('fxt', i), ('fss', i), 'fnw'], w=[('fyt', i)])
                kb.dma('sp', out[(t - 2) * 128:(t - 1) * 128, :], yt[i][:], r=[('fyt', i)], w=[('out', t)])
            kb.release(m)

        def finish_dbg(hT_=True, oT=None, mT=None):
            kb.barrier()
            for t in range(NT):
                pass
            kb.dma('sp', dbg['d_x'], xbuf[:, :], w=['d_x'])
            if hT_:
                kb.dma('sp', dbg['d_hT'], hT[:], w=['d_hT'])
            if oT is not None:
                kb.dma('sp', dbg['d_oT'], oT[:], w=['d_oT'])
            if mT is not None:
                kb.dma('sp', dbg['d_mT'], mT[:], w=['d_mT'])
            kb.finish()
            return nc

        kb.dma('sp', xbuf[0:L, :], ctx_in[:, :], w=['xinit0'])
        kb.dma('sp', xbuf[L:T, :], x_in[:, :], w=['xinit1'])
        for l in range(nlayers):
            phase_ada(l)
        kb.barrier()
        if stop == 'ada':
            kb.dma('sp', dbg['d_mod'], modbuf[0], w=['d_mod'])
            return finish_dbg()
        for l in range(nlayers):
            phase_norm(l, 1)
            if stop == f'norm{l}':
                return finish_dbg()
            mm = kb.mark()
            oT = kb.sb('oT', [128, 4, T], BF16, top=True)
            phase_ret(l, oT)
            if stop == f'ret{l}':
                return finish_dbg(oT=oT)
            mT = kb.sb('mT', [128, 8, T], BF16)
            merge(l, 2048, w_br, oT, 'retT', mT, True)
            phase_attn(l, oT)
            if stop == f'attn{l}':
                return finish_dbg(oT=oT)
            merge(l, 0, w_ba, oT, 'oaT', mT, False)
            phase_four(l, oT)
            if stop == f'four{l}':
                return finish_dbg(oT=oT)
            merge(l, 1024, w_bf, oT, 'ofT', mT, False)
            if stop == f'merge{l}':
                return finish_dbg(mT=mT)
            phase_out(l, mT)
            kb.release(mm)
            if stop == f'mix{l}':
                return finish_dbg()
            if os.environ.get('MOE_DENSE'):
                phase_moe(l)
            else:
                phase_moe_sparse(l)
            if stop == f'moe{l}':
                return finish_dbg()
        phase_final()
        kb.finish()
    print("SBUF peak bytes", kb.peak, "instr counts", kb.cnt)
    return nc


def _consts():
    bf = ml_dtypes.bfloat16
    f32 = np.float32
    n = np.arange(N)
    inv16 = (10000.0 ** (-(np.arange(16, dtype=f32)) / f32(16))).astype(f32)
    row = (n // 64).astype(f32); col = (n % 64).astype(f32)
    ropeA = np.zeros((2, 128, N), f32)
    for p in range(128):
        d = p % 64
        pos = row if d < 32 else col
        dd = d % 32
        ang = (pos * inv16[dd % 16]).astype(f32)
        ropeA[0, p] = np.cos(ang); ropeA[1, p] = np.sin(ang) * (-1.0 if dd < 16 else 1.0)
    inv32 = (10000.0 ** (-(np.arange(32, dtype=f32)) / f32(32))).astype(f32)
    ropeR = np.zeros((2, 128, N), f32)
    for p in range(128):
        d = p % 64
        ang = (n.astype(f32) * inv32[d % 32]).astype(f32)
        ropeR[0, p] = np.cos(ang); ropeR[1, p] = np.sin(ang) * (-1.0 if d < 32 else 1.0)
    k = np.arange(N, dtype=np.int64)
    ph = (np.outer(k, k) % N).astype(np.float64) * (2 * np.pi / N)
    dftN = np.stack([np.cos(ph), -np.sin(ph)]) / np.sqrt(N)
    k2 = np.arange(256, dtype=np.int64)
    ph2 = (np.outer(k2, k2) % 256).astype(np.float64) * (2 * np.pi / 256)
    dft256 = np.stack([np.cos(ph2), -np.sin(ph2)]) / np.sqrt(256)
    k3 = np.arange(128, dtype=np.int64)
    ph3 = (np.outer(k3, k3) % 128).astype(np.float64) * (2 * np.pi / 128)
    dftC = np.concatenate([np.cos(ph3), np.sin(ph3)], axis=1) / np.sqrt(128)
    b = np.arange(128)[:, None]; a = np.arange(128)[None, :]
    amask = np.concatenate([(b >= a), (b <= a)], axis=1).astype(f32)
    j = b; i = a
    rconst = np.concatenate([np.maximum(i - j, 0), np.maximum(j - i, 0), (i >= j), (j > i), (i + 1) + 0 * j, (128 - i) + 0 * j,
                             127 - np.arange(128)[:, None], np.arange(128)[:, None]], axis=1).astype(f32)
    mconst = np.concatenate([np.tile((np.arange(32) * CAP)[None, :], (128, 1)), np.arange(128)[:, None] + 128 * np.arange(NT)[None, :]], axis=1).astype(f32)
    return dict(mconst=mconst, ropeA=ropeA, ropeR=ropeR, dftN=dftN.astype(bf), dft256=dft256.astype(bf), dftC=dftC.astype(bf),
                amask=amask.astype(bf), rconst=rconst)


def _wext_index():
    idx = []
    swa = np.array([d + 16 if (d % 32) < 16 else d - 16 for d in range(64)])
    swr = np.array([d + 32 if d < 32 else d - 32 for d in range(64)])
    base = 0
    qa = [np.concatenate([np.arange(g * 64, (g + 1) * 64), np.arange((4 + g) * 64, (5 + g) * 64)]) for g in range(4)]
    qas = [np.concatenate([g * 64 + swa, (4 + g) * 64 + swa]) for g in range(4)]
    idx += qa + qas
    idx += [512 + np.arange(128), 512 + np.concatenate([swa, 64 + swa])]
    idx += [640 + np.arange(128)]
    idx += [768 + np.arange(512), 768 + np.concatenate([h * 64 + swr for h in range(8)])]
    idx += [1280 + np.arange(512), 1280 + np.concatenate([h * 64 + swr for h in range(8)])]
    idx += [1792 + np.arange(512), 2304 + np.arange(512), 2816 + np.arange(512), 3328 + np.arange(3072)]
    idx = np.concatenate(idx)
    assert idx.shape[0] == WEXT
    return idx


_CACHE = {}


def kernel(x, c, ctx, c_ctx, norm_mix, norm_ffn, w_ada, b_ada, w_in, attn_sink, ret_decay_fwd, ret_decay_bwd,
           w_branch_attn, w_branch_fourier, w_branch_ret, w_out, w_router_group, b_router_group,
           w_router_expert, b_router_expert, w_exp_gate, w_exp_up, w_exp_down, norm_final):
    f = lambda a: np.ascontiguousarray(np.asarray(a, dtype=np.float32))
    if 'nc' not in _CACHE:
        _CACHE['nc'] = build()
        _CACHE['consts'] = _consts()
        _CACHE['idx'] = _wext_index()
    nc = _CACHE['nc']
    shared = dict(_CACHE['consts'])
    w_in = f(w_in)
    shared.update(
        c_ctx=f(c_ctx), norm_mix=f(norm_mix), norm_ffn=f(norm_ffn), w_ada=f(w_ada), b_ada=f(b_ada),
        w_ext=np.ascontiguousarray(w_in[:, :, _CACHE['idx']]), attn_sink=f(attn_sink),
        ret_decay_fwd=f(ret_decay_fwd), ret_decay_bwd=f(ret_decay_bwd), w_branch_attn=f(w_branch_attn),
        w_branch_fourier=f(w_branch_fourier), w_branch_ret=f(w_branch_ret), w_out=f(w_out),
        w_rt=np.ascontiguousarray(np.concatenate([f(w_router_group), f(w_router_expert)], axis=-1)),
        b_rt=np.ascontiguousarray(np.concatenate([f(b_router_group), f(b_router_expert)], axis=-1)),
        w_exp_gate=f(w_exp_gate), w_exp_up=f(w_exp_up), w_exp_down=f(w_exp_down), norm_final=f(norm_final))
    x = f(x); c = f(c); ctx = f(ctx)
    B = x.shape[0]
    in_maps = []
    for b in range(B):
        d = dict(shared)
        d.update(x=x[b], ctx=ctx[b], c=c[b])
        in_maps.append(d)
    res = run_bass_kernel_spmd(nc, in_maps, core_ids=list(range(B)))
    return np.stack([np.asarray(r["out"], dtype=np.float32) for r in res.results], axis=0)
```

```python
import contextlib, os
import numpy as np
import ml_dtypes
import concourse.bass as bass
import concourse.mybir as mybir
from concourse.bass_utils import run_bass_kernel_spmd

F32 = mybir.dt.float32
BF16 = mybir.dt.bfloat16
AF = mybir.ActivationFunctionType
ALU = mybir.AluOpType
AX = mybir.AxisListType

ARENA_BASE = 20736
SBUF_BYTES = 229376 - ARENA_BASE - 128
SAME_ENGINE_SYNC = bool(int(os.environ.get("SES", "1")))
NDMA_SEM = 12
EPOCH = 30000


def _dsize(dt):
    return 2 if dt == BF16 else 4


class KB:
    def __init__(self, nc):
        self.nc = nc
        self.es = contextlib.ExitStack()
        self.eng = {'pe': nc.tensor, 'act': nc.scalar, 'dve': nc.vector, 'pool': nc.gpsimd, 'sp': nc.sync}
        self.cnt = {e: 0 for e in self.eng}
        self.sems = {e: [] for e in self.eng}
        self.seen = {e: {} for e in self.eng}
        self.res = {}
        self.dma_sems = {}
        self.dma_i = {}
        self.bot = ARENA_BASE
        self.top = ARENA_BASE + SBUF_BYTES
        self.nps = 0
        self.n_sem = 0
        self.peak = 0
        self.off = {}

    def __enter__(self):
        self.es.__enter__()
        return self

    def __exit__(self, *a):
        return self.es.__exit__(*a)

    def sb(self, name, shape, dtype, top=False):
        n = 1
        for s in shape[1:]:
            n *= s
        nbytes = (n * _dsize(dtype) + 63) // 64 * 64
        if top:
            self.top -= nbytes
            off = self.top
        else:
            off = self.bot
            self.bot += nbytes
        assert self.bot <= self.top, f"SBUF overflow allocating {name}: bot={self.bot} top={self.top}"
        self.peak = max(self.peak, self.bot + SBUF_BYTES - self.top)
        self.nps += 1
        self.off[name] = off
        return self.nc.alloc_sbuf_tensor_at(f"{name}_{self.nps}", list(shape), dtype, offset=off)

    def alias(self, name, shape, dtype, off):
        self.nps += 1
        return self.nc.alloc_sbuf_tensor_at(f"{name}_{self.nps}", list(shape), dtype, offset=off)

    def mark(self):
        return (self.bot, self.top)

    def release(self, m):
        self.barrier()
        self.bot, self.top = m

    def ps(self, name, shape, dtype=F32):
        return self.nc.alloc_psum_tensor(name, list(shape), dtype)

    def _newsem(self, name):
        self.n_sem += 1
        return self.es.enter_context(self.nc.semaphore(f"{name}_{self.n_sem}"))

    def _tick(self, e):
        c = self.cnt[e]
        ep, v = divmod(c, EPOCH)
        while len(self.sems[e]) <= ep:
            self.sems[e].append(self._newsem(f"s_{e}"))
        self.cnt[e] = c + 1
        return (self.sems[e][ep], v + 1, e)

    def _wait(self, e, tick):
        sem, val, owner = tick
        if owner == e and (e == 'pe' or not SAME_ENGINE_SYNC):
            return
        k = sem.num
        if self.seen[e].get(k, 0) >= val:
            return
        self.eng[e].wait_ge(sem, val)
        self.seen[e][k] = val

    def _deps(self, e, r, w):
        ticks = []
        for k in r:
            rec = self.res.get(k)
            if rec and rec[0]:
                ticks.append(rec[0])
        for k in w:
            rec = self.res.get(k)
            if rec:
                if rec[0]:
                    ticks.append(rec[0])
                ticks.extend(rec[1])
        for t in ticks:
            self._wait(e, t)

    def _record(self, tick, r, w):
        for k in r:
            rec = self.res.setdefault(k, [None, []])
            rec[1] = [t for t in rec[1] if not (t[0] is tick[0])] + [tick]
        for k in w:
            self.res[k] = [tick, []]

    def op(self, e, fn, r=(), w=()):
        self._deps(e, r, w)
        tick = self._tick(e)
        fn(self.eng[e]).then_inc(tick[0], 1)
        self._record(tick, r, w)

    def dma(self, q, out, in_, r=(), w=(), fn=None, **kw):
        self._deps(q, r, w)
        pool = self.dma_sems.setdefault(q, [])
        i = self.dma_i.get(q, 0)
        self.dma_i[q] = i + 1
        slot = i % NDMA_SEM
        if len(pool) <= slot:
            pool.append([self._newsem(f"d_{q}"), 0])
        ent = pool[slot]
        if ent[1] > 0:
            self._wait(q, (ent[0], ent[1], 'dma'))
        ent[1] += 16
        tick = (ent[0], ent[1], 'dma')
        if fn is not None:
            fn(self.eng[q]).then_inc(ent[0], 16)
        else:
            self.eng[q].dma_start(out=out, in_=in_, **kw).then_inc(ent[0], 16)
        self._record(tick, r, w)

    def all_ticks(self):
        ticks = []
        for e in self.eng:
            c = self.cnt[e]
            if c > 0:
                ep, v = divmod(c - 1, EPOCH)
                ticks.append((self.sems[e][ep], v + 1, e))
        for q, pool in self.dma_sems.items():
            for ent in pool:
                if ent[1] > 0:
                    ticks.append((ent[0], ent[1], 'dma'))
        return ticks

    def barrier(self):
        ticks = self.all_ticks()
        for e in self.eng:
            for t in ticks:
                self._wait(e, t)
        self.res = {}

    def finish(self):
        ticks = self.all_ticks()
        for t in ticks:
            if t[2] != 'sp':
                self._wait('sp', t)

    def make_ident(self, ident):
        nc = self.nc
        self.op('pool', lambda e: e.memset(ident[:], 1.0), w=['ident'])
        self.op('pool', lambda e: e.affine_select(out=ident[:], in_=ident[:], pattern=[[-1, 128]],
                                                  compare_op=ALU.is_equal, fill=0.0, base=0,
                                                  channel_multiplier=1), r=['ident'], w=['ident'])

D = 1024; L = 256; N = 2048; T = 2304; NT = 18
QA, QAS, KA, KAS, VA, QR, QRS, KR, KRS, VR, GR, FU, GT = 0, 512, 1024, 1152, 1280, 1408, 1920, 2432, 2944, 3456, 3968, 4480, 4992
WEXT = 8064
CAP = int(os.environ.get('MOE_CAP', '1024'))
NS = CAP // 128
I32 = mybir.dt.int32
TB = [(0, 256), (256, 512), (768, 512), (1280, 512), (1792, 512)]


def bc(ap, free):
    return bass.AP(ap.tensor, ap.offset, [list(ap.ap[0])] + [list(f) for f in free])


def build(stop=None, nlayers=2):
    nc = bass.Bass("TRN2", target_bir_lowering=False)

    def din(name, shape, dt=F32):
        return nc.dram_tensor(name, list(shape), dt, kind="ExternalInput").ap()
    x_in = din("x", [N, D]); ctx_in = din("ctx", [L, D]); c_in = din("c", [D]); cctx_in = din("c_ctx", [D])
    norm_mix = din("norm_mix", [2, D]); norm_ffn = din("norm_ffn", [2, D])
    w_ada = din("w_ada", [2, D, 6 * D]); b_ada = din("b_ada", [2, 6 * D])
    w_ext = din("w_ext", [2, D, WEXT]); sink_in = din("attn_sink", [2, 8])
    dec_f = din("ret_decay_fwd", [2, 8]); dec_b = din("ret_decay_bwd", [2, 8])
    w_ba = din("w_branch_attn", [2, 512, D]); w_bf = din("w_branch_fourier", [2, 512, D]); w_br = din("w_branch_ret", [2, 512, D])
    w_out = din("w_out", [2, D, D]); w_rt = din("w_rt", [2, D, 36]); b_rt = din("b_rt", [2, 36])
    if stop is None or stop.startswith('moe'):
        w_eg = din("w_exp_gate", [2, 32, D, 512]); w_eu = din("w_exp_up", [2, 32, D, 512]); w_ed = din("w_exp_down", [2, 32, 512, D])
    norm_final = din("norm_final", [D])
    ropeA = din("ropeA", [2, 128, N]); ropeR = din("ropeR", [2, 128, N])
    dftN = din("dftN", [2, N, N], BF16); dft256 = din("dft256", [2, 256, 256], BF16); dftC = din("dftC", [128, 256], BF16)
    amask = din("amask", [128, 256], BF16); rconst = din("rconst", [128, 770])
    out = nc.dram_tensor("out", [N, D], F32, kind="ExternalOutput").ap()
    xbuf = nc.dram_tensor("xbuf", [T, D], F32).ap()
    mconst = din("mconst", [128, 32 + NT])
    h2d = nc.dram_tensor("h2d", [T, D], BF16).ap()
    rec_d = nc.dram_tensor("rec_d", [32 * CAP, 4], F32).ap()
    AB = nc.dram_tensor("AB", [2 * T, D], F32).ap()
    modbuf = nc.dram_tensor("modbuf", [2, 2, 6 * D], F32).ap()
    dbg = {}
    if stop is not None:
        dbg['d_x'] = nc.dram_tensor("d_x", [T, D], F32, kind="ExternalOutput").ap()
        dbg['d_mod'] = nc.dram_tensor("d_mod", [2, 6 * D], F32, kind="ExternalOutput").ap()
        dbg['d_hT'] = nc.dram_tensor("d_hT", [128, 8, T], BF16, kind="ExternalOutput").ap()
        dbg['d_oT'] = nc.dram_tensor("d_oT", [128, 4, T], BF16, kind="ExternalOutput").ap()
        dbg['d_mT'] = nc.dram_tensor("d_mT", [128, 8, T], BF16, kind="ExternalOutput").ap()

    kb = KB(nc)
    with kb:
        PSA = kb.ps("psa", [128, 7, 512], F32)
        PST = kb.ps("pst", [128, 8, 128], BF16)
        ident = kb.sb("ident", [128, 128], BF16)
        ident32 = kb.sb("ident32", [128, 128], F32)
        kb.make_ident(ident)
        kb.op('pool', lambda e: e.memset(ident32[:], 1.0), w=['ident32'])
        kb.op('pool', lambda e: e.affine_select(out=ident32[:], in_=ident32[:], pattern=[[-1, 128]], compare_op=ALU.is_equal,
                                                fill=0.0, base=0, channel_multiplier=1), r=['ident32'], w=['ident32'])
        cst = kb.sb("cst", [128, 4], F32)
        kb.op('pool', lambda e: e.memset(cst[:, 0:1], 1e-6), w=['cst'])
        kb.op('pool', lambda e: e.memset(cst[:, 1:2], 1e-5), w=['cst'])
        kb.op('pool', lambda e: e.memset(cst[:, 2:3], 1.0), w=['cst'])
        epsN = cst[:, 0:1]; epsG = cst[:, 1:2]; one1 = cst[:, 2:3]
        stg = [kb.sb(f"stg{i}", [128, 2048], F32) for i in range(2)]
        stg_i = [0]; cast_rr = [0]
        hT = kb.sb("hT", [128, 8, T], BF16)
        stgx = [kb.alias(f'stgx{i}', [128, 2048], F32, kb.off['hT'] + i * 8192) for i in range(4)]
        kb.barrier()

        stg_all = [list(stg)]

        def load_w(dst, dkey, src2d, K, W, engs=('pool',)):
            kk = max(1, 2048 // W)
            stg = stg_all[0]
            for k0 in range(0, K, kk):
                k1 = min(K, k0 + kk)
                i = stg_i[0] % len(stg); stg_i[0] += 1
                sv = stg[i][:, 0:(k1 - k0) * W].rearrange("p (k c) -> p k c", c=W)
                kb.dma('sp', sv, src2d[k0 * 128:k1 * 128, :].rearrange("(k p) c -> p k c", p=128), w=[('stg', i)])
                en = engs[cast_rr[0] % len(engs)]; cast_rr[0] += 1
                if en == 'act':
                    kb.op('act', lambda e: e.copy(out=dst[:, k0:k1, :], in_=sv), r=[('stg', i)], w=[dkey])
                else:
                    kb.op(en, lambda e: e.tensor_copy(out=dst[:, k0:k1, :], in_=sv), r=[('stg', i)], w=[dkey])

        def proj_fm(wt, wkey, c0, tok0, ntok, bank):
            for k in range(8):
                kb.op('pe', lambda e: e.matmul(PSA[:, bank, 0:ntok], lhsT=wt[:, k, c0:c0 + 128], rhs=hT[:, k, tok0:tok0 + ntok],
                                               start=(k == 0), stop=(k == 7)), r=[wkey, 'hT'], w=[('ps', bank)])

        def dump(name, src, r):
            kb.dma('sp', dbg[name], src, r=r, w=[name])

        def xdump(name, t, shape, dt):
            if stop is None or not os.environ.get('XDUMP'):
                return
            kb.barrier()
            d_ = nc.dram_tensor("x_" + name, list(shape), dt, kind="ExternalOutput").ap()
            kb.dma('sp', d_, t[:], w=["x_" + name])

        def phase_ada(l):
            m = kb.mark()
            stg_all[0] = list(stg) + stgx
            stg_i[0] = 0
            cT = kb.sb('cT', [128, 8, 2], F32); cTb = kb.sb('cTb', [128, 8, 2], BF16)
            bsb = kb.sb('bsb', [2, 6 * D], F32); modsb = kb.sb('modsb', [2, 6 * D], F32)
            wa = [kb.sb(f'wa{i}', [128, 8, 512], BF16) for i in range(2)]
            kb.dma('sp', cT[:, :, 0], cctx_in.rearrange("(k p) -> p k", p=128), w=['cT'], allow_slow_non_contiguous=True)
            kb.dma('sp', cT[:, :, 1], c_in.rearrange("(k p) -> p k", p=128), w=['cT'], allow_slow_non_contiguous=True)
            kb.dma('sp', bsb[:], b_ada[l].partition_broadcast(2), w=['bsb'])
            kb.op('act', lambda e: e.activation(out=cTb[:], in_=cT[:], func=AF.Silu), r=['cT'], w=['cTb'])
            for nb in range(12):
                i = nb % 2
                load_w(wa[i], ('wa', i), w_ada[l][:, nb * 512:(nb + 1) * 512], 8, 512, engs=('pool', 'act'))
                for k in range(8):
                    kb.op('pe', lambda e: e.matmul(PSA[0:2, i, :], lhsT=cTb[:, k, :], rhs=wa[i][:, k, :], start=(k == 0), stop=(k == 7)),
                          r=['cTb', ('wa', i)], w=[('ps', i)])
                kb.op('dve', lambda e: e.tensor_tensor(out=modsb[:, nb * 512:(nb + 1) * 512], in0=PSA[0:2, i, :],
                                                       in1=bsb[:, nb * 512:(nb + 1) * 512], op=ALU.add), r=[('ps', i), 'bsb'], w=['modsb'])
            kb.dma('sp', modbuf[l], modsb[:], r=['modsb'], w=[('mod', l)])
            if stop == f'ada{l}':
                dump('d_mod', modsb[:], ['modsb'])
            stg_all[0] = list(stg)
            stg_i[0] = 0
            kb.release(m)
            kb.res[('mod', l)] = None
            kb.res.pop(('mod', l))

        def phase_norm(l, which, logits=None, sparse=False):
            m = kb.mark()
            nw = norm_mix if which == 1 else norm_ffn
            si, ci = (0, 1) if which == 1 else (3, 4)
            tiles = range(NT) if (which == 1 or l == 0) else range(2, NT)
            nwb = kb.sb('nwb', [128, D], F32)
            kb.dma('sp', nwb[:], nw[l].partition_broadcast(128), w=['nwb'])
            G = []; S = []
            for row in (0, 1):
                g_ = kb.sb(f'G{row}', [128, D], F32); s_ = kb.sb(f'S{row}', [128, D], F32)
                kb.dma('sp', g_[:], modbuf[l, row, ci * D:(ci + 1) * D].partition_broadcast(128), w=[f'G{row}'])
                kb.dma('sp', s_[:], modbuf[l, row, si * D:(si + 1) * D].partition_broadcast(128), w=[f'S{row}'])
                kb.op('dve', lambda e: e.scalar_tensor_tensor(out=g_[:], in0=g_[:], scalar=1.0, in1=nwb[:], op0=ALU.add, op1=ALU.mult),
                      r=[f'G{row}', 'nwb'], w=[f'G{row}'])
                G.append(g_); S.append(s_)
            xt = [kb.sb(f'xt{i}', [128, D], F32) for i in range(3)]
            tmp = [kb.sb(f'tmp{i}', [128, D], F32) for i in range(3)]
            hb = [kb.sb(f'hb{i}', [128, D], BF16 if which == 1 else F32) for i in range(3)]
            ssq = [kb.sb(f'ssq{i}', [128, 1], F32) for i in range(3)]
            junk = kb.sb('junk', [128, D], F32)
            if which == 2:
                hbb = [kb.sb(f'hbb{i}', [128, D], BF16) for i in range(3)]
                h32 = [kb.sb(f'h32{i}', [128, 8, 128], F32) for i in range(3)]
                wrt32 = kb.sb('wrt32', [128, 8, 36], F32); brt = kb.sb('brt', [128, 36], F32)
                kb.dma('sp', wrt32[:], w_rt[l].rearrange("(k p) c -> p k c", p=128), w=['wrt32'])
                kb.dma('sp', brt[:], b_rt[l].partition_broadcast(128), w=['brt'])
            def stA(t):
                i = t % 3; row = 0 if t < 2 else 1
                kb.dma('sp', xt[i][:], xbuf[t * 128:(t + 1) * 128, :], r=[('x', t)], w=[('xt', i)])
                kb.op('act', lambda e: e.activation(out=junk[:], in_=xt[i][:], func=AF.Square, accum_out=ssq[i][:]),
                      r=[('xt', i)], w=['junk', ('ssq', i)])
                kb.op('act', lambda e: e.activation(out=ssq[i][:], in_=ssq[i][:], func=AF.Sqrt, scale=1.0 / D, bias=epsN),
                      r=[('ssq', i)], w=[('ssq', i)])
                kb.op('dve', lambda e: e.reciprocal(out=ssq[i][:], in_=ssq[i][:]), r=[('ssq', i)], w=[('ssq', i)])
                kb.op('dve', lambda e: e.scalar_tensor_tensor(out=tmp[i][:], in0=xt[i][:], scalar=ssq[i][:, 0:1], in1=G[row][:],
                                                              op0=ALU.mult, op1=ALU.mult), r=[('xt', i), ('ssq', i), f'G{row}'], w=[('tmp', i)])
                kb.op('pool', lambda e: e.tensor_tensor(out=hb[i][:], in0=tmp[i][:], in1=S[row][:], op=ALU.add),
                      r=[('tmp', i), f'S{row}'], w=[('hb', i)])

            def stB(t):
                i = t % 3; row = 0 if t < 2 else 1
                if which == 1:
                    for k in range(8):
                        kb.op('pe', lambda e: e.transpose(out=PST[:, k, :], in_=hb[i][:, k * 128:(k + 1) * 128], identity=ident[:]),
                              r=[('hb', i), 'ident'], w=['pst'])
                    kb.op('act', lambda e: e.copy(out=hT[:, :, t * 128:(t + 1) * 128], in_=PST[:]), r=['pst'], w=['hT'])
                else:
                    for k in range(8):
                        kb.op('pe', lambda e: e.matmul(PSA[:, 5 + k // 4, (k % 4) * 128:(k % 4 + 1) * 128], lhsT=hb[i][:, k * 128:(k + 1) * 128],
                                                       rhs=ident32[:], start=True, stop=True), r=[('hb', i), 'ident32'], w=[('ps', 5 + k // 4)])
                    pv = PSA[:, 5:7, :].rearrange("p a (b c) -> p (a b) c", c=128)
                    kb.op('dve', lambda e: e.tensor_copy(out=h32[i][:], in_=pv), r=[('ps', 5), ('ps', 6)], w=[('h32', i)])
                    if sparse:
                        kb.op('act', lambda e: e.copy(out=hbb[i][:], in_=hb[i][:]), r=[('hb', i)], w=[('hbb', i)])
                        kb.dma('sp', h2d[t * 128:(t + 1) * 128, :], hbb[i][:], r=[('hbb', i)], w=[('h2d', t)])
                    else:
                        kb.op('act', lambda e: e.copy(out=hT[:, :, t * 128:(t + 1) * 128], in_=h32[i][:]), r=[('h32', i)], w=['hT'])
                    for k in range(8):
                        kb.op('pe', lambda e: e.matmul(PSA[:, 4, 0:36], lhsT=h32[i][:, k, :], rhs=wrt32[:, k, :], start=(k == 0), stop=(k == 7)),
                              r=[('h32', i), 'wrt32'], w=[('ps', 4)])
                    kb.op('dve', lambda e: e.tensor_tensor(out=logits[:, t, :], in0=PSA[:, 4, 0:36], in1=brt[:], op=ALU.add),
                          r=[('ps', 4), 'brt'], w=['logits'])

            tl_ = list(tiles)
            stA(tl_[0])
            for n_, t in enumerate(tl_):
                if n_ + 1 < len(tl_):
                    stA(tl_[n_ + 1])
                stB(t)
            kb.release(m)

        def merge(l, gate_off, wb_dram, oT, okey, mT, first):
            m = kb.mark()
            wg = [kb.sb(f'mwg{i}', [128, 8, 128], BF16) for i in range(2)]
            wb = [kb.sb(f'mwb{i}', [128, 4, 128], BF16) for i in range(2)]
            sig = [kb.sb(f'sig{i}', [128, 512], F32) for i in range(2)]
            mtmp = [kb.sb(f'mtmp{i}', [128, 512], F32) for i in range(2)]
            it = 0
            for fc in range(8):
                j = fc % 2
                load_w(wg[j], ('mwg', j), w_ext[l][:, GT + gate_off + fc * 128: GT + gate_off + (fc + 1) * 128], 8, 128)
                load_w(wb[j], ('mwb', j), wb_dram[l][:, fc * 128:(fc + 1) * 128], 4, 128)
                for (s0, n) in (TB if l == 0 else TB[1:]):
                    i = it % 2; it += 1
                    proj_fm(wg[j], ('mwg', j), 0, s0, n, i)
                    kb.op('act', lambda e: e.activation(out=sig[i][:, 0:n], in_=PSA[:, i, 0:n], func=AF.Sigmoid), r=[('ps', i)], w=[('sig', i)])
                    for k in range(4):
                        kb.op('pe', lambda e: e.matmul(PSA[:, 2 + i, 0:n], lhsT=wb[j][:, k, :], rhs=oT[:, k, s0:s0 + n], start=(k == 0), stop=(k == 3)),
                              r=[('mwb', j), okey], w=[('ps', 2 + i)])
                    if first:
                        kb.op('dve', lambda e: e.tensor_tensor(out=mT[:, fc, s0:s0 + n], in0=sig[i][:, 0:n], in1=PSA[:, 2 + i, 0:n], op=ALU.mult),
                              r=[('sig', i), ('ps', 2 + i)], w=['mT'])
                    else:
                        kb.op('dve', lambda e: e.tensor_tensor(out=mtmp[i][:, 0:n], in0=sig[i][:, 0:n], in1=PSA[:, 2 + i, 0:n], op=ALU.mult),
                              r=[('sig', i), ('ps', 2 + i)], w=[('mtmp', i)])
                        kb.op('pool', lambda e: e.tensor_tensor(out=mT[:, fc, s0:s0 + n], in0=mT[:, fc, s0:s0 + n], in1=mtmp[i][:, 0:n], op=ALU.add),
                              r=[('mtmp', i), 'mT'], w=['mT'])
            kb.release(m)

        def rope_evac(bA, bB, dst, dkey, tC, tS, n, t1, t2, j):
            kb.op('dve', lambda e: e.tensor_tensor(out=t1[:, 0:n], in0=PSA[:, bA, 0:n], in1=tC[:, 0:n], op=ALU.mult), r=[('ps', bA), ('rc', j)], w=[('t1', j)])
            kb.op('dve', lambda e: e.tensor_tensor(out=t2[:, 0:n], in0=PSA[:, bB, 0:n], in1=tS[:, 0:n], op=ALU.mult), r=[('ps', bB), ('rs', j)], w=[('t2', j)])
            kb.op('pool', lambda e: e.tensor_tensor(out=dst, in0=t1[:, 0:n], in1=t2[:, 0:n], op=ALU.add), r=[('t1', j), ('t2', j)], w=[dkey])

        def phase_ret(l, retT):
            need_ctx = (l == 0)
            m = kb.mark()
            RC = kb.sb('RC', [128, 770], F32)
            kb.dma('sp', RC[:], rconst[:, :], w=['RC'])
            dpos = RC[:, 0:128]; dneg = RC[:, 128:256]; mge = RC[:, 256:384]; mlt = RC[:, 384:512]
            io1 = RC[:, 512:640]; iob = RC[:, 640:768]; pc127 = RC[:, 768:769]; pcol = RC[:, 769:770]
            lg = kb.sb('lg', [128, 16], F32)
            kb.dma('sp', lg[:, 0:8], dec_f[l].partition_broadcast(128), w=['lg'])
            kb.dma('sp', lg[:, 8:16], dec_b[l].partition_broadcast(128), w=['lg'])
            kb.op('act', lambda e: e.activation(out=lg[:], in_=lg[:], func=AF.Exp, scale=-1.0), r=['lg'], w=['lg'])
            kb.op('act', lambda e: e.activation(out=lg[:], in_=lg[:], func=AF.Ln, bias=one1), r=['lg', 'cst'], w=['lg'])
            kb.op('dve', lambda e: e.tensor_scalar(out=lg[:], in0=lg[:], scalar1=-1.0, scalar2=None, op0=ALU.mult), r=['lg'], w=['lg'])
            lgp = kb.sb('lgp', [128, 8], F32)
            for r in range(4):
                for hh in range(2):
                    for d in range(2):
                        kb.op('pool', lambda e: e.tensor_copy(out=lgp[hh * 64:(hh + 1) * 64, d * 4 + r:d * 4 + r + 1],
                                                              in_=lg[hh * 64:(hh + 1) * 64, d * 8 + 2 * r + hh:d * 8 + 2 * r + hh + 1]), r=['lg'], w=['lgp'])
            g128 = kb.sb('g128', [128, 8], F32)
            kb.op('act', lambda e: e.activation(out=g128[:], in_=lgp[:], func=AF.Exp, scale=128.0), r=['lgp'], w=['g128'])
            Z = kb.sb('Z', [128, 16], F32)
            kb.op('act', lambda e: e.activation(out=Z[:, 0:8], in_=lg[:, 0:8], func=AF.Exp, scale=pc127), r=['lg', 'RC'], w=['Z'])
            kb.op('act', lambda e: e.activation(out=Z[:, 8:16], in_=lg[:, 8:16], func=AF.Exp, scale=pcol), r=['lg', 'RC'], w=['Z'])
            kb.op('dve', lambda e: e.tensor_scalar(out=Z[:], in0=Z[:], scalar1=0.125, scalar2=None, op0=ALU.mult), r=['Z'], w=['Z'])
            DecT = kb.sb('DecT', [128, 8, 128], F32)
            d1 = kb.sb('d1', [128, 128], F32); d2 = kb.sb('d2', [128, 128], F32)
            for h in range(8):
                kb.op('act', lambda e: e.activation(out=d1[:], in_=dpos, func=AF.Exp, scale=lg[:, h:h + 1]), r=['lg', 'RC'], w=['d1'])
                kb.op('pool', lambda e: e.tensor_tensor(out=d1[:], in0=d1[:], in1=mge, op=ALU.mult), r=['d1', 'RC'], w=['d1'])
                kb.op('act', lambda e: e.activation(out=d2[:], in_=dneg, func=AF.Exp, scale=lg[:, 8 + h:9 + h]), r=['lg', 'RC'], w=['d2'])
                kb.op('pool', lambda e: e.tensor_tensor(out=d2[:], in0=d2[:], in1=mlt, op=ALU.mult), r=['d2', 'RC'], w=['d2'])
                kb.op('pool', lambda e: e.tensor_tensor(out=d1[:], in0=d1[:], in1=d2[:], op=ALU.add), r=['d1', 'd2'], w=['d1'])
                kb.op('dve', lambda e: e.tensor_scalar(out=DecT[:, h, :], in0=d1[:], scalar1=0.125, scalar2=None, op0=ALU.mult), r=['d1'], w=['DecT'])
            X = kb.sb('X', [128, 8, 128], F32)
            for r in range(4):
                kb.op('act', lambda e: e.activation(out=X[:, r, :], in_=io1, func=AF.Exp, scale=lgp[:, r:r + 1]), r=['lgp', 'RC'], w=['X'])
                kb.op('act', lambda e: e.activation(out=X[:, 4 + r, :], in_=iob, func=AF.Exp, scale=lgp[:, 4 + r:5 + r]), r=['lgp', 'RC'], w=['X'])
            if os.environ.get('RET_CUT') == '1':
                kb.release(m); return
            qT = kb.sb('qT', [128, T], BF16); kT = kb.sb('kT', [128, T], BF16)
            ktok = kb.sb('ktok', [128, NT, 128], BF16); vtok = kb.sb('vtok', [128, NT, 128], BF16)
            vf = kb.sb('vf', [128, NT, 128], BF16); vb = kb.sb('vb', [128, NT, 128], BF16)
            sg = kb.sb('sg', [128, NT, 128], F32)
            Sf = kb.sb('Sf', [128, NT, 128], BF16); Rb = kb.sb('Rb', [128, NT, 128], BF16)
            Srun = kb.sb('Srun', [128, 128], F32); Rrun = kb.sb('Rrun', [128, 128], F32)
            ws = {nm: kb.sb('w' + nm, [128, 8, 128], BF16) for nm in ('q', 'qs', 'k', 'ks', 'v', 'g')}
            rc = [kb.sb(f'rc{i}', [128, 512], F32) for i in range(2)]; rs = [kb.sb(f'rs{i}', [128, 512], F32) for i in range(2)]
            t1 = [kb.sb(f't1{i}', [128, 512], F32) for i in range(2)]; t2 = [kb.sb(f't2{i}', [128, 512], F32) for i in range(2)]
            AT = [kb.sb(f'AT{i}', [128, 2, 128], BF16) for i in range(2)]
            qxf = [kb.sb(f'qxf{i}', [128, 128], BF16) for i in range(2)]; qxb = [kb.sb(f'qxb{i}', [128, 128], BF16) for i in range(2)]
            oc = [kb.sb(f'oc{i}', [128, 128], F32) for i in range(2)]; sq = [kb.sb(f'sq{i}', [128, 128], F32) for i in range(2)]
            st = [kb.sb(f'st{i}', [128, 4], F32) for i in range(2)]
            rtok = [kb.sb(f'rtok{i}', [128, 128], BF16) for i in range(2)]
            o_all = kb.sb('o_all', [128, NT, 128], F32); sq_all = kb.sb('sq_all', [128, NT, 128], F32); rt_all = kb.sb('rt_all', [128, NT, 128], BF16)
            mu_ = kb.sb('mu_', [128, 2 * NT], F32); va_ = kb.sb('va_', [128, 2 * NT], F32)
            for r in range(4):
                for nm, c0 in (('q', QR), ('qs', QRS), ('k', KR), ('ks', KRS), ('v', VR), ('g', GR)):
                    load_w(ws[nm], 'w' + nm, w_ext[l][:, c0 + r * 128:c0 + (r + 1) * 128], 8, 128)
                for bi, (s0, n) in enumerate(TB):
                    if s0 == 0:
                        proj_fm(ws['q'], 'wq', 0, s0, n, 0)
                        kb.op('act', lambda e: e.copy(out=qT[:, s0:s0 + n], in_=PSA[:, 0, 0:n]), r=[('ps', 0)], w=['qT'])
                        proj_fm(ws['k'], 'wk', 0, s0, n, 1)
                        kb.op('act', lambda e: e.copy(out=kT[:, s0:s0 + n], in_=PSA[:, 1, 0:n]), r=[('ps', 1)], w=['kT'])
                    else:
                        j = bi % 2; p0 = s0 - 256
                        kb.dma('sp', rc[j][:, 0:n], ropeR[0, :, p0:p0 + n], w=[('rc', j)])
                        kb.dma('sp', rs[j][:, 0:n], ropeR[1, :, p0:p0 + n], w=[('rs', j)])
                        proj_fm(ws['q'], 'wq', 0, s0, n, 0); proj_fm(ws['qs'], 'wqs', 0, s0, n, 1)
                        rope_evac(0, 1, qT[:, s0:s0 + n], 'qT', rc[j], rs[j], n, t1[j], t2[j], j)
                        proj_fm(ws['k'], 'wk', 0, s0, n, 2); proj_fm(ws['ks'], 'wks', 0, s0, n, 3)
                        rope_evac(2, 3, kT[:, s0:s0 + n], 'kT', rc[j], rs[j], n, t1[j], t2[j], j)
                    for t in range(s0 // 128, (s0 + n) // 128):
                        bank = 4 + (t % 2)
                        for k in range(8):
                            kb.op('pe', lambda e: e.matmul(PSA[:, bank, 0:128], lhsT=hT[:, k, t * 128:(t + 1) * 128], rhs=ws['v'][:, k, :],
                                                           start=(k == 0), stop=(k == 7)), r=['hT', 'wv'], w=[('ps', bank)])
                        for k in range(8):
                            kb.op('pe', lambda e: e.matmul(PSA[:, bank, 128:256], lhsT=hT[:, k, t * 128:(t + 1) * 128], rhs=ws['g'][:, k, :],
                                                           start=(k == 0), stop=(k == 7)), r=['hT', 'wg'], w=[('ps', bank)])
                        kb.op('act', lambda e: e.copy(out=vtok[:, t, :], in_=PSA[:, bank, 0:128]), r=[('ps', bank)], w=['vtok'])
                        kb.op('act', lambda e: e.activation(out=sg[:, t, :], in_=PSA[:, bank, 128:256], func=AF.Silu), r=[('ps', bank)], w=['sg'])
                if os.environ.get('RET_CUT') == '2':
                    kb.release(m); return
                for t in range(NT):
                    kb.op('pe', lambda e: e.transpose(out=PST[:, t % 8, :], in_=kT[:, t * 128:(t + 1) * 128], identity=ident[:]), r=['kT', 'ident'], w=['pst'])
                    if t % 8 == 7 or t == NT - 1:
                        n8 = t % 8 + 1; t0 = t - n8 + 1
                        kb.op('act', lambda e: e.copy(out=ktok[:, t0:t + 1, :], in_=PST[:, 0:n8, :]), r=['pst'], w=['ktok'])
                v4 = lambda a: a[:].rearrange("p c (h e) -> p c h e", h=2)
                kb.op('pool', lambda e: e.tensor_tensor(out=v4(vf), in0=v4(vtok), in1=bc(Z[:, 2 * r:2 * r + 2], [[0, NT], [1, 2], [0, 64]]), op=ALU.mult),
                      r=['vtok', 'Z'], w=['vf'])
                kb.op('pool', lambda e: e.tensor_tensor(out=v4(vb), in0=v4(vtok), in1=bc(Z[:, 8 + 2 * r:8 + 2 * r + 2], [[0, NT], [1, 2], [0, 64]]), op=ALU.mult),
                      r=['vtok', 'Z'], w=['vb'])
                if os.environ.get('RET_CUT') == '3':
                    kb.release(m); return
                kb.op('pool', lambda e: e.memset(Srun[:], 0.0), w=['Srun'])
                kb.op('pool', lambda e: e.memset(Sf[:, 0, :], 0.0), w=['Sf'])
                for c in range(NT - 1):
                    bank = 4 + c % 2
                    kb.op('pe', lambda e: e.matmul(PSA[:, bank, 0:128], lhsT=ktok[:, c, :], rhs=vf[:, c, :], start=True, stop=True),
                          r=['ktok', 'vf'], w=[('ps', bank)])
                    kb.op('dve', lambda e: e.scalar_tensor_tensor(out=Srun[:], in0=Srun[:], scalar=g128[:, r:r + 1], in1=PSA[:, bank, 0:128],
                                                                  op0=ALU.mult, op1=ALU.add), r=['Srun', 'g128', ('ps', bank)], w=['Srun'])
                    kb.op('act', lambda e: e.copy(out=Sf[:, c + 1, :], in_=Srun[:]), r=['Srun'], w=['Sf'])
                kb.op('pool', lambda e: e.memset(Rrun[:], 0.0), w=['Rrun'])
                kb.op('pool', lambda e: e.memset(Rb[:, 1, :], 0.0), w=['Rb'])
                order = [1, 0] + list(range(17, 2, -1)); dest = [0, 17] + list(range(16, 1, -1))
                for ii, (c, dd) in enumerate(zip(order, dest)):
                    bank = 4 + ii % 2
                    kb.op('pe', lambda e: e.matmul(PSA[:, bank, 0:128], lhsT=ktok[:, c, :], rhs=vb[:, c, :], start=True, stop=True),
                          r=['ktok', 'vb'], w=[('ps', bank)])
                    kb.op('dve', lambda e: e.scalar_tensor_tensor(out=Rrun[:], in0=Rrun[:], scalar=g128[:, 4 + r:5 + r], in1=PSA[:, bank, 0:128],
                                                                  op0=ALU.mult, op1=ALU.add), r=['Rrun', 'g128', ('ps', bank)], w=['Rrun'])
                    kb.op('act', lambda e: e.copy(out=Rb[:, dd, :], in_=Rrun[:]), r=['Rrun'], w=['Rb'])
                if os.environ.get('RET_CUT') == '4':
                    kb.release(m); return
                chunks_ = list(range(NT) if need_ctx else range(2, NT))

                def inner_(c):
                    i = c % 2; cs = slice(c * 128, (c + 1) * 128)
                    for hh in range(2):
                        ps_ = slice(hh * 64, (hh + 1) * 64)
                        kb.op('pe', lambda e: e.matmul(PSA[:, i + 4 * hh, 0:128], lhsT=kT[ps_, cs], rhs=qT[ps_, cs], start=True, stop=True),
                              r=['kT', 'qT'], w=[('ps', i + 4 * hh)])

                def rest_(c):
                    i = c % 2; cs = slice(c * 128, (c + 1) * 128)
                    for hh in range(2):
                        kb.op('dve', lambda e: e.tensor_tensor(out=AT[i][:, hh, :], in0=PSA[:, i + 4 * hh, 0:128],
                                                               in1=DecT[:, 2 * r + hh, :], op=ALU.mult), r=[('ps', i + 4 * hh), 'DecT'], w=[('AT', i)])
                    kb.op('pool', lambda e: e.tensor_tensor(out=qxf[i][:], in0=qT[:, cs], in1=X[:, r, :], op=ALU.mult), r=['qT', 'X'], w=[('qxf', i)])
                    kb.op('pool', lambda e: e.tensor_tensor(out=qxb[i][:], in0=qT[:, cs], in1=X[:, 4 + r, :], op=ALU.mult), r=['qT', 'X'], w=[('qxb', i)])
                    for hh in range(2):
                        ps_ = slice(hh * 64, (hh + 1) * 64)
                        o_ = PSA[:, 2 + i, hh * 64:(hh + 1) * 64]
                        kb.op('pe', lambda e: e.matmul(o_, lhsT=AT[i][:, hh, :], rhs=vtok[:, c, hh * 64:(hh + 1) * 64], start=True, stop=False),
                              r=[('AT', i), 'vtok'], w=[('ps', 2 + i)])
                        kb.op('pe', lambda e: e.matmul(o_, lhsT=qxf[i][ps_, :], rhs=Sf[ps_, c, hh * 64:(hh + 1) * 64], start=False, stop=False),
                              r=[('qxf', i), 'Sf'], w=[('ps', 2 + i)])
                        kb.op('pe', lambda e: e.matmul(o_, lhsT=qxb[i][ps_, :], rhs=Rb[ps_, c, hh * 64:(hh + 1) * 64], start=False, stop=True),
                              r=[('qxb', i), 'Rb'], w=[('ps', 2 + i)])
                    kb.op('act', lambda e: e.copy(out=o_all[:, c, :], in_=PSA[:, 2 + i, 0:128]), r=[('ps', 2 + i)], w=['o_all'])

                inner_(chunks_[0])
                for n_, c in enumerate(chunks_):
                    if n_ + 1 < len(chunks_):
                        inner_(chunks_[n_ + 1])
                    rest_(c)
                c0_ = 0 if need_ctx else 2
                G_ = (NT - c0_) * 2
                og = o_all[:, c0_:NT, :].rearrange("p c (h e) -> p (c h) e", h=2)
                sqg = sq_all[:, c0_:NT, :].rearrange("p c (h e) -> p (c h) e", h=2)
                kb.op('dve', lambda e: e.reduce_sum(out=mu_[:, 0:G_], in_=og, axis=AX.X), r=['o_all'], w=['mu_'])
                kb.op('dve', lambda e: e.tensor_scalar(out=mu_[:, 0:G_], in0=mu_[:, 0:G_], scalar1=-1.0 / 64, scalar2=None, op0=ALU.mult), r=['mu_'], w=['mu_'])
                kb.op('pool', lambda e: e.tensor_tensor(out=og, in0=og, in1=bc(mu_[:, 0:G_], [[1, G_], [0, 64]]), op=ALU.add), r=['o_all', 'mu_'], w=['o_all'])
                kb.op('pool', lambda e: e.tensor_tensor(out=sqg, in0=og, in1=og, op=ALU.mult), r=['o_all'], w=['sq_all'])
                kb.op('dve', lambda e: e.reduce_sum(out=va_[:, 0:G_], in_=sqg, axis=AX.X), r=['sq_all'], w=['va_'])
                kb.op('act', lambda e: e.activation(out=va_[:, 0:G_], in_=va_[:, 0:G_], func=AF.Sqrt, scale=1.0 / 64, bias=epsG), r=['va_'], w=['va_'])
                kb.op('dve', lambda e: e.reciprocal(out=va_[:, 0:G_], in_=va_[:, 0:G_]), r=['va_'], w=['va_'])
                kb.op('pool', lambda e: e.tensor_tensor(out=og, in0=og, in1=bc(va_[:, 0:G_], [[1, G_], [0, 64]]), op=ALU.mult), r=['o_all', 'va_'], w=['o_all'])
                kb.op('pool', lambda e: e.tensor_tensor(out=rt_all[:, c0_:NT, :], in0=o_all[:, c0_:NT, :], in1=sg[:, c0_:NT, :], op=ALU.mult),
                      r=['o_all', 'sg'], w=['rt_all'])
                cl_ = list(range(c0_, NT))
                for n0 in range(0, len(cl_), 8):
                    grp_ = cl_[n0:n0 + 8]
                    for ii_, c in enumerate(grp_):
                        kb.op('pe', lambda e: e.transpose(out=PST[:, ii_, :], in_=rt_all[:, c, :], identity=ident[:]), r=['rt_all', 'ident'], w=['pst'])
                    kb.op('act', lambda e: e.copy(out=retT[:, r, grp_[0] * 128:(grp_[-1] + 1) * 128].rearrange("p (a b) -> p a b", b=128),
                                                  in_=PST[:, 0:len(grp_), :]), r=['pst'], w=['retT'])
                if os.environ.get('RET_CUT') in ('5', '6', '7'):
                    kb.release(m); return
                if r == 3:
                    for nm_, t_, sh_, dt_ in (('sg', sg, [128, NT, 128], F32), ('DecT', DecT, [128, 8, 128], F32), ('X', X, [128, 8, 128], F32),
                                              ('Z', Z, [128, 16], F32), ('g128', g128, [128, 8], F32), ('lg', lg, [128, 16], F32),
                                              ('Sf', Sf, [128, NT, 128], BF16), ('Rb', Rb, [128, NT, 128], BF16), ('qT', qT, [128, T], BF16),
                                              ('kT', kT, [128, T], BF16), ('vtok', vtok, [128, NT, 128], BF16), ('ktok', ktok, [128, NT, 128], BF16),
                                              ('vf', vf, [128, NT, 128], BF16), ('AT1', AT[1], [128, 2, 128], BF16), ('qxf1', qxf[1], [128, 128], BF16),
                                              ('qxb1', qxb[1], [128, 128], BF16)):
                        xdump(nm_, t_, sh_, dt_)
            kb.release(m)

        def phase_attn(l, oaT):
            need_ctx = (l == 0)
            m = kb.mark()
            qT = kb.sb('aqT', [128, 4, T], BF16); kT = kb.sb('akT', [128, T], BF16)
            Va = kb.sb('Va', [128, NT, 2, 66], BF16)
            msk = kb.sb('msk', [128, 256], BF16)
            kb.dma('sp', msk[:], amask[:, :], w=['msk'])
            snk = kb.sb('snk', [128, 8], F32)
            kb.dma('sp', snk[:], sink_in[l].partition_broadcast(128), w=['snk'])
            kb.op('act', lambda e: e.activation(out=snk[:], in_=snk[:], func=AF.Exp), r=['snk'], w=['snk'])
            kb.op('pool', lambda e: e.memset(Va[:, :, :, 64:66], 1.0), w=['Va'])
            wq = kb.sb('awq', [128, 8, 1024], BF16); wk = kb.sb('awk', [128, 8, 256], BF16); wv = kb.sb('awv', [128, 8, 128], BF16)
            load_w(wq, 'awq', w_ext[l][:, QA:QA + 1024], 8, 1024)
            load_w(wk, 'awk', w_ext[l][:, KA:KA + 256], 8, 256)
            load_w(wv, 'awv', w_ext[l][:, VA:VA + 128], 8, 128)
            rc = [kb.sb(f'arc{i}', [128, 512], F32) for i in range(2)]; rs = [kb.sb(f'ars{i}', [128, 512], F32) for i in range(2)]
            t1 = [kb.sb(f'at1{i}', [128, 512], F32) for i in range(2)]; t2 = [kb.sb(f'at2{i}', [128, 512], F32) for i in range(2)]
            for bi, (s0, n) in enumerate(TB):
                if s0 == 0:
                    for g in range(4):
                        proj_fm(wq, 'awq', g * 128, s0, n, g % 2)
                        kb.op('act', lambda e: e.copy(out=qT[:, g, s0:s0 + n], in_=PSA[:, g % 2, 0:n]), r=[('ps', g % 2)], w=['aqT'])
                    proj_fm(wk, 'awk', 0, s0, n, 2)
                    kb.op('act', lambda e: e.copy(out=kT[:, s0:s0 + n], in_=PSA[:, 2, 0:n]), r=[('ps', 2)], w=['akT'])
                else:
                    j = bi % 2; p0 = s0 - 256
                    kb.dma('sp', rc[j][:, 0:n], ropeA[0, :, p0:p0 + n], w=[('rc', j)])
                    kb.dma('sp', rs[j][:, 0:n], ropeA[1, :, p0:p0 + n], w=[('rs', j)])
                    for g in range(4):
                        b0 = 2 * (g % 2)
                        proj_fm(wq, 'awq', g * 128, s0, n, b0); proj_fm(wq, 'awq', 512 + g * 128, s0, n, b0 + 1)
                        rope_evac(b0, b0 + 1, qT[:, g, s0:s0 + n], 'aqT', rc[j], rs[j], n, t1[j], t2[j], j)
                    proj_fm(wk, 'awk', 0, s0, n, 4); proj_fm(wk, 'awk', 128, s0, n, 5)
                    rope_evac(4, 5, kT[:, s0:s0 + n], 'akT', rc[j], rs[j], n, t1[j], t2[j], j)
                for t in range(s0 // 128, (s0 + n) // 128):
                    for k in range(8):
                        kb.op('pe', lambda e: e.matmul(PSA[:, 6, 0:128], lhsT=hT[:, k, t * 128:(t + 1) * 128], rhs=wv[:, k, :],
                                                       start=(k == 0), stop=(k == 7)), r=['hT', 'awv'], w=[('ps', 6)])
                    kb.op('act', lambda e: e.copy(out=Va[:, t, :, 0:64], in_=PSA[:, 6, 0:128].rearrange("p (h e) -> p h e", h=2)),
                          r=[('ps', 6)], w=['Va'])
            PT = [[kb.sb(f'PT{i}_{j}', [128, 4, 128], BF16) for j in range(5)] for i in range(2)]
            oat = [kb.sb(f'oat{i}', [128, 8, 64], BF16) for i in range(2)]
            den = [kb.sb(f'den{i}', [128, 4], F32) for i in range(2)]
            def keys_of(t):
                if t < 2:
                    return [(0, None), (1, None)]
                keys = []
                if t > 2: keys.append((t - 1, 0))
                keys.append((t, None))
                if t < NT - 1: keys.append((t + 1, 1))
                return keys + [(0, None), (1, None)]

            groups = [(t, h2) for t in (range(NT) if need_ctx else range(2, NT)) for h2 in range(2)]
            SB = [0, 1, 2, 5, 6]
            sbc = [0]

            def SE(n):
                t, h2 = groups[n]; i = n % 2
                ps_ = slice(h2 * 64, (h2 + 1) * 64)
                for ki, (kt, mk) in enumerate(keys_of(t)):
                    bank = SB[sbc[0] % 5]; sbc[0] += 1
                    kb.op('pe', lambda e: e.matmul(PSA[:, bank, :].rearrange("p (g q) -> p g q", g=4), lhsT=kT[ps_, kt * 128:(kt + 1) * 128],
                                                   rhs=qT[ps_, :, t * 128:(t + 1) * 128], start=True, stop=True), r=['akT', 'aqT'], w=[('ps', bank)])
                    kb.op('act', lambda e: e.activation(out=PT[i][ki][:], in_=PSA[:, bank, :].rearrange("p (g q) -> p g q", g=4), func=AF.Exp, scale=0.125),
                          r=[('ps', bank)], w=[('PT', i, ki)])
                    if mk is not None:
                        kb.op('pool', lambda e: e.tensor_tensor(out=PT[i][ki][:], in0=PT[i][ki][:], in1=bc(msk[:, mk * 128:(mk + 1) * 128], [[0, 4], [1, 128]]),
                                                                op=ALU.mult), r=[('PT', i, ki), 'msk'], w=[('PT', i, ki)])

            def PVN(n):
                t, h2 = groups[n]; i = n % 2; ti = t % 2
                keys = keys_of(t)
                ob = 3 + i
                for g in range(4):
                    for ki, (kt, mk) in enumerate(keys):
                        kb.op('pe', lambda e: e.matmul(PSA[:, ob, g * 66:g * 66 + 65], lhsT=PT[i][ki][:, g, :], rhs=Va[:, kt, h2, 0:65],
                                                       start=(ki == 0), stop=(ki == len(keys) - 1)), r=[('PT', i, ki), 'Va'], w=[('ps', ob)])
                ov = PSA[:, ob, 0:264].rearrange("p (g e) -> p g e", g=4)
                kb.op('dve', lambda e: e.tensor_tensor(out=den[i][:], in0=ov[:, :, 64], in1=snk[:, h2 * 4:(h2 + 1) * 4], op=ALU.add),
                      r=[('ps', ob), 'snk'], w=[('den', i)])
                kb.op('dve', lambda e: e.reciprocal(out=den[i][:], in_=den[i][:]), r=[('den', i)], w=[('den', i)])
                kb.op('dve', lambda e: e.tensor_tensor(out=oat[ti][:, h2 * 4:(h2 + 1) * 4, :], in0=ov[:, :, 0:64], in1=bc(den[i][:, 0:4], [[1, 4], [0, 64]]),
                                                       op=ALU.mult), r=[('ps', ob), ('den', i)], w=[('oat', ti)])
                if h2 == 1:
                    for k in range(4):
                        kb.op('pe', lambda e: e.transpose(out=PST[:, k, :], in_=oat[ti][:, 2 * k:2 * k + 2, :].rearrange("p h e -> p (h e)"), identity=ident[:]),
                              r=[('oat', ti), 'ident'], w=['pst'])
                    kb.op('act', lambda e: e.copy(out=oaT[:, :, t * 128:(t + 1) * 128], in_=PST[:, 0:4, :]), r=['pst'], w=['oaT'])

            SE(0)
            for n in range(len(groups)):
                if n + 1 < len(groups):
                    SE(n + 1)
                PVN(n)
            kb.release(m)

        def phase_four(l, ofT):
            need_ctx = (l == 0)
            m = kb.mark()
            wfu = kb.sb('wfu', [128, 8, 512], BF16)
            load_w(wfu, 'wfu', w_ext[l][:, FU:FU + 512], 8, 512)
            dC = kb.sb('dC', [128, 256], BF16)
            kb.dma('sp', dC[:], dftC[:, :], w=['dC'])
            W = kb.sb('W', [128, NT, 4, 256], BF16)
            uT = [kb.sb(f'uT{i}', [128, T], BF16) for i in range(2)]
            for g in range(4):
                i = g % 2
                for bi, (s0, n) in enumerate(TB):
                    proj_fm(wfu, 'wfu', g * 128, s0, n, bi % 2)
                    kb.op('act', lambda e: e.copy(out=uT[i][:, s0:s0 + n], in_=PSA[:, bi % 2, 0:n]), r=[('ps', bi % 2)], w=[('uT', i)])
                for t in range(NT):
                    bank = 2 + t % 2
                    kb.op('pe', lambda e: e.matmul(PSA[:, bank, 0:256], lhsT=uT[i][:, t * 128:(t + 1) * 128], rhs=dC[:], start=True, stop=True),
                          r=[('uT', i), 'dC'], w=[('ps', bank)])
                    kb.op('dve', lambda e: e.tensor_copy(out=W[:, t, g, :], in_=PSA[:, bank, 0:256]), r=[('ps', bank)], w=['W'])
            Cb = [kb.sb(f'Cb{i}', [128, 16, 256], BF16) for i in range(2)]
            Nb = [kb.sb(f'Nb{i}', [128, 16, 256], BF16) for i in range(2)]
            it = 0
            for nb in range(8):
                i = nb % 2
                kb.dma('sp', Cb[i][:], dftN[0, :, nb * 256:(nb + 1) * 256].rearrange("(t p) c -> p t c", p=128), w=[('Cb', i)])
                kb.dma('sp', Nb[i][:], dftN[1, :, nb * 256:(nb + 1) * 256].rearrange("(t p) c -> p t c", p=128), w=[('Nb', i)])
                for g in range(4):
                    bank = 4 + it % 2; it += 1
                    for t in range(16):
                        kb.op('pe', lambda e: e.matmul(PSA[:, bank, 0:256], lhsT=W[:, 2 + t, g, 0:128], rhs=Cb[i][:, t, :], start=(t == 0), stop=False),
                              r=['W', ('Cb', i)], w=[('ps', bank)])
                        kb.op('pe', lambda e: e.matmul(PSA[:, bank, 0:256], lhsT=W[:, 2 + t, g, 128:256], rhs=Nb[i][:, t, :], start=False, stop=(t == 15)),
                              r=['W', ('Nb', i)], w=[('ps', bank)])
                    kb.op('act', lambda e: e.copy(out=ofT[:, g, 256 + nb * 256:256 + (nb + 1) * 256], in_=PSA[:, bank, 0:256]), r=[('ps', bank)], w=['ofT'])
            if need_ctx:
                kb.dma('sp', Cb[0][:, 0:2, :], dft256[0].rearrange("(t p) c -> p t c", p=128), w=[('Cb', 0)])
                kb.dma('sp', Nb[0][:, 0:2, :], dft256[1].rearrange("(t p) c -> p t c", p=128), w=[('Nb', 0)])
                for g in range(4):
                    bank = 4 + g % 2
                    for t in range(2):
                        kb.op('pe', lambda e: e.matmul(PSA[:, bank, 0:256], lhsT=W[:, t, g, 0:128], rhs=Cb[0][:, t, :], start=(t == 0), stop=False),
                              r=['W', ('Cb', 0)], w=[('ps', bank)])
                        kb.op('pe', lambda e: e.matmul(PSA[:, bank, 0:256], lhsT=W[:, t, g, 128:256], rhs=Nb[0][:, t, :], start=False, stop=(t == 1)),
                              r=['W', ('Nb', 0)], w=[('ps', bank)])
                    kb.op('act', lambda e: e.copy(out=ofT[:, g, 0:256], in_=PSA[:, bank, 0:256]), r=[('ps', bank)], w=['ofT'])
            kb.release(m)

        def resid_update(l, gi, tiles, src_fn, src_keys):
            pass

        def phase_out(l, mT):
            m = kb.mark()
            wo = kb.sb('wo', [128, 8, D], BF16)
            load_w(wo, 'wo', w_out[l][:, :], 8, D)
            g1 = []
            for row in (0, 1):
                g_ = kb.sb(f'g1_{row}', [128, D], F32)
                kb.dma('sp', g_[:], modbuf[l, row, 2 * D:3 * D].partition_broadcast(128), w=[f'g1_{row}'])
                g1.append(g_)
            xt = [kb.sb(f'oxt{i}', [128, D], F32) for i in range(3)]
            yt = [kb.sb(f'oyt{i}', [128, D], F32) for i in range(3)]
            otl = list(range(NT) if l == 0 else range(2, NT))
            kb.dma('sp', xt[otl[0] % 3][:], xbuf[otl[0] * 128:(otl[0] + 1) * 128, :], r=[('x', otl[0])], w=[('oxt', otl[0] % 3)])
            for n_, t in enumerate(otl):
                i = t % 3; row = 0 if t < 2 else 1
                if n_ + 1 < len(otl):
                    t2 = otl[n_ + 1]
                    kb.dma('sp', xt[t2 % 3][:], xbuf[t2 * 128:(t2 + 1) * 128, :], r=[('x', t2)], w=[('oxt', t2 % 3)])
                for hf in range(2):
                    bank = 2 * i + hf
                    for fc in range(8):
                        kb.op('pe', lambda e: e.matmul(PSA[:, bank, :], lhsT=mT[:, fc, t * 128:(t + 1) * 128], rhs=wo[:, fc, hf * 512:(hf + 1) * 512],
                                                       start=(fc == 0), stop=(fc == 7)), r=['mT', 'wo'], w=[('ps', bank)])
                    kb.op('dve', lambda e: e.tensor_tensor(out=yt[i][:, hf * 512:(hf + 1) * 512], in0=PSA[:, bank, :], in1=g1[row][:, hf * 512:(hf + 1) * 512],
                                                           op=ALU.mult), r=[('ps', bank), f'g1_{row}'], w=[('oyt', i)])
                kb.op('pool', lambda e: e.tensor_tensor(out=yt[i][:], in0=yt[i][:], in1=xt[i][:], op=ALU.add), r=[('oyt', i), ('oxt', i)], w=[('oyt', i)])
                kb.dma('sp', xbuf[t * 128:(t + 1) * 128, :], yt[i][:], r=[('oyt', i)], w=[('x', t)])
            kb.release(m)

        def phase_moe(l):
            m = kb.mark()
            tiles = list(range(NT) if l == 0 else range(2, NT))
            blocks = TB if l == 0 else TB[1:]
            logits = kb.sb('logits', [128, NT, 36], F32)
            Wt = kb.sb('Wt', [128, NT, 32], F32)
            kb.op('pool', lambda e: e.memset(logits[:], 0.0), w=['logits'])
            phase_norm(l, 2, logits)
            if os.environ.get('MOE_CUT') == '1':
                kb.release(m); return
            m2 = kb.mark()
            lgG = logits[:, :, 0:4]; lgE = logits[:, :, 4:36]
            gmax = kb.sb('gmax', [128, NT], F32); ohg = kb.sb('ohg', [128, NT, 4], F32); eg = kb.sb('eg', [128, NT, 4], F32)
            pg = kb.sb('pg', [128, NT], F32); me = kb.sb('me', [128, NT, 32], F32); oh1 = kb.sb('oh1', [128, NT, 32], F32)
            oh2 = kb.sb('oh2', [128, NT, 32], F32); m1 = kb.sb('m1', [128, NT], F32); m2_ = kb.sb('m2', [128, NT], F32)
            w1 = kb.sb('w1', [128, NT], F32); w2 = kb.sb('w2', [128, NT], F32)
            b1 = lambda a, n_: bc(a, [[1, NT], [0, n_]])
            kb.op('dve', lambda e: e.reduce_max(out=gmax[:], in_=lgG, axis=AX.X), r=['logits'], w=['gmax'])
            kb.op('dve', lambda e: e.tensor_tensor(out=ohg[:], in0=lgG, in1=b1(gmax[:, 0:NT], 4), op=ALU.is_equal), r=['logits', 'gmax'], w=['ohg'])
            kb.op('dve', lambda e: e.tensor_tensor(out=eg[:], in0=lgG, in1=b1(gmax[:, 0:NT], 4), op=ALU.subtract), r=['logits', 'gmax'], w=['eg'])
            kb.op('act', lambda e: e.activation(out=eg[:], in_=eg[:], func=AF.Exp), r=['eg'], w=['eg'])
            kb.op('dve', lambda e: e.reduce_sum(out=pg[:], in_=eg[:], axis=AX.X), r=['eg'], w=['pg'])
            kb.op('dve', lambda e: e.reciprocal(out=pg[:], in_=pg[:]), r=['pg'], w=['pg'])
            kb.op('dve', lambda e: e.tensor_scalar(out=ohg[:], in0=ohg[:], scalar1=-1.0, scalar2=1e30, op0=ALU.add, op1=ALU.mult), r=['ohg'], w=['ohg'])
            kb.op('dve', lambda e: e.tensor_tensor(out=me[:].rearrange("p t (g x) -> p t g x", g=4), in0=lgE.rearrange("p t (g x) -> p t g x", g=4),
                                                   in1=bc(ohg[:, 0:NT, :], [[4, NT], [1, 4], [0, 8]]), op=ALU.add), r=['logits', 'ohg'], w=['me'])
            kb.op('dve', lambda e: e.reduce_max(out=m1[:], in_=me[:], axis=AX.X), r=['me'], w=['m1'])
            kb.op('dve', lambda e: e.tensor_tensor(out=oh1[:], in0=me[:], in1=b1(m1[:, 0:NT], 32), op=ALU.is_equal), r=['me', 'm1'], w=['oh1'])
            kb.op('dve', lambda e: e.scalar_tensor_tensor(out=me[:], in0=oh1[:], scalar=-1e30, in1=me[:], op0=ALU.mult, op1=ALU.add), r=['oh1', 'me'], w=['me'])
            kb.op('dve', lambda e: e.reduce_max(out=m2_[:], in_=me[:], axis=AX.X), r=['me'], w=['m2'])
            kb.op('dve', lambda e: e.tensor_tensor(out=oh2[:], in0=me[:], in1=b1(m2_[:, 0:NT], 32), op=ALU.is_equal), r=['me', 'm2'], w=['oh2'])
            kb.op('dve', lambda e: e.tensor_tensor(out=w1[:], in0=m1[:], in1=m2_[:], op=ALU.subtract), r=['m1', 'm2'], w=['w1'])
            kb.op('act', lambda e: e.activation(out=w2[:], in_=w1[:], func=AF.Sigmoid, scale=-1.0), r=['w1'], w=['w2'])
            kb.op('act', lambda e: e.activation(out=w1[:], in_=w1[:], func=AF.Sigmoid), r=['w1'], w=['w1'])
            kb.op('dve', lambda e: e.tensor_tensor(out=w1[:], in0=w1[:], in1=pg[:], op=ALU.mult), r=['w1', 'pg'], w=['w1'])
            kb.op('dve', lambda e: e.tensor_tensor(out=w2[:], in0=w2[:], in1=pg[:], op=ALU.mult), r=['w2', 'pg'], w=['w2'])
            kb.op('dve', lambda e: e.tensor_tensor(out=oh1[:], in0=oh1[:], in1=b1(w1[:, 0:NT], 32), op=ALU.mult), r=['oh1', 'w1'], w=['oh1'])
            kb.op('dve', lambda e: e.tensor_tensor(out=oh2[:], in0=oh2[:], in1=b1(w2[:, 0:NT], 32), op=ALU.mult), r=['oh2', 'w2'], w=['oh2'])
            kb.op('dve', lambda e: e.tensor_tensor(out=Wt[:], in0=oh1[:], in1=oh2[:], op=ALU.add), r=['oh1', 'oh2'], w=['Wt'])
            kb.release(m2)
            if os.environ.get('MOE_CUT') == '2':
                kb.release(m); return
            acc = kb.sb('acc', [128, NT, D], F32)
            m3 = kb.mark()
            wg = [kb.sb(f'ewg{i}', [128, 8, 512], BF16) for i in range(2)]
            wu = [kb.sb(f'ewu{i}', [128, 8, 512], BF16) for i in range(2)]
            wd = [kb.sb(f'ewd{i}', [128, 4, D], BF16) for i in range(2)]
            aT = [kb.sb(f'aT{i}', [128, 4, 512], BF16) for i in range(2)]
            sgt = [kb.sb(f'sgt{i}', [128, 512], F32) for i in range(2)]
            it = 0; ih = 0
            for ex in range(int(os.environ.get('MOE_NEXP', '32'))):
                j = ex % 2
                load_w(wg[j], ('ewg', j), w_eg[l, ex], 8, 512, engs=('pool', 'act'))
                load_w(wu[j], ('ewu', j), w_eu[l, ex], 8, 512, engs=('pool', 'act'))
                load_w(wd[j], ('ewd', j), w_ed[l, ex], 4, D, engs=('pool', 'act'))
                for (s0, n) in blocks:
                    i = it % 2; it += 1
                    for hc in range(4):
                        ii = ih % 2; ih += 1
                        for k in range(8):
                            kb.op('pe', lambda e: e.matmul(PSA[:, ii, 0:n], lhsT=wg[j][:, k, hc * 128:(hc + 1) * 128], rhs=hT[:, k, s0:s0 + n],
                                                           start=(k == 0), stop=(k == 7)), r=[('ewg', j), 'hT'], w=[('ps', ii)])
                        for k in range(8):
                            kb.op('pe', lambda e: e.matmul(PSA[:, 2 + ii, 0:n], lhsT=wu[j][:, k, hc * 128:(hc + 1) * 128], rhs=hT[:, k, s0:s0 + n],
                                                           start=(k == 0), stop=(k == 7)), r=[('ewu', j), 'hT'], w=[('ps', 2 + ii)])
                        kb.op('act', lambda e: e.activation(out=sgt[ii][:, 0:n], in_=PSA[:, ii, 0:n], func=AF.Silu), r=[('ps', ii)], w=[('sgt', ii)])
                        kb.op('dve', lambda e: e.tensor_tensor(out=aT[i][:, hc, 0:n], in0=sgt[ii][:, 0:n], in1=PSA[:, 2 + ii, 0:n], op=ALU.mult),
                              r=[('sgt', ii), ('ps', 2 + ii)], w=[('aT', i)])
                    for t in range(s0 // 128, (s0 + n) // 128):
                        tl = t * 128 - s0
                        for hf in range(2):
                            bank = 4 + (2 * t + hf) % 3
                            for hc in range(4):
                                kb.op('pe', lambda e: e.matmul(PSA[:, bank, :], lhsT=aT[i][:, hc, tl:tl + 128], rhs=wd[j][:, hc, hf * 512:(hf + 1) * 512],
                                                               start=(hc == 0), stop=(hc == 3)), r=[('aT', i), ('ewd', j)], w=[('ps', bank)])
                            a_ = acc[:, t, hf * 512:(hf + 1) * 512]
                            if ex == 0:
                                kb.op('dve', lambda e: e.tensor_scalar(out=a_, in0=PSA[:, bank, :], scalar1=Wt[:, t, ex:ex + 1], scalar2=None, op0=ALU.mult),
                                      r=[('ps', bank), 'Wt'], w=[('acc', t)])
                            else:
                                kb.op('dve', lambda e: e.scalar_tensor_tensor(out=a_, in0=PSA[:, bank, :], scalar=Wt[:, t, ex:ex + 1], in1=a_,
                                                                              op0=ALU.mult, op1=ALU.add), r=[('ps', bank), 'Wt', ('acc', t)], w=[('acc', t)])
            kb.release(m3)
            g2 = []
            for row in (0, 1):
                g_ = kb.sb(f'g2_{row}', [128, D], F32)
                kb.dma('sp', g_[:], modbuf[l, row, 5 * D:6 * D].partition_broadcast(128), w=[f'g2_{row}'])
                g2.append(g_)
            xt = [kb.sb(f'mxt{i}', [128, D], F32) for i in range(2)]
            for t in tiles:
                i = t % 2; row = 0 if t < 2 else 1
                kb.dma('sp', xt[i][:], xbuf[t * 128:(t + 1) * 128, :], r=[('x', t)], w=[('mxt', i)])
                kb.op('dve', lambda e: e.tensor_tensor(out=acc[:, t, :], in0=acc[:, t, :], in1=g2[row][:], op=ALU.mult), r=[('acc', t), f'g2_{row}'], w=[('acc', t)])
                kb.op('pool', lambda e: e.tensor_tensor(out=xt[i][:], in0=xt[i][:], in1=acc[:, t, :], op=ALU.add), r=[('acc', t), ('mxt', i)], w=[('mxt', i)])
                kb.dma('sp', xbuf[t * 128:(t + 1) * 128, :], xt[i][:], r=[('mxt', i)], w=[('x', t)])
            kb.release(m)

        moe_state = {}

        def phase_moe_sparse(l):
            IOA = bass.IndirectOffsetOnAxis
            ABv = AB.rearrange("r (h c) -> (r h) c", h=2)
            if 'bregs' not in moe_state:
                regs = {}
                for nm_, v_ in (('rec', 32 * CAP - 1), ('tok', T - 1), ('ab', 2 * T - 1)):
                    rg = nc.gpsimd.alloc_register('bnd_' + nm_)
                    nc.gpsimd.reg_mov(rg, v_)
                    regs[nm_] = rg
                moe_state['bregs'] = regs
            BR = moe_state['bregs']
            m = kb.mark()
            t0 = 0 if l == 0 else 2
            tiles = list(range(t0, NT))
            logits = kb.sb('logits', [128, NT, 36], F32)
            kb.op('pool', lambda e: e.memset(logits[:], 0.0), w=['logits'])
            zt = kb.sb('zt', [128, D], F32)
            kb.op('pool', lambda e: e.memset(zt[:], 0.0), w=['zt'])
            for q in range(2 * NT):
                kb.dma('act', AB[q * 128:(q + 1) * 128, :], zt[:], r=['zt'], w=[('ABz', q)])
            ri_ = kb.sb('recinit', [128, (32 * CAP) // 128, 4], F32)
            kb.op('pool', lambda e: e.memset(ri_[:], 1.0e6), w=['recinit'])
            kb.op('pool', lambda e: e.memset(ri_[:, :, 2:3], 0.0), r=['recinit'], w=['recinit'])
            kb.dma('act', rec_d.rearrange("(p s) c -> p s c", p=128), ri_[:], r=['recinit'], w=['rec_d'])
            phase_norm(l, 2, logits, sparse=True)
            wg = [kb.sb(f'ewg{i}', [128, 8, 512], BF16) for i in range(2)]
            wu = [kb.sb(f'ewu{i}', [128, 8, 512], BF16) for i in range(2)]
            wd = [kb.sb(f'ewd{i}', [128, 4, D], BF16) for i in range(2)]
            stgs = list(stg) + stgx

            def w_issue(ex):
                j = ex % 2; pieces = []
                for (dst, dkey, src, K, W) in ((wg[j], ('ewg', j), w_eg[l, ex], 8, 512), (wu[j], ('ewu', j), w_eu[l, ex], 8, 512), (wd[j], ('ewd', j), w_ed[l, ex], 4, D)):
                    kk = 2048 // W
                    for k0 in range(0, K, kk):
                        i = len(pieces)
                        sv = stgs[i][:, 0:kk * W].rearrange("p (k c) -> p k c", c=W)
                        kb.dma('sp', sv, src[k0 * 128:(k0 + kk) * 128, :].rearrange("(k p) c -> p k c", p=128), w=[('stg', i)])
                        pieces.append((dst, dkey, k0, k0 + kk, sv, i))
                return pieces
            pcs0 = w_issue(0)
            MC = kb.sb('MC', [128, 32 + NT], F32)
            kb.dma('sp', MC[:], mconst[:, :], w=['MC'])
            eC = MC[:, 0:32]; tokid = MC[:, 32:32 + NT]
            lgG = logits[:, :, 0:4]; lgE = logits[:, :, 4:36]
            gmax = kb.sb('gmax', [128, NT], F32); ohg = kb.sb('ohg', [128, NT, 4], F32); eg = kb.sb('eg', [128, NT, 4], F32)
            pg = kb.sb('pg', [128, NT], F32); me = kb.sb('me', [128, NT, 32], F32); oh1 = kb.sb('oh1', [128, NT, 32], F32)
            oh2 = kb.sb('oh2', [128, NT, 32], F32); m1 = kb.sb('m1', [128, NT], F32); m2_ = kb.sb('m2', [128, NT], F32)
            w1 = kb.sb('w1', [128, NT], F32); w2 = kb.sb('w2', [128, NT], F32)
            b1 = lambda a, n_: bc(a, [[1, NT], [0, n_]])
            kb.op('dve', lambda e: e.reduce_max(out=gmax[:], in_=lgG, axis=AX.X), r=['logits'], w=['gmax'])
            kb.op('dve', lambda e: e.tensor_tensor(out=ohg[:], in0=lgG, in1=b1(gmax[:, 0:NT], 4), op=ALU.is_equal), r=['logits', 'gmax'], w=['ohg'])
            kb.op('dve', lambda e: e.tensor_tensor(out=eg[:], in0=lgG, in1=b1(gmax[:, 0:NT], 4), op=ALU.subtract), r=['logits', 'gmax'], w=['eg'])
            kb.op('act', lambda e: e.activation(out=eg[:], in_=eg[:], func=AF.Exp), r=['eg'], w=['eg'])
            kb.op('dve', lambda e: e.reduce_sum(out=pg[:], in_=eg[:], axis=AX.X), r=['eg'], w=['pg'])
            kb.op('dve', lambda e: e.reciprocal(out=pg[:], in_=pg[:]), r=['pg'], w=['pg'])
            kb.op('dve', lambda e: e.tensor_scalar(out=ohg[:], in0=ohg[:], scalar1=-1.0, scalar2=1e30, op0=ALU.add, op1=ALU.mult), r=['ohg'], w=['ohg'])
            kb.op('dve', lambda e: e.tensor_tensor(out=me[:].rearrange("p t (g x) -> p t g x", g=4), in0=lgE.rearrange("p t (g x) -> p t g x", g=4),
                                                   in1=bc(ohg[:, 0:NT, :], [[4, NT], [1, 4], [0, 8]]), op=ALU.add), r=['logits', 'ohg'], w=['me'])
            kb.op('dve', lambda e: e.reduce_max(out=m1[:], in_=me[:], axis=AX.X), r=['me'], w=['m1'])
            kb.op('dve', lambda e: e.tensor_tensor(out=oh1[:], in0=me[:], in1=b1(m1[:, 0:NT], 32), op=ALU.is_equal), r=['me', 'm1'], w=['oh1'])
            kb.op('dve', lambda e: e.scalar_tensor_tensor(out=me[:], in0=oh1[:], scalar=-1e30, in1=me[:], op0=ALU.mult, op1=ALU.add), r=['oh1', 'me'], w=['me'])
            kb.op('dve', lambda e: e.reduce_max(out=m2_[:], in_=me[:], axis=AX.X), r=['me'], w=['m2'])
            kb.op('dve', lambda e: e.tensor_tensor(out=oh2[:], in0=me[:], in1=b1(m2_[:, 0:NT], 32), op=ALU.is_equal), r=['me', 'm2'], w=['oh2'])
            kb.op('dve', lambda e: e.tensor_tensor(out=w1[:], in0=m1[:], in1=m2_[:], op=ALU.subtract), r=['m1', 'm2'], w=['w1'])
            kb.op('act', lambda e: e.activation(out=w2[:], in_=w1[:], func=AF.Sigmoid, scale=-1.0), r=['w1'], w=['w2'])
            kb.op('act', lambda e: e.activation(out=w1[:], in_=w1[:], func=AF.Sigmoid), r=['w1'], w=['w1'])
            kb.op('dve', lambda e: e.tensor_tensor(out=w1[:], in0=w1[:], in1=pg[:], op=ALU.mult), r=['w1', 'pg'], w=['w1'])
            kb.op('dve', lambda e: e.tensor_tensor(out=w2[:], in0=w2[:], in1=pg[:], op=ALU.mult), r=['w2', 'pg'], w=['w2'])
            if t0 > 0:
                kb.op('pool', lambda e: e.memset(oh1[:, 0:t0, :], 0.0), r=['oh1'], w=['oh1'])
                kb.op('pool', lambda e: e.memset(oh2[:, 0:t0, :], 0.0), r=['oh2'], w=['oh2'])
            selb = kb.sb('selb', [128, NT * 32], BF16)
            kb.op('pool', lambda e: e.tensor_tensor(out=selb[:], in0=oh1[:].rearrange("p t e -> p (t e)"), in1=oh2[:].rearrange("p t e -> p (t e)"), op=ALU.add),
                  r=['oh1', 'oh2'], w=['selb'])
            LT = kb.sb('LT', [128, 128], BF16); ones = kb.sb('ones', [128, 128], BF16)
            kb.op('pool', lambda e: e.memset(ones[:], 1.0), w=['ones'])
            kb.op('pool', lambda e: e.memset(LT[:], 1.0), w=['LT'])
            kb.op('pool', lambda e: e.affine_select(out=LT[:], in_=LT[:], pattern=[[1, 128]], compare_op=ALU.is_gt, fill=0.0, base=0,
                                                    channel_multiplier=-1), r=['LT'], w=['LT'])
            slot = kb.sb('slot', [128, NT, 32], F32); tot = kb.sb('tot', [128, NT, 32], F32); cum = kb.sb('cum', [128, NT, 32], F32)
            sl2 = slot[:].rearrange("p t e -> p (t e)"); to2 = tot[:].rearrange("p t e -> p (t e)")
            for (c0, c1, bank) in ((0, 512, 0), (512, NT * 32, 1)):
                kb.op('pe', lambda e: e.matmul(PSA[:, bank, 0:c1 - c0], lhsT=LT[:], rhs=selb[:, c0:c1], start=True, stop=True), r=['LT', 'selb'], w=[('ps', bank)])
                kb.op('dve', lambda e: e.tensor_copy(out=sl2[:, c0:c1], in_=PSA[:, bank, 0:c1 - c0]), r=[('ps', bank)], w=['slot'])
                kb.op('pe', lambda e: e.matmul(PSA[:, 2 + bank, 0:c1 - c0], lhsT=ones[:], rhs=selb[:, c0:c1], start=True, stop=True), r=['ones', 'selb'], w=[('ps', 2 + bank)])
                kb.op('dve', lambda e: e.tensor_copy(out=to2[:, c0:c1], in_=PSA[:, 2 + bank, 0:c1 - c0]), r=[('ps', 2 + bank)], w=['tot'])
            kb.op('pool', lambda e: e.memset(cum[:, 0, :], 0.0), w=['cum'])
            for t in range(1, NT):
                kb.op('dve', lambda e: e.tensor_tensor(out=cum[:, t, :], in0=cum[:, t - 1, :], in1=tot[:, t - 1, :], op=ALU.add), r=['cum', 'tot'], w=['cum'])
            kb.op('dve', lambda e: e.tensor_tensor(out=slot[:], in0=slot[:], in1=cum[:], op=ALU.add), r=['slot', 'cum'], w=['slot'])
            rec = kb.sb('rec', [128, NT, 2, 4], F32)
            rr = kb.sb('rr', [128, NT, 2], F32); rri = kb.sb('rri', [128, NT, 2], I32)
            s_ = kb.sb('s_', [128, NT], F32); e_ = kb.sb('e_', [128, NT], F32)
            kb.op('pool', lambda e: e.memset(rec[:], 0.0), w=['rec'])
            for k, (oh, wk) in enumerate(((oh1, w1), (oh2, w2))):
                kb.op('dve', lambda e: e.tensor_tensor(out=me[:], in0=oh[:], in1=slot[:], op=ALU.mult), r=['oh1', 'oh2', 'slot', 'me'], w=['me'])
                kb.op('dve', lambda e: e.reduce_sum(out=s_[:], in_=me[:], axis=AX.X), r=['me'], w=['s_'])
                kb.op('dve', lambda e: e.tensor_tensor(out=me[:], in0=oh[:], in1=bc(eC, [[0, NT], [1, 32]]), op=ALU.mult), r=['oh1', 'oh2', 'MC', 'me'], w=['me'])
                kb.op('dve', lambda e: e.reduce_sum(out=e_[:], in_=me[:], axis=AX.X), r=['me'], w=['e_'])
                kb.op('dve', lambda e: e.tensor_tensor(out=e_[:], in0=e_[:], in1=s_[:], op=ALU.add), r=['e_', 's_'], w=['e_'])
                kb.op('dve', lambda e: e.tensor_scalar(out=s_[:], in0=s_[:], scalar1=float(CAP), scalar2=1.0e6, op0=ALU.is_ge, op1=ALU.mult), r=['s_'], w=['s_'])
                kb.op('dve', lambda e: e.tensor_tensor(out=rr[:, :, k], in0=e_[:], in1=s_[:], op=ALU.add), r=['e_', 's_'], w=['rr'])
                kb.op('pool', lambda e: e.tensor_copy(out=rec[:, :, k, 0], in_=tokid), r=['MC', 'rec'], w=['rec'])
                kb.op('pool', lambda e: e.tensor_scalar(out=rec[:, :, k, 1], in0=tokid, scalar1=float(k * T), scalar2=None, op0=ALU.add), r=['MC', 'rec'], w=['rec'])
                kb.op('pool', lambda e: e.tensor_scalar(out=rec[:, :, k, 3], in0=tokid, scalar1=2.0, scalar2=float(2 * k * T + 1), op0=ALU.mult, op1=ALU.add), r=['MC', 'rec'], w=['rec'])
                kb.op('pool', lambda e: e.tensor_copy(out=rec[:, :, k, 2], in_=wk[:]), r=['w1', 'w2', 'rec'], w=['rec'])
            kb.op('dve', lambda e: e.tensor_copy(out=rri[:], in_=rr[:]), r=['rr'], w=['rri'])
            if stop == f'moe{l}' and os.environ.get('XDUMP'):
                xdump('rr', rr, [128, NT, 2], F32); xdump('rri', rri, [128, NT, 2], I32); xdump('rec', rec, [128, NT, 2, 4], F32)
                xdump('slot', slot, [128, NT, 32], F32)
            kb.barrier()
            for t in tiles:
                for k in range(2):
                    kb.dma('pool', None, None, r=['rec', 'rri'], w=[('recs', t, k)],
                           fn=lambda g: g.indirect_dma_start(out=rec_d[:, :], out_offset=IOA(ap=rri[:, t, k:k + 1], axis=0), in_=rec[:, t, k, :],
                                                             in_offset=None, bounds_check=BR['rec'], oob_is_err=False))
            kb.barrier()
            if os.environ.get('MOE_CUT') == '3':
                kb.release(m); return
            m3 = kb.mark()
            XT = [kb.sb(f'XT{i}', [128, 8, CAP], BF16) for i in range(2)]
            aT = kb.sb('aT', [128, 4, CAP], BF16)
            sgt = [kb.sb(f'sgt{i}', [128, 512], F32) for i in range(2)]
            rsb = [kb.sb(f'rsb{i}', [128, NS, 4], F32) for i in range(2)]
            gi = [kb.sb(f'gi{i}', [128, NS, 4], I32) for i in range(2)]
            xg = [kb.sb(f'xg{i}', [128, D], BF16) for i in range(NS)]
            yw = [kb.sb(f'yw{i}', [128, D], F32) for i in range(3)]
            for i in range(NS):
                kb.op('pool', lambda e: e.memset(xg[i][:], 0.0), w=[('xg', i)])
            PST2 = PSA[:, 6, :].bitcast(BF16).rearrange("p (k c) -> p k c", c=128)
            nexp = int(os.environ.get('MOE_NEXP', '32'))
            cnt = {'iy': 0, 'ih': 0}

            def w_cast(pieces):
                for n_, (dst, dkey, k0, k1, sv, i) in enumerate(pieces):
                    if n_ % 2 == 0:
                        kb.op('act', lambda e: e.copy(out=dst[:, k0:k1, :], in_=sv), r=[('stg', i)], w=[dkey])
                    else:
                        kb.op('dve', lambda e: e.tensor_copy(out=dst[:, k0:k1, :], in_=sv), r=[('stg', i)], w=[dkey])

            def G(ex):
                j = ex % 2
                kb.dma('sp', rsb[j][:], rec_d[ex * CAP:(ex + 1) * CAP, :].rearrange("(s p) c -> p s c", p=128), w=[('rsb', j)])
                kb.op('dve', lambda e: e.tensor_copy(out=gi[j][:], in_=rsb[j][:]), r=[('rsb', j)], w=[('gi', j)])
                for s_i in range(NS):
                    kb.dma('pool', None, None, r=[('gi', j)], w=[('xg', s_i)],
                           fn=lambda g: g.indirect_dma_start(out=xg[s_i][:], out_offset=None, in_=h2d[:, :],
                                                             in_offset=IOA(ap=gi[j][:, s_i, 0:1], axis=0), bounds_check=BR['tok'], oob_is_err=False))

            def TR(ex):
                j = ex % 2
                for s_i in range(NS):
                    pt_, pk_ = (PST, 'pst') if s_i % 2 == 0 else (PST2, ('ps', 6))
                    for k in range(8):
                        kb.op('pe', lambda e: e.transpose(out=pt_[:, k, :], in_=xg[s_i][:, k * 128:(k + 1) * 128], identity=ident[:]),
                              r=[('xg', s_i), 'ident'], w=[pk_])
                    kb.op('act', lambda e: e.copy(out=XT[j][:, :, s_i * 128:(s_i + 1) * 128], in_=pt_[:]), r=[pk_], w=[('XT', j)])

            def F1(ex):
                j = ex % 2
                for c0 in range(0, CAP, 512):
                    n = min(512, CAP - c0)
                    for hc in range(4):
                        ii = cnt['ih'] % 2; cnt['ih'] += 1
                        for k in range(8):
                            kb.op('pe', lambda e: e.matmul(PSA[:, ii, 0:n], lhsT=wg[j][:, k, hc * 128:(hc + 1) * 128], rhs=XT[j][:, k, c0:c0 + n],
                                                           start=(k == 0), stop=(k == 7)), r=[('ewg', j), ('XT', j)], w=[('ps', ii)])
                        for k in range(8):
                            kb.op('pe', lambda e: e.matmul(PSA[:, 2 + ii, 0:n], lhsT=wu[j][:, k, hc * 128:(hc + 1) * 128], rhs=XT[j][:, k, c0:c0 + n],
                                                           start=(k == 0), stop=(k == 7)), r=[('ewu', j), ('XT', j)], w=[('ps', 2 + ii)])
                        kb.op('act', lambda e: e.activation(out=sgt[ii][:, 0:n], in_=PSA[:, ii, 0:n], func=AF.Silu), r=[('ps', ii)], w=[('sgt', ii)])
                        kb.op('dve', lambda e: e.tensor_tensor(out=aT[:, hc, c0:c0 + n], in0=sgt[ii][:, 0:n], in1=PSA[:, 2 + ii, 0:n], op=ALU.mult),
                              r=[('sgt', ii), ('ps', 2 + ii)], w=['aT'])

            def F2(ex):
                j = ex % 2
                for s_i in range(NS):
                    q = cnt['iy'] % 3; cnt['iy'] += 1
                    for hf in range(2):
                        bank = 4 + hf
                        for hc in range(4):
                            kb.op('pe', lambda e: e.matmul(PSA[:, bank, :], lhsT=aT[:, hc, s_i * 128:(s_i + 1) * 128], rhs=wd[j][:, hc, hf * 512:(hf + 1) * 512],
                                                           start=(hc == 0), stop=(hc == 3)), r=['aT', ('ewd', j)], w=[('ps', bank)])
                        kb.op('dve', lambda e: e.tensor_scalar(out=yw[q][:, hf * 512:(hf + 1) * 512], in0=PSA[:, bank, :], scalar1=rsb[j][:, s_i, 2:3], scalar2=None,
                                                               op0=ALU.mult), r=[('ps', bank), ('rsb', j)], w=[('yw', q)])
                    kb.dma('pool', None, None, r=[('yw', q), ('gi', j)], w=[('ABs', ex, s_i)],
                           fn=lambda g: g.indirect_dma_start(out=AB[:, :], out_offset=IOA(ap=gi[j][:, s_i, 1:2], axis=0),
                                                             in_=yw[q][:], in_offset=None, bounds_check=BR['ab'], oob_is_err=False))

            w_cast(pcs0); G(0); TR(0)
            for ex in range(nexp):
                nxt = ex + 1 < nexp
                if nxt:
                    pcs = w_issue(ex + 1)
                    G(ex + 1)
                F1(ex)
                if nxt:
                    w_cast(pcs)
                    TR(ex + 1)
                F2(ex)
            stg_all[0] = list(stg)
            stg_i[0] = 0
            kb.release(m3)
            g2 = []
            for row in (0, 1):
                g_ = kb.sb(f'g2_{row}', [128, D], F32)
                kb.dma('sp', g_[:], modbuf[l, row, 5 * D:6 * D].partition_broadcast(128), w=[f'g2_{row}'])
                g2.append(g_)
            xt = [kb.sb(f'mxt{i}', [128, D], F32) for i in range(2)]
            ya = [kb.sb(f'mya{i}', [128, D], F32) for i in range(2)]
            yb = [kb.sb(f'myb{i}', [128, D], F32) for i in range(2)]
            def rload(t):
                i = t % 2
                kb.dma('sp', xt[i][:], xbuf[t * 128:(t + 1) * 128, :], r=[('x', t)], w=[('mxt', i)])
                kb.dma('sp', ya[i][:], AB[t * 128:(t + 1) * 128, :], w=[('mya', i)])
                kb.dma('sp', yb[i][:], AB[T + t * 128:T + (t + 1) * 128, :], w=[('myb', i)])
            rload(tiles[0])
            for n_, t in enumerate(tiles):
                i = t % 2; row = 0 if t < 2 else 1
                if n_ + 1 < len(tiles):
                    rload(tiles[n_ + 1])
                kb.op('pool', lambda e: e.tensor_tensor(out=ya[i][:], in0=ya[i][:], in1=yb[i][:], op=ALU.add), r=[('mya', i), ('myb', i)], w=[('mya', i)])
                kb.op('dve', lambda e: e.tensor_tensor(out=ya[i][:], in0=ya[i][:], in1=g2[row][:], op=ALU.mult), r=[('mya', i), f'g2_{row}'], w=[('mya', i)])
                kb.op('pool', lambda e: e.tensor_tensor(out=xt[i][:], in0=xt[i][:], in1=ya[i][:], op=ALU.add), r=[('mya', i), ('mxt', i)], w=[('mxt', i)])
                kb.dma('sp', xbuf[t * 128:(t + 1) * 128, :], xt[i][:], r=[('mxt', i)], w=[('x', t)])
            kb.release(m)

        def phase_final():
            m = kb.mark()
            nwb = kb.sb('fnw', [128, D], F32)
            kb.dma('sp', nwb[:], norm_final.partition_broadcast(128), w=['fnw'])
            xt = [kb.sb(f'fxt{i}', [128, D], F32) for i in range(3)]
            yt = [kb.sb(f'fyt{i}', [128, D], F32) for i in range(3)]
            ssq = [kb.sb(f'fss{i}', [128, 1], F32) for i in range(3)]
            junk = kb.sb('fjunk', [128, D], F32)
            kb.dma('sp', xt[2 % 3][:], xbuf[2 * 128:3 * 128, :], r=[('x', 2)], w=[('fxt', 2 % 3)])
            for t in range(2, NT):
                i = t % 3
                if t + 1 < NT:
                    kb.dma('sp', xt[(t + 1) % 3][:], xbuf[(t + 1) * 128:(t + 2) * 128, :], r=[('x', t + 1)], w=[('fxt', (t + 1) % 3)])
                kb.op('act', lambda e: e.activation(out=junk[:], in_=xt[i][:], func=AF.Square, accum_out=ssq[i][:]), r=[('fxt', i)], w=['fjunk', ('fss', i)])
                kb.op('act', lambda e: e.activation(out=ssq[i][:], in_=ssq[i][:], func=AF.Sqrt, scale=1.0 / D, bias=epsN), r=[('fss', i)], w=[('fss', i)])
                kb.op('dve', lambda e: e.reciprocal(out=ssq[i][:], in_=ssq[i][:]), r=[('fss', i)], w=[('fss', i)])
                kb.op('dve', lambda e: e.scalar_tensor_tensor(out=yt[i][:], in0=xt[i][:], scalar=ssq[i][:, 0:1], in1=nwb[:], op0=ALU.mult, op1=ALU.mult),
                      r=[('fxt', i), ('fss', i), 'fnw'], w=[('fyt', i)])
                kb.dma('sp', out[(t - 2) * 128:(t - 1) * 128, :], yt[i][:], r=[('fyt', i)], w=[('out', t)])
            kb.release(m)

        def finish_dbg(hT_=True, oT=None, mT=None):
            kb.barrier()
            for t in range(NT):
                pass
            kb.dma('sp', dbg['d_x'], xbuf[:, :], w=['d_x'])
            if hT_:
                kb.dma('sp', dbg['d_hT'], hT[:], w=['d_hT'])
            if oT is not None:
                kb.dma('sp', dbg['d_oT'], oT[:], w=['d_oT'])
            if mT is not None:
                kb.dma('sp', dbg['d_mT'], mT[:], w=['d_mT'])
            kb.finish()
            return nc

        kb.dma('sp', xbuf[0:L, :], ctx_in[:, :], w=['xinit0'])
        kb.dma('sp', xbuf[L:T, :], x_in[:, :], w=['xinit1'])
        for l in range(nlayers):
            phase_ada(l)
        kb.barrier()
        if stop == 'ada':
            kb.dma('sp', dbg['d_mod'], modbuf[0], w=['d_mod'])
            return finish_dbg()
        for l in range(nlayers):
            phase_norm(l, 1)
            if stop == f'norm{l}':
                return finish_dbg()
            mm = kb.mark()
            oT = kb.sb('oT', [128, 4, T], BF16, top=True)
            phase_ret(l, oT)
            if stop == f'ret{l}':
                return finish_dbg(oT=oT)
            mT = kb.sb('mT', [128, 8, T], BF16)
            merge(l, 2048, w_br, oT, 'retT', mT, True)
            phase_attn(l, oT)
            if stop == f'attn{l}':
                return finish_dbg(oT=oT)
            merge(l, 0, w_ba, oT, 'oaT', mT, False)
            phase_four(l, oT)
            if stop == f'four{l}':
                return finish_dbg(oT=oT)
            merge(l, 1024, w_bf, oT, 'ofT', mT, False)
            if stop == f'merge{l}':
                return finish_dbg(mT=mT)
            phase_out(l, mT)
            kb.release(mm)
            if stop == f'mix{l}':
                return finish_dbg()
            if os.environ.get('MOE_DENSE'):
                phase_moe(l)
            else:
                phase_moe_sparse(l)
            if stop == f'moe{l}':
                return finish_dbg()
        phase_final()
        kb.finish()
    print("SBUF peak bytes", kb.peak, "instr counts", kb.cnt)
    return nc


def _consts():
    bf = ml_dtypes.bfloat16
    f32 = np.float32
    n = np.arange(N)
    inv16 = (10000.0 ** (-(np.arange(16, dtype=f32)) / f32(16))).astype(f32)
    row = (n // 64).astype(f32); col = (n % 64).astype(f32)
    ropeA = np.zeros((2, 128, N), f32)
    for p in range(128):
        d = p % 64
        pos = row if d < 32 else col
        dd = d % 32
        ang = (pos * inv16[dd % 16]).astype(f32)
        ropeA[0, p] = np.cos(ang); ropeA[1, p] = np.sin(ang) * (-1.0 if dd < 16 else 1.0)
    inv32 = (10000.0 ** (-(np.arange(32, dtype=f32)) / f32(32))).astype(f32)
    ropeR = np.zeros((2, 128, N), f32)
    for p in range(128):
        d = p % 64
        ang = (n.astype(f32) * inv32[d % 32]).astype(f32)
        ropeR[0, p] = np.cos(ang); ropeR[1, p] = np.sin(ang) * (-1.0 if d < 32 else 1.0)
    k = np.arange(N, dtype=np.int64)
    ph = (np.outer(k, k) % N).astype(np.float64) * (2 * np.pi / N)
    dftN = np.stack([np.cos(ph), -np.sin(ph)]) / np.sqrt(N)
    k2 = np.arange(256, dtype=np.int64)
    ph2 = (np.outer(k2, k2) % 256).astype(np.float64) * (2 * np.pi / 256)
    dft256 = np.stack([np.cos(ph2), -np.sin(ph2)]) / np.sqrt(256)
    k3 = np.arange(128, dtype=np.int64)
    ph3 = (np.outer(k3, k3) % 128).astype(np.float64) * (2 * np.pi / 128)
    dftC = np.concatenate([np.cos(ph3), np.sin(ph3)], axis=1) / np.sqrt(128)
    b = np.arange(128)[:, None]; a = np.arange(128)[None, :]
    amask = np.concatenate([(b >= a), (b <= a)], axis=1).astype(f32)
    j = b; i = a
    rconst = np.concatenate([np.maximum(i - j, 0), np.maximum(j - i, 0), (i >= j), (j > i), (i + 1) + 0 * j, (128 - i) + 0 * j,
                             127 - np.arange(128)[:, None], np.arange(128)[:, None]], axis=1).astype(f32)
    mconst = np.concatenate([np.tile((np.arange(32) * CAP)[None, :], (128, 1)), np.arange(128)[:, None] + 128 * np.arange(NT)[None, :]], axis=1).astype(f32)
    return dict(mconst=mconst, ropeA=ropeA, ropeR=ropeR, dftN=dftN.astype(bf), dft256=dft256.astype(bf), dftC=dftC.astype(bf),
                amask=amask.astype(bf), rconst=rconst)


def _wext_index():
    idx = []
    swa = np.array([d + 16 if (d % 32) < 16 else d - 16 for d in range(64)])
    swr = np.array([d + 32 if d < 32 else d - 32 for d in range(64)])
    base = 0
    qa = [np.concatenate([np.arange(g * 64, (g + 1) * 64), np.arange((4 + g) * 64, (5 + g) * 64)]) for g in range(4)]
    qas = [np.concatenate([g * 64 + swa, (4 + g) * 64 + swa]) for g in range(4)]
    idx += qa + qas
    idx += [512 + np.arange(128), 512 + np.concatenate([swa, 64 + swa])]
    idx += [640 + np.arange(128)]
    idx += [768 + np.arange(512), 768 + np.concatenate([h * 64 + swr for h in range(8)])]
    idx += [1280 + np.arange(512), 1280 + np.concatenate([h * 64 + swr for h in range(8)])]
    idx += [1792 + np.arange(512), 2304 + np.arange(512), 2816 + np.arange(512), 3328 + np.arange(3072)]
    idx = np.concatenate(idx)
    assert idx.shape[0] == WEXT
    return idx


_CACHE = {}


def kernel(x, c, ctx, c_ctx, norm_mix, norm_ffn, w_ada, b_ada, w_in, attn_sink, ret_decay_fwd, ret_decay_bwd,
           w_branch_attn, w_branch_fourier, w_branch_ret, w_out, w_router_group, b_router_group,
           w_router_expert, b_router_expert, w_exp_gate, w_exp_up, w_exp_down, norm_final):
    f = lambda a: np.ascontiguousarray(np.asarray(a, dtype=np.float32))
    if 'nc' not in _CACHE:
        _CACHE['nc'] = build()
        _CACHE['consts'] = _consts()
        _CACHE['idx'] = _wext_index()
    nc = _CACHE['nc']
    shared = dict(_CACHE['consts'])
    w_in = f(w_in)
    shared.update(
        c_ctx=f(c_ctx), norm_mix=f(norm_mix), norm_ffn=f(norm_ffn), w_ada=f(w_ada), b_ada=f(b_ada),
        w_ext=np.ascontiguousarray(w_in[:, :, _CACHE['idx']]), attn_sink=f(attn_sink),
        ret_decay_fwd=f(ret_decay_fwd), ret_decay_bwd=f(ret_decay_bwd), w_branch_attn=f(w_branch_attn),
        w_branch_fourier=f(w_branch_fourier), w_branch_ret=f(w_branch_ret), w_out=f(w_out),
        w_rt=np.ascontiguousarray(np.concatenate([f(w_router_group), f(w_router_expert)], axis=-1)),
        b_rt=np.ascontiguousarray(np.concatenate([f(b_router_group), f(b_router_expert)], axis=-1)),
        w_exp_gate=f(w_exp_gate), w_exp_up=f(w_exp_up), w_exp_down=f(w_exp_down), norm_final=f(norm_final))
    x = f(x); c = f(c); ctx = f(ctx)
    B = x.shape[0]
    in_maps = []
    for b in range(B):
        d = dict(shared)
        d.update(x=x[b], ctx=ctx[b], c=c[b])
        in_maps.append(d)
    res = run_bass_kernel_spmd(nc, in_maps, core_ids=list(range(B)))
    return np.stack([np.asarray(r["out"], dtype=np.float32) for r in res.results], axis=0)
```
